# Optimizing a Trainium2 kernel written in Bass

```python
import math
import jax, jax.numpy as jnp
from jax import lax
import numpy as np

D_MODEL = 1024
BATCH = 8
SEQ = 2048
DEPTH = 1

A_HEADS = 8
A_HEAD_DIM = 64
A_WIDTH = A_HEADS * A_HEAD_DIM
IDX_HEADS = 8
IDX_DIM = 32
INDEX_TOPK = 256
SPARSE_Q_BLOCK = 64
N_BUCKETS = 32
MAX_DISTANCE = 128

B_HEADS = 4
B_KEY_DIM = 64
B_VAL_DIM = 128
B_KEY_WIDTH = B_HEADS * B_KEY_DIM
B_WIDTH = B_HEADS * B_VAL_DIM
GATE_RANK = 16
GATE_TEMP = 16.0
GLA_CHUNK = 64

MIX_WIDTH = A_WIDTH + B_WIDTH

PROJ_SPLITS = (A_WIDTH, A_WIDTH, A_WIDTH,
               IDX_HEADS * IDX_DIM, IDX_DIM, IDX_HEADS,
               B_KEY_WIDTH, B_KEY_WIDTH, B_WIDTH,
               GATE_RANK, B_WIDTH)
PROJ_WIDTH = 3 * A_WIDTH + IDX_HEADS * IDX_DIM + IDX_DIM + IDX_HEADS + 2 * B_KEY_WIDTH + B_WIDTH + GATE_RANK + B_WIDTH

N_GROUPS = 4
EXPERTS_PER_GROUP = 8
N_EXPERTS = N_GROUPS * EXPERTS_PER_GROUP
EXPERT_TOPK = 2
D_EXPERT = 256

EPS = 1e-6

kernel_name = "hymba_dsa_gla_hier_moe_layer"


def rmsnorm(x, g):
    xf = x.astype(jnp.float32)
    y = xf * lax.rsqrt(jnp.mean(xf * xf, axis=-1, keepdims=True) + EPS)
    return (y * g.astype(jnp.float32)).astype(x.dtype)


def t5_bucket(dist):
    max_exact = N_BUCKETS // 2
    d_f = jnp.maximum(dist, 1).astype(jnp.float32)
    large = max_exact + (jnp.log(d_f / max_exact) / math.log(MAX_DISTANCE / max_exact)
                         * (N_BUCKETS - max_exact)).astype(jnp.int32)
    large = jnp.minimum(large, N_BUCKETS - 1)
    return jnp.where(dist < max_exact, dist, large)


def dsa_attention(q, k, v, iq, ik, iw, rel_bias):
    bsz, s_len = q.shape[0], q.shape[1]
    topk = min(INDEX_TOPK, s_len // 4)
    nb = s_len // SPARSE_Q_BLOCK

    def blocks(a):
        return a.reshape(bsz, nb, SPARSE_Q_BLOCK, *a.shape[2:]).swapaxes(0, 1)

    q_pos = jnp.arange(s_len, dtype=jnp.int32).reshape(nb, SPARSE_Q_BLOCK)
    key_pos = jnp.arange(s_len, dtype=jnp.int32)
    ik_f = ik.astype(jnp.float32)
    gather = jax.vmap(lambda a, i: a[i])

    def one_block(args):
        qb, iqb, iwb, tb = args
        idx_logits = jnp.einsum('bqhd,bsd->bqhs', iqb.astype(jnp.float32), ik_f) * IDX_DIM ** -0.5
        score = jnp.einsum('bqhs,bqh->bqs', jax.nn.relu(idx_logits),
                           iwb.astype(jnp.float32) * IDX_HEADS ** -0.5)
        causal = key_pos[None, :] <= tb[:, None]
        score = jnp.where(causal[None], score, -jnp.inf)
        _, idx = lax.top_k(score, topk)
        valid = idx <= tb[None, :, None]
        k_sel = gather(k, idx)
        v_sel = gather(v, idx)
        logits = jnp.einsum('bqhd,bqkhd->bhqk', qb, k_sel).astype(jnp.float32) * A_HEAD_DIM ** -0.5
        dist = jnp.maximum(tb[None, :, None] - idx, 0)
        bias = rel_bias[t5_bucket(dist)]
        logits = logits + jnp.transpose(bias, (0, 3, 1, 2)).astype(jnp.float32)
        logits = jnp.where(valid[:, None], logits, -jnp.inf)
        p = jax.nn.softmax(logits, axis=-1).astype(v.dtype)
        return jnp.einsum('bhqk,bqkhd->bqhd', p, v_sel)

    out = lax.map(one_block, (blocks(q), blocks(iq), blocks(iw), q_pos))
    return out.swapaxes(0, 1).reshape(bsz, s_len, A_HEADS * A_HEAD_DIM)


def gla_chunked(q, k, v, log_a):
    bsz, s_len, h, dk = q.shape
    dv = v.shape[-1]
    nc = s_len // GLA_CHUNK

    def chunks(a):
        return a.reshape(bsz, nc, GLA_CHUNK, h, a.shape[-1]).transpose(1, 0, 3, 2, 4)

    tri = jnp.tril(jnp.ones((GLA_CHUNK, GLA_CHUNK), dtype=bool))

    def step(state, inp):
        qc, kc, vc, gc = inp
        b = jnp.cumsum(gc, axis=2)
        o_inter = jnp.einsum('bhcd,bhdv->bhcv', qc * jnp.exp(b), state)
        diff = b[:, :, :, None, :] - b[:, :, None, :, :]
        decay = jnp.exp(jnp.where(tri[None, None, :, :, None], diff, -jnp.inf))
        attn = jnp.einsum('bhtd,bhsd,bhtsd->bhts', qc, kc, decay)
        o_intra = jnp.einsum('bhts,bhsv->bhtv', attn, vc)
        b_last = b[:, :, -1:, :]
        state = (jnp.exp(b_last[:, :, 0, :])[..., None] * state
                 + jnp.einsum('bhsd,bhsv->bhdv', kc * jnp.exp(b_last - b), vc))
        return state, o_inter + o_intra

    init = jnp.zeros((bsz, h, dk, dv), jnp.float32)
    _, o = lax.scan(step, init, (chunks(q * dk ** -0.5), chunks(k), chunks(v), chunks(log_a)))
    return o.transpose(1, 0, 3, 2, 4).reshape(bsz, s_len, h, dv)


def hier_moe(h, w_rg, b_rg, w_re, b_re, w_g, w_u, w_d):
    bsz, s_len, d = h.shape
    hf = h.reshape(-1, d)
    g_logits = (hf @ w_rg).astype(jnp.float32) + b_rg.astype(jnp.float32)
    g_prob = jax.nn.softmax(g_logits, axis=-1)
    _, g_sel = lax.top_k(g_logits, 1)
    g_onehot = jax.nn.one_hot(g_sel[:, 0], N_GROUPS, dtype=jnp.float32)
    g_weight = jnp.sum(g_prob * g_onehot, axis=-1, keepdims=True)
    e_logits = ((hf @ w_re).astype(jnp.float32) + b_re.astype(jnp.float32)).reshape(-1, N_GROUPS, EXPERTS_PER_GROUP)
    e_logits_sel = jnp.einsum('nge,ng->ne', e_logits, g_onehot)
    top_val, top_idx = lax.top_k(e_logits_sel, EXPERT_TOPK)
    e_weight = jax.nn.softmax(top_val, axis=-1) * g_weight
    e_idx = g_sel * EXPERTS_PER_GROUP + top_idx
    gates = jnp.sum(jax.nn.one_hot(e_idx, N_EXPERTS, dtype=jnp.float32) * e_weight[..., None], axis=1)
    out = jnp.zeros(hf.shape, jnp.float32)
    for g in range(N_GROUPS):
        sl = slice(g * EXPERTS_PER_GROUP, (g + 1) * EXPERTS_PER_GROUP)
        a = jnp.einsum('nd,edf->nef', hf, w_g[sl])
        u = jnp.einsum('nd,edf->nef', hf, w_u[sl])
        hid = jax.nn.silu(a) * u * gates[:, sl, None].astype(hf.dtype)
        out = out + jnp.einsum('nef,efd->nd', hid, w_d[sl]).astype(jnp.float32)
    return out.astype(h.dtype).reshape(bsz, s_len, d)


def setup_inputs(seed: int = 0) -> dict:
    key = jax.random.key(seed)
    ks = jax.random.split(key, 20)
    f32 = jnp.float32
    nrm = lambda k, shape, scale: jax.random.normal(k, shape, f32) * scale
    return {
        "x": nrm(ks[0], (BATCH, SEQ, D_MODEL), 1.0),
        "norm1_g": 1.0 + nrm(ks[1], (DEPTH, D_MODEL), 0.01),
        "w_in": nrm(ks[2], (DEPTH, D_MODEL, PROJ_WIDTH), D_MODEL ** -0.5),
        "q_norm_g": 1.0 + nrm(ks[3], (DEPTH, A_HEAD_DIM), 0.01),
        "k_norm_g": 1.0 + nrm(ks[4], (DEPTH, A_HEAD_DIM), 0.01),
        "rel_bias": nrm(ks[5], (N_BUCKETS, A_HEADS), 0.5),
        "gla_gate_w2": nrm(ks[6], (DEPTH, GATE_RANK, B_KEY_WIDTH), GATE_RANK ** -0.5),
        "gla_gate_b": nrm(ks[7], (DEPTH, B_KEY_WIDTH), 0.1),
        "gla_out_norm_g": 1.0 + nrm(ks[8], (DEPTH, B_VAL_DIM), 0.01),
        "w_out": nrm(ks[9], (DEPTH, MIX_WIDTH, D_MODEL), MIX_WIDTH ** -0.5),
        "norm2_g": 1.0 + nrm(ks[10], (DEPTH, D_MODEL), 0.01),
        "w_router_group": nrm(ks[11], (DEPTH, D_MODEL, N_GROUPS), D_MODEL ** -0.5),
        "b_router_group": nrm(ks[12], (DEPTH, N_GROUPS), 0.01),
        "w_router_expert": nrm(ks[13], (DEPTH, D_MODEL, N_EXPERTS), D_MODEL ** -0.5),
        "b_router_expert": nrm(ks[14], (DEPTH, N_EXPERTS), 0.01),
        "w_exp_gate": nrm(ks[15], (DEPTH, N_EXPERTS, D_MODEL, D_EXPERT), D_MODEL ** -0.5),
        "w_exp_up": nrm(ks[16], (DEPTH, N_EXPERTS, D_MODEL, D_EXPERT), D_MODEL ** -0.5),
        "w_exp_down": nrm(ks[17], (DEPTH, N_EXPERTS, D_EXPERT, D_MODEL), D_EXPERT ** -0.5),
    }


def reference(x, norm1_g, w_in, q_norm_g, k_norm_g, rel_bias, gla_gate_w2, gla_gate_b,
              gla_out_norm_g, w_out, norm2_g, w_router_group, b_router_group,
              w_router_expert, b_router_expert, w_exp_gate, w_exp_up, w_exp_down):
    bsz, s_len, _ = x.shape
    split_points = np.cumsum(PROJ_SPLITS)[:-1].tolist()
    for l in range(DEPTH):
        h = rmsnorm(x, norm1_g[l])
        proj = h @ w_in[l]
        qa, ka, va, iq, ik, iw, qb, kb, vb, g_lr, r_gate = jnp.split(proj, split_points, axis=-1)

        qa = rmsnorm(qa.reshape(bsz, s_len, A_HEADS, A_HEAD_DIM), q_norm_g[l])
        ka = rmsnorm(ka.reshape(bsz, s_len, A_HEADS, A_HEAD_DIM), k_norm_g[l])
        va = va.reshape(bsz, s_len, A_HEADS, A_HEAD_DIM)
        o_a = dsa_attention(qa, ka, va, iq.reshape(bsz, s_len, IDX_HEADS, IDX_DIM), ik, iw, rel_bias)

        log_a = jax.nn.log_sigmoid((g_lr @ gla_gate_w2[l]).astype(jnp.float32)
                                   + gla_gate_b[l].astype(jnp.float32)) / GATE_TEMP
        o_b = gla_chunked(qb.reshape(bsz, s_len, B_HEADS, B_KEY_DIM).astype(jnp.float32),
                          kb.reshape(bsz, s_len, B_HEADS, B_KEY_DIM).astype(jnp.float32),
                          vb.reshape(bsz, s_len, B_HEADS, B_VAL_DIM).astype(jnp.float32),
                          log_a.reshape(bsz, s_len, B_HEADS, B_KEY_DIM))
        o_b = rmsnorm(o_b, gla_out_norm_g[l]).reshape(bsz, s_len, B_WIDTH).astype(x.dtype) * jax.nn.silu(r_gate)

        x = x + jnp.concatenate([o_a, o_b], axis=-1) @ w_out[l]

        x = x + hier_moe(rmsnorm(x, norm2_g[l]), w_router_group[l], b_router_group[l],
                         w_router_expert[l], b_router_expert[l],
                         w_exp_gate[l], w_exp_up[l], w_exp_down[l])
    return x
```

```python
import os
import numpy as np
import ml_dtypes
from contextlib import ExitStack
import concourse.bass as bass
import concourse.mybir as mybir
from concourse.bass_utils import run_bass_kernel_spmd

F32 = mybir.dt.float32
BF16 = mybir.dt.bfloat16
U8 = mybir.dt.uint8
AF = mybir.ActivationFunctionType
ALU = mybir.AluOpType
AX = mybir.AxisListType

P = 128
S = 2048
D = 1024
NT = 16
NE = 32
EPS = 1e-6
KITER = 18
DEBUG = {}
STOP_AFTER = None


class _Op:
    __slots__ = ("eng", "fn", "reads", "writes", "dma", "deps", "waits", "signal", "idx", "flag", "slotwait", "after")

    def __init__(self, eng, fn, reads, writes, dma):
        self.eng = eng
        self.fn = fn
        self.reads = reads
        self.writes = writes
        self.dma = dma
        self.deps = ()
        self.waits = []
        self.signal = None
        self.flag = False
        self.slotwait = None
        self.after = []


def _nofn(e):
    return None


class Sched:
    EPOCH = 12000
    RING = 8

    def __init__(self, nc, stack):
        self.nc = nc
        self.stack = stack
        self.ops = []

    def add(self, eng, fn, reads=(), writes=(), dma=False):
        reads = list(reads)
        writes = list(writes)
        for k in reads:
            if isinstance(k, tuple) and k and k[0] == "ps" and k not in writes:
                writes.append(k)
        self.ops.append(_Op(eng, fn, tuple(reads), tuple(writes), dma))

    def barrier(self):
        pos = getattr(self, "_barpos", 0)
        last = {}
        dmas = []
        for i, op in enumerate(self.ops):
            if i < pos:
                continue
            if op.dma:
                dmas.append(op)
            elif op.fn is not _nofn:
                last[op.eng] = op
        for eng in ("pe", "act", "dve", "pool", "sp"):
            b = _Op(eng, _nofn, (), (), False)
            b.after = [p for k, p in last.items() if k != eng] + dmas
            self.ops.append(b)
        self._barpos = len(self.ops)

    def finalize(self):
        nc = self.nc
        last_w = {}
        readers = {}
        for i, op in enumerate(self.ops):
            op.idx = i
            raw = set()
            other = set()
            for k in op.reads:
                if k in last_w:
                    raw.add(last_w[k])
            for k in op.writes:
                if k in last_w:
                    raw.add(last_w[k])
                for r in readers.get(k, ()):
                    other.add(r)
            raw.discard(i)
            other.discard(i)
            for k in op.reads:
                readers.setdefault(k, set()).add(i)
            for k in op.writes:
                last_w[k] = i
                readers[k] = set()
            best = {}
            deps = []
            for d in raw | other:
                p = self.ops[d]
                if p.dma:
                    deps.append(d)
                    continue
                if p.eng == op.eng and not op.dma:
                    if p.eng == "pe":
                        continue
                    if d not in raw:
                        continue
                if p.eng not in best or best[p.eng] < d:
                    best[p.eng] = d
            deps.extend(best.values())
            for a in op.after:
                deps.append(a.idx)
            op.deps = deps
            for d in deps:
                self.ops[d].flag = True
        cnt = {}
        self.sems = {}
        dcount = {}
        for op in self.ops:
            if op.dma:
                q = op.eng
                k = dcount.get(q, 0)
                dcount[q] = k + 1
                slot = k % self.RING
                name = "dq_%s_%d" % (q, slot)
                if name not in self.sems:
                    self.sems[name] = self.stack.enter_context(nc.semaphore(name))
                op.signal = (name, 16 * (k // self.RING + 1), 16)
                if k >= self.RING:
                    op.slotwait = (name, 16 * (k // self.RING))
            elif op.flag:
                c = cnt.get(op.eng, 0)
                ep = c // self.EPOCH
                name = "s_%s_%d" % (op.eng, ep)
                if name not in self.sems:
                    self.sems[name] = self.stack.enter_context(nc.semaphore(name))
                op.signal = (name, c % self.EPOCH + 1, 1)
                cnt[op.eng] = c + 1
        for op in self.ops:
            w = []
            if op.slotwait is not None:
                w.append(op.slotwait)
            for d in op.deps:
                sg = self.ops[d].signal
                w.append((sg[0], sg[1]))
            op.waits = w

    def emit(self, block):
        table = [("pe", block.tensor), ("act", block.scalar), ("dve", block.vector),
                 ("pool", block.gpsimd), ("sp", block.sync)]
        for engname, deco in table:
            ops = [op for op in self.ops if op.eng == engname]

            def body(e, ops=ops):
                seen = {}
                for op in ops:
                    for (sn, val) in op.waits:
                        if seen.get(sn, 0) >= val:
                            continue
                        e.wait_ge(self.sems[sn], val)
                        seen[sn] = val
                    ins = op.fn(e)
                    if op.signal is not None and ins is not None:
                        ins.then_inc(self.sems[op.signal[0]], op.signal[2])

            deco(body)


IQ_TILES = [(0, 3), (3, 6), (6, 8)]


def _t5_bucket_np(dist):
    max_exact = 16
    d_f = np.maximum(dist, 1).astype(np.float32)
    large = max_exact + (np.log(d_f / max_exact) / np.log(128 / max_exact) * (32 - max_exact)).astype(np.int32)
    large = np.minimum(large, 31)
    return np.where(dist < max_exact, dist, large)


def _w1_columns():
    A = 512
    off = {}
    o = 0
    for name, n in [("qa", 512), ("ka", 512), ("va", 512), ("iq", 256), ("ik", 32), ("iw", 8),
                    ("qb", 256), ("kb", 256), ("vb", 512), ("glr", 16), ("rg", 512)]:
        off[name] = o
        o += n
    cols = []
    groups = []

    def grp(name, kind, cl):
        groups.append((name, kind, len(cols), len(cl)))
        cols.extend(cl)

    r = lambda name, a, b: list(range(off[name] + a, off[name] + b))
    grp("qbkb", "fm", r("qb", 0, 256) + r("kb", 0, 256) + r("glr", 0, 16))
    grp("vb", "tm", r("vb", 0, 512))
    grp("rg", "tm", r("rg", 0, 512))
    grp("kbiw", "tm", r("kb", 0, 256) + r("iw", 0, 8))
    grp("qa", "fm", r("qa", 0, 512))
    grp("ka", "fm", r("ka", 0, 512))
    iqc = []
    for (h0, h1) in IQ_TILES:
        iqc += r("iq", h0 * 32, h1 * 32)
    grp("iqik", "fm", iqc + r("ik", 0, 32) * 3)
    grp("va", "tm", r("va", 0, 512))
    return np.array(cols, np.int64), groups


W1_COLS, W1_GROUPS = _w1_columns()
NW1 = len(W1_COLS)


def _selT_off(i):
    return sum((16 - ii) * 128 for ii in range(i))


SELT_TOTAL = _selT_off(16)


def host_constants():
    c = {}
    idx = np.arange(128)
    tri = (idx[:, None] <= idx[None, :])
    c["ident_bf"] = np.eye(128, dtype=np.float32).astype(ml_dtypes.bfloat16)
    c["ident_f"] = np.eye(128, dtype=np.float32)
    c["tri_bf"] = tri.astype(np.float32).astype(ml_dtypes.bfloat16)
    c["tri_f"] = tri.astype(np.float32)
    c["after_f"] = (idx[:, None] > idx[None, :]).astype(np.float32)
    c["negmask"] = np.where(idx[None, :] <= idx[:, None], 0.0, -1e30).astype(np.float32)
    bo = np.zeros((128, 128), np.float32)
    bo[:64, :64] = 1.0
    bo[64:, 64:] = 1.0
    c["blockones"] = bo.astype(ml_dtypes.bfloat16)
    c["pow2"] = np.tile((2.0 ** -np.arange(KITER + 1, dtype=np.float64)).astype(np.float32)[None, :], (128, 1))
    oh = np.zeros((32, 32, 128), np.float32)
    for e in range(32):
        oh[e, e, :] = 1.0
    c["onehot"] = oh.astype(ml_dtypes.bfloat16)
    return c


def build_program():
    nc = bass.Bass("TRN2", target_bir_lowering=False)
    stack = ExitStack()
    sch = Sched(nc, stack)

    def dram(name, shape, dt, kind="ExternalInput"):
        return nc.dram_tensor(name, list(shape), dt, kind=kind).ap()

    x_d = dram("x", [S, D], F32)
    w1_d = dram("w1", [D, NW1], F32)
    wout_d = dram("wout", [D, D], F32)
    wr_d = dram("wr", [D, 36], F32)
    wg_d = dram("wg", [NE, D, 256], F32)
    wu_d = dram("wu", [NE, D, 256], F32)
    wd_d = dram("wd", [NE, 256, D], F32)
    w2_d = dram("w2aug", [17, 256], F32)
    g1bc_d = dram("g1bc", [P, D], F32)
    g2bc_d = dram("g2bc", [P, D], F32)
    qkg_d = dram("qkg", [P, 2], F32)
    gout_d = dram("goutbc", [P, 512], F32)
    brbc_d = dram("brbc", [P, 36], F32)
    biasT_d = dram("biasT", [P, 8 * 2 * 128], F32)
    rb31_d = dram("rb31", [P, 8], F32)
    ident_bf_d = dram("ident_bf", [P, P], BF16)
    ident_f_d = dram("ident_f", [P, P], F32)
    tri_bf_d = dram("tri_bf", [P, P], BF16)
    tri_f_d = dram("tri_f", [P, P], F32)
    after_f_d = dram("after_f", [P, P], F32)
    negmask_d = dram("negmask", [P, P], F32)
    blockones_d = dram("blockones", [P, P], BF16)
    pow2_d = dram("pow2", [P, KITER + 1], F32)
    onehot_d = dram("onehot", [32, 32 * 128], BF16)
    out_d = dram("out", [S, D], F32, kind="ExternalOutput")
    dbg = {}
    for name, (shape, dt) in DEBUG.items():
        dbg[name] = dram("dbg_" + name, shape, dt, kind="ExternalOutput")

    def sb(name, shape, dt):
        return stack.enter_context(nc.sbuf_tensor(name, list(shape), dt))

    ident_bf = sb("ident_bf_s", [P, P], BF16)
    ident_f = sb("ident_f_s", [P, P], F32)
    tri_bf = sb("tri_bf_s", [P, P], BF16)
    tri_f = sb("tri_f_s", [P, P], F32)
    after_f = sb("after_f_s", [P, P], F32)
    negmask = sb("negmask_s", [P, P], F32)
    blockones = sb("blockones_s", [P, P], BF16)
    pow2 = sb("pow2_s", [P, KITER + 1], F32)
    g1bc = sb("g1bc_s", [P, D], F32)
    qkg = sb("qkg_s", [P, 2], F32)
    gout = sb("gout_s", [P, 512], F32)
    brbc = sb("brbc_s", [P, 36], F32)
    rb31 = sb("rb31_s", [P, 8], F32)
    Etile = sb("Etile", [P, 8, 2, 128], BF16)
    w2aug = sb("w2aug_s", [17, 256], BF16)
    iw_s = sb("iw_s", [P, NT, 8], F32)
    ssq1 = sb("ssq1", [P, NT], F32)
    rstd1 = sb("rstd1", [P, NT], F32)
    ones_col = sb("ones_col", [P, 1], F32)

    R1 = sb("R1", [P, 32 * 1024], U8)
    R2 = sb("R2", [P, 16 * 1024], U8)
    R3 = sb("R3", [P, 16 * 1024], U8)
    R4 = sb("R4", [P, 112 * 1024], U8)

    def view(arena, off, shape, dt, parts=P):
        nb = int(np.prod(shape[1:])) * (4 if dt == F32 else (2 if dt == BF16 else 1))
        ap = arena[0:parts, off:off + nb].bitcast(dt)
        if len(shape) == 3:
            ap = ap.rearrange("p (a b) -> p a b", a=shape[1])
        elif len(shape) == 4:
            ap = ap.rearrange("p (a b c) -> p a b c", a=shape[1], b=shape[2])
        return ap

    KB = 1024
    hT = view(R1, 0, [P, 8, S], BF16)
    mixT_b = view(R2, 0, [P, 4, S], BF16)
    mixT_a = view(R3, 0, [P, 4, S], BF16)
    qbT = view(R4, 0, [P, 2, S], BF16)
    kbT = view(R4, 8 * KB, [P, 2, S], BF16)
    glrT = view(R4, 16 * KB, [32, S], BF16, parts=32)
    vb = view(R4, 20 * KB, [P, NT, 512], BF16)
    Gt = view(R4, 36 * KB, [P, NT, 512], BF16)
    kbtm = view(R4, 52 * KB, [P, NT, 256], BF16)
    wst = [view(R4, 60 * KB + i * 9 * KB, [P, 8, 528], BF16) for i in range(2)]
    xst = [view(R4, 78 * KB + i * 4 * KB, [P, D], F32) for i in range(2)]
    xs = [view(R4, 86 * KB + i * 2 * KB, [P, D], BF16) for i in range(2)]
    junk = view(R4, 90 * KB, [P, 2048], BF16)
    tmpA = [view(R4, 94 * KB + i * 2 * KB, [P, 512], F32) for i in range(4)]
    glatmp = view(R4, 102 * KB, [P, 10 * 256], F32)

    ps = [stack.enter_context(nc.psum_tensor("ps%d" % i, [P, 512], F32)) for i in range(8)]

    def psk(i):
        return ("ps", i)

    def load_const(dst, src, key, eng="sp"):
        sch.add(eng, lambda e, dst=dst, src=src: e.dma_start(out=dst, in_=src), writes=[key], dma=True)

    load_const(ident_bf[:], ident_bf_d[:, :], "ident_bf")
    load_const(ident_f[:], ident_f_d[:, :], "ident_f")
    load_const(tri_bf[:], tri_bf_d[:, :], "tri_bf")
    load_const(tri_f[:], tri_f_d[:, :], "tri_f")
    load_const(after_f[:], after_f_d[:, :], "after_f")
    load_const(negmask[:], negmask_d[:, :], "negmask")
    load_const(blockones[:], blockones_d[:, :], "blockones")
    load_const(pow2[:], pow2_d[:, :], "pow2")
    load_const(g1bc[:], g1bc_d[:, :], "g1bc")
    load_const(qkg[:], qkg_d[:, :], "qkg")
    load_const(gout[:], gout_d[:, :], "gout")
    load_const(brbc[:], brbc_d[:, :], "brbc")
    load_const(rb31[:], rb31_d[:, :], "rb31")
    load_const(w2aug[:], w2_d[:, :], "w2aug", eng="pool")
    sch.add("dve", lambda e: e.memset(ones_col[:], 1.0), writes=["ones_col"])
    sch.add("dve", lambda e: e.memset(glrT[0:32, :], 1.0), writes=["glrT_init"])

    def x_tile(T):
        b = T % 2
        tsl = slice(T * P, (T + 1) * P)
        sch.add("sp", lambda e, b=b, tsl=tsl: e.dma_start(out=xst[b], in_=x_d[tsl, :]),
                writes=[("xst", b)], dma=True)
        sch.add("act", lambda e, b=b, T=T: e.activation(out=junk[:, 0:D], in_=xst[b], func=AF.Square,
                                                        accum_out=ssq1[:, T:T + 1]),
                reads=[("xst", b)], writes=["junk", ("ssq1", T)])
        sch.add("act", lambda e, T=T: e.activation(out=rstd1[:, T:T + 1], in_=ssq1[:, T:T + 1], func=AF.Sqrt,
                                                   scale=1.0 / D, bias=eps_col[:, 0:1]),
                reads=[("ssq1", T), "eps_col"], writes=[("rstd1", T)])
        sch.add("dve", lambda e, T=T: e.reciprocal(out=rstd1[:, T:T + 1], in_=rstd1[:, T:T + 1]),
                reads=[("rstd1", T)], writes=[("rstd1", T)])
        sch.add("dve", lambda e, b=b, T=T: e.scalar_tensor_tensor(out=xs[b], in0=xst[b], scalar=rstd1[:, T:T + 1],
                                                                  in1=g1bc[:], op0=ALU.mult, op1=ALU.mult),
                reads=[("xst", b), ("rstd1", T), "g1bc"], writes=[("xs", b)])
        pb = T % 2

        def tr(e, b=b, pb=pb):
            o = ps[pb][:].bitcast(BF16).rearrange("p (c t) -> p c t", c=8)
            ins = None
            for c in range(8):
                ins = e.transpose(o[:, c, :], xs[b][:, c * P:(c + 1) * P], ident_bf[:])
            return ins
        sch.add("pe", tr, reads=[("xs", b), "ident_bf"], writes=[psk(pb)])
        sch.add("act", lambda e, pb=pb, tsl=tsl: e.activation(
            out=hT[:, :, tsl], in_=ps[pb][:].bitcast(BF16).rearrange("p (c t) -> p c t", c=8), func=AF.Copy),
            reads=[psk(pb)], writes=[("hT", T)])


    wcount = [0]

    def load_wgroup(gi):
        name, kind, c0, n = W1_GROUPS[gi]
        b = wcount[0] % 2
        wcount[0] += 1
        src = w1_d[:, c0:c0 + n].rearrange("(c p) n -> p c n", p=P)
        dst = wst[b][:, :, 0:n]
        sch.add("pool", lambda e, dst=dst, src=src: e.dma_start(out=dst, in_=src),
                writes=[("wst", b)], dma=True)
        return b

    bankrot = {}

    def next_bank(lo=2, hi=6):
        k = (lo, hi)
        c = bankrot.get(k, 0)
        bankrot[k] = c + 1
        return lo + c % (hi - lo)

    def fm_matmul(wb, col0, m, tc, bank, parts=None):
        wt = wst[wb]

        def fn(e):
            ins = None
            for c in range(8):
                ins = e.matmul(ps[bank][0:m, :], lhsT=wt[:, c, col0:col0 + m],
                               rhs=hT[:, c, tc * 512:(tc + 1) * 512], start=(c == 0), stop=(c == 7))
            return ins
        sch.add("pe", fn, reads=[("wst", wb)] + [("hT", tc * 4 + k) for k in range(4)], writes=[psk(bank)])

    def tm_matmul(wb, col0, n, T, bank):
        wt = wst[wb]

        def fn(e):
            ins = None
            for c in range(8):
                ins = e.matmul(ps[bank][:, 0:n], lhsT=hT[:, c, T * P:(T + 1) * P],
                               rhs=wt[:, c, col0:col0 + n], start=(c == 0), stop=(c == 7))
            return ins
        sch.add("pe", fn, reads=[("wst", wb), ("hT", T)], writes=[psk(bank)])

    eps_col = sb("eps_col", [P, 1], F32)
    sch.ops.insert(0, _Op("dve", lambda e: e.memset(eps_col[:], EPS), (), ("eps_col",), False))

    wb = load_wgroup(0)
    for tc in range(4):
        for T_ in range(4 * tc, 4 * tc + 4):
            x_tile(T_)
        tsl = slice(tc * 512, (tc + 1) * 512)
        for k in range(4):
            bank = next_bank()
            fm_matmul(wb, k * 128, 128, tc, bank)
            dst = (qbT if k < 2 else kbT)[:, k % 2, tsl]
            key = ("qbT" if k < 2 else "kbT", tc)
            eng = "act" if k % 2 == 0 else "dve"
            if eng == "act":
                sch.add("act", lambda e, dst=dst, bank=bank: e.activation(out=dst, in_=ps[bank][:], func=AF.Copy),
                        reads=[psk(bank)], writes=[key + (k % 2,)])
            else:
                sch.add("dve", lambda e, dst=dst, bank=bank: e.tensor_copy(out=dst, in_=ps[bank][:]),
                        reads=[psk(bank)], writes=[key + (k % 2,)])
        bank = next_bank()
        fm_matmul(wb, 512, 16, tc, bank)
        sch.add("dve", lambda e, tsl=tsl, bank=bank: e.tensor_copy(out=glrT[0:16, tsl], in_=ps[bank][0:16, :]),
                reads=[psk(bank), "glrT_init"], writes=[("glrT", tc)])
    wb = load_wgroup(1)
    for T in range(NT):
        bank = next_bank()
        tm_matmul(wb, 0, 512, T, bank)
        eng = "act" if T % 2 == 0 else "dve"
        if eng == "act":
            sch.add("act", lambda e, T=T, bank=bank: e.activation(out=vb[:, T, :], in_=ps[bank][:], func=AF.Copy),
                    reads=[psk(bank)], writes=[("vb", T)])
        else:
            sch.add("dve", lambda e, T=T, bank=bank: e.tensor_copy(out=vb[:, T, :], in_=ps[bank][:]),
                    reads=[psk(bank)], writes=[("vb", T)])
    wb = load_wgroup(2)
    for T in range(NT):
        bank = next_bank()
        tm_matmul(wb, 0, 512, T, bank)
        tb = T % 2
        sch.add("act", lambda e, tb=tb, bank=bank: e.activation(out=tmpA[tb][:], in_=ps[bank][:], func=AF.Silu),
                reads=[psk(bank)], writes=[("tmpA", tb)])
        sch.add("pool", lambda e, tb=tb, T=T: e.tensor_tensor(out=Gt[:, T, :], in0=tmpA[tb][:], in1=gout[:], op=ALU.mult),
                reads=[("tmpA", tb), "gout"], writes=[("Gt", T)])
    wb = load_wgroup(3)
    for T in range(NT):
        bank = next_bank()
        tm_matmul(wb, 0, 264, T, bank)
        sch.add("dve", lambda e, T=T, bank=bank: e.tensor_copy(out=kbtm[:, T, :], in_=ps[bank][:, 0:256]),
                reads=[psk(bank)], writes=[("kbtm", T)])
        sch.add("dve", lambda e, T=T, bank=bank: e.tensor_copy(out=iw_s[:, T, :], in_=ps[bank][:, 256:264]),
                reads=[psk(bank)], writes=[("iw", T)])

    def dump(name, src_ap, dst_ap, reads):
        if name in dbg:
            sch.add("sp", lambda e: e.dma_start(out=dst_ap, in_=src_ap), reads=reads, writes=["dbg_" + name], dma=True)

    def finish():
        import os
        if os.environ.get("KSTOP_OPS"):
            print("total ops", len(sch.ops))
            sch.ops = sch.ops[:int(os.environ["KSTOP_OPS"])]
        outs = ["dbg_" + n for n in dbg] + ["out_%d" % T for T in range(NT)]
        sch.add("sp", lambda e: None, reads=outs)
        sch.finalize()
        with nc.Block() as block:
            sch.emit(block)
        stack.close()
        return nc

    dump("hT", hT, dbg.get("hT").rearrange("(c p) t -> p c t", p=P) if "hT" in dbg else None, [("hT", T) for T in range(NT)])
    dump("qbT", qbT[:, 0, :], dbg.get("qbT"), [("qbT", tc, 0) for tc in range(4)])
    dump("vb", vb, dbg.get("vb").rearrange("(t p) c -> p t c", p=P) if "vb" in dbg else None, [("vb", T) for T in range(NT)])
    dump("Gt", Gt, dbg.get("Gt").rearrange("(t p) c -> p t c", p=P) if "Gt" in dbg else None, [("Gt", T) for T in range(NT)])
    if STOP_AFTER == "p1":
        return finish()

    Sst = sb("Sst", [P, 4, 128], F32)
    Sbf = sb("Sbf", [P, 4, 128], BF16)
    g_e1 = sb("g_e1", [P, 256], F32)
    g_l = sb("g_l", [P, 256], F32)
    g_eb = [sb("g_eb%d" % i, [P, 2, 128], F32) for i in range(2)]
    g_einv = sb("g_einv", [P, 2, 128], F32)
    g_erem = sb("g_erem", [P, 256], F32)
    g_qt = [sb("g_qt%d" % i, [P, 2, 128], BF16) for i in range(2)]
    g_kt = sb("g_kt", [P, 2, 128], BF16)
    g_kh = sb("g_kh", [P, 256], BF16)
    g_A = [sb("g_A%d" % i, [P, 4, 128], BF16) for i in range(2)]
    g_ob = sb("g_ob", [P, 512], BF16)
    g_ssq = sb("g_ssq", [P, 4], F32)
    g_rs = sb("g_rs", [P, 4], F32)
    sch.add("dve", lambda e: e.memset(Sst[:], 0.0), writes=[("Sst", h) for h in range(4)])
    sch.add("dve", lambda e: e.memset(Sbf[:], 0.0), writes=[("Sbf", h) for h in range(4)])
    BZ, BC, BA, BO, BT = 0, 1, 3, 4, 6
    BUS = [5, 2]
    print("ops before GLA", len(sch.ops))

    def gla_stage1(T):
        b = T % 2
        tsl = slice(T * P, (T + 1) * P)
        eb, qt, A, BU = g_eb[b], g_qt[b], g_A[b], BUS[b]
        sch.add("pe", lambda e: e.matmul(ps[BZ][:, 0:256], lhsT=glrT[0:17, tsl], rhs=w2aug[0:17, :], start=True, stop=True),
                reads=[("glrT", T // 4), "glrT_init", "w2aug"], writes=[psk(BZ)])
        sch.add("act", lambda e: e.activation(out=g_e1[:], in_=ps[BZ][:, 0:256], func=AF.Exp, scale=-1.0),
                reads=[psk(BZ)], writes=["g_e1"])
        sch.add("act", lambda e: e.activation(out=g_l[:], in_=g_e1[:], func=AF.Ln, scale=1.0, bias=ones_col[:, 0:1]),
                reads=["g_e1", "ones_col"], writes=["g_l"])

        def cum(e):
            e.matmul(ps[BC][:, 0:128], lhsT=g_l[:, 0:128], rhs=tri_f[:], start=True, stop=True)
            return e.matmul(ps[BC][:, 128:256], lhsT=g_l[:, 128:256], rhs=tri_f[:], start=True, stop=True)
        sch.add("pe", cum, reads=["g_l", "tri_f"], writes=[psk(BC)])
        sch.add("pe", lambda e: e.matmul(ps[BZ][:, 256:512], lhsT=after_f[:], rhs=g_l[:], start=True, stop=True),
                reads=["g_l", "after_f"], writes=[psk(BZ)])
        cview = ps[BC][:, 0:256].rearrange("p (a b) -> p a b", a=2)
        sch.add("act", lambda e: e.activation(out=eb[:], in_=cview, func=AF.Exp, scale=-1.0 / 16),
                reads=[psk(BC)], writes=[("g_eb", b)])
        sch.add("act", lambda e: e.activation(out=g_einv[:], in_=cview, func=AF.Exp, scale=1.0 / 16),
                reads=[psk(BC)], writes=["g_einv"])
        sch.add("act", lambda e: e.activation(out=g_erem[:], in_=ps[BZ][:, 256:512], func=AF.Exp, scale=-1.0 / 16),
                reads=[psk(BZ)], writes=["g_erem"])
        sch.add("dve", lambda e: e.scalar_tensor_tensor(out=qt[:], in0=qbT[:, :, tsl], scalar=0.125, in1=eb[:],
                                                        op0=ALU.mult, op1=ALU.mult),
                reads=[("qbT", T // 4, 0), ("qbT", T // 4, 1), ("g_eb", b)], writes=[("g_qt", b)])
        sch.add("dve", lambda e: e.scalar_tensor_tensor(out=g_kt[:], in0=kbT[:, :, tsl], scalar=1.0, in1=g_einv[:],
                                                        op0=ALU.mult, op1=ALU.mult),
                reads=[("kbT", T // 4, 0), ("kbT", T // 4, 1), "g_einv"], writes=["g_kt"])
        sch.add("dve", lambda e: e.scalar_tensor_tensor(out=g_kh[:], in0=kbtm[:, T, :], scalar=1.0, in1=g_erem[:],
                                                        op0=ALU.mult, op1=ALU.mult),
                reads=[("kbtm", T), "g_erem"], writes=["g_kh"])

        def attn(e):
            ins = None
            for h in (0, 2, 1, 3):
                p, r = h // 2, h % 2
                rs = slice(r * 64, (r + 1) * 64)
                bk = BA if r == 0 else 7
                ins = e.matmul(ps[bk][:, p * 128:(p + 1) * 128], lhsT=g_kt[rs, p, :], rhs=qt[rs, p, :], start=True, stop=True)
            return ins
        sch.add("pe", attn, reads=["g_kt", ("g_qt", b)], writes=[psk(BA), psk(7)])
        for r in range(2):
            bk = BA if r == 0 else 7
            sch.add("dve", lambda e, r=r, bk=bk: e.tensor_tensor(
                out=A[:, r::2, :], in0=ps[bk][:, 0:256].rearrange("p (h t) -> p h t", h=2),
                in1=tri_bf[:, :].unsqueeze(1).to_broadcast([P, 2, 128]), op=ALU.mult),
                reads=[psk(bk), "tri_bf"], writes=[("g_A", b, r)])

        def umm(e):
            ins = None
            for h in range(4):
                p = h // 2
                ins = e.matmul(ps[BU][:, h * 128:(h + 1) * 128], lhsT=g_kh[:, p * 128:(p + 1) * 128], rhs=vb[:, T, h * 128:(h + 1) * 128],
                               start=True, stop=True)
            return ins
        sch.add("pe", umm, reads=["g_kh", ("vb", T)], writes=[psk(BU)])

    def gla_stage2(T):
        b = T % 2
        tsl = slice(T * P, (T + 1) * P)
        eb, qt, A, BU = g_eb[b], g_qt[b], g_A[b], BUS[b]

        def omm(e):
            ins = None
            for h in range(4):
                p = h // 2
                e.matmul(ps[BO][:, h * 128:(h + 1) * 128], lhsT=A[:, h, :], rhs=vb[:, T, h * 128:(h + 1) * 128], start=True, stop=False)
                ins = e.matmul(ps[BO][:, h * 128:(h + 1) * 128], lhsT=qt[:, p, :], rhs=Sbf[:, h, :], start=False, stop=True)
            return ins
        sch.add("pe", omm, reads=[("g_A", b, 0), ("g_A", b, 1), ("vb", T), ("g_qt", b)] + [("Sbf", h) for h in range(4)], writes=[psk(BO)])
        for h in range(4):
            p, r = h // 2, h % 2
            rs = slice(r * 64, (r + 1) * 64)
            sch.add("dve", lambda e, h=h, p=p, rs=rs: e.scalar_tensor_tensor(
                out=Sst[rs, h, :], in0=Sst[rs, h, :], scalar=eb[rs, p, 127:128], in1=ps[BU][rs, h * 128:(h + 1) * 128],
                op0=ALU.mult, op1=ALU.add), reads=[psk(BU), ("g_eb", b), ("Sst", h)], writes=[("Sst", h), psk(BU)])
            sch.add("act", lambda e, h=h, rs=rs: e.activation(out=Sbf[rs, h, :], in_=Sst[rs, h, :], func=AF.Copy),
                    reads=[("Sst", h)], writes=[("Sbf", h)])
        for h in range(4):
            sch.add("act", lambda e, h=h: e.activation(out=junk[:, 0:128], in_=ps[BO][:, h * 128:(h + 1) * 128], func=AF.Square,
                                                       accum_out=g_ssq[:, h:h + 1]),
                    reads=[psk(BO)], writes=["junk", ("g_ssq", h), psk(BO)])
        sch.add("act", lambda e: e.activation(out=g_rs[:], in_=g_ssq[:], func=AF.Sqrt, scale=1.0 / 128, bias=eps_col[:, 0:1]),
                reads=[("g_ssq", h) for h in range(4)] + ["eps_col"], writes=["g_rs"])
        sch.add("dve", lambda e: e.reciprocal(out=g_rs[:], in_=g_rs[:]), reads=["g_rs"], writes=["g_rs"])
        for h in range(4):
            hs = slice(h * 128, (h + 1) * 128)
            sch.add("dve", lambda e, h=h, hs=hs: e.scalar_tensor_tensor(
                out=g_ob[:, hs], in0=ps[BO][:, hs], scalar=g_rs[:, h:h + 1], in1=Gt[:, T, hs], op0=ALU.mult, op1=ALU.mult),
                reads=[psk(BO), "g_rs", ("Gt", T)], writes=[("g_ob", h), psk(BO)])
        if "ob" in dbg:
            sch.add("sp", lambda e: e.dma_start(out=dbg["ob"][tsl, :], in_=g_ob[:]), reads=[("g_ob", h) for h in range(4)],
                    writes=["dbg_ob"], dma=True)

        def trb(e):
            o = ps[BT][:].bitcast(BF16).rearrange("p (c t) -> p c t", c=8)
            ins = None
            for c in range(4):
                ins = e.transpose(o[:, c, :], g_ob[:, c * P:(c + 1) * P], ident_bf[:])
            return ins
        sch.add("pe", trb, reads=[("g_ob", h) for h in range(4)] + ["ident_bf"], writes=[psk(BT)])
        sch.add("act", lambda e: e.activation(
            out=mixT_b[:, :, tsl], in_=ps[BT][:].bitcast(BF16).rearrange("p (c t) -> p c t", c=8)[:, 0:4, :], func=AF.Copy),
            reads=[psk(BT)], writes=[("mixT_b", T)])

    gla_stage1(0)
    for T in range(NT):
        if T + 1 < NT:
            gla_stage1(T + 1)
        gla_stage2(T)
    if STOP_AFTER == "gla":
        return finish()
    sch.barrier()

    qaT = view(R4, 0, [P, 4, S], BF16)
    kaT = view(R4, 16 * KB, [P, 4, S], BF16)
    iqT = view(R4, 32 * KB, [P, 3, S], BF16)
    ikT = view(R4, 44 * KB, [P, S], BF16)
    vaT = view(R4, 48 * KB, [P, NT, 8 * 65], BF16)
    wst2 = [view(R4, 66 * KB + i * 9 * KB, [P, 8, 528], BF16) for i in range(2)]
    n_sq = [view(R4, 84 * KB + i * KB, [P, 512], BF16) for i in range(2)]
    n_ln = [view(R4, 86 * KB + i * 2 * KB, [P, 512], F32) for i in range(2)]
    n_rs = [view(R4, 90 * KB + i * 2 * KB, [P, 512], F32) for i in range(2)]
    wst[0], wst[1] = wst2[0], wst2[1]
    sch.add("dve", lambda e: e.memset(vaT[:], 1.0), writes=[("va", T) for T in range(NT)])
    ncnt = [0]
    for gi, which in [(4, 0), (5, 1)]:
        wb = load_wgroup(gi)
        dstT = qaT if which == 0 else kaT
        kname = "qaT" if which == 0 else "kaT"
        for tc in range(4):
            tsl = slice(tc * 512, (tc + 1) * 512)
            for p in range(4):
                bank = next_bank(0, 4)
                sbank = next_bank(4, 8)
                nb = ncnt[0] % 2
                ncnt[0] += 1
                fm_matmul(wb, p * 128, 128, tc, bank)
                sch.add("act", lambda e, nb=nb, bank=bank: e.activation(out=n_sq[nb], in_=ps[bank][:], func=AF.Square),
                        reads=[psk(bank)], writes=[("n_sq", nb), psk(bank)])
                sch.add("pe", lambda e, nb=nb, sbank=sbank: e.matmul(ps[sbank][:], lhsT=blockones[:], rhs=n_sq[nb], start=True, stop=True),
                        reads=[("n_sq", nb), "blockones"], writes=[psk(sbank)])
                sch.add("act", lambda e, nb=nb, sbank=sbank: e.activation(out=n_ln[nb], in_=ps[sbank][:], func=AF.Ln, scale=1.0 / 64,
                                                                          bias=eps_col[:, 0:1]),
                        reads=[psk(sbank), "eps_col"], writes=[("n_ln", nb)])
                sch.add("act", lambda e, nb=nb: e.activation(out=n_rs[nb], in_=n_ln[nb], func=AF.Exp, scale=-0.5),
                        reads=[("n_ln", nb)], writes=[("n_rs", nb)])
                sch.add("dve", lambda e, nb=nb, bank=bank, p=p, tsl=tsl, dstT=dstT, which=which: e.scalar_tensor_tensor(
                    out=dstT[:, p, tsl], in0=ps[bank][:], scalar=qkg[:, which:which + 1], in1=n_rs[nb], op0=ALU.mult, op1=ALU.mult),
                    reads=[psk(bank), ("n_rs", nb), "qkg"], writes=[(kname, p, tc), psk(bank)])
    wb = load_wgroup(6)
    for tc in range(4):
        tsl = slice(tc * 512, (tc + 1) * 512)
        col = 0
        for ti, (h0, h1) in enumerate(IQ_TILES):
            m = (h1 - h0) * 32
            bank = next_bank(0, 8)
            fm_matmul(wb, col, m, tc, bank)
            col += m
            sch.add("act", lambda e, ti=ti, m=m, tsl=tsl, bank=bank: e.activation(out=iqT[0:m, ti, tsl], in_=ps[bank][0:m, :], func=AF.Copy),
                    reads=[psk(bank)], writes=[("iqT", ti, tc)])
        bank = next_bank(0, 8)
        fm_matmul(wb, col, 96, tc, bank)
        sch.add("dve", lambda e, tsl=tsl, bank=bank: e.tensor_copy(out=ikT[0:96, tsl], in_=ps[bank][0:96, :]),
                reads=[psk(bank)], writes=[("ikT", tc)])
    wb = load_wgroup(7)
    for T in range(NT):
        bank = next_bank(0, 8)
        tm_matmul(wb, 0, 512, T, bank)
        dstv = vaT[:, T, :].rearrange("p (h d) -> p h d", h=8)[:, :, 0:64]
        srcv = ps[bank][:].rearrange("p (h d) -> p h d", h=8)
        if T % 2 == 0:
            sch.add("act", lambda e, dstv=dstv, srcv=srcv: e.activation(out=dstv, in_=srcv, func=AF.Copy),
                    reads=[psk(bank)], writes=[("va", T)])
        else:
            sch.add("dve", lambda e, dstv=dstv, srcv=srcv: e.tensor_copy(out=dstv, in_=srcv),
                    reads=[psk(bank)], writes=[("va", T)])
    dump("qaT", qaT[:, 0, :], dbg.get("qaT"), [("qaT", 0, tc) for tc in range(4)])
    dump("kaT", kaT[:, 0, :], dbg.get("kaT"), [("kaT", 0, tc) for tc in range(4)])
    if STOP_AFTER == "p2":
        return finish()
    sch.barrier()

    selT = view(R4, 66 * KB, [P, SELT_TOTAL], BF16)
    selts = [view(R4, 100 * KB + i * 4 * KB, [P, S], BF16) for i in range(2)]
    PTb = [view(R4, 108 * KB + i * KB, [P, 512], BF16) for i in range(4)]
    junk_tk = view(R4, 108 * KB, [P, 2048], BF16)
    score_all = R1[:, :].bitcast(F32)
    thr = sb("thr", [P, NT], F32)
    thr2 = sb("thr2", [P, NT], F32)
    thrB = sb("thrB", [P, NT], F32)
    cnt = sb("cnt", [P, NT], F32)
    sgn = sb("sgn", [P, NT], F32)
    amax = sb("amax", [P, NT], F32)
    mrow = sb("mrow", [P, 1], F32)
    mtab = sb("mtab", [P, KITER + 1], F32)
    biasT_s = view(R4, 100 * KB, [P, 8, 2, 128], F32)
    sch.add("sp", lambda e: e.dma_start(out=biasT_s, in_=biasT_d.rearrange("p (h d t) -> p h d t", h=8, d=2)),
            writes=["biasT"], dma=True)
    sch.add("act", lambda e: e.activation(out=Etile[:], in_=biasT_s, func=AF.Exp), reads=["biasT"], writes=["Etile"])
    for h in range(8):
        sch.add("dve", lambda e, h=h: e.tensor_tensor(out=Etile[:, h, 0, :], in0=Etile[:, h, 0, :], in1=tri_bf[:], op=ALU.mult),
                reads=["Etile", "tri_bf"], writes=["Etile"])
    sch.add("dve", lambda e: e.memset(selts[0][:], 0.0), reads=["Etile"], writes=[("selts", 0)])
    sch.add("dve", lambda e: e.tensor_copy(out=selT[:, _selT_off(0):_selT_off(0) + 128], in_=tri_bf[:]), reads=["tri_bf"], writes=[("selT", 0, 0)])
    sch.add("dve", lambda e: e.memset(selT[:, _selT_off(0) + 128:_selT_off(0) + 256], 1.0), writes=[("selT", 0, 1)])
    sch.add("dve", lambda e: e.tensor_copy(out=selT[:, _selT_off(1):_selT_off(1) + 128], in_=tri_bf[:]), reads=["tri_bf"], writes=[("selT", 1, 1)])

    print("ops before topk loops", len(sch.ops))
    batches = [list(range(2, 8)), list(range(8, 12)), list(range(12, 16))]
    ACT_SHARE = [4, 2, 2]
    sumA = sb("sumA", [P, NT], F32)
    nhalf = sb("nhalf", [P, NT], F32)
    junk_act = view(R3, 0, [P, 2048], BF16)
    for j in range(NT):
        sch.add("dve", lambda e, j=j: e.memset(nhalf[:, j:j + 1], 64.0 * (j + 1)), writes=["nhalf"])
    hd_loc = []
    for ti, (h0, h1) in enumerate(IQ_TILES):
        for k in range(h1 - h0):
            hd_loc.append((ti, k))
    lbank = [0]
    for bi, batch in enumerate(batches):
        soff = {}
        o = 0
        for j in batch:
            soff[j] = o
            o += (j + 1) * 128
        nb = len(batch)
        j0 = batch[0]
        for j in batch:
            n = (j + 1) * 128
            sc_j = score_all[:, soff[j]:soff[j] + n]
            nsc = (n + 511) // 512
            for sc in range(nsc):
                w = min(512, n - sc * 512)
                ssl = slice(sc * 512, sc * 512 + w)
                for h in range(8):
                    ti, k = hd_loc[h]
                    rs = slice(k * 32, (k + 1) * 32)
                    lb = lbank[0] % 4
                    rb = 4 + lbank[0] % 4
                    lbank[0] += 1
                    sch.add("pe", lambda e, lb=lb, rs=rs, ti=ti, j=j, ssl=ssl, w=w: e.matmul(
                        ps[lb][:, 0:w], lhsT=iqT[rs, ti, j * 128:(j + 1) * 128], rhs=ikT[rs, ssl], start=True, stop=True),
                        reads=[("iqT", ti, j // 4)] + [("ikT", c) for c in range(sc * 4 // 4, (sc * 512 + w - 1) // 512 + 1)],
                        writes=[psk(lb)])
                    sch.add("act", lambda e, lb=lb, rb=rb, w=w: e.activation(out=ps[rb][:, 0:w], in_=ps[lb][:, 0:w], func=AF.Relu),
                            reads=[psk(lb)], writes=[psk(rb), psk(lb)])
                    dst = sc_j[:, ssl]
                    if h == 0:
                        sch.add("dve", lambda e, rb=rb, w=w, dst=dst, j=j, h=h: e.tensor_scalar(
                            out=dst, in0=ps[rb][:, 0:w], scalar1=iw_s[:, j, h:h + 1], scalar2=None, op0=ALU.mult),
                            reads=[psk(rb), ("iw", j)], writes=[("score", j, sc), psk(rb)])
                    else:
                        sch.add("dve", lambda e, rb=rb, w=w, dst=dst, j=j, h=h: e.scalar_tensor_tensor(
                            out=dst, in0=ps[rb][:, 0:w], scalar=iw_s[:, j, h:h + 1], in1=dst, op0=ALU.mult, op1=ALU.add),
                            reads=[psk(rb), ("iw", j), ("score", j, sc)], writes=[("score", j, sc), psk(rb)])
            skeys = [("score", j, sc) for sc in range(nsc)]
            sch.add("dve", lambda e, sc_j=sc_j, j=j: e.tensor_reduce(out=amax[:, j:j + 1], in_=sc_j, axis=AX.X, op=ALU.max,
                                                                    apply_absolute_value=True),
                    reads=skeys, writes=[("amax", j)])
            dsl = slice(soff[j] + j * 128, soff[j] + (j + 1) * 128)
            sch.add("dve", lambda e, dsl=dsl: e.tensor_tensor(out=score_all[:, dsl], in0=score_all[:, dsl], in1=negmask[:], op=ALU.add),
                    reads=skeys + ["negmask", ("amax", j)], writes=skeys)
        if "score" in dbg and bi == 1:
            sch.add("sp", lambda e, soff=soff: e.dma_start(out=dbg["score"][:, 0:1280], in_=score_all[:, soff[9]:soff[9] + 1280]),
                    reads=[("score", 9, sc) for sc in range(3)], writes=["dbg_score"], dma=True)
        bsl = slice(j0, j0 + nb)
        sch.add("dve", lambda e, bsl=bsl: e.tensor_reduce(out=mrow[:], in_=amax[:, bsl], axis=AX.X, op=ALU.max),
                reads=[("amax", j) for j in batch], writes=["mrow"])
        sch.add("dve", lambda e: e.tensor_scalar(out=mtab[:], in0=pow2[:], scalar1=mrow[:, 0:1], scalar2=None, op0=ALU.mult),
                reads=["mrow", "pow2"], writes=["mtab"])
        sch.add("dve", lambda e, bsl=bsl: e.memset(thr[:, bsl], 0.0), writes=["thr"])
        nact = ACT_SHARE[bi]
        act_tiles = batch[:nact]
        asl = slice(batch[0], batch[0] + nact)
        cur, ckey = thr, "thr"
        for k in range(KITER):
            for j in batch:
                n = (j + 1) * 128
                sc_j = score_all[:, soff[j]:soff[j] + n]
                skeys_j = [("score", j, sc) for sc in range((n + 511) // 512)]
                if j in act_tiles:
                    sch.add("act", lambda e, sc_j=sc_j, n=n, j=j, cur=cur: e.activation(
                        out=junk_act[:, 0:n], in_=sc_j, func=AF.Sign, scale=-1.0, bias=cur[:, j:j + 1], accum_out=sumA[:, j:j + 1]),
                        reads=skeys_j + [ckey], writes=["junk_act", ("sumA", j)])
                else:
                    sch.add("dve", lambda e, sc_j=sc_j, n=n, j=j, cur=cur: e.tensor_scalar(
                        out=junk_tk[:, 0:n], in0=sc_j, scalar1=cur[:, j:j + 1], scalar2=None, op0=ALU.is_ge, op1=ALU.add,
                        accum_out=cnt[:, j:j + 1]),
                        reads=skeys_j + [ckey], writes=["junk", ("cnt", j)])
            if nact > 0:
                sch.add("dve", lambda e, asl=asl: e.scalar_tensor_tensor(out=cnt[:, asl], in0=sumA[:, asl], scalar=-0.5, in1=nhalf[:, asl],
                                                                        op0=ALU.mult, op1=ALU.add),
                        reads=[("sumA", j) for j in act_tiles] + ["nhalf"], writes=[("cnt", j) for j in act_tiles])
            sch.add("dve", lambda e, bsl=bsl: e.tensor_scalar(out=sgn[:, bsl], in0=cnt[:, bsl], scalar1=256.0, scalar2=0.5,
                                                             op0=ALU.is_ge, op1=ALU.subtract),
                    reads=[("cnt", j) for j in batch], writes=["sgn"])
            nxt, nkey = (thrB, "thrB") if cur is thr else (thr, "thr")
            sch.add("dve", lambda e, bsl=bsl, k=k, cur=cur, nxt=nxt: e.scalar_tensor_tensor(
                out=nxt[:, bsl], in0=sgn[:, bsl], scalar=mtab[:, k:k + 1], in1=cur[:, bsl], op0=ALU.mult, op1=ALU.add),
                reads=["sgn", "mtab", ckey], writes=[nkey])
            cur, ckey = nxt, nkey
        sch.add("dve", lambda e, bsl=bsl, cur=cur: e.tensor_scalar(out=thr2[:, bsl], in0=cur[:, bsl], scalar1=mtab[:, KITER:KITER + 1], scalar2=None,
                                                                  op0=ALU.subtract),
                reads=[ckey, "mtab"], writes=["thr2"])
        if "thr" in dbg and bi == 1:
            sch.add("sp", lambda e: e.dma_start(out=dbg["thr"][:, :], in_=thr2[:]), reads=["thr2"], writes=["dbg_thr"], dma=True)
        for j in batch:
            n = (j + 1) * 128
            sc_j = score_all[:, soff[j]:soff[j] + n]
            sb_ = j % 2
            sch.add("dve", lambda e, sc_j=sc_j, n=n, j=j, sb_=sb_: e.tensor_scalar(
                out=selts[sb_][:, 0:n], in0=sc_j, scalar1=thr2[:, j:j + 1], scalar2=None, op0=ALU.is_ge),
                reads=[("score", j, sc) for sc in range((n + 511) // 512)] + ["thr2"], writes=[("selts", sb_)])
            for i0 in range(0, j + 1, 8):
                i1 = min(j + 1, i0 + 8)
                tb = next_bank(0, 8)

                def trs(e, i0=i0, i1=i1, tb=tb, sb_=sb_):
                    o = ps[tb][:].bitcast(BF16).rearrange("p (c t) -> p c t", c=8)
                    ins = None
                    for i in range(i0, i1):
                        ins = e.transpose(o[:, i - i0, :], selts[sb_][:, i * 128:(i + 1) * 128], ident_bf[:])
                    return ins
                sch.add("pe", trs, reads=[("selts", sb_), "ident_bf"], writes=[psk(tb)])
                for i in range(i0, i1):
                    o = ps[tb][:].bitcast(BF16).rearrange("p (c t) -> p c t", c=8)[:, i - i0, :]
                    off = _selT_off(i) + (j - i) * 128
                    if i % 2 == 0:
                        sch.add("act", lambda e, o=o, off=off: e.activation(out=selT[:, off:off + 128], in_=o, func=AF.Copy),
                                reads=[psk(tb)], writes=[("selT", i, j), psk(tb)])
                    else:
                        sch.add("dve", lambda e, o=o, off=off: e.tensor_copy(out=selT[:, off:off + 128], in_=o),
                                reads=[psk(tb)], writes=[("selT", i, j), psk(tb)])
    dump("selT", selT, dbg.get("selT"), [("selT", i, j) for i in range(16) for j in range(i, 16)])
    if STOP_AFTER == "topk":
        return finish()
    sch.barrier()

    mixa = view(R1, 0, [P, NT, 512], BF16)
    rden = sb("rden", [P, 4], F32)
    abank = [0]
    LOOKAHEAD = 2
    pend = []

    def flush(keep):
        while len(pend) > keep:
            pend.pop(0)()

    for h in range(8):
        p, r = h // 2, h % 2
        rs = slice(r * 64, (r + 1) * 64)
        for J in range(4):
            accb = 6 + (abank[0] % 2)
            abank[0] += 1
            accv = ps[accb][:, 0:260].rearrange("p (j d) -> p j d", j=4)
            first = [True]
            for i in range(4 * J + 4):
                jlo = max(i, 4 * J)
                t0 = jlo * 128
                n = (4 * J + 4 - jlo) * 128
                sbk = next_bank(0, 6)
                pb = next_bank(100, 104) - 100
                sch.add("pe", lambda e, sbk=sbk, rs=rs, p=p, i=i, t0=t0, n=n: e.matmul(
                    ps[sbk][:, 0:n], lhsT=kaT[rs, p, i * 128:(i + 1) * 128], rhs=qaT[rs, p, t0:t0 + n], start=True, stop=True),
                    reads=[("kaT", p, i // 4), ("qaT", p, J)], writes=[psk(sbk)])
                nnear = max(0, min(4 * J + 4, i + 2) - jlo) * 128
                if nnear > 0:
                    sch.add("act", lambda e, sbk=sbk, pb=pb, nnear=nnear: e.activation(out=PTb[pb][:, 0:nnear], in_=ps[sbk][:, 0:nnear],
                                                                                       func=AF.Exp, scale=0.125),
                            reads=[psk(sbk)], writes=[("PT", pb), psk(sbk)])
                if n > nnear:
                    sch.add("act", lambda e, sbk=sbk, pb=pb, nnear=nnear, n=n, h=h: e.activation(
                        out=PTb[pb][:, nnear:n], in_=ps[sbk][:, nnear:n], func=AF.Exp, scale=0.125, bias=rb31[:, h:h + 1]),
                        reads=[psk(sbk), "rb31"], writes=[("PT", pb), psk(sbk)])
                soff_ = _selT_off(i) + (jlo - i) * 128
                sch.add("dve", lambda e, pb=pb, n=n, soff_=soff_: e.tensor_tensor(out=PTb[pb][:, 0:n], in0=PTb[pb][:, 0:n],
                                                                                 in1=selT[:, soff_:soff_ + n], op=ALU.mult),
                        reads=[("PT", pb)] + [("selT", i, j) for j in range(jlo, 4 * J + 4)], writes=[("PT", pb)])
                for j in range(jlo, min(4 * J + 4, i + 2)):
                    dlt = j - i
                    cs = slice((j - jlo) * 128, (j - jlo + 1) * 128)
                    sch.add("dve", lambda e, pb=pb, cs=cs, h=h, dlt=dlt: e.tensor_tensor(out=PTb[pb][:, cs], in0=PTb[pb][:, cs],
                                                                                         in1=Etile[:, h, dlt, :], op=ALU.mult),
                            reads=[("PT", pb), "Etile"], writes=[("PT", pb)])

                def back(pb=pb, jlo=jlo, J=J, i=i, h=h, accv=accv, first=first, accb=accb):
                    def pv(e):
                        ins = None
                        for j in range(jlo, 4 * J + 4):
                            cs = slice((j - jlo) * 128, (j - jlo + 1) * 128)
                            ins = e.matmul(accv[:, j - 4 * J, :], lhsT=PTb[pb][:, cs], rhs=vaT[:, i, h * 65:(h + 1) * 65],
                                           start=first[0], stop=False, skip_group_check=True)
                            first[0] = False
                        return ins
                    sch.add("pe", pv, reads=[("PT", pb), ("va", i)], writes=[psk(accb)])
                    if i == 4 * J + 3:
                        sch.add("dve", lambda e: e.reciprocal(out=rden[:], in_=accv[:, :, 64]), reads=[psk(accb)], writes=["rden", psk(accb)])
                        sch.add("dve", lambda e: e.tensor_tensor(
                            out=mixa[:, 4 * J:4 * J + 4, h * 64:(h + 1) * 64], in0=accv[:, :, 0:64],
                            in1=rden[:, :].unsqueeze(2).to_broadcast([P, 4, 64]), op=ALU.mult),
                            reads=[psk(accb), "rden"], writes=[("mixa", 4 * J + jj, h) for jj in range(4)] + [psk(accb)])
                pend.append(back)
                flush(LOOKAHEAD)
    flush(0)
    dump("mixa", mixa, dbg.get("mixa").rearrange("(t p) c -> p t c", p=P) if "mixa" in dbg else None,
         [("mixa", T, h) for T in range(NT) for h in range(8)])
    for T in range(NT):
        tsl = slice(T * P, (T + 1) * P)
        tb = next_bank(0, 6)

        def tra(e, T=T, tb=tb):
            o = ps[tb][:].bitcast(BF16).rearrange("p (c t) -> p c t", c=8)
            ins = None
            for c in range(4):
                ins = e.transpose(o[:, c, :], mixa[:, T, c * P:(c + 1) * P], ident_bf[:])
            return ins
        sch.add("pe", tra, reads=[("mixa", T, h) for h in range(8)] + ["ident_bf"], writes=[psk(tb)])
        sch.add("act", lambda e, tsl=tsl, tb=tb: e.activation(
            out=mixT_a[:, :, tsl], in_=ps[tb][:].bitcast(BF16).rearrange("p (c t) -> p c t", c=8)[:, 0:4, :], func=AF.Copy),
            reads=[psk(tb)], writes=[("mixT_a", T)])
    if STOP_AFTER == "attn":
        return finish()
    sch.barrier()

    x1 = view(R4, 0, [P, NT, D], F32)
    woutb = view(R4, 64 * KB, [P, 8, D], BF16)
    xst5 = [view(R4, 80 * KB + i * 4 * KB, [P, D], F32) for i in range(2)]
    xs5 = [view(R4, 88 * KB + i * 2 * KB, [P, D], BF16) for i in range(2)]
    junk5 = view(R4, 92 * KB, [P, 2048], BF16)
    g2bc = view(R4, 96 * KB, [P, D], F32)
    wrb = view(R4, 100 * KB, [P, 8, 36], BF16)
    h2T = view(R1, 0, [P, 8, S], BF16)
    ssq2 = sb("ssq2", [P, NT], F32)
    rstd2 = sb("rstd2", [P, NT], F32)
    logit = sb("logit", [P, NT, 36], F32)
    for hh in range(2):
        sch.add("pool", lambda e, hh=hh: e.dma_start(out=woutb[:, :, hh * 512:(hh + 1) * 512],
                                                     in_=wout_d[:, hh * 512:(hh + 1) * 512].rearrange("(c p) n -> p c n", p=P)),
                writes=[("wout", hh)], dma=True)
    sch.add("pool", lambda e: e.dma_start(out=wrb, in_=wr_d.rearrange("(c p) n -> p c n", p=P)), writes=["wrb"], dma=True)
    sch.add("sp", lambda e: e.dma_start(out=g2bc, in_=g2bc_d[:, :]), writes=["g2bc"], dma=True)
    pend5 = []
    for T in range(NT):
        b = T % 2
        tsl = slice(T * P, (T + 1) * P)
        sch.add("sp", lambda e, b=b, tsl=tsl: e.dma_start(out=xst5[b], in_=x_d[tsl, :]), writes=[("xst5", b)], dma=True)
        for hh in range(2):
            bank = next_bank(0, 4)

            def om(e, T=T, hh=hh, bank=bank):
                ins = None
                for c in range(8):
                    src = mixT_a if c < 4 else mixT_b
                    ins = e.matmul(ps[bank][:], lhsT=src[:, c % 4, T * P:(T + 1) * P], rhs=woutb[:, c, hh * 512:(hh + 1) * 512],
                                   start=(c == 0), stop=(c == 7))
                return ins
            sch.add("pe", om, reads=[("mixT_a", T), ("mixT_b", T), ("wout", hh)], writes=[psk(bank)])
            sch.add("dve", lambda e, T=T, hh=hh, bank=bank, b=b: e.tensor_tensor(
                out=x1[:, T, hh * 512:(hh + 1) * 512], in0=ps[bank][:], in1=xst5[b][:, hh * 512:(hh + 1) * 512], op=ALU.add),
                reads=[psk(bank), ("xst5", b)], writes=[("x1", T, hh)])
        sch.add("act", lambda e, T=T: e.activation(out=junk5[:, 0:D], in_=x1[:, T, :], func=AF.Square, accum_out=ssq2[:, T:T + 1]),
                reads=[("x1", T, 0), ("x1", T, 1)], writes=["junk5", ("ssq2", T)])
        sch.add("act", lambda e, T=T: e.activation(out=rstd2[:, T:T + 1], in_=ssq2[:, T:T + 1], func=AF.Sqrt, scale=1.0 / D,
                                                   bias=eps_col[:, 0:1]),
                reads=[("ssq2", T), "eps_col"], writes=[("rstd2", T)])
        sch.add("dve", lambda e, T=T: e.reciprocal(out=rstd2[:, T:T + 1], in_=rstd2[:, T:T + 1]), reads=[("rstd2", T)], writes=[("rstd2", T)])
        sch.add("dve", lambda e, T=T, b=b: e.scalar_tensor_tensor(out=xs5[b], in0=x1[:, T, :], scalar=rstd2[:, T:T + 1], in1=g2bc,
                                                                  op0=ALU.mult, op1=ALU.mult),
                reads=[("x1", T, 0), ("x1", T, 1), ("rstd2", T), "g2bc"], writes=[("xs5", b)])
        def back5(T=T, b=b, tsl=tsl):
            tb = next_bank(4, 6)

            def tr5(e, b=b, tb=tb):
                o = ps[tb][:].bitcast(BF16).rearrange("p (c t) -> p c t", c=8)
                ins = None
                for c in range(8):
                    ins = e.transpose(o[:, c, :], xs5[b][:, c * P:(c + 1) * P], ident_bf[:])
                return ins
            sch.add("pe", tr5, reads=[("xs5", b), "ident_bf"], writes=[psk(tb)])
            sch.add("act", lambda e, tb=tb, tsl=tsl: e.activation(
                out=h2T[:, :, tsl], in_=ps[tb][:].bitcast(BF16).rearrange("p (c t) -> p c t", c=8), func=AF.Copy),
                reads=[psk(tb)], writes=[("h2T", T)])
            lbk = next_bank(6, 8)

            def rmm(e, T=T, lbk=lbk):
                ins = None
                for c in range(8):
                    ins = e.matmul(ps[lbk][:, 0:36], lhsT=h2T[:, c, T * P:(T + 1) * P], rhs=wrb[:, c, :], start=(c == 0), stop=(c == 7))
                return ins
            sch.add("pe", rmm, reads=[("h2T", T), "wrb"], writes=[psk(lbk)])
            sch.add("dve", lambda e, T=T, lbk=lbk: e.tensor_tensor(out=logit[:, T, :], in0=ps[lbk][:, 0:36], in1=brbc[:], op=ALU.add),
                    reads=[psk(lbk), "brbc"], writes=["logit"])
        pend5.append(back5)
        while len(pend5) > 1:
            pend5.pop(0)()
    while pend5:
        pend5.pop(0)()
    dump("x1", x1, dbg.get("x1").rearrange("(t p) c -> p t c", p=P) if "x1" in dbg else None,
         [("x1", T, hh) for T in range(NT) for hh in range(2)])
    if STOP_AFTER == "x1":
        return finish()

    gl = logit[:, :, 0:4]
    el = logit[:, :, 4:36].rearrange("p t (g e) -> p t g e", g=4)
    _ro = [101 * KB]

    def rv(shape):
        nb = int(np.prod(shape[1:])) * 4
        v = view(R4, _ro[0], shape, F32)
        _ro[0] += nb
        return v
    gmax = rv([P, NT])
    goh = rv([P, NT, 4])
    gsh = rv([P, NT, 4])
    gsum = rv([P, NT])
    gw = rv([P, NT])
    etmp = rv([P, NT, 4, 8])
    esel = rv([P, NT, 8])
    esel2 = rv([P, NT, 8])
    m1 = rv([P, NT])
    m2 = rv([P, NT])
    oh1 = rv([P, NT, 8])
    oh2 = rv([P, NT, 8])
    dd = rv([P, NT])
    w1 = rv([P, NT])
    w2 = rv([P, NT])
    gsel = rv([P, NT, 8])
    assert _ro[0] <= 110 * KB
    gates = view(R4, 110 * KB, [P, NT, 4, 8], F32)

    def D_(fn, reads, writes):
        sch.add("dve", fn, reads=reads, writes=writes)

    def bc3(ap2, n):
        return ap2.unsqueeze(2).to_broadcast([P, NT, n])
    D_(lambda e: e.tensor_reduce(out=gmax[:], in_=gl, axis=AX.X, op=ALU.max), ["logit"], ["gmax"])
    D_(lambda e: e.tensor_tensor(out=goh[:], in0=gl, in1=bc3(gmax[:, :], 4), op=ALU.is_equal), ["logit", "gmax"], ["goh"])
    D_(lambda e: e.tensor_tensor(out=gsh[:], in0=gl, in1=bc3(gmax[:, :], 4), op=ALU.subtract), ["logit", "gmax"], ["gsh"])
    sch.add("act", lambda e: e.activation(out=gsh[:], in_=gsh[:], func=AF.Exp), reads=["gsh"], writes=["gsh"])
    D_(lambda e: e.tensor_reduce(out=gsum[:], in_=gsh[:], axis=AX.X, op=ALU.add), ["gsh"], ["gsum"])
    D_(lambda e: e.reciprocal(out=gw[:], in_=gsum[:]), ["gsum"], ["gw"])
    D_(lambda e: e.tensor_tensor(out=etmp[:], in0=el, in1=goh[:, :, :].unsqueeze(3).to_broadcast([P, NT, 4, 8]), op=ALU.mult),
       ["logit", "goh"], ["etmp"])
    D_(lambda e: e.tensor_reduce(out=esel[:], in_=etmp[:, :, :, :].rearrange("p t g e -> p t e g"), axis=AX.X, op=ALU.add), ["etmp"], ["esel"])
    D_(lambda e: e.tensor_reduce(out=m1[:], in_=esel[:], axis=AX.X, op=ALU.max), ["esel"], ["m1"])
    D_(lambda e: e.tensor_tensor(out=oh1[:], in0=esel[:], in1=bc3(m1[:, :], 8), op=ALU.is_equal), ["esel", "m1"], ["oh1"])
    D_(lambda e: e.scalar_tensor_tensor(out=esel2[:], in0=oh1[:], scalar=-1e30, in1=esel[:], op0=ALU.mult, op1=ALU.add), ["oh1", "esel"], ["esel2"])
    D_(lambda e: e.tensor_reduce(out=m2[:], in_=esel2[:], axis=AX.X, op=ALU.max), ["esel2"], ["m2"])
    D_(lambda e: e.tensor_tensor(out=oh2[:], in0=esel2[:], in1=bc3(m2[:, :], 8), op=ALU.is_equal), ["esel2", "m2"], ["oh2"])
    D_(lambda e: e.tensor_tensor(out=dd[:], in0=m2[:], in1=m1[:], op=ALU.subtract), ["m1", "m2"], ["dd"])
    sch.add("act", lambda e: e.activation(out=dd[:], in_=dd[:], func=AF.Exp), reads=["dd"], writes=["dd"])
    D_(lambda e: e.tensor_scalar(out=w1[:], in0=dd[:], scalar1=1.0, scalar2=None, op0=ALU.add), ["dd"], ["w1"])
    D_(lambda e: e.reciprocal(out=w1[:], in_=w1[:]), ["w1"], ["w1"])
    D_(lambda e: e.tensor_tensor(out=w2[:], in0=dd[:], in1=w1[:], op=ALU.mult), ["dd", "w1"], ["w2"])
    D_(lambda e: e.tensor_tensor(out=w1[:], in0=w1[:], in1=gw[:], op=ALU.mult), ["w1", "gw"], ["w1"])
    D_(lambda e: e.tensor_tensor(out=w2[:], in0=w2[:], in1=gw[:], op=ALU.mult), ["w2", "gw"], ["w2"])
    D_(lambda e: e.tensor_tensor(out=oh1[:], in0=oh1[:], in1=bc3(w1[:, :], 8), op=ALU.mult), ["oh1", "w1"], ["oh1"])
    D_(lambda e: e.tensor_tensor(out=oh2[:], in0=oh2[:], in1=bc3(w2[:, :], 8), op=ALU.mult), ["oh2", "w2"], ["oh2"])
    D_(lambda e: e.tensor_tensor(out=gsel[:], in0=oh1[:], in1=oh2[:], op=ALU.add), ["oh1", "oh2"], ["gsel"])
    D_(lambda e: e.tensor_tensor(out=gates[:], in0=goh[:, :, :].unsqueeze(3).to_broadcast([P, NT, 4, 8]),
                                 in1=gsel[:, :, :].unsqueeze(2).to_broadcast([P, NT, 4, 8]), op=ALU.mult), ["goh", "gsel"], ["gates"])
    dump("gates", gates[:, :, :, :].rearrange("p t g e -> p t (g e)"),
         dbg.get("gates").rearrange("(t p) c -> p t c", p=P) if "gates" in dbg else None, ["gates"])
    if STOP_AFTER == "router":
        return finish()
    sch.barrier()

    hid = [view(R4, 64 * KB + i * 8 * KB, [P, 2, S], BF16) for i in range(2)]
    gT = view(R4, 80 * KB, [32, 2, S], BF16, parts=32)
    onehot = view(R4, 88 * KB, [32, 32, 128], BF16, parts=32)
    sa = [view(R4, 96 * KB + i * 2 * KB, [P, 512], F32) for i in range(2)]
    t1 = [view(R4, 100 * KB + i * 2 * KB, [P, 512], F32) for i in range(2)]
    wbuf = []
    for RR in (R2, R3):
        wbuf.append((view(RR, 0, [P, 8, 256], BF16), view(RR, 4 * KB, [P, 8, 256], BF16), view(RR, 8 * KB, [P, 2, D], BF16)))
    sch.add("sp", lambda e: e.dma_start(out=onehot, in_=onehot_d.rearrange("e (k m) -> e k m", k=32)), writes=["onehot"], dma=True)
    for T4 in range(4):
        tb = next_bank(0, 4)

        def trg(e, T4=T4, tb=tb):
            ins = None
            for k in range(4):
                T = T4 * 4 + k
                ins = e.transpose(ps[tb][0:32, k * 128:(k + 1) * 128], gates[:, T, :, :].rearrange("p g e -> p (g e)"), ident_f[:])
            return ins
        sch.add("pe", trg, reads=["gates", "ident_f"], writes=[psk(tb)])
        sl = slice(T4 * 512, (T4 + 1) * 512)
        sch.add("act", lambda e, tb=tb, sl=sl: e.activation(out=gT[0:32, 0, sl], in_=ps[tb][0:32, :], func=AF.Copy),
                reads=[psk(tb)], writes=[("gThi", T4), psk(tb)])
        sch.add("dve", lambda e, tb=tb, sl=sl: e.tensor_tensor(out=gT[0:32, 1, sl], in0=ps[tb][0:32, :], in1=gT[0:32, 0, sl], op=ALU.subtract),
                reads=[psk(tb), ("gThi", T4)], writes=[("gTlo", T4), psk(tb)])
    mcnt = [0]
    for pr in range(NE // 2):
      for ex in (2 * pr, 2 * pr + 1):
        wbi = ex % 2
        Wg, Wu, Wd = wbuf[wbi]
        sch.add("pool", lambda e, Wg=Wg, ex=ex: e.dma_start(out=Wg, in_=wg_d[ex].rearrange("(c p) f -> p c f", p=P)),
                writes=[("Wg", wbi)], dma=True)
        sch.add("pool", lambda e, Wu=Wu, ex=ex: e.dma_start(out=Wu, in_=wu_d[ex].rearrange("(c p) f -> p c f", p=P)),
                writes=[("Wu", wbi)], dma=True)
        for hh in range(2):
            sch.add("pool", lambda e, Wd=Wd, ex=ex, hh=hh: e.dma_start(
                out=Wd[:, :, hh * 512:(hh + 1) * 512], in_=wd_d[ex][:, hh * 512:(hh + 1) * 512].rearrange("(c p) n -> p c n", p=P)),
                writes=[("Wd", wbi, hh)], dma=True)
        hb = ex % 2
        for tc in range(4):
            sl = slice(tc * 512, (tc + 1) * 512)
            gb = mcnt[0] % 2
            mcnt[0] += 1

            def gmm(e, ex=ex, gb=gb, sl=sl):
                e.matmul(ps[gb][:], lhsT=onehot[0:32, ex, :], rhs=gT[0:32, 0, sl], start=True, stop=False)
                return e.matmul(ps[gb][:], lhsT=onehot[0:32, ex, :], rhs=gT[0:32, 1, sl], start=False, stop=True)
            sch.add("pe", gmm, reads=["onehot", ("gThi", tc), ("gTlo", tc)], writes=[psk(gb)])
            for ft in range(2):
                ab = 2 + (mcnt[0] % 2)
                ub = 4 + (mcnt[0] % 2)
                tb_ = mcnt[0] % 2
                mcnt[0] += 1

                def amm(e, Wg=Wg, ft=ft, sl=sl, ab=ab):
                    ins = None
                    for c in range(8):
                        ins = e.matmul(ps[ab][:], lhsT=Wg[:, c, ft * 128:(ft + 1) * 128], rhs=h2T[:, c, sl], start=(c == 0), stop=(c == 7))
                    return ins

                def umm2(e, Wu=Wu, ft=ft, sl=sl, ub=ub):
                    ins = None
                    for c in range(8):
                        ins = e.matmul(ps[ub][:], lhsT=Wu[:, c, ft * 128:(ft + 1) * 128], rhs=h2T[:, c, sl], start=(c == 0), stop=(c == 7))
                    return ins
                hkeys = [("h2T", tc * 4 + k) for k in range(4)]
                sch.add("pe", amm, reads=[("Wg", wbi)] + hkeys, writes=[psk(ab)])
                sch.add("pe", umm2, reads=[("Wu", wbi)] + hkeys, writes=[psk(ub)])
                sch.add("act", lambda e, ab=ab, tb_=tb_: e.activation(out=sa[tb_], in_=ps[ab][:], func=AF.Silu),
                        reads=[psk(ab)], writes=[("sa", tb_), psk(ab)])
                sch.add("dve", lambda e, ub=ub, tb_=tb_: e.tensor_tensor(out=t1[tb_], in0=ps[ub][:], in1=sa[tb_], op=ALU.mult),
                        reads=[psk(ub), ("sa", tb_)], writes=[("t1", tb_), psk(ub)])
                sch.add("dve", lambda e, gb=gb, tb_=tb_, hb=hb, ft=ft, sl=sl: e.tensor_tensor(out=hid[hb][:, ft, sl], in0=ps[gb][:], in1=t1[tb_],
                                                                                             op=ALU.mult),
                        reads=[psk(gb), ("t1", tb_)], writes=[("hid", hb, tc), psk(gb)])
      WdA, WdB = wbuf[0][2], wbuf[1][2]
      for T in range(NT):
            for hh in range(2):
                yb = 6 + (mcnt[0] % 2)
                mcnt[0] += 1

                def dmm(e, T=T, hh=hh, yb=yb):
                    ins = None
                    k = 0
                    for (hd, Wd_) in ((hid[0], WdA), (hid[1], WdB)):
                        for ft in range(2):
                            ins = e.matmul(ps[yb][:], lhsT=hd[:, ft, T * P:(T + 1) * P], rhs=Wd_[:, ft, hh * 512:(hh + 1) * 512],
                                           start=(k == 0), stop=(k == 3))
                            k += 1
                    return ins
                sch.add("pe", dmm, reads=[("hid", 0, T // 4), ("hid", 1, T // 4), ("Wd", 0, hh), ("Wd", 1, hh)], writes=[psk(yb)])
                sch.add("dve", lambda e, T=T, hh=hh, yb=yb: e.tensor_tensor(out=x1[:, T, hh * 512:(hh + 1) * 512], in0=ps[yb][:],
                                                                           in1=x1[:, T, hh * 512:(hh + 1) * 512], op=ALU.add),
                        reads=[psk(yb), ("x1", T, hh)], writes=[("x1", T, hh), psk(yb)])
    for T in range(NT):
        tsl = slice(T * P, (T + 1) * P)
        sch.add("sp", lambda e, T=T, tsl=tsl: e.dma_start(out=out_d[tsl, :], in_=x1[:, T, :]), reads=[("x1", T, 0), ("x1", T, 1)],
                writes=["out_%d" % T], dma=True)
    return finish()


_CACHE = {}


def _prep_shared(inputs):
    f32 = np.float32
    c = host_constants()
    w_in = np.asarray(inputs["w_in"][0], f32)
    shared = {}
    shared["w1"] = np.ascontiguousarray(w_in[:, W1_COLS])
    shared["wout"] = np.ascontiguousarray(np.asarray(inputs["w_out"][0], f32))
    shared["wr"] = np.ascontiguousarray(np.concatenate([np.asarray(inputs["w_router_group"][0], f32),
                                                        np.asarray(inputs["w_router_expert"][0], f32)], axis=1))
    shared["wg"] = np.ascontiguousarray(np.asarray(inputs["w_exp_gate"][0], f32))
    shared["wu"] = np.ascontiguousarray(np.asarray(inputs["w_exp_up"][0], f32))
    shared["wd"] = np.ascontiguousarray(np.asarray(inputs["w_exp_down"][0], f32))
    shared["w2aug"] = np.ascontiguousarray(np.concatenate([np.asarray(inputs["gla_gate_w2"][0], f32),
                                                           np.asarray(inputs["gla_gate_b"], f32).reshape(1, 256)], axis=0))
    shared["g1bc"] = np.ascontiguousarray(np.tile(np.asarray(inputs["norm1_g"], f32).reshape(1, D), (P, 1)))
    shared["g2bc"] = np.ascontiguousarray(np.tile(np.asarray(inputs["norm2_g"], f32).reshape(1, D), (P, 1)))
    qg = np.tile(np.asarray(inputs["q_norm_g"], f32).reshape(64), 2)
    kg = np.tile(np.asarray(inputs["k_norm_g"], f32).reshape(64), 2)
    shared["qkg"] = np.ascontiguousarray(np.stack([qg, kg], axis=1))
    shared["goutbc"] = np.ascontiguousarray(np.tile(np.asarray(inputs["gla_out_norm_g"], f32).reshape(1, 128), (P, 4)))
    br = np.concatenate([np.asarray(inputs["b_router_group"], f32).reshape(4),
                         np.asarray(inputs["b_router_expert"], f32).reshape(32)])
    shared["brbc"] = np.ascontiguousarray(np.tile(br.reshape(1, 36), (P, 1)))
    rb = np.asarray(inputs["rel_bias"], f32)
    idx = np.arange(128)
    bT = np.zeros((P, 8, 2, 128), f32)
    for dlt in range(2):
        dist = 128 * dlt + idx[None, :] - idx[:, None]
        bk = _t5_bucket_np(np.maximum(dist, 0))
        for h in range(8):
            bT[:, h, dlt, :] = rb[bk, h]
    shared["biasT"] = np.ascontiguousarray(bT.reshape(P, -1))
    shared["rb31"] = np.ascontiguousarray(np.tile(rb[31:32, :], (P, 1)))
    for k in ["ident_bf", "ident_f", "tri_bf", "tri_f", "after_f", "negmask", "blockones", "pow2"]:
        shared[k] = c[k]
    shared["onehot"] = np.ascontiguousarray(c["onehot"].reshape(32, -1))
    return shared


def kernel(**inputs):
    x = np.asarray(inputs["x"], np.float32)
    if "nc" not in _CACHE:
        _CACHE["nc"] = build_program()
    nc = _CACHE["nc"]
    shared = _prep_shared(inputs)
    in_maps = []
    for b in range(8):
        m = dict(shared)
        m["x"] = np.ascontiguousarray(x[b])
        in_maps.append(m)
    res = run_bass_kernel_spmd(nc, in_maps, core_ids=list(range(8)))
    _CACHE["last"] = res
    out = np.stack([np.asarray(r["out"], np.float32) for r in res.results], axis=0)
    return out
```

```python
import os
import numpy as np
import ml_dtypes
from contextlib import ExitStack
import concourse.bass as bass
import concourse.mybir as mybir
from concourse.bass_utils import run_bass_kernel_spmd

F32 = mybir.dt.float32
BF16 = mybir.dt.bfloat16
U8 = mybir.dt.uint8
AF = mybir.ActivationFunctionType
ALU = mybir.AluOpType
AX = mybir.AxisListType

P = 128
S = 2048
D = 1024
NT = 16
NE = 32
EPS = 1e-6
KITER = 18
DEBUG = {}
STOP_AFTER = None


class _Op:
    __slots__ = ("eng", "fn", "reads", "writes", "dma", "deps", "waits", "signal", "idx", "flag", "slotwait", "after")

    def __init__(self, eng, fn, reads, writes, dma):
        self.eng = eng
        self.fn = fn
        self.reads = reads
        self.writes = writes
        self.dma = dma
        self.deps = ()
        self.waits = []
        self.signal = None
        self.flag = False
        self.slotwait = None
        self.after = []


def _nofn(e):
    return None


class Sched:
    EPOCH = 12000
    RING = 8

    def __init__(self, nc, stack):
        self.nc = nc
        self.stack = stack
        self.ops = []

    def add(self, eng, fn, reads=(), writes=(), dma=False):
        reads = list(reads)
        writes = list(writes)
        for k in reads:
            if isinstance(k, tuple) and k and k[0] == "ps" and k not in writes:
                writes.append(k)
        self.ops.append(_Op(eng, fn, tuple(reads), tuple(writes), dma))

    def barrier(self):
        pos = getattr(self, "_barpos", 0)
        last = {}
        dmas = []
        for i, op in enumerate(self.ops):
            if i < pos:
                continue
            if op.dma:
                dmas.append(op)
            elif op.fn is not _nofn:
                last[op.eng] = op
        for eng in ("pe", "act", "dve", "pool", "sp"):
            b = _Op(eng, _nofn, (), (), False)
            b.after = [p for k, p in last.items() if k != eng] + dmas
            self.ops.append(b)
        self._barpos = len(self.ops)

    def finalize(self):
        nc = self.nc
        last_w = {}
        readers = {}
        for i, op in enumerate(self.ops):
            op.idx = i
            raw = set()
            other = set()
            for k in op.reads:
                if k in last_w:
                    raw.add(last_w[k])
            for k in op.writes:
                if k in last_w:
                    raw.add(last_w[k])
                for r in readers.get(k, ()):
                    other.add(r)
            raw.discard(i)
            other.discard(i)
            for k in op.reads:
                readers.setdefault(k, set()).add(i)
            for k in op.writes:
                last_w[k] = i
                readers[k] = set()
            best = {}
            deps = []
            for d in raw | other:
                p = self.ops[d]
                if p.dma:
                    deps.append(d)
                    continue
                if p.eng == op.eng and not op.dma:
                    if p.eng == "pe":
                        continue
                    if d not in raw:
                        continue
                if p.eng not in best or best[p.eng] < d:
                    best[p.eng] = d
            deps.extend(best.values())
            for a in op.after:
                deps.append(a.idx)
            op.deps = deps
            for d in deps:
                self.ops[d].flag = True
        cnt = {}
        self.sems = {}
        dcount = {}
        for op in self.ops:
            if op.dma:
                q = op.eng
                k = dcount.get(q, 0)
                dcount[q] = k + 1
                slot = k % self.RING
                name = "dq_%s_%d" % (q, slot)
                if name not in self.sems:
                    self.sems[name] = self.stack.enter_context(nc.semaphore(name))
                op.signal = (name, 16 * (k // self.RING + 1), 16)
                if k >= self.RING:
                    op.slotwait = (name, 16 * (k // self.RING))
            elif op.flag:
                c = cnt.get(op.eng, 0)
                ep = c // self.EPOCH
                name = "s_%s_%d" % (op.eng, ep)
                if name not in self.sems:
                    self.sems[name] = self.stack.enter_context(nc.semaphore(name))
                op.signal = (name, c % self.EPOCH + 1, 1)
                cnt[op.eng] = c + 1
        for op in self.ops:
            w = []
            if op.slotwait is not None:
                w.append(op.slotwait)
            for d in op.deps:
                sg = self.ops[d].signal
                w.append((sg[0], sg[1]))
            op.waits = w

    def emit(self, block):
        table = [("pe", block.tensor), ("act", block.scalar), ("dve", block.vector),
                 ("pool", block.gpsimd), ("sp", block.sync)]
        for engname, deco in table:
            ops = [op for op in self.ops if op.eng == engname]

            def body(e, ops=ops):
                seen = {}
                for op in ops:
                    for (sn, val) in op.waits:
                        if seen.get(sn, 0) >= val:
                            continue
                        e.wait_ge(self.sems[sn], val)
                        seen[sn] = val
                    ins = op.fn(e)
                    if op.signal is not None and ins is not None:
                        ins.then_inc(self.sems[op.signal[0]], op.signal[2])

            deco(body)


IQ_TILES = [(0, 3), (3, 6), (6, 8)]


def _t5_bucket_np(dist):
    max_exact = 16
    d_f = np.maximum(dist, 1).astype(np.float32)
    large = max_exact + (np.log(d_f / max_exact) / np.log(128 / max_exact) * (32 - max_exact)).astype(np.int32)
    large = np.minimum(large, 31)
    return np.where(dist < max_exact, dist, large)


def _w1_columns():
    A = 512
    off = {}
    o = 0
    for name, n in [("qa", 512), ("ka", 512), ("va", 512), ("iq", 256), ("ik", 32), ("iw", 8),
                    ("qb", 256), ("kb", 256), ("vb", 512), ("glr", 16), ("rg", 512)]:
        off[name] = o
        o += n
    cols = []
    groups = []

    def grp(name, kind, cl):
        groups.append((name, kind, len(cols), len(cl)))
        cols.extend(cl)

    r = lambda name, a, b: list(range(off[name] + a, off[name] + b))
    grp("qbkb", "fm", r("qb", 0, 256) + r("kb", 0, 256) + r("glr", 0, 16))
    grp("vb", "tm", r("vb", 0, 512))
    grp("rg", "tm", r("rg", 0, 512))
    grp("kbiw", "tm", r("kb", 0, 256) + r("iw", 0, 8))
    grp("qa", "fm", r("qa", 0, 512))
    grp("ka", "fm", r("ka", 0, 512))
    iqc = []
    for (h0, h1) in IQ_TILES:
        iqc += r("iq", h0 * 32, h1 * 32)
    grp("iqik", "fm", iqc + r("ik", 0, 32) * 3)
    grp("va", "tm", r("va", 0, 512))
    return np.array(cols, np.int64), groups


W1_COLS, W1_GROUPS = _w1_columns()
NW1 = len(W1_COLS)


def _selT_off(i):
    return sum((16 - ii) * 128 for ii in range(i))


SELT_TOTAL = _selT_off(16)


def host_constants():
    c = {}
    idx = np.arange(128)
    tri = (idx[:, None] <= idx[None, :])
    c["ident_bf"] = np.eye(128, dtype=np.float32).astype(ml_dtypes.bfloat16)
    c["ident_f"] = np.eye(128, dtype=np.float32)
    c["tri_bf"] = tri.astype(np.float32).astype(ml_dtypes.bfloat16)
    c["tri_f"] = tri.astype(np.float32)
    c["after_f"] = (idx[:, None] > idx[None, :]).astype(np.float32)
    c["negmask"] = np.where(idx[None, :] <= idx[:, None], 0.0, -1e30).astype(np.float32)
    bo = np.zeros((128, 128), np.float32)
    bo[:64, :64] = 1.0
    bo[64:, 64:] = 1.0
    c["blockones"] = bo.astype(ml_dtypes.bfloat16)
    c["pow2"] = np.tile((2.0 ** -np.arange(KITER + 1, dtype=np.float64)).astype(np.float32)[None, :], (128, 1))
    oh = np.zeros((32, 32, 128), np.float32)
    for e in range(32):
        oh[e, e, :] = 1.0
    c["onehot"] = oh.astype(ml_dtypes.bfloat16)
    return c


def build_program():
    nc = bass.Bass("TRN2", target_bir_lowering=False)
    stack = ExitStack()
    sch = Sched(nc, stack)

    def dram(name, shape, dt, kind="ExternalInput"):
        return nc.dram_tensor(name, list(shape), dt, kind=kind).ap()

    x_d = dram("x", [S, D], F32)
    w1_d = dram("w1", [D, NW1], F32)
    wout_d = dram("wout", [D, D], F32)
    wr_d = dram("wr", [D, 36], F32)
    wg_d = dram("wg", [NE, D, 256], F32)
    wu_d = dram("wu", [NE, D, 256], F32)
    wd_d = dram("wd", [NE, 256, D], F32)
    w2_d = dram("w2aug", [17, 256], F32)
    g1bc_d = dram("g1bc", [P, D], F32)
    g2bc_d = dram("g2bc", [P, D], F32)
    qkg_d = dram("qkg", [P, 2], F32)
    gout_d = dram("goutbc", [P, 512], F32)
    brbc_d = dram("brbc", [P, 36], F32)
    biasT_d = dram("biasT", [P, 8 * 2 * 128], F32)
    rb31_d = dram("rb31", [P, 8], F32)
    ident_bf_d = dram("ident_bf", [P, P], BF16)
    ident_f_d = dram("ident_f", [P, P], F32)
    tri_bf_d = dram("tri_bf", [P, P], BF16)
    tri_f_d = dram("tri_f", [P, P], F32)
    after_f_d = dram("after_f", [P, P], F32)
    negmask_d = dram("negmask", [P, P], F32)
    blockones_d = dram("blockones", [P, P], BF16)
    pow2_d = dram("pow2", [P, KITER + 1], F32)
    onehot_d = dram("onehot", [32, 32 * 128], BF16)
    out_d = dram("out", [S, D], F32, kind="ExternalOutput")
    dbg = {}
    for name, (shape, dt) in DEBUG.items():
        dbg[name] = dram("dbg_" + name, shape, dt, kind="ExternalOutput")

    def sb(name, shape, dt):
        return stack.enter_context(nc.sbuf_tensor(name, list(shape), dt))

    ident_bf = sb("ident_bf_s", [P, P], BF16)
    ident_f = sb("ident_f_s", [P, P], F32)
    tri_bf = sb("tri_bf_s", [P, P], BF16)
    tri_f = sb("tri_f_s", [P, P], F32)
    after_f = sb("after_f_s", [P, P], F32)
    negmask = sb("negmask_s", [P, P], F32)
    blockones = sb("blockones_s", [P, P], BF16)
    pow2 = sb("pow2_s", [P, KITER + 1], F32)
    g1bc = sb("g1bc_s", [P, D], F32)
    qkg = sb("qkg_s", [P, 2], F32)
    gout = sb("gout_s", [P, 512], F32)
    brbc = sb("brbc_s", [P, 36], F32)
    rb31 = sb("rb31_s", [P, 8], F32)
    Etile = sb("Etile", [P, 8, 2, 128], BF16)
    w2aug = sb("w2aug_s", [17, 256], BF16)
    iw_s = sb("iw_s", [P, NT, 8], F32)
    ssq1 = sb("ssq1", [P, NT], F32)
    rstd1 = sb("rstd1", [P, NT], F32)
    ones_col = sb("ones_col", [P, 1], F32)

    R1 = sb("R1", [P, 32 * 1024], U8)
    R2 = sb("R2", [P, 16 * 1024], U8)
    R3 = sb("R3", [P, 16 * 1024], U8)
    R4 = sb("R4", [P, 112 * 1024], U8)

    def view(arena, off, shape, dt, parts=P):
        nb = int(np.prod(shape[1:])) * (4 if dt == F32 else (2 if dt == BF16 else 1))
        ap = arena[0:parts, off:off + nb].bitcast(dt)
        if len(shape) == 3:
            ap = ap.rearrange("p (a b) -> p a b", a=shape[1])
        elif len(shape) == 4:
            ap = ap.rearrange("p (a b c) -> p a b c", a=shape[1], b=shape[2])
        return ap

    KB = 1024
    hT = view(R1, 0, [P, 8, S], BF16)
    mixT_b = view(R2, 0, [P, 4, S], BF16)
    mixT_a = view(R3, 0, [P, 4, S], BF16)
    qbT = view(R4, 0, [P, 2, S], BF16)
    kbT = view(R4, 8 * KB, [P, 2, S], BF16)
    glrT = view(R4, 16 * KB, [32, S], BF16, parts=32)
    vb = view(R4, 20 * KB, [P, NT, 512], BF16)
    Gt = view(R4, 36 * KB, [P, NT, 512], BF16)
    kbtm = view(R4, 52 * KB, [P, NT, 256], BF16)
    wst = [view(R4, 60 * KB + i * 9 * KB, [P, 8, 528], BF16) for i in range(2)]
    xst = [view(R4, 78 * KB + i * 4 * KB, [P, D], F32) for i in range(2)]
    xs = [view(R4, 86 * KB + i * 2 * KB, [P, D], BF16) for i in range(2)]
    junk = view(R4, 90 * KB, [P, 2048], BF16)
    tmpA = [view(R4, 94 * KB + i * 2 * KB, [P, 512], F32) for i in range(4)]
    glatmp = view(R4, 102 * KB, [P, 10 * 256], F32)

    ps = [stack.enter_context(nc.psum_tensor("ps%d" % i, [P, 512], F32)) for i in range(8)]

    def psk(i):
        return ("ps", i)

    def load_const(dst, src, key, eng="sp"):
        sch.add(eng, lambda e, dst=dst, src=src: e.dma_start(out=dst, in_=src), writes=[key], dma=True)

    load_const(ident_bf[:], ident_bf_d[:, :], "ident_bf")
    load_const(ident_f[:], ident_f_d[:, :], "ident_f")
    load_const(tri_bf[:], tri_bf_d[:, :], "tri_bf")
    load_const(tri_f[:], tri_f_d[:, :], "tri_f")
    load_const(after_f[:], after_f_d[:, :], "after_f")
    load_const(negmask[:], negmask_d[:, :], "negmask")
    load_const(blockones[:], blockones_d[:, :], "blockones")
    load_const(pow2[:], pow2_d[:, :], "pow2")
    load_const(g1bc[:], g1bc_d[:, :], "g1bc")
    load_const(qkg[:], qkg_d[:, :], "qkg")
    load_const(gout[:], gout_d[:, :], "gout")
    load_const(brbc[:], brbc_d[:, :], "brbc")
    load_const(rb31[:], rb31_d[:, :], "rb31")
    load_const(w2aug[:], w2_d[:, :], "w2aug", eng="pool")
    sch.add("dve", lambda e: e.memset(ones_col[:], 1.0), writes=["ones_col"])
    sch.add("dve", lambda e: e.memset(glrT[0:32, :], 1.0), writes=["glrT_init"])

    for T in range(NT):
        b = T % 2
        tsl = slice(T * P, (T + 1) * P)
        sch.add("sp", lambda e, b=b, tsl=tsl: e.dma_start(out=xst[b], in_=x_d[tsl, :]),
                writes=[("xst", b)], dma=True)
        sch.add("act", lambda e, b=b, T=T: e.activation(out=junk[:, 0:D], in_=xst[b], func=AF.Square,
                                                        accum_out=ssq1[:, T:T + 1]),
                reads=[("xst", b)], writes=["junk", ("ssq1", T)])
        sch.add("act", lambda e, T=T: e.activation(out=rstd1[:, T:T + 1], in_=ssq1[:, T:T + 1], func=AF.Sqrt,
                                                   scale=1.0 / D, bias=eps_col[:, 0:1]),
                reads=[("ssq1", T), "eps_col"], writes=[("rstd1", T)])
        sch.add("dve", lambda e, T=T: e.reciprocal(out=rstd1[:, T:T + 1], in_=rstd1[:, T:T + 1]),
                reads=[("rstd1", T)], writes=[("rstd1", T)])
        sch.add("dve", lambda e, b=b, T=T: e.scalar_tensor_tensor(out=xs[b], in0=xst[b], scalar=rstd1[:, T:T + 1],
                                                                  in1=g1bc[:], op0=ALU.mult, op1=ALU.mult),
                reads=[("xst", b), ("rstd1", T), "g1bc"], writes=[("xs", b)])
        pb = T % 2

        def tr(e, b=b, pb=pb):
            o = ps[pb][:].bitcast(BF16).rearrange("p (c t) -> p c t", c=8)
            ins = None
            for c in range(8):
                ins = e.transpose(o[:, c, :], xs[b][:, c * P:(c + 1) * P], ident_bf[:])
            return ins
        sch.add("pe", tr, reads=[("xs", b), "ident_bf"], writes=[psk(pb)])
        sch.add("act", lambda e, pb=pb, tsl=tsl: e.activation(
            out=hT[:, :, tsl], in_=ps[pb][:].bitcast(BF16).rearrange("p (c t) -> p c t", c=8), func=AF.Copy),
            reads=[psk(pb)], writes=[("hT", T)])

    wcount = [0]

    def load_wgroup(gi):
        name, kind, c0, n = W1_GROUPS[gi]
        b = wcount[0] % 2
        wcount[0] += 1
        src = w1_d[:, c0:c0 + n].rearrange("(c p) n -> p c n", p=P)
        dst = wst[b][:, :, 0:n]
        sch.add("pool", lambda e, dst=dst, src=src: e.dma_start(out=dst, in_=src),
                writes=[("wst", b)], dma=True)
        return b

    bankrot = {}

    def next_bank(lo=2, hi=6):
        k = (lo, hi)
        c = bankrot.get(k, 0)
        bankrot[k] = c + 1
        return lo + c % (hi - lo)

    def fm_matmul(wb, col0, m, tc, bank, parts=None):
        wt = wst[wb]

        def fn(e):
            ins = None
            for c in range(8):
                ins = e.matmul(ps[bank][0:m, :], lhsT=wt[:, c, col0:col0 + m],
                               rhs=hT[:, c, tc * 512:(tc + 1) * 512], start=(c == 0), stop=(c == 7))
            return ins
        sch.add("pe", fn, reads=[("wst", wb)] + [("hT", tc * 4 + k) for k in range(4)], writes=[psk(bank)])

    def tm_matmul(wb, col0, n, T, bank):
        wt = wst[wb]

        def fn(e):
            ins = None
            for c in range(8):
                ins = e.matmul(ps[bank][:, 0:n], lhsT=hT[:, c, T * P:(T + 1) * P],
                               rhs=wt[:, c, col0:col0 + n], start=(c == 0), stop=(c == 7))
            return ins
        sch.add("pe", fn, reads=[("wst", wb), ("hT", T)], writes=[psk(bank)])

    eps_col = sb("eps_col", [P, 1], F32)
    sch.ops.insert(0, _Op("dve", lambda e: e.memset(eps_col[:], EPS), (), ("eps_col",), False))

    wb = load_wgroup(0)
    for tc in range(4):
        tsl = slice(tc * 512, (tc + 1) * 512)
        for k in range(4):
            bank = next_bank()
            fm_matmul(wb, k * 128, 128, tc, bank)
            dst = (qbT if k < 2 else kbT)[:, k % 2, tsl]
            key = ("qbT" if k < 2 else "kbT", tc)
            eng = "act" if k % 2 == 0 else "dve"
            if eng == "act":
                sch.add("act", lambda e, dst=dst, bank=bank: e.activation(out=dst, in_=ps[bank][:], func=AF.Copy),
                        reads=[psk(bank)], writes=[key + (k % 2,)])
            else:
                sch.add("dve", lambda e, dst=dst, bank=bank: e.tensor_copy(out=dst, in_=ps[bank][:]),
                        reads=[psk(bank)], writes=[key + (k % 2,)])
        bank = next_bank()
        fm_matmul(wb, 512, 16, tc, bank)
        sch.add("dve", lambda e, tsl=tsl, bank=bank: e.tensor_copy(out=glrT[0:16, tsl], in_=ps[bank][0:16, :]),
                reads=[psk(bank), "glrT_init"], writes=[("glrT", tc)])
    wb = load_wgroup(1)
    for T in range(NT):
        bank = next_bank()
        tm_matmul(wb, 0, 512, T, bank)
        eng = "act" if T % 2 == 0 else "dve"
        if eng == "act":
            sch.add("act", lambda e, T=T, bank=bank: e.activation(out=vb[:, T, :], in_=ps[bank][:], func=AF.Copy),
                    reads=[psk(bank)], writes=[("vb", T)])
        else:
            sch.add("dve", lambda e, T=T, bank=bank: e.tensor_copy(out=vb[:, T, :], in_=ps[bank][:]),
                    reads=[psk(bank)], writes=[("vb", T)])
    wb = load_wgroup(2)
    for T in range(NT):
        bank = next_bank()
        tm_matmul(wb, 0, 512, T, bank)
        tb = T % 2
        sch.add("act", lambda e, tb=tb, bank=bank: e.activation(out=tmpA[tb][:], in_=ps[bank][:], func=AF.Silu),
                reads=[psk(bank)], writes=[("tmpA", tb)])
        sch.add("pool", lambda e, tb=tb, T=T: e.tensor_tensor(out=Gt[:, T, :], in0=tmpA[tb][:], in1=gout[:], op=ALU.mult),
                reads=[("tmpA", tb), "gout"], writes=[("Gt", T)])
    wb = load_wgroup(3)
    for T in range(NT):
        bank = next_bank()
        tm_matmul(wb, 0, 264, T, bank)
        sch.add("dve", lambda e, T=T, bank=bank: e.tensor_copy(out=kbtm[:, T, :], in_=ps[bank][:, 0:256]),
                reads=[psk(bank)], writes=[("kbtm", T)])
        sch.add("dve", lambda e, T=T, bank=bank: e.tensor_copy(out=iw_s[:, T, :], in_=ps[bank][:, 256:264]),
                reads=[psk(bank)], writes=[("iw", T)])

    def dump(name, src_ap, dst_ap, reads):
        if name in dbg:
            sch.add("sp", lambda e: e.dma_start(out=dst_ap, in_=src_ap), reads=reads, writes=["dbg_" + name], dma=True)

    def finish():
        import os
        if os.environ.get("KSTOP_OPS"):
            print("total ops", len(sch.ops))
            sch.ops = sch.ops[:int(os.environ["KSTOP_OPS"])]
        outs = ["dbg_" + n for n in dbg] + ["out_%d" % T for T in range(NT)]
        sch.add("sp", lambda e: None, reads=outs)
        sch.finalize()
        with nc.Block() as block:
            sch.emit(block)
        stack.close()
        return nc

    dump("hT", hT, dbg.get("hT").rearrange("(c p) t -> p c t", p=P) if "hT" in dbg else None, [("hT", T) for T in range(NT)])
    dump("qbT", qbT[:, 0, :], dbg.get("qbT"), [("qbT", tc, 0) for tc in range(4)])
    dump("vb", vb, dbg.get("vb").rearrange("(t p) c -> p t c", p=P) if "vb" in dbg else None, [("vb", T) for T in range(NT)])
    dump("Gt", Gt, dbg.get("Gt").rearrange("(t p) c -> p t c", p=P) if "Gt" in dbg else None, [("Gt", T) for T in range(NT)])
    if STOP_AFTER == "p1":
        return finish()

    Sst = sb("Sst", [P, 4, 128], F32)
    Sbf = sb("Sbf", [P, 4, 128], BF16)
    g_e1 = sb("g_e1", [P, 256], F32)
    g_l = sb("g_l", [P, 256], F32)
    g_eb = [sb("g_eb%d" % i, [P, 2, 128], F32) for i in range(2)]
    g_einv = sb("g_einv", [P, 2, 128], F32)
    g_erem = sb("g_erem", [P, 256], F32)
    g_qt = [sb("g_qt%d" % i, [P, 2, 128], BF16) for i in range(2)]
    g_kt = sb("g_kt", [P, 2, 128], BF16)
    g_kh = sb("g_kh", [P, 256], BF16)
    g_A = [sb("g_A%d" % i, [P, 4, 128], BF16) for i in range(2)]
    g_ob = sb("g_ob", [P, 512], BF16)
    g_ssq = sb("g_ssq", [P, 4], F32)
    g_rs = sb("g_rs", [P, 4], F32)
    sch.add("dve", lambda e: e.memset(Sst[:], 0.0), writes=[("Sst", h) for h in range(4)])
    sch.add("dve", lambda e: e.memset(Sbf[:], 0.0), writes=[("Sbf", h) for h in range(4)])
    BZ, BC, BA, BO, BT = 0, 1, 3, 4, 6
    BUS = [5, 2]
    print("ops before GLA", len(sch.ops))

    def gla_stage1(T):
        b = T % 2
        tsl = slice(T * P, (T + 1) * P)
        eb, qt, A, BU = g_eb[b], g_qt[b], g_A[b], BUS[b]
        sch.add("pe", lambda e: e.matmul(ps[BZ][:, 0:256], lhsT=glrT[0:17, tsl], rhs=w2aug[0:17, :], start=True, stop=True),
                reads=[("glrT", T // 4), "glrT_init", "w2aug"], writes=[psk(BZ)])
        sch.add("act", lambda e: e.activation(out=g_e1[:], in_=ps[BZ][:, 0:256], func=AF.Exp, scale=-1.0),
                reads=[psk(BZ)], writes=["g_e1"])
        sch.add("act", lambda e: e.activation(out=g_l[:], in_=g_e1[:], func=AF.Ln, scale=1.0, bias=ones_col[:, 0:1]),
                reads=["g_e1", "ones_col"], writes=["g_l"])

        def cum(e):
            e.matmul(ps[BC][:, 0:128], lhsT=g_l[:, 0:128], rhs=tri_f[:], start=True, stop=True)
            return e.matmul(ps[BC][:, 128:256], lhsT=g_l[:, 128:256], rhs=tri_f[:], start=True, stop=True)
        sch.add("pe", cum, reads=["g_l", "tri_f"], writes=[psk(BC)])
        sch.add("pe", lambda e: e.matmul(ps[BZ][:, 256:512], lhsT=after_f[:], rhs=g_l[:], start=True, stop=True),
                reads=["g_l", "after_f"], writes=[psk(BZ)])
        cview = ps[BC][:, 0:256].rearrange("p (a b) -> p a b", a=2)
        sch.add("act", lambda e: e.activation(out=eb[:], in_=cview, func=AF.Exp, scale=-1.0 / 16),
                reads=[psk(BC)], writes=[("g_eb", b)])
        sch.add("act", lambda e: e.activation(out=g_einv[:], in_=cview, func=AF.Exp, scale=1.0 / 16),
                reads=[psk(BC)], writes=["g_einv"])
        sch.add("act", lambda e: e.activation(out=g_erem[:], in_=ps[BZ][:, 256:512], func=AF.Exp, scale=-1.0 / 16),
                reads=[psk(BZ)], writes=["g_erem"])
        sch.add("dve", lambda e: e.scalar_tensor_tensor(out=qt[:], in0=qbT[:, :, tsl], scalar=0.125, in1=eb[:],
                                                        op0=ALU.mult, op1=ALU.mult),
                reads=[("qbT", T // 4, 0), ("qbT", T // 4, 1), ("g_eb", b)], writes=[("g_qt", b)])
        sch.add("dve", lambda e: e.scalar_tensor_tensor(out=g_kt[:], in0=kbT[:, :, tsl], scalar=1.0, in1=g_einv[:],
                                                        op0=ALU.mult, op1=ALU.mult),
                reads=[("kbT", T // 4, 0), ("kbT", T // 4, 1), "g_einv"], writes=["g_kt"])
        sch.add("dve", lambda e: e.scalar_tensor_tensor(out=g_kh[:], in0=kbtm[:, T, :], scalar=1.0, in1=g_erem[:],
                                                        op0=ALU.mult, op1=ALU.mult),
                reads=[("kbtm", T), "g_erem"], writes=["g_kh"])

        def attn(e):
            ins = None
            for h in (0, 2, 1, 3):
                p, r = h // 2, h % 2
                rs = slice(r * 64, (r + 1) * 64)
                bk = BA if r == 0 else 7
                ins = e.matmul(ps[bk][:, p * 128:(p + 1) * 128], lhsT=g_kt[rs, p, :], rhs=qt[rs, p, :], start=True, stop=True)
            return ins
        sch.add("pe", attn, reads=["g_kt", ("g_qt", b)], writes=[psk(BA), psk(7)])
        for r in range(2):
            bk = BA if r == 0 else 7
            sch.add("dve", lambda e, r=r, bk=bk: e.tensor_tensor(
                out=A[:, r::2, :], in0=ps[bk][:, 0:256].rearrange("p (h t) -> p h t", h=2),
                in1=tri_bf[:, :].unsqueeze(1).to_broadcast([P, 2, 128]), op=ALU.mult),
                reads=[psk(bk), "tri_bf"], writes=[("g_A", b, r)])

        def umm(e):
            ins = None
            for h in range(4):
                p = h // 2
                ins = e.matmul(ps[BU][:, h * 128:(h + 1) * 128], lhsT=g_kh[:, p * 128:(p + 1) * 128], rhs=vb[:, T, h * 128:(h + 1) * 128],
                               start=True, stop=True)
            return ins
        sch.add("pe", umm, reads=["g_kh", ("vb", T)], writes=[psk(BU)])

    def gla_stage2(T):
        b = T % 2
        tsl = slice(T * P, (T + 1) * P)
        eb, qt, A, BU = g_eb[b], g_qt[b], g_A[b], BUS[b]

        def omm(e):
            ins = None
            for h in range(4):
                p = h // 2
                e.matmul(ps[BO][:, h * 128:(h + 1) * 128], lhsT=A[:, h, :], rhs=vb[:, T, h * 128:(h + 1) * 128], start=True, stop=False)
                ins = e.matmul(ps[BO][:, h * 128:(h + 1) * 128], lhsT=qt[:, p, :], rhs=Sbf[:, h, :], start=False, stop=True)
            return ins
        sch.add("pe", omm, reads=[("g_A", b, 0), ("g_A", b, 1), ("vb", T), ("g_qt", b)] + [("Sbf", h) for h in range(4)], writes=[psk(BO)])
        for h in range(4):
            p, r = h // 2, h % 2
            rs = slice(r * 64, (r + 1) * 64)
            sch.add("dve", lambda e, h=h, p=p, rs=rs: e.scalar_tensor_tensor(
                out=Sst[rs, h, :], in0=Sst[rs, h, :], scalar=eb[rs, p, 127:128], in1=ps[BU][rs, h * 128:(h + 1) * 128],
                op0=ALU.mult, op1=ALU.add), reads=[psk(BU), ("g_eb", b), ("Sst", h)], writes=[("Sst", h), psk(BU)])
            sch.add("act", lambda e, h=h, rs=rs: e.activation(out=Sbf[rs, h, :], in_=Sst[rs, h, :], func=AF.Copy),
                    reads=[("Sst", h)], writes=[("Sbf", h)])
        for h in range(4):
            sch.add("act", lambda e, h=h: e.activation(out=junk[:, 0:128], in_=ps[BO][:, h * 128:(h + 1) * 128], func=AF.Square,
                                                       accum_out=g_ssq[:, h:h + 1]),
                    reads=[psk(BO)], writes=["junk", ("g_ssq", h), psk(BO)])
        sch.add("act", lambda e: e.activation(out=g_rs[:], in_=g_ssq[:], func=AF.Sqrt, scale=1.0 / 128, bias=eps_col[:, 0:1]),
                reads=[("g_ssq", h) for h in range(4)] + ["eps_col"], writes=["g_rs"])
        sch.add("dve", lambda e: e.reciprocal(out=g_rs[:], in_=g_rs[:]), reads=["g_rs"], writes=["g_rs"])
        for h in range(4):
            hs = slice(h * 128, (h + 1) * 128)
            sch.add("dve", lambda e, h=h, hs=hs: e.scalar_tensor_tensor(
                out=g_ob[:, hs], in0=ps[BO][:, hs], scalar=g_rs[:, h:h + 1], in1=Gt[:, T, hs], op0=ALU.mult, op1=ALU.mult),
                reads=[psk(BO), "g_rs", ("Gt", T)], writes=[("g_ob", h), psk(BO)])
        if "ob" in dbg:
            sch.add("sp", lambda e: e.dma_start(out=dbg["ob"][tsl, :], in_=g_ob[:]), reads=[("g_ob", h) for h in range(4)],
                    writes=["dbg_ob"], dma=True)

        def trb(e):
            o = ps[BT][:].bitcast(BF16).rearrange("p (c t) -> p c t", c=8)
            ins = None
            for c in range(4):
                ins = e.transpose(o[:, c, :], g_ob[:, c * P:(c + 1) * P], ident_bf[:])
            return ins
        sch.add("pe", trb, reads=[("g_ob", h) for h in range(4)] + ["ident_bf"], writes=[psk(BT)])
        sch.add("act", lambda e: e.activation(
            out=mixT_b[:, :, tsl], in_=ps[BT][:].bitcast(BF16).rearrange("p (c t) -> p c t", c=8)[:, 0:4, :], func=AF.Copy),
            reads=[psk(BT)], writes=[("mixT_b", T)])

    gla_stage1(0)
    for T in range(NT):
        if T + 1 < NT:
            gla_stage1(T + 1)
        gla_stage2(T)
    if STOP_AFTER == "gla":
        return finish()
    sch.barrier()

    qaT = view(R4, 0, [P, 4, S], BF16)
    kaT = view(R4, 16 * KB, [P, 4, S], BF16)
    iqT = view(R4, 32 * KB, [P, 3, S], BF16)
    ikT = view(R4, 44 * KB, [P, S], BF16)
    vaT = view(R4, 48 * KB, [P, NT, 8 * 65], BF16)
    wst2 = [view(R4, 66 * KB + i * 9 * KB, [P, 8, 528], BF16) for i in range(2)]
    n_sq = [view(R4, 84 * KB + i * KB, [P, 512], BF16) for i in range(2)]
    n_ln = [view(R4, 86 * KB + i * 2 * KB, [P, 512], F32) for i in range(2)]
    n_rs = [view(R4, 90 * KB + i * 2 * KB, [P, 512], F32) for i in range(2)]
    wst[0], wst[1] = wst2[0], wst2[1]
    sch.add("dve", lambda e: e.memset(vaT[:], 1.0), writes=[("va", T) for T in range(NT)])
    ncnt = [0]
    for gi, which in [(4, 0), (5, 1)]:
        wb = load_wgroup(gi)
        dstT = qaT if which == 0 else kaT
        kname = "qaT" if which == 0 else "kaT"
        for tc in range(4):
            tsl = slice(tc * 512, (tc + 1) * 512)
            for p in range(4):
                bank = next_bank(0, 4)
                sbank = next_bank(4, 8)
                nb = ncnt[0] % 2
                ncnt[0] += 1
                fm_matmul(wb, p * 128, 128, tc, bank)
                sch.add("act", lambda e, nb=nb, bank=bank: e.activation(out=n_sq[nb], in_=ps[bank][:], func=AF.Square),
                        reads=[psk(bank)], writes=[("n_sq", nb), psk(bank)])
                sch.add("pe", lambda e, nb=nb, sbank=sbank: e.matmul(ps[sbank][:], lhsT=blockones[:], rhs=n_sq[nb], start=True, stop=True),
                        reads=[("n_sq", nb), "blockones"], writes=[psk(sbank)])
                sch.add("act", lambda e, nb=nb, sbank=sbank: e.activation(out=n_ln[nb], in_=ps[sbank][:], func=AF.Ln, scale=1.0 / 64,
                                                                          bias=eps_col[:, 0:1]),
                        reads=[psk(sbank), "eps_col"], writes=[("n_ln", nb)])
                sch.add("act", lambda e, nb=nb: e.activation(out=n_rs[nb], in_=n_ln[nb], func=AF.Exp, scale=-0.5),
                        reads=[("n_ln", nb)], writes=[("n_rs", nb)])
                sch.add("dve", lambda e, nb=nb, bank=bank, p=p, tsl=tsl, dstT=dstT, which=which: e.scalar_tensor_tensor(
                    out=dstT[:, p, tsl], in0=ps[bank][:], scalar=qkg[:, which:which + 1], in1=n_rs[nb], op0=ALU.mult, op1=ALU.mult),
                    reads=[psk(bank), ("n_rs", nb), "qkg"], writes=[(kname, p, tc), psk(bank)])
    wb = load_wgroup(6)
    for tc in range(4):
        tsl = slice(tc * 512, (tc + 1) * 512)
        col = 0
        for ti, (h0, h1) in enumerate(IQ_TILES):
            m = (h1 - h0) * 32
            bank = next_bank(0, 8)
            fm_matmul(wb, col, m, tc, bank)
            col += m
            sch.add("act", lambda e, ti=ti, m=m, tsl=tsl, bank=bank: e.activation(out=iqT[0:m, ti, tsl], in_=ps[bank][0:m, :], func=AF.Copy),
                    reads=[psk(bank)], writes=[("iqT", ti, tc)])
        bank = next_bank(0, 8)
        fm_matmul(wb, col, 96, tc, bank)
        sch.add("dve", lambda e, tsl=tsl, bank=bank: e.tensor_copy(out=ikT[0:96, tsl], in_=ps[bank][0:96, :]),
                reads=[psk(bank)], writes=[("ikT", tc)])
    wb = load_wgroup(7)
    for T in range(NT):
        bank = next_bank(0, 8)
        tm_matmul(wb, 0, 512, T, bank)
        dstv = vaT[:, T, :].rearrange("p (h d) -> p h d", h=8)[:, :, 0:64]
        srcv = ps[bank][:].rearrange("p (h d) -> p h d", h=8)
        if T % 2 == 0:
            sch.add("act", lambda e, dstv=dstv, srcv=srcv: e.activation(out=dstv, in_=srcv, func=AF.Copy),
                    reads=[psk(bank)], writes=[("va", T)])
        else:
            sch.add("dve", lambda e, dstv=dstv, srcv=srcv: e.tensor_copy(out=dstv, in_=srcv),
                    reads=[psk(bank)], writes=[("va", T)])
    dump("qaT", qaT[:, 0, :], dbg.get("qaT"), [("qaT", 0, tc) for tc in range(4)])
    dump("kaT", kaT[:, 0, :], dbg.get("kaT"), [("kaT", 0, tc) for tc in range(4)])
    if STOP_AFTER == "p2":
        return finish()
    sch.barrier()

    selT = view(R4, 66 * KB, [P, SELT_TOTAL], BF16)
    selts = [view(R4, 100 * KB + i * 4 * KB, [P, S], BF16) for i in range(2)]
    PTb = [view(R4, 108 * KB + i * KB, [P, 512], BF16) for i in range(4)]
    junk_tk = view(R4, 108 * KB, [P, 2048], BF16)
    score_all = R1[:, :].bitcast(F32)
    thr = sb("thr", [P, NT], F32)
    thr2 = sb("thr2", [P, NT], F32)
    cnt = sb("cnt", [P, NT], F32)
    sgn = sb("sgn", [P, NT], F32)
    amax = sb("amax", [P, NT], F32)
    mrow = sb("mrow", [P, 1], F32)
    mtab = sb("mtab", [P, KITER + 1], F32)
    biasT_s = view(R4, 100 * KB, [P, 8, 2, 128], F32)
    sch.add("sp", lambda e: e.dma_start(out=biasT_s, in_=biasT_d.rearrange("p (h d t) -> p h d t", h=8, d=2)),
            writes=["biasT"], dma=True)
    sch.add("act", lambda e: e.activation(out=Etile[:], in_=biasT_s, func=AF.Exp), reads=["biasT"], writes=["Etile"])
    for h in range(8):
        sch.add("dve", lambda e, h=h: e.tensor_tensor(out=Etile[:, h, 0, :], in0=Etile[:, h, 0, :], in1=tri_bf[:], op=ALU.mult),
                reads=["Etile", "tri_bf"], writes=["Etile"])
    sch.add("dve", lambda e: e.memset(selts[0][:], 0.0), reads=["Etile"], writes=[("selts", 0)])
    sch.add("dve", lambda e: e.tensor_copy(out=selT[:, _selT_off(0):_selT_off(0) + 128], in_=tri_bf[:]), reads=["tri_bf"], writes=[("selT", 0, 0)])
    sch.add("dve", lambda e: e.memset(selT[:, _selT_off(0) + 128:_selT_off(0) + 256], 1.0), writes=[("selT", 0, 1)])
    sch.add("dve", lambda e: e.tensor_copy(out=selT[:, _selT_off(1):_selT_off(1) + 128], in_=tri_bf[:]), reads=["tri_bf"], writes=[("selT", 1, 1)])

    print("ops before topk loops", len(sch.ops))
    batches = [list(range(2, 8)), list(range(8, 12)), list(range(12, 16))]
    ACT_SHARE = [4, 2, 2]
    sumA = sb("sumA", [P, NT], F32)
    nhalf = sb("nhalf", [P, NT], F32)
    junk_act = view(R3, 0, [P, 2048], BF16)
    for j in range(NT):
        sch.add("dve", lambda e, j=j: e.memset(nhalf[:, j:j + 1], 64.0 * (j + 1)), writes=["nhalf"])
    hd_loc = []
    for ti, (h0, h1) in enumerate(IQ_TILES):
        for k in range(h1 - h0):
            hd_loc.append((ti, k))
    lbank = [0]
    for bi, batch in enumerate(batches):
        soff = {}
        o = 0
        for j in batch:
            soff[j] = o
            o += (j + 1) * 128
        nb = len(batch)
        j0 = batch[0]
        for j in batch:
            n = (j + 1) * 128
            sc_j = score_all[:, soff[j]:soff[j] + n]
            nsc = (n + 511) // 512
            for sc in range(nsc):
                w = min(512, n - sc * 512)
                ssl = slice(sc * 512, sc * 512 + w)
                for h in range(8):
                    ti, k = hd_loc[h]
                    rs = slice(k * 32, (k + 1) * 32)
                    lb = lbank[0] % 4
                    rb = 4 + lbank[0] % 4
                    lbank[0] += 1
                    sch.add("pe", lambda e, lb=lb, rs=rs, ti=ti, j=j, ssl=ssl, w=w: e.matmul(
                        ps[lb][:, 0:w], lhsT=iqT[rs, ti, j * 128:(j + 1) * 128], rhs=ikT[rs, ssl], start=True, stop=True),
                        reads=[("iqT", ti, j // 4)] + [("ikT", c) for c in range(sc * 4 // 4, (sc * 512 + w - 1) // 512 + 1)],
                        writes=[psk(lb)])
                    sch.add("act", lambda e, lb=lb, rb=rb, w=w: e.activation(out=ps[rb][:, 0:w], in_=ps[lb][:, 0:w], func=AF.Relu),
                            reads=[psk(lb)], writes=[psk(rb), psk(lb)])
                    dst = sc_j[:, ssl]
                    if h == 0:
                        sch.add("dve", lambda e, rb=rb, w=w, dst=dst, j=j, h=h: e.tensor_scalar(
                            out=dst, in0=ps[rb][:, 0:w], scalar1=iw_s[:, j, h:h + 1], scalar2=None, op0=ALU.mult),
                            reads=[psk(rb), ("iw", j)], writes=[("score", j, sc), psk(rb)])
                    else:
                        sch.add("dve", lambda e, rb=rb, w=w, dst=dst, j=j, h=h: e.scalar_tensor_tensor(
                            out=dst, in0=ps[rb][:, 0:w], scalar=iw_s[:, j, h:h + 1], in1=dst, op0=ALU.mult, op1=ALU.add),
                            reads=[psk(rb), ("iw", j), ("score", j, sc)], writes=[("score", j, sc), psk(rb)])
            skeys = [("score", j, sc) for sc in range(nsc)]
            sch.add("dve", lambda e, sc_j=sc_j, j=j: e.tensor_reduce(out=amax[:, j:j + 1], in_=sc_j, axis=AX.X, op=ALU.max,
                                                                    apply_absolute_value=True),
                    reads=skeys, writes=[("amax", j)])
            dsl = slice(soff[j] + j * 128, soff[j] + (j + 1) * 128)
            sch.add("dve", lambda e, dsl=dsl: e.tensor_tensor(out=score_all[:, dsl], in0=score_all[:, dsl], in1=negmask[:], op=ALU.add),
                    reads=skeys + ["negmask", ("amax", j)], writes=skeys)
        if "score" in dbg and bi == 1:
            sch.add("sp", lambda e, soff=soff: e.dma_start(out=dbg["score"][:, 0:1280], in_=score_all[:, soff[9]:soff[9] + 1280]),
                    reads=[("score", 9, sc) for sc in range(3)], writes=["dbg_score"], dma=True)
        bsl = slice(j0, j0 + nb)
        sch.add("dve", lambda e, bsl=bsl: e.tensor_reduce(out=mrow[:], in_=amax[:, bsl], axis=AX.X, op=ALU.max),
                reads=[("amax", j) for j in batch], writes=["mrow"])
        sch.add("dve", lambda e: e.tensor_scalar(out=mtab[:], in0=pow2[:], scalar1=mrow[:, 0:1], scalar2=None, op0=ALU.mult),
                reads=["mrow", "pow2"], writes=["mtab"])
        sch.add("dve", lambda e, bsl=bsl: e.memset(thr[:, bsl], 0.0), writes=["thr"])
        nact = ACT_SHARE[bi]
        act_tiles = batch[:nact]
        asl = slice(batch[0], batch[0] + nact)
        for k in range(KITER):
            for j in batch:
                n = (j + 1) * 128
                sc_j = score_all[:, soff[j]:soff[j] + n]
                skeys_j = [("score", j, sc) for sc in range((n + 511) // 512)]
                if j in act_tiles:
                    sch.add("act", lambda e, sc_j=sc_j, n=n, j=j: e.activation(
                        out=junk_act[:, 0:n], in_=sc_j, func=AF.Sign, scale=-1.0, bias=thr[:, j:j + 1], accum_out=sumA[:, j:j + 1]),
                        reads=skeys_j + ["thr"], writes=["junk_act", ("sumA", j)])
                else:
                    sch.add("dve", lambda e, sc_j=sc_j, n=n, j=j: e.tensor_scalar(
                        out=junk_tk[:, 0:n], in0=sc_j, scalar1=thr[:, j:j + 1], scalar2=None, op0=ALU.is_ge, op1=ALU.add,
                        accum_out=cnt[:, j:j + 1]),
                        reads=skeys_j + ["thr"], writes=["junk", ("cnt", j)])
            if nact > 0:
                sch.add("dve", lambda e, asl=asl: e.scalar_tensor_tensor(out=cnt[:, asl], in0=sumA[:, asl], scalar=-0.5, in1=nhalf[:, asl],
                                                                        op0=ALU.mult, op1=ALU.add),
                        reads=[("sumA", j) for j in act_tiles] + ["nhalf"], writes=[("cnt", j) for j in act_tiles])
            sch.add("dve", lambda e, bsl=bsl: e.tensor_scalar(out=sgn[:, bsl], in0=cnt[:, bsl], scalar1=256.0, scalar2=0.5,
                                                             op0=ALU.is_ge, op1=ALU.subtract),
                    reads=[("cnt", j) for j in batch], writes=["sgn"])
            sch.add("dve", lambda e, bsl=bsl, k=k: e.scalar_tensor_tensor(out=thr2[:, bsl], in0=sgn[:, bsl], scalar=mtab[:, k:k + 1],
                                                                       in1=thr[:, bsl], op0=ALU.mult, op1=ALU.add),
                    reads=["sgn", "mtab", "thr"], writes=["thr2"])
            sch.add("dve", lambda e, bsl=bsl: e.tensor_copy(out=thr[:, bsl], in_=thr2[:, bsl]), reads=["thr2"], writes=["thr"])
        sch.add("dve", lambda e, bsl=bsl: e.tensor_scalar(out=thr2[:, bsl], in0=thr[:, bsl], scalar1=mtab[:, KITER:KITER + 1], scalar2=None,
                                                         op0=ALU.subtract),
                reads=["thr", "mtab"], writes=["thr2"])
        if "thr" in dbg and bi == 1:
            sch.add("sp", lambda e: e.dma_start(out=dbg["thr"][:, :], in_=thr2[:]), reads=["thr2"], writes=["dbg_thr"], dma=True)
        for j in batch:
            n = (j + 1) * 128
            sc_j = score_all[:, soff[j]:soff[j] + n]
            sb_ = j % 2
            sch.add("dve", lambda e, sc_j=sc_j, n=n, j=j, sb_=sb_: e.tensor_scalar(
                out=selts[sb_][:, 0:n], in0=sc_j, scalar1=thr2[:, j:j + 1], scalar2=None, op0=ALU.is_ge),
                reads=[("score", j, sc) for sc in range((n + 511) // 512)] + ["thr2"], writes=[("selts", sb_)])
            for i0 in range(0, j + 1, 8):
                i1 = min(j + 1, i0 + 8)
                tb = next_bank(0, 8)

                def trs(e, i0=i0, i1=i1, tb=tb, sb_=sb_):
                    o = ps[tb][:].bitcast(BF16).rearrange("p (c t) -> p c t", c=8)
                    ins = None
                    for i in range(i0, i1):
                        ins = e.transpose(o[:, i - i0, :], selts[sb_][:, i * 128:(i + 1) * 128], ident_bf[:])
                    return ins
                sch.add("pe", trs, reads=[("selts", sb_), "ident_bf"], writes=[psk(tb)])
                for i in range(i0, i1):
                    o = ps[tb][:].bitcast(BF16).rearrange("p (c t) -> p c t", c=8)[:, i - i0, :]
                    off = _selT_off(i) + (j - i) * 128
                    if i % 2 == 0:
                        sch.add("act", lambda e, o=o, off=off: e.activation(out=selT[:, off:off + 128], in_=o, func=AF.Copy),
                                reads=[psk(tb)], writes=[("selT", i, j), psk(tb)])
                    else:
                        sch.add("dve", lambda e, o=o, off=off: e.tensor_copy(out=selT[:, off:off + 128], in_=o),
                                reads=[psk(tb)], writes=[("selT", i, j), psk(tb)])
    dump("selT", selT, dbg.get("selT"), [("selT", i, j) for i in range(16) for j in range(i, 16)])
    if STOP_AFTER == "topk":
        return finish()
    sch.barrier()

    mixa = view(R1, 0, [P, NT, 512], BF16)
    rden = sb("rden", [P, 4], F32)
    abank = [0]
    LOOKAHEAD = 3
    pend = []

    def flush(keep):
        while len(pend) > keep:
            pend.pop(0)()

    for h in range(8):
        p, r = h // 2, h % 2
        rs = slice(r * 64, (r + 1) * 64)
        for J in range(4):
            accb = 6 + (abank[0] % 2)
            abank[0] += 1
            accv = ps[accb][:, 0:260].rearrange("p (j d) -> p j d", j=4)
            first = [True]
            for i in range(4 * J + 4):
                jlo = max(i, 4 * J)
                t0 = jlo * 128
                n = (4 * J + 4 - jlo) * 128
                sbk = next_bank(0, 6)
                pb = next_bank(100, 104) - 100
                sch.add("pe", lambda e, sbk=sbk, rs=rs, p=p, i=i, t0=t0, n=n: e.matmul(
                    ps[sbk][:, 0:n], lhsT=kaT[rs, p, i * 128:(i + 1) * 128], rhs=qaT[rs, p, t0:t0 + n], start=True, stop=True),
                    reads=[("kaT", p, i // 4), ("qaT", p, J)], writes=[psk(sbk)])
                nnear = max(0, min(4 * J + 4, i + 2) - jlo) * 128
                if nnear > 0:
                    sch.add("act", lambda e, sbk=sbk, pb=pb, nnear=nnear: e.activation(out=PTb[pb][:, 0:nnear], in_=ps[sbk][:, 0:nnear],
                                                                                       func=AF.Exp, scale=0.125),
                            reads=[psk(sbk)], writes=[("PT", pb), psk(sbk)])
                if n > nnear:
                    sch.add("act", lambda e, sbk=sbk, pb=pb, nnear=nnear, n=n, h=h: e.activation(
                        out=PTb[pb][:, nnear:n], in_=ps[sbk][:, nnear:n], func=AF.Exp, scale=0.125, bias=rb31[:, h:h + 1]),
                        reads=[psk(sbk), "rb31"], writes=[("PT", pb), psk(sbk)])
                soff_ = _selT_off(i) + (jlo - i) * 128
                sch.add("dve", lambda e, pb=pb, n=n, soff_=soff_: e.tensor_tensor(out=PTb[pb][:, 0:n], in0=PTb[pb][:, 0:n],
                                                                                 in1=selT[:, soff_:soff_ + n], op=ALU.mult),
                        reads=[("PT", pb)] + [("selT", i, j) for j in range(jlo, 4 * J + 4)], writes=[("PT", pb)])
                for j in range(jlo, min(4 * J + 4, i + 2)):
                    dlt = j - i
                    cs = slice((j - jlo) * 128, (j - jlo + 1) * 128)
                    sch.add("dve", lambda e, pb=pb, cs=cs, h=h, dlt=dlt: e.tensor_tensor(out=PTb[pb][:, cs], in0=PTb[pb][:, cs],
                                                                                         in1=Etile[:, h, dlt, :], op=ALU.mult),
                            reads=[("PT", pb), "Etile"], writes=[("PT", pb)])

                def back(pb=pb, jlo=jlo, J=J, i=i, h=h, accv=accv, first=first, accb=accb):
                    def pv(e):
                        ins = None
                        for j in range(jlo, 4 * J + 4):
                            cs = slice((j - jlo) * 128, (j - jlo + 1) * 128)
                            ins = e.matmul(accv[:, j - 4 * J, :], lhsT=PTb[pb][:, cs], rhs=vaT[:, i, h * 65:(h + 1) * 65],
                                           start=first[0], stop=False, skip_group_check=True)
                            first[0] = False
                        return ins
                    sch.add("pe", pv, reads=[("PT", pb), ("va", i)], writes=[psk(accb)])
                    if i == 4 * J + 3:
                        sch.add("dve", lambda e: e.reciprocal(out=rden[:], in_=accv[:, :, 64]), reads=[psk(accb)], writes=["rden", psk(accb)])
                        sch.add("dve", lambda e: e.tensor_tensor(
                            out=mixa[:, 4 * J:4 * J + 4, h * 64:(h + 1) * 64], in0=accv[:, :, 0:64],
                            in1=rden[:, :].unsqueeze(2).to_broadcast([P, 4, 64]), op=ALU.mult),
                            reads=[psk(accb), "rden"], writes=[("mixa", 4 * J + jj, h) for jj in range(4)] + [psk(accb)])
                pend.append(back)
                flush(LOOKAHEAD)
    flush(0)
    dump("mixa", mixa, dbg.get("mixa").rearrange("(t p) c -> p t c", p=P) if "mixa" in dbg else None,
         [("mixa", T, h) for T in range(NT) for h in range(8)])
    for T in range(NT):
        tsl = slice(T * P, (T + 1) * P)
        tb = next_bank(0, 6)

        def tra(e, T=T, tb=tb):
            o = ps[tb][:].bitcast(BF16).rearrange("p (c t) -> p c t", c=8)
            ins = None
            for c in range(4):
                ins = e.transpose(o[:, c, :], mixa[:, T, c * P:(c + 1) * P], ident_bf[:])
            return ins
        sch.add("pe", tra, reads=[("mixa", T, h) for h in range(8)] + ["ident_bf"], writes=[psk(tb)])
        sch.add("act", lambda e, tsl=tsl, tb=tb: e.activation(
            out=mixT_a[:, :, tsl], in_=ps[tb][:].bitcast(BF16).rearrange("p (c t) -> p c t", c=8)[:, 0:4, :], func=AF.Copy),
            reads=[psk(tb)], writes=[("mixT_a", T)])
    if STOP_AFTER == "attn":
        return finish()
    sch.barrier()

    x1 = view(R4, 0, [P, NT, D], F32)
    woutb = view(R4, 64 * KB, [P, 8, D], BF16)
    xst5 = [view(R4, 80 * KB + i * 4 * KB, [P, D], F32) for i in range(2)]
    xs5 = [view(R4, 88 * KB + i * 2 * KB, [P, D], BF16) for i in range(2)]
    junk5 = view(R4, 92 * KB, [P, 2048], BF16)
    g2bc = view(R4, 96 * KB, [P, D], F32)
    wrb = view(R4, 100 * KB, [P, 8, 36], BF16)
    h2T = view(R1, 0, [P, 8, S], BF16)
    ssq2 = sb("ssq2", [P, NT], F32)
    rstd2 = sb("rstd2", [P, NT], F32)
    logit = sb("logit", [P, NT, 36], F32)
    for hh in range(2):
        sch.add("pool", lambda e, hh=hh: e.dma_start(out=woutb[:, :, hh * 512:(hh + 1) * 512],
                                                     in_=wout_d[:, hh * 512:(hh + 1) * 512].rearrange("(c p) n -> p c n", p=P)),
                writes=[("wout", hh)], dma=True)
    sch.add("pool", lambda e: e.dma_start(out=wrb, in_=wr_d.rearrange("(c p) n -> p c n", p=P)), writes=["wrb"], dma=True)
    sch.add("sp", lambda e: e.dma_start(out=g2bc, in_=g2bc_d[:, :]), writes=["g2bc"], dma=True)
    pend5 = []
    for T in range(NT):
        b = T % 2
        tsl = slice(T * P, (T + 1) * P)
        sch.add("sp", lambda e, b=b, tsl=tsl: e.dma_start(out=xst5[b], in_=x_d[tsl, :]), writes=[("xst5", b)], dma=True)
        for hh in range(2):
            bank = next_bank(0, 4)

            def om(e, T=T, hh=hh, bank=bank):
                ins = None
                for c in range(8):
                    src = mixT_a if c < 4 else mixT_b
                    ins = e.matmul(ps[bank][:], lhsT=src[:, c % 4, T * P:(T + 1) * P], rhs=woutb[:, c, hh * 512:(hh + 1) * 512],
                                   start=(c == 0), stop=(c == 7))
                return ins
            sch.add("pe", om, reads=[("mixT_a", T), ("mixT_b", T), ("wout", hh)], writes=[psk(bank)])
            sch.add("dve", lambda e, T=T, hh=hh, bank=bank, b=b: e.tensor_tensor(
                out=x1[:, T, hh * 512:(hh + 1) * 512], in0=ps[bank][:], in1=xst5[b][:, hh * 512:(hh + 1) * 512], op=ALU.add),
                reads=[psk(bank), ("xst5", b)], writes=[("x1", T, hh)])
        sch.add("act", lambda e, T=T: e.activation(out=junk5[:, 0:D], in_=x1[:, T, :], func=AF.Square, accum_out=ssq2[:, T:T + 1]),
                reads=[("x1", T, 0), ("x1", T, 1)], writes=["junk5", ("ssq2", T)])
        sch.add("act", lambda e, T=T: e.activation(out=rstd2[:, T:T + 1], in_=ssq2[:, T:T + 1], func=AF.Sqrt, scale=1.0 / D,
                                                   bias=eps_col[:, 0:1]),
                reads=[("ssq2", T), "eps_col"], writes=[("rstd2", T)])
        sch.add("dve", lambda e, T=T: e.reciprocal(out=rstd2[:, T:T + 1], in_=rstd2[:, T:T + 1]), reads=[("rstd2", T)], writes=[("rstd2", T)])
        sch.add("dve", lambda e, T=T, b=b: e.scalar_tensor_tensor(out=xs5[b], in0=x1[:, T, :], scalar=rstd2[:, T:T + 1], in1=g2bc,
                                                                  op0=ALU.mult, op1=ALU.mult),
                reads=[("x1", T, 0), ("x1", T, 1), ("rstd2", T), "g2bc"], writes=[("xs5", b)])
        def back5(T=T, b=b, tsl=tsl):
            tb = next_bank(4, 6)

            def tr5(e, b=b, tb=tb):
                o = ps[tb][:].bitcast(BF16).rearrange("p (c t) -> p c t", c=8)
                ins = None
                for c in range(8):
                    ins = e.transpose(o[:, c, :], xs5[b][:, c * P:(c + 1) * P], ident_bf[:])
                return ins
            sch.add("pe", tr5, reads=[("xs5", b), "ident_bf"], writes=[psk(tb)])
            sch.add("act", lambda e, tb=tb, tsl=tsl: e.activation(
                out=h2T[:, :, tsl], in_=ps[tb][:].bitcast(BF16).rearrange("p (c t) -> p c t", c=8), func=AF.Copy),
                reads=[psk(tb)], writes=[("h2T", T)])
            lbk = next_bank(6, 8)

            def rmm(e, T=T, lbk=lbk):
                ins = None
                for c in range(8):
                    ins = e.matmul(ps[lbk][:, 0:36], lhsT=h2T[:, c, T * P:(T + 1) * P], rhs=wrb[:, c, :], start=(c == 0), stop=(c == 7))
                return ins
            sch.add("pe", rmm, reads=[("h2T", T), "wrb"], writes=[psk(lbk)])
            sch.add("dve", lambda e, T=T, lbk=lbk: e.tensor_tensor(out=logit[:, T, :], in0=ps[lbk][:, 0:36], in1=brbc[:], op=ALU.add),
                    reads=[psk(lbk), "brbc"], writes=["logit"])
        pend5.append(back5)
        while len(pend5) > 1:
            pend5.pop(0)()
    while pend5:
        pend5.pop(0)()
    dump("x1", x1, dbg.get("x1").rearrange("(t p) c -> p t c", p=P) if "x1" in dbg else None,
         [("x1", T, hh) for T in range(NT) for hh in range(2)])
    if STOP_AFTER == "x1":
        return finish()

    gl = logit[:, :, 0:4]
    el = logit[:, :, 4:36].rearrange("p t (g e) -> p t g e", g=4)
    _ro = [101 * KB]

    def rv(shape):
        nb = int(np.prod(shape[1:])) * 4
        v = view(R4, _ro[0], shape, F32)
        _ro[0] += nb
        return v
    gmax = rv([P, NT])
    goh = rv([P, NT, 4])
    gsh = rv([P, NT, 4])
    gsum = rv([P, NT])
    gw = rv([P, NT])
    etmp = rv([P, NT, 4, 8])
    esel = rv([P, NT, 8])
    esel2 = rv([P, NT, 8])
    m1 = rv([P, NT])
    m2 = rv([P, NT])
    oh1 = rv([P, NT, 8])
    oh2 = rv([P, NT, 8])
    dd = rv([P, NT])
    w1 = rv([P, NT])
    w2 = rv([P, NT])
    gsel = rv([P, NT, 8])
    assert _ro[0] <= 110 * KB
    gates = view(R4, 110 * KB, [P, NT, 4, 8], F32)

    def D_(fn, reads, writes):
        sch.add("dve", fn, reads=reads, writes=writes)

    def bc3(ap2, n):
        return ap2.unsqueeze(2).to_broadcast([P, NT, n])
    D_(lambda e: e.tensor_reduce(out=gmax[:], in_=gl, axis=AX.X, op=ALU.max), ["logit"], ["gmax"])
    D_(lambda e: e.tensor_tensor(out=goh[:], in0=gl, in1=bc3(gmax[:, :], 4), op=ALU.is_equal), ["logit", "gmax"], ["goh"])
    D_(lambda e: e.tensor_tensor(out=gsh[:], in0=gl, in1=bc3(gmax[:, :], 4), op=ALU.subtract), ["logit", "gmax"], ["gsh"])
    sch.add("act", lambda e: e.activation(out=gsh[:], in_=gsh[:], func=AF.Exp), reads=["gsh"], writes=["gsh"])
    D_(lambda e: e.tensor_reduce(out=gsum[:], in_=gsh[:], axis=AX.X, op=ALU.add), ["gsh"], ["gsum"])
    D_(lambda e: e.reciprocal(out=gw[:], in_=gsum[:]), ["gsum"], ["gw"])
    D_(lambda e: e.tensor_tensor(out=etmp[:], in0=el, in1=goh[:, :, :].unsqueeze(3).to_broadcast([P, NT, 4, 8]), op=ALU.mult),
       ["logit", "goh"], ["etmp"])
    D_(lambda e: e.tensor_reduce(out=esel[:], in_=etmp[:, :, :, :].rearrange("p t g e -> p t e g"), axis=AX.X, op=ALU.add), ["etmp"], ["esel"])
    D_(lambda e: e.tensor_reduce(out=m1[:], in_=esel[:], axis=AX.X, op=ALU.max), ["esel"], ["m1"])
    D_(lambda e: e.tensor_tensor(out=oh1[:], in0=esel[:], in1=bc3(m1[:, :], 8), op=ALU.is_equal), ["esel", "m1"], ["oh1"])
    D_(lambda e: e.scalar_tensor_tensor(out=esel2[:], in0=oh1[:], scalar=-1e30, in1=esel[:], op0=ALU.mult, op1=ALU.add), ["oh1", "esel"], ["esel2"])
    D_(lambda e: e.tensor_reduce(out=m2[:], in_=esel2[:], axis=AX.X, op=ALU.max), ["esel2"], ["m2"])
    D_(lambda e: e.tensor_tensor(out=oh2[:], in0=esel2[:], in1=bc3(m2[:, :], 8), op=ALU.is_equal), ["esel2", "m2"], ["oh2"])
    D_(lambda e: e.tensor_tensor(out=dd[:], in0=m2[:], in1=m1[:], op=ALU.subtract), ["m1", "m2"], ["dd"])
    sch.add("act", lambda e: e.activation(out=dd[:], in_=dd[:], func=AF.Exp), reads=["dd"], writes=["dd"])
    D_(lambda e: e.tensor_scalar(out=w1[:], in0=dd[:], scalar1=1.0, scalar2=None, op0=ALU.add), ["dd"], ["w1"])
    D_(lambda e: e.reciprocal(out=w1[:], in_=w1[:]), ["w1"], ["w1"])
    D_(lambda e: e.tensor_tensor(out=w2[:], in0=dd[:], in1=w1[:], op=ALU.mult), ["dd", "w1"], ["w2"])
    D_(lambda e: e.tensor_tensor(out=w1[:], in0=w1[:], in1=gw[:], op=ALU.mult), ["w1", "gw"], ["w1"])
    D_(lambda e: e.tensor_tensor(out=w2[:], in0=w2[:], in1=gw[:], op=ALU.mult), ["w2", "gw"], ["w2"])
    D_(lambda e: e.tensor_tensor(out=oh1[:], in0=oh1[:], in1=bc3(w1[:, :], 8), op=ALU.mult), ["oh1", "w1"], ["oh1"])
    D_(lambda e: e.tensor_tensor(out=oh2[:], in0=oh2[:], in1=bc3(w2[:, :], 8), op=ALU.mult), ["oh2", "w2"], ["oh2"])
    D_(lambda e: e.tensor_tensor(out=gsel[:], in0=oh1[:], in1=oh2[:], op=ALU.add), ["oh1", "oh2"], ["gsel"])
    D_(lambda e: e.tensor_tensor(out=gates[:], in0=goh[:, :, :].unsqueeze(3).to_broadcast([P, NT, 4, 8]),
                                 in1=gsel[:, :, :].unsqueeze(2).to_broadcast([P, NT, 4, 8]), op=ALU.mult), ["goh", "gsel"], ["gates"])
    dump("gates", gates[:, :, :, :].rearrange("p t g e -> p t (g e)"),
         dbg.get("gates").rearrange("(t p) c -> p t c", p=P) if "gates" in dbg else None, ["gates"])
    if STOP_AFTER == "router":
        return finish()
    sch.barrier()

    hid = [view(R4, 64 * KB + i * 8 * KB, [P, 2, S], BF16) for i in range(2)]
    gT = view(R4, 80 * KB, [32, 2, S], BF16, parts=32)
    onehot = view(R4, 88 * KB, [32, 32, 128], BF16, parts=32)
    sa = [view(R4, 96 * KB + i * 2 * KB, [P, 512], F32) for i in range(2)]
    t1 = [view(R4, 100 * KB + i * 2 * KB, [P, 512], F32) for i in range(2)]
    wbuf = []
    for RR in (R2, R3):
        wbuf.append((view(RR, 0, [P, 8, 256], BF16), view(RR, 4 * KB, [P, 8, 256], BF16), view(RR, 8 * KB, [P, 2, D], BF16)))
    sch.add("sp", lambda e: e.dma_start(out=onehot, in_=onehot_d.rearrange("e (k m) -> e k m", k=32)), writes=["onehot"], dma=True)
    for T4 in range(4):
        tb = next_bank(0, 4)

        def trg(e, T4=T4, tb=tb):
            ins = None
            for k in range(4):
                T = T4 * 4 + k
                ins = e.transpose(ps[tb][0:32, k * 128:(k + 1) * 128], gates[:, T, :, :].rearrange("p g e -> p (g e)"), ident_f[:])
            return ins
        sch.add("pe", trg, reads=["gates", "ident_f"], writes=[psk(tb)])
        sl = slice(T4 * 512, (T4 + 1) * 512)
        sch.add("act", lambda e, tb=tb, sl=sl: e.activation(out=gT[0:32, 0, sl], in_=ps[tb][0:32, :], func=AF.Copy),
                reads=[psk(tb)], writes=[("gThi", T4), psk(tb)])
        sch.add("dve", lambda e, tb=tb, sl=sl: e.tensor_tensor(out=gT[0:32, 1, sl], in0=ps[tb][0:32, :], in1=gT[0:32, 0, sl], op=ALU.subtract),
                reads=[psk(tb), ("gThi", T4)], writes=[("gTlo", T4), psk(tb)])
    mcnt = [0]
    for pr in range(NE // 2):
      for ex in (2 * pr, 2 * pr + 1):
        wbi = ex % 2
        Wg, Wu, Wd = wbuf[wbi]
        sch.add("pool", lambda e, Wg=Wg, ex=ex: e.dma_start(out=Wg, in_=wg_d[ex].rearrange("(c p) f -> p c f", p=P)),
                writes=[("Wg", wbi)], dma=True)
        sch.add("pool", lambda e, Wu=Wu, ex=ex: e.dma_start(out=Wu, in_=wu_d[ex].rearrange("(c p) f -> p c f", p=P)),
                writes=[("Wu", wbi)], dma=True)
        for hh in range(2):
            sch.add("pool", lambda e, Wd=Wd, ex=ex, hh=hh: e.dma_start(
                out=Wd[:, :, hh * 512:(hh + 1) * 512], in_=wd_d[ex][:, hh * 512:(hh + 1) * 512].rearrange("(c p) n -> p c n", p=P)),
                writes=[("Wd", wbi, hh)], dma=True)
        hb = ex % 2
        for tc in range(4):
            sl = slice(tc * 512, (tc + 1) * 512)
            gb = mcnt[0] % 2
            mcnt[0] += 1

            def gmm(e, ex=ex, gb=gb, sl=sl):
                e.matmul(ps[gb][:], lhsT=onehot[0:32, ex, :], rhs=gT[0:32, 0, sl], start=True, stop=False)
                return e.matmul(ps[gb][:], lhsT=onehot[0:32, ex, :], rhs=gT[0:32, 1, sl], start=False, stop=True)
            sch.add("pe", gmm, reads=["onehot", ("gThi", tc), ("gTlo", tc)], writes=[psk(gb)])
            for ft in range(2):
                ab = 2 + (mcnt[0] % 2)
                ub = 4 + (mcnt[0] % 2)
                tb_ = mcnt[0] % 2
                mcnt[0] += 1

                def amm(e, Wg=Wg, ft=ft, sl=sl, ab=ab):
                    ins = None
                    for c in range(8):
                        ins = e.matmul(ps[ab][:], lhsT=Wg[:, c, ft * 128:(ft + 1) * 128], rhs=h2T[:, c, sl], start=(c == 0), stop=(c == 7))
                    return ins

                def umm2(e, Wu=Wu, ft=ft, sl=sl, ub=ub):
                    ins = None
                    for c in range(8):
                        ins = e.matmul(ps[ub][:], lhsT=Wu[:, c, ft * 128:(ft + 1) * 128], rhs=h2T[:, c, sl], start=(c == 0), stop=(c == 7))
                    return ins
                hkeys = [("h2T", tc * 4 + k) for k in range(4)]
                sch.add("pe", amm, reads=[("Wg", wbi)] + hkeys, writes=[psk(ab)])
                sch.add("pe", umm2, reads=[("Wu", wbi)] + hkeys, writes=[psk(ub)])
                sch.add("act", lambda e, ab=ab, tb_=tb_: e.activation(out=sa[tb_], in_=ps[ab][:], func=AF.Silu),
                        reads=[psk(ab)], writes=[("sa", tb_), psk(ab)])
                sch.add("dve", lambda e, ub=ub, tb_=tb_: e.tensor_tensor(out=t1[tb_], in0=ps[ub][:], in1=sa[tb_], op=ALU.mult),
                        reads=[psk(ub), ("sa", tb_)], writes=[("t1", tb_), psk(ub)])
                sch.add("dve", lambda e, gb=gb, tb_=tb_, hb=hb, ft=ft, sl=sl: e.tensor_tensor(out=hid[hb][:, ft, sl], in0=ps[gb][:], in1=t1[tb_],
                                                                                             op=ALU.mult),
                        reads=[psk(gb), ("t1", tb_)], writes=[("hid", hb, tc), psk(gb)])
      WdA, WdB = wbuf[0][2], wbuf[1][2]
      for T in range(NT):
            for hh in range(2):
                yb = 6 + (mcnt[0] % 2)
                mcnt[0] += 1

                def dmm(e, T=T, hh=hh, yb=yb):
                    ins = None
                    k = 0
                    for (hd, Wd_) in ((hid[0], WdA), (hid[1], WdB)):
                        for ft in range(2):
                            ins = e.matmul(ps[yb][:], lhsT=hd[:, ft, T * P:(T + 1) * P], rhs=Wd_[:, ft, hh * 512:(hh + 1) * 512],
                                           start=(k == 0), stop=(k == 3))
                            k += 1
                    return ins
                sch.add("pe", dmm, reads=[("hid", 0, T // 4), ("hid", 1, T // 4), ("Wd", 0, hh), ("Wd", 1, hh)], writes=[psk(yb)])
                sch.add("dve", lambda e, T=T, hh=hh, yb=yb: e.tensor_tensor(out=x1[:, T, hh * 512:(hh + 1) * 512], in0=ps[yb][:],
                                                                           in1=x1[:, T, hh * 512:(hh + 1) * 512], op=ALU.add),
                        reads=[psk(yb), ("x1", T, hh)], writes=[("x1", T, hh), psk(yb)])
    for T in range(NT):
        tsl = slice(T * P, (T + 1) * P)
        sch.add("sp", lambda e, T=T, tsl=tsl: e.dma_start(out=out_d[tsl, :], in_=x1[:, T, :]), reads=[("x1", T, 0), ("x1", T, 1)],
                writes=["out_%d" % T], dma=True)
    return finish()


_CACHE = {}


def _prep_shared(inputs):
    f32 = np.float32
    c = host_constants()
    w_in = np.asarray(inputs["w_in"][0], f32)
    shared = {}
    shared["w1"] = np.ascontiguousarray(w_in[:, W1_COLS])
    shared["wout"] = np.ascontiguousarray(np.asarray(inputs["w_out"][0], f32))
    shared["wr"] = np.ascontiguousarray(np.concatenate([np.asarray(inputs["w_router_group"][0], f32),
                                                        np.asarray(inputs["w_router_expert"][0], f32)], axis=1))
    shared["wg"] = np.ascontiguousarray(np.asarray(inputs["w_exp_gate"][0], f32))
    shared["wu"] = np.ascontiguousarray(np.asarray(inputs["w_exp_up"][0], f32))
    shared["wd"] = np.ascontiguousarray(np.asarray(inputs["w_exp_down"][0], f32))
    shared["w2aug"] = np.ascontiguousarray(np.concatenate([np.asarray(inputs["gla_gate_w2"][0], f32),
                                                           np.asarray(inputs["gla_gate_b"], f32).reshape(1, 256)], axis=0))
    shared["g1bc"] = np.ascontiguousarray(np.tile(np.asarray(inputs["norm1_g"], f32).reshape(1, D), (P, 1)))
    shared["g2bc"] = np.ascontiguousarray(np.tile(np.asarray(inputs["norm2_g"], f32).reshape(1, D), (P, 1)))
    qg = np.tile(np.asarray(inputs["q_norm_g"], f32).reshape(64), 2)
    kg = np.tile(np.asarray(inputs["k_norm_g"], f32).reshape(64), 2)
    shared["qkg"] = np.ascontiguousarray(np.stack([qg, kg], axis=1))
    shared["goutbc"] = np.ascontiguousarray(np.tile(np.asarray(inputs["gla_out_norm_g"], f32).reshape(1, 128), (P, 4)))
    br = np.concatenate([np.asarray(inputs["b_router_group"], f32).reshape(4),
                         np.asarray(inputs["b_router_expert"], f32).reshape(32)])
    shared["brbc"] = np.ascontiguousarray(np.tile(br.reshape(1, 36), (P, 1)))
    rb = np.asarray(inputs["rel_bias"], f32)
    idx = np.arange(128)
    bT = np.zeros((P, 8, 2, 128), f32)
    for dlt in range(2):
        dist = 128 * dlt + idx[None, :] - idx[:, None]
        bk = _t5_bucket_np(np.maximum(dist, 0))
        for h in range(8):
            bT[:, h, dlt, :] = rb[bk, h]
    shared["biasT"] = np.ascontiguousarray(bT.reshape(P, -1))
    shared["rb31"] = np.ascontiguousarray(np.tile(rb[31:32, :], (P, 1)))
    for k in ["ident_bf", "ident_f", "tri_bf", "tri_f", "after_f", "negmask", "blockones", "pow2"]:
        shared[k] = c[k]
    shared["onehot"] = np.ascontiguousarray(c["onehot"].reshape(32, -1))
    return shared


def kernel(**inputs):
    x = np.asarray(inputs["x"], np.float32)
    if "nc" not in _CACHE:
        _CACHE["nc"] = build_program()
    nc = _CACHE["nc"]
    shared = _prep_shared(inputs)
    in_maps = []
    for b in range(8):
        m = dict(shared)
        m["x"] = np.ascontiguousarray(x[b])
        in_maps.append(m)
    res = run_bass_kernel_spmd(nc, in_maps, core_ids=list(range(8)))
    _CACHE["last"] = res
    out = np.stack([np.asarray(r["out"], np.float32) for r in res.results], axis=0)
    return out
```

```python
import os
import numpy as np
import ml_dtypes
from contextlib import ExitStack
import concourse.bass as bass
import concourse.mybir as mybir
from concourse.bass_utils import run_bass_kernel_spmd

F32 = mybir.dt.float32
BF16 = mybir.dt.bfloat16
U8 = mybir.dt.uint8
AF = mybir.ActivationFunctionType
ALU = mybir.AluOpType
AX = mybir.AxisListType

P = 128
S = 2048
D = 1024
NT = 16
NE = 32
EPS = 1e-6
KITER = 18
DEBUG = {}
STOP_AFTER = None


class _Op:
    __slots__ = ("eng", "fn", "reads", "writes", "dma", "deps", "waits", "signal", "idx", "flag", "slotwait", "after")

    def __init__(self, eng, fn, reads, writes, dma):
        self.eng = eng
        self.fn = fn
        self.reads = reads
        self.writes = writes
        self.dma = dma
        self.deps = ()
        self.waits = []
        self.signal = None
        self.flag = False
        self.slotwait = None
        self.after = []


def _nofn(e):
    return None


class Sched:
    EPOCH = 12000
    RING = 8

    def __init__(self, nc, stack):
        self.nc = nc
        self.stack = stack
        self.ops = []

    def add(self, eng, fn, reads=(), writes=(), dma=False):
        reads = list(reads)
        writes = list(writes)
        for k in reads:
            if isinstance(k, tuple) and k and k[0] == "ps" and k not in writes:
                writes.append(k)
        self.ops.append(_Op(eng, fn, tuple(reads), tuple(writes), dma))

    def barrier(self):
        pos = getattr(self, "_barpos", 0)
        last = {}
        dmas = []
        for i, op in enumerate(self.ops):
            if i < pos:
                continue
            if op.dma:
                dmas.append(op)
            elif op.fn is not _nofn:
                last[op.eng] = op
        for eng in ("pe", "act", "dve", "pool", "sp"):
            b = _Op(eng, _nofn, (), (), False)
            b.after = [p for k, p in last.items() if k != eng] + dmas
            self.ops.append(b)
        self._barpos = len(self.ops)

    def finalize(self):
        nc = self.nc
        last_w = {}
        readers = {}
        for i, op in enumerate(self.ops):
            op.idx = i
            raw = set()
            other = set()
            for k in op.reads:
                if k in last_w:
                    raw.add(last_w[k])
            for k in op.writes:
                if k in last_w:
                    raw.add(last_w[k])
                for r in readers.get(k, ()):
                    other.add(r)
            raw.discard(i)
            other.discard(i)
            for k in op.reads:
                readers.setdefault(k, set()).add(i)
            for k in op.writes:
                last_w[k] = i
                readers[k] = set()
            best = {}
            deps = []
            for d in raw | other:
                p = self.ops[d]
                if p.dma:
                    deps.append(d)
                    continue
                if p.eng == op.eng and not op.dma:
                    if p.eng == "pe":
                        continue
                    if d not in raw:
                        continue
                if p.eng not in best or best[p.eng] < d:
                    best[p.eng] = d
            deps.extend(best.values())
            for a in op.after:
                deps.append(a.idx)
            op.deps = deps
            for d in deps:
                self.ops[d].flag = True
        cnt = {}
        self.sems = {}
        dcount = {}
        for op in self.ops:
            if op.dma:
                q = op.eng
                k = dcount.get(q, 0)
                dcount[q] = k + 1
                slot = k % self.RING
                name = "dq_%s_%d" % (q, slot)
                if name not in self.sems:
                    self.sems[name] = self.stack.enter_context(nc.semaphore(name))
                op.signal = (name, 16 * (k // self.RING + 1), 16)
                if k >= self.RING:
                    op.slotwait = (name, 16 * (k // self.RING))
            elif op.flag:
                c = cnt.get(op.eng, 0)
                ep = c // self.EPOCH
                name = "s_%s_%d" % (op.eng, ep)
                if name not in self.sems:
                    self.sems[name] = self.stack.enter_context(nc.semaphore(name))
                op.signal = (name, c % self.EPOCH + 1, 1)
                cnt[op.eng] = c + 1
        for op in self.ops:
            w = []
            if op.slotwait is not None:
                w.append(op.slotwait)
            for d in op.deps:
                sg = self.ops[d].signal
                w.append((sg[0], sg[1]))
            op.waits = w

    def emit(self, block):
        table = [("pe", block.tensor), ("act", block.scalar), ("dve", block.vector),
                 ("pool", block.gpsimd), ("sp", block.sync)]
        for engname, deco in table:
            ops = [op for op in self.ops if op.eng == engname]

            def body(e, ops=ops):
                seen = {}
                for op in ops:
                    for (sn, val) in op.waits:
                        if seen.get(sn, 0) >= val:
                            continue
                        e.wait_ge(self.sems[sn], val)
                        seen[sn] = val
                    ins = op.fn(e)
                    if op.signal is not None and ins is not None:
                        ins.then_inc(self.sems[op.signal[0]], op.signal[2])

            deco(body)


IQ_TILES = [(0, 3), (3, 6), (6, 8)]


def _t5_bucket_np(dist):
    max_exact = 16
    d_f = np.maximum(dist, 1).astype(np.float32)
    large = max_exact + (np.log(d_f / max_exact) / np.log(128 / max_exact) * (32 - max_exact)).astype(np.int32)
    large = np.minimum(large, 31)
    return np.where(dist < max_exact, dist, large)


def _w1_columns():
    A = 512
    off = {}
    o = 0
    for name, n in [("qa", 512), ("ka", 512), ("va", 512), ("iq", 256), ("ik", 32), ("iw", 8),
                    ("qb", 256), ("kb", 256), ("vb", 512), ("glr", 16), ("rg", 512)]:
        off[name] = o
        o += n
    cols = []
    groups = []

    def grp(name, kind, cl):
        groups.append((name, kind, len(cols), len(cl)))
        cols.extend(cl)

    r = lambda name, a, b: list(range(off[name] + a, off[name] + b))
    grp("qbkb", "fm", r("qb", 0, 256) + r("kb", 0, 256) + r("glr", 0, 16))
    grp("vb", "tm", r("vb", 0, 512))
    grp("rg", "tm", r("rg", 0, 512))
    grp("kbiw", "tm", r("kb", 0, 256) + r("iw", 0, 8))
    grp("qa", "fm", r("qa", 0, 512))
    grp("ka", "fm", r("ka", 0, 512))
    iqc = []
    for (h0, h1) in IQ_TILES:
        iqc += r("iq", h0 * 32, h1 * 32)
    grp("iqik", "fm", iqc + r("ik", 0, 32) * 3)
    grp("va", "tm", r("va", 0, 512))
    return np.array(cols, np.int64), groups


W1_COLS, W1_GROUPS = _w1_columns()
NW1 = len(W1_COLS)


def _selT_off(i):
    return sum((16 - ii) * 128 for ii in range(i))


SELT_TOTAL = _selT_off(16)


def host_constants():
    c = {}
    idx = np.arange(128)
    tri = (idx[:, None] <= idx[None, :])
    c["ident_bf"] = np.eye(128, dtype=np.float32).astype(ml_dtypes.bfloat16)
    c["ident_f"] = np.eye(128, dtype=np.float32)
    c["tri_bf"] = tri.astype(np.float32).astype(ml_dtypes.bfloat16)
    c["tri_f"] = tri.astype(np.float32)
    c["after_f"] = (idx[:, None] > idx[None, :]).astype(np.float32)
    c["negmask"] = np.where(idx[None, :] <= idx[:, None], 0.0, -1e30).astype(np.float32)
    bo = np.zeros((128, 128), np.float32)
    bo[:64, :64] = 1.0
    bo[64:, 64:] = 1.0
    c["blockones"] = bo.astype(ml_dtypes.bfloat16)
    c["pow2"] = np.tile((2.0 ** -np.arange(KITER + 1, dtype=np.float64)).astype(np.float32)[None, :], (128, 1))
    oh = np.zeros((32, 32, 128), np.float32)
    for e in range(32):
        oh[e, e, :] = 1.0
    c["onehot"] = oh.astype(ml_dtypes.bfloat16)
    return c


def build_program():
    nc = bass.Bass("TRN2", target_bir_lowering=False)
    stack = ExitStack()
    sch = Sched(nc, stack)

    def dram(name, shape, dt, kind="ExternalInput"):
        return nc.dram_tensor(name, list(shape), dt, kind=kind).ap()

    x_d = dram("x", [S, D], F32)
    w1_d = dram("w1", [D, NW1], F32)
    wout_d = dram("wout", [D, D], F32)
    wr_d = dram("wr", [D, 36], F32)
    wg_d = dram("wg", [NE, D, 256], F32)
    wu_d = dram("wu", [NE, D, 256], F32)
    wd_d = dram("wd", [NE, 256, D], F32)
    w2_d = dram("w2aug", [17, 256], F32)
    g1bc_d = dram("g1bc", [P, D], F32)
    g2bc_d = dram("g2bc", [P, D], F32)
    qkg_d = dram("qkg", [P, 2], F32)
    gout_d = dram("goutbc", [P, 512], F32)
    brbc_d = dram("brbc", [P, 36], F32)
    biasT_d = dram("biasT", [P, 8 * 2 * 128], F32)
    rb31_d = dram("rb31", [P, 8], F32)
    ident_bf_d = dram("ident_bf", [P, P], BF16)
    ident_f_d = dram("ident_f", [P, P], F32)
    tri_bf_d = dram("tri_bf", [P, P], BF16)
    tri_f_d = dram("tri_f", [P, P], F32)
    after_f_d = dram("after_f", [P, P], F32)
    negmask_d = dram("negmask", [P, P], F32)
    blockones_d = dram("blockones", [P, P], BF16)
    pow2_d = dram("pow2", [P, KITER + 1], F32)
    onehot_d = dram("onehot", [32, 32 * 128], BF16)
    out_d = dram("out", [S, D], F32, kind="ExternalOutput")
    dbg = {}
    for name, (shape, dt) in DEBUG.items():
        dbg[name] = dram("dbg_" + name, shape, dt, kind="ExternalOutput")

    def sb(name, shape, dt):
        return stack.enter_context(nc.sbuf_tensor(name, list(shape), dt))

    ident_bf = sb("ident_bf_s", [P, P], BF16)
    ident_f = sb("ident_f_s", [P, P], F32)
    tri_bf = sb("tri_bf_s", [P, P], BF16)
    tri_f = sb("tri_f_s", [P, P], F32)
    after_f = sb("after_f_s", [P, P], F32)
    negmask = sb("negmask_s", [P, P], F32)
    blockones = sb("blockones_s", [P, P], BF16)
    pow2 = sb("pow2_s", [P, KITER + 1], F32)
    g1bc = sb("g1bc_s", [P, D], F32)
    qkg = sb("qkg_s", [P, 2], F32)
    gout = sb("gout_s", [P, 512], F32)
    brbc = sb("brbc_s", [P, 36], F32)
    rb31 = sb("rb31_s", [P, 8], F32)
    Etile = sb("Etile", [P, 8, 2, 128], BF16)
    w2aug = sb("w2aug_s", [17, 256], BF16)
    iw_s = sb("iw_s", [P, NT, 8], F32)
    ssq1 = sb("ssq1", [P, NT], F32)
    rstd1 = sb("rstd1", [P, NT], F32)
    ones_col = sb("ones_col", [P, 1], F32)

    R1 = sb("R1", [P, 32 * 1024], U8)
    R2 = sb("R2", [P, 16 * 1024], U8)
    R3 = sb("R3", [P, 16 * 1024], U8)
    R4 = sb("R4", [P, 112 * 1024], U8)

    def view(arena, off, shape, dt, parts=P):
        nb = int(np.prod(shape[1:])) * (4 if dt == F32 else (2 if dt == BF16 else 1))
        ap = arena[0:parts, off:off + nb].bitcast(dt)
        if len(shape) == 3:
            ap = ap.rearrange("p (a b) -> p a b", a=shape[1])
        elif len(shape) == 4:
            ap = ap.rearrange("p (a b c) -> p a b c", a=shape[1], b=shape[2])
        return ap

    KB = 1024
    hT = view(R1, 0, [P, 8, S], BF16)
    mixT_b = view(R2, 0, [P, 4, S], BF16)
    mixT_a = view(R3, 0, [P, 4, S], BF16)
    qbT = view(R4, 0, [P, 2, S], BF16)
    kbT = view(R4, 8 * KB, [P, 2, S], BF16)
    glrT = view(R4, 16 * KB, [32, S], BF16, parts=32)
    vb = view(R4, 20 * KB, [P, NT, 512], BF16)
    Gt = view(R4, 36 * KB, [P, NT, 512], BF16)
    kbtm = view(R4, 52 * KB, [P, NT, 256], BF16)
    wst = [view(R4, 60 * KB + i * 9 * KB, [P, 8, 528], BF16) for i in range(2)]
    xst = [view(R4, 78 * KB + i * 4 * KB, [P, D], F32) for i in range(2)]
    xs = [view(R4, 86 * KB + i * 2 * KB, [P, D], BF16) for i in range(2)]
    junk = view(R4, 90 * KB, [P, 2048], BF16)
    tmpA = [view(R4, 94 * KB + i * 2 * KB, [P, 512], F32) for i in range(4)]
    glatmp = view(R4, 102 * KB, [P, 10 * 256], F32)

    ps = [stack.enter_context(nc.psum_tensor("ps%d" % i, [P, 512], F32)) for i in range(8)]

    def psk(i):
        return ("ps", i)

    def load_const(dst, src, key, eng="sp"):
        sch.add(eng, lambda e, dst=dst, src=src: e.dma_start(out=dst, in_=src), writes=[key], dma=True)

    load_const(ident_bf[:], ident_bf_d[:, :], "ident_bf")
    load_const(ident_f[:], ident_f_d[:, :], "ident_f")
    load_const(tri_bf[:], tri_bf_d[:, :], "tri_bf")
    load_const(tri_f[:], tri_f_d[:, :], "tri_f")
    load_const(after_f[:], after_f_d[:, :], "after_f")
    load_const(negmask[:], negmask_d[:, :], "negmask")
    load_const(blockones[:], blockones_d[:, :], "blockones")
    load_const(pow2[:], pow2_d[:, :], "pow2")
    load_const(g1bc[:], g1bc_d[:, :], "g1bc")
    load_const(qkg[:], qkg_d[:, :], "qkg")
    load_const(gout[:], gout_d[:, :], "gout")
    load_const(brbc[:], brbc_d[:, :], "brbc")
    load_const(rb31[:], rb31_d[:, :], "rb31")
    load_const(w2aug[:], w2_d[:, :], "w2aug", eng="pool")
    sch.add("dve", lambda e: e.memset(ones_col[:], 1.0), writes=["ones_col"])
    sch.add("dve", lambda e: e.memset(glrT[0:32, :], 1.0), writes=["glrT_init"])

    for T in range(NT):
        b = T % 2
        tsl = slice(T * P, (T + 1) * P)
        sch.add("sp", lambda e, b=b, tsl=tsl: e.dma_start(out=xst[b], in_=x_d[tsl, :]),
                writes=[("xst", b)], dma=True)
        sch.add("act", lambda e, b=b, T=T: e.activation(out=junk[:, 0:D], in_=xst[b], func=AF.Square,
                                                        accum_out=ssq1[:, T:T + 1]),
                reads=[("xst", b)], writes=["junk", ("ssq1", T)])
        sch.add("act", lambda e, T=T: e.activation(out=rstd1[:, T:T + 1], in_=ssq1[:, T:T + 1], func=AF.Sqrt,
                                                   scale=1.0 / D, bias=eps_col[:, 0:1]),
                reads=[("ssq1", T), "eps_col"], writes=[("rstd1", T)])
        sch.add("dve", lambda e, T=T: e.reciprocal(out=rstd1[:, T:T + 1], in_=rstd1[:, T:T + 1]),
                reads=[("rstd1", T)], writes=[("rstd1", T)])
        sch.add("dve", lambda e, b=b, T=T: e.scalar_tensor_tensor(out=xs[b], in0=xst[b], scalar=rstd1[:, T:T + 1],
                                                                  in1=g1bc[:], op0=ALU.mult, op1=ALU.mult),
                reads=[("xst", b), ("rstd1", T), "g1bc"], writes=[("xs", b)])
        pb = T % 2

        def tr(e, b=b, pb=pb):
            o = ps[pb][:].bitcast(BF16).rearrange("p (c t) -> p c t", c=8)
            ins = None
            for c in range(8):
                ins = e.transpose(o[:, c, :], xs[b][:, c * P:(c + 1) * P], ident_bf[:])
            return ins
        sch.add("pe", tr, reads=[("xs", b), "ident_bf"], writes=[psk(pb)])
        sch.add("act", lambda e, pb=pb, tsl=tsl: e.activation(
            out=hT[:, :, tsl], in_=ps[pb][:].bitcast(BF16).rearrange("p (c t) -> p c t", c=8), func=AF.Copy),
            reads=[psk(pb)], writes=[("hT", T)])

    wcount = [0]

    def load_wgroup(gi):
        name, kind, c0, n = W1_GROUPS[gi]
        b = wcount[0] % 2
        wcount[0] += 1
        src = w1_d[:, c0:c0 + n].rearrange("(c p) n -> p c n", p=P)
        dst = wst[b][:, :, 0:n]
        sch.add("pool", lambda e, dst=dst, src=src: e.dma_start(out=dst, in_=src),
                writes=[("wst", b)], dma=True)
        return b

    bankrot = {}

    def next_bank(lo=2, hi=6):
        k = (lo, hi)
        c = bankrot.get(k, 0)
        bankrot[k] = c + 1
        return lo + c % (hi - lo)

    def fm_matmul(wb, col0, m, tc, bank, parts=None):
        wt = wst[wb]

        def fn(e):
            ins = None
            for c in range(8):
                ins = e.matmul(ps[bank][0:m, :], lhsT=wt[:, c, col0:col0 + m],
                               rhs=hT[:, c, tc * 512:(tc + 1) * 512], start=(c == 0), stop=(c == 7))
            return ins
        sch.add("pe", fn, reads=[("wst", wb)] + [("hT", tc * 4 + k) for k in range(4)], writes=[psk(bank)])

    def tm_matmul(wb, col0, n, T, bank):
        wt = wst[wb]

        def fn(e):
            ins = None
            for c in range(8):
                ins = e.matmul(ps[bank][:, 0:n], lhsT=hT[:, c, T * P:(T + 1) * P],
                               rhs=wt[:, c, col0:col0 + n], start=(c == 0), stop=(c == 7))
            return ins
        sch.add("pe", fn, reads=[("wst", wb), ("hT", T)], writes=[psk(bank)])

    eps_col = sb("eps_col", [P, 1], F32)
    sch.ops.insert(0, _Op("dve", lambda e: e.memset(eps_col[:], EPS), (), ("eps_col",), False))

    wb = load_wgroup(0)
    for tc in range(4):
        tsl = slice(tc * 512, (tc + 1) * 512)
        for k in range(4):
            bank = next_bank()
            fm_matmul(wb, k * 128, 128, tc, bank)
            dst = (qbT if k < 2 else kbT)[:, k % 2, tsl]
            key = ("qbT" if k < 2 else "kbT", tc)
            eng = "act" if k % 2 == 0 else "dve"
            if eng == "act":
                sch.add("act", lambda e, dst=dst, bank=bank: e.activation(out=dst, in_=ps[bank][:], func=AF.Copy),
                        reads=[psk(bank)], writes=[key + (k % 2,)])
            else:
                sch.add("dve", lambda e, dst=dst, bank=bank: e.tensor_copy(out=dst, in_=ps[bank][:]),
                        reads=[psk(bank)], writes=[key + (k % 2,)])
        bank = next_bank()
        fm_matmul(wb, 512, 16, tc, bank)
        sch.add("dve", lambda e, tsl=tsl, bank=bank: e.tensor_copy(out=glrT[0:16, tsl], in_=ps[bank][0:16, :]),
                reads=[psk(bank), "glrT_init"], writes=[("glrT", tc)])
    wb = load_wgroup(1)
    for T in range(NT):
        bank = next_bank()
        tm_matmul(wb, 0, 512, T, bank)
        eng = "act" if T % 2 == 0 else "dve"
        if eng == "act":
            sch.add("act", lambda e, T=T, bank=bank: e.activation(out=vb[:, T, :], in_=ps[bank][:], func=AF.Copy),
                    reads=[psk(bank)], writes=[("vb", T)])
        else:
            sch.add("dve", lambda e, T=T, bank=bank: e.tensor_copy(out=vb[:, T, :], in_=ps[bank][:]),
                    reads=[psk(bank)], writes=[("vb", T)])
    wb = load_wgroup(2)
    for T in range(NT):
        bank = next_bank()
        tm_matmul(wb, 0, 512, T, bank)
        tb = T % 2
        sch.add("act", lambda e, tb=tb, bank=bank: e.activation(out=tmpA[tb][:], in_=ps[bank][:], func=AF.Silu),
                reads=[psk(bank)], writes=[("tmpA", tb)])
        sch.add("pool", lambda e, tb=tb, T=T: e.tensor_tensor(out=Gt[:, T, :], in0=tmpA[tb][:], in1=gout[:], op=ALU.mult),
                reads=[("tmpA", tb), "gout"], writes=[("Gt", T)])
    wb = load_wgroup(3)
    for T in range(NT):
        bank = next_bank()
        tm_matmul(wb, 0, 264, T, bank)
        sch.add("dve", lambda e, T=T, bank=bank: e.tensor_copy(out=kbtm[:, T, :], in_=ps[bank][:, 0:256]),
                reads=[psk(bank)], writes=[("kbtm", T)])
        sch.add("dve", lambda e, T=T, bank=bank: e.tensor_copy(out=iw_s[:, T, :], in_=ps[bank][:, 256:264]),
                reads=[psk(bank)], writes=[("iw", T)])

    def dump(name, src_ap, dst_ap, reads):
        if name in dbg:
            sch.add("sp", lambda e: e.dma_start(out=dst_ap, in_=src_ap), reads=reads, writes=["dbg_" + name], dma=True)

    def finish():
        import os
        if os.environ.get("KSTOP_OPS"):
            print("total ops", len(sch.ops))
            sch.ops = sch.ops[:int(os.environ["KSTOP_OPS"])]
        outs = ["dbg_" + n for n in dbg] + ["out_%d" % T for T in range(NT)]
        sch.add("sp", lambda e: None, reads=outs)
        sch.finalize()
        with nc.Block() as block:
            sch.emit(block)
        stack.close()
        return nc

    dump("hT", hT, dbg.get("hT").rearrange("(c p) t -> p c t", p=P) if "hT" in dbg else None, [("hT", T) for T in range(NT)])
    dump("qbT", qbT[:, 0, :], dbg.get("qbT"), [("qbT", tc, 0) for tc in range(4)])
    dump("vb", vb, dbg.get("vb").rearrange("(t p) c -> p t c", p=P) if "vb" in dbg else None, [("vb", T) for T in range(NT)])
    dump("Gt", Gt, dbg.get("Gt").rearrange("(t p) c -> p t c", p=P) if "Gt" in dbg else None, [("Gt", T) for T in range(NT)])
    if STOP_AFTER == "p1":
        return finish()

    Sst = sb("Sst", [P, 4, 128], F32)
    Sbf = sb("Sbf", [P, 4, 128], BF16)
    g_e1 = sb("g_e1", [P, 256], F32)
    g_l = sb("g_l", [P, 256], F32)
    g_eb = [sb("g_eb%d" % i, [P, 2, 128], F32) for i in range(2)]
    g_einv = sb("g_einv", [P, 2, 128], F32)
    g_erem = sb("g_erem", [P, 256], F32)
    g_qt = [sb("g_qt%d" % i, [P, 2, 128], BF16) for i in range(2)]
    g_kt = sb("g_kt", [P, 2, 128], BF16)
    g_kh = sb("g_kh", [P, 256], BF16)
    g_A = [sb("g_A%d" % i, [P, 4, 128], BF16) for i in range(2)]
    g_ob = sb("g_ob", [P, 512], BF16)
    g_ssq = sb("g_ssq", [P, 4], F32)
    g_rs = sb("g_rs", [P, 4], F32)
    sch.add("dve", lambda e: e.memset(Sst[:], 0.0), writes=[("Sst", h) for h in range(4)])
    sch.add("dve", lambda e: e.memset(Sbf[:], 0.0), writes=[("Sbf", h) for h in range(4)])
    BZ, BC, BA, BO, BT = 0, 1, 3, 4, 6
    BUS = [5, 2]
    print("ops before GLA", len(sch.ops))

    def gla_stage1(T):
        b = T % 2
        tsl = slice(T * P, (T + 1) * P)
        eb, qt, A, BU = g_eb[b], g_qt[b], g_A[b], BUS[b]
        sch.add("pe", lambda e: e.matmul(ps[BZ][:, 0:256], lhsT=glrT[0:17, tsl], rhs=w2aug[0:17, :], start=True, stop=True),
                reads=[("glrT", T // 4), "glrT_init", "w2aug"], writes=[psk(BZ)])
        sch.add("act", lambda e: e.activation(out=g_e1[:], in_=ps[BZ][:, 0:256], func=AF.Exp, scale=-1.0),
                reads=[psk(BZ)], writes=["g_e1"])
        sch.add("act", lambda e: e.activation(out=g_l[:], in_=g_e1[:], func=AF.Ln, scale=1.0, bias=ones_col[:, 0:1]),
                reads=["g_e1", "ones_col"], writes=["g_l"])

        def cum(e):
            e.matmul(ps[BC][:, 0:128], lhsT=g_l[:, 0:128], rhs=tri_f[:], start=True, stop=True)
            return e.matmul(ps[BC][:, 128:256], lhsT=g_l[:, 128:256], rhs=tri_f[:], start=True, stop=True)
        sch.add("pe", cum, reads=["g_l", "tri_f"], writes=[psk(BC)])
        sch.add("pe", lambda e: e.matmul(ps[BZ][:, 256:512], lhsT=after_f[:], rhs=g_l[:], start=True, stop=True),
                reads=["g_l", "after_f"], writes=[psk(BZ)])
        cview = ps[BC][:, 0:256].rearrange("p (a b) -> p a b", a=2)
        sch.add("act", lambda e: e.activation(out=eb[:], in_=cview, func=AF.Exp, scale=-1.0 / 16),
                reads=[psk(BC)], writes=[("g_eb", b)])
        sch.add("act", lambda e: e.activation(out=g_einv[:], in_=cview, func=AF.Exp, scale=1.0 / 16),
                reads=[psk(BC)], writes=["g_einv"])
        sch.add("act", lambda e: e.activation(out=g_erem[:], in_=ps[BZ][:, 256:512], func=AF.Exp, scale=-1.0 / 16),
                reads=[psk(BZ)], writes=["g_erem"])
        sch.add("dve", lambda e: e.scalar_tensor_tensor(out=qt[:], in0=qbT[:, :, tsl], scalar=0.125, in1=eb[:],
                                                        op0=ALU.mult, op1=ALU.mult),
                reads=[("qbT", T // 4, 0), ("qbT", T // 4, 1), ("g_eb", b)], writes=[("g_qt", b)])
        sch.add("dve", lambda e: e.scalar_tensor_tensor(out=g_kt[:], in0=kbT[:, :, tsl], scalar=1.0, in1=g_einv[:],
                                                        op0=ALU.mult, op1=ALU.mult),
                reads=[("kbT", T // 4, 0), ("kbT", T // 4, 1), "g_einv"], writes=["g_kt"])
        sch.add("dve", lambda e: e.scalar_tensor_tensor(out=g_kh[:], in0=kbtm[:, T, :], scalar=1.0, in1=g_erem[:],
                                                        op0=ALU.mult, op1=ALU.mult),
                reads=[("kbtm", T), "g_erem"], writes=["g_kh"])

        def attn(e):
            ins = None
            for h in (0, 2, 1, 3):
                p, r = h // 2, h % 2
                rs = slice(r * 64, (r + 1) * 64)
                bk = BA if r == 0 else 7
                ins = e.matmul(ps[bk][:, p * 128:(p + 1) * 128], lhsT=g_kt[rs, p, :], rhs=qt[rs, p, :], start=True, stop=True)
            return ins
        sch.add("pe", attn, reads=["g_kt", ("g_qt", b)], writes=[psk(BA), psk(7)])
        for r in range(2):
            bk = BA if r == 0 else 7
            sch.add("dve", lambda e, r=r, bk=bk: e.tensor_tensor(
                out=A[:, r::2, :], in0=ps[bk][:, 0:256].rearrange("p (h t) -> p h t", h=2),
                in1=tri_bf[:, :].unsqueeze(1).to_broadcast([P, 2, 128]), op=ALU.mult),
                reads=[psk(bk), "tri_bf"], writes=[("g_A", b, r)])

        def umm(e):
            ins = None
            for h in range(4):
                p = h // 2
                ins = e.matmul(ps[BU][:, h * 128:(h + 1) * 128], lhsT=g_kh[:, p * 128:(p + 1) * 128], rhs=vb[:, T, h * 128:(h + 1) * 128],
                               start=True, stop=True)
            return ins
        sch.add("pe", umm, reads=["g_kh", ("vb", T)], writes=[psk(BU)])

    def gla_stage2(T):
        b = T % 2
        tsl = slice(T * P, (T + 1) * P)
        eb, qt, A, BU = g_eb[b], g_qt[b], g_A[b], BUS[b]

        def omm(e):
            ins = None
            for h in range(4):
                p = h // 2
                e.matmul(ps[BO][:, h * 128:(h + 1) * 128], lhsT=A[:, h, :], rhs=vb[:, T, h * 128:(h + 1) * 128], start=True, stop=False)
                ins = e.matmul(ps[BO][:, h * 128:(h + 1) * 128], lhsT=qt[:, p, :], rhs=Sbf[:, h, :], start=False, stop=True)
            return ins
        sch.add("pe", omm, reads=[("g_A", b, 0), ("g_A", b, 1), ("vb", T), ("g_qt", b)] + [("Sbf", h) for h in range(4)], writes=[psk(BO)])
        for h in range(4):
            p, r = h // 2, h % 2
            rs = slice(r * 64, (r + 1) * 64)
            sch.add("dve", lambda e, h=h, p=p, rs=rs: e.scalar_tensor_tensor(
                out=Sst[rs, h, :], in0=Sst[rs, h, :], scalar=eb[rs, p, 127:128], in1=ps[BU][rs, h * 128:(h + 1) * 128],
                op0=ALU.mult, op1=ALU.add), reads=[psk(BU), ("g_eb", b), ("Sst", h)], writes=[("Sst", h), psk(BU)])
            sch.add("act", lambda e, h=h, rs=rs: e.activation(out=Sbf[rs, h, :], in_=Sst[rs, h, :], func=AF.Copy),
                    reads=[("Sst", h)], writes=[("Sbf", h)])
        for h in range(4):
            sch.add("act", lambda e, h=h: e.activation(out=junk[:, 0:128], in_=ps[BO][:, h * 128:(h + 1) * 128], func=AF.Square,
                                                       accum_out=g_ssq[:, h:h + 1]),
                    reads=[psk(BO)], writes=["junk", ("g_ssq", h), psk(BO)])
        sch.add("act", lambda e: e.activation(out=g_rs[:], in_=g_ssq[:], func=AF.Sqrt, scale=1.0 / 128, bias=eps_col[:, 0:1]),
                reads=[("g_ssq", h) for h in range(4)] + ["eps_col"], writes=["g_rs"])
        sch.add("dve", lambda e: e.reciprocal(out=g_rs[:], in_=g_rs[:]), reads=["g_rs"], writes=["g_rs"])
        for h in range(4):
            hs = slice(h * 128, (h + 1) * 128)
            sch.add("dve", lambda e, h=h, hs=hs: e.scalar_tensor_tensor(
                out=g_ob[:, hs], in0=ps[BO][:, hs], scalar=g_rs[:, h:h + 1], in1=Gt[:, T, hs], op0=ALU.mult, op1=ALU.mult),
                reads=[psk(BO), "g_rs", ("Gt", T)], writes=[("g_ob", h), psk(BO)])
        if "ob" in dbg:
            sch.add("sp", lambda e: e.dma_start(out=dbg["ob"][tsl, :], in_=g_ob[:]), reads=[("g_ob", h) for h in range(4)],
                    writes=["dbg_ob"], dma=True)

        def trb(e):
            o = ps[BT][:].bitcast(BF16).rearrange("p (c t) -> p c t", c=8)
            ins = None
            for c in range(4):
                ins = e.transpose(o[:, c, :], g_ob[:, c * P:(c + 1) * P], ident_bf[:])
            return ins
        sch.add("pe", trb, reads=[("g_ob", h) for h in range(4)] + ["ident_bf"], writes=[psk(BT)])
        sch.add("act", lambda e: e.activation(
            out=mixT_b[:, :, tsl], in_=ps[BT][:].bitcast(BF16).rearrange("p (c t) -> p c t", c=8)[:, 0:4, :], func=AF.Copy),
            reads=[psk(BT)], writes=[("mixT_b", T)])

    gla_stage1(0)
    for T in range(NT):
        if T + 1 < NT:
            gla_stage1(T + 1)
        gla_stage2(T)
    if STOP_AFTER == "gla":
        return finish()
    sch.barrier()

    qaT = view(R4, 0, [P, 4, S], BF16)
    kaT = view(R4, 16 * KB, [P, 4, S], BF16)
    iqT = view(R4, 32 * KB, [P, 3, S], BF16)
    ikT = view(R4, 44 * KB, [P, S], BF16)
    vaT = view(R4, 48 * KB, [P, NT, 8 * 65], BF16)
    wst2 = [view(R4, 66 * KB + i * 9 * KB, [P, 8, 528], BF16) for i in range(2)]
    n_sq = [view(R4, 84 * KB + i * KB, [P, 512], BF16) for i in range(2)]
    n_ln = [view(R4, 86 * KB + i * 2 * KB, [P, 512], F32) for i in range(2)]
    n_rs = [view(R4, 90 * KB + i * 2 * KB, [P, 512], F32) for i in range(2)]
    wst[0], wst[1] = wst2[0], wst2[1]
    sch.add("dve", lambda e: e.memset(vaT[:], 1.0), writes=[("va", T) for T in range(NT)])
    ncnt = [0]
    for gi, which in [(4, 0), (5, 1)]:
        wb = load_wgroup(gi)
        dstT = qaT if which == 0 else kaT
        kname = "qaT" if which == 0 else "kaT"
        for tc in range(4):
            tsl = slice(tc * 512, (tc + 1) * 512)
            for p in range(4):
                bank = next_bank(0, 4)
                sbank = next_bank(4, 8)
                nb = ncnt[0] % 2
                ncnt[0] += 1
                fm_matmul(wb, p * 128, 128, tc, bank)
                sch.add("act", lambda e, nb=nb, bank=bank: e.activation(out=n_sq[nb], in_=ps[bank][:], func=AF.Square),
                        reads=[psk(bank)], writes=[("n_sq", nb), psk(bank)])
                sch.add("pe", lambda e, nb=nb, sbank=sbank: e.matmul(ps[sbank][:], lhsT=blockones[:], rhs=n_sq[nb], start=True, stop=True),
                        reads=[("n_sq", nb), "blockones"], writes=[psk(sbank)])
                sch.add("act", lambda e, nb=nb, sbank=sbank: e.activation(out=n_ln[nb], in_=ps[sbank][:], func=AF.Ln, scale=1.0 / 64,
                                                                          bias=eps_col[:, 0:1]),
                        reads=[psk(sbank), "eps_col"], writes=[("n_ln", nb)])
                sch.add("act", lambda e, nb=nb: e.activation(out=n_rs[nb], in_=n_ln[nb], func=AF.Exp, scale=-0.5),
                        reads=[("n_ln", nb)], writes=[("n_rs", nb)])
                sch.add("dve", lambda e, nb=nb, bank=bank, p=p, tsl=tsl, dstT=dstT, which=which: e.scalar_tensor_tensor(
                    out=dstT[:, p, tsl], in0=ps[bank][:], scalar=qkg[:, which:which + 1], in1=n_rs[nb], op0=ALU.mult, op1=ALU.mult),
                    reads=[psk(bank), ("n_rs", nb), "qkg"], writes=[(kname, p, tc), psk(bank)])
    wb = load_wgroup(6)
    for tc in range(4):
        tsl = slice(tc * 512, (tc + 1) * 512)
        col = 0
        for ti, (h0, h1) in enumerate(IQ_TILES):
            m = (h1 - h0) * 32
            bank = next_bank(0, 8)
            fm_matmul(wb, col, m, tc, bank)
            col += m
            sch.add("act", lambda e, ti=ti, m=m, tsl=tsl, bank=bank: e.activation(out=iqT[0:m, ti, tsl], in_=ps[bank][0:m, :], func=AF.Copy),
                    reads=[psk(bank)], writes=[("iqT", ti, tc)])
        bank = next_bank(0, 8)
        fm_matmul(wb, col, 96, tc, bank)
        sch.add("dve", lambda e, tsl=tsl, bank=bank: e.tensor_copy(out=ikT[0:96, tsl], in_=ps[bank][0:96, :]),
                reads=[psk(bank)], writes=[("ikT", tc)])
    wb = load_wgroup(7)
    for T in range(NT):
        bank = next_bank(0, 8)
        tm_matmul(wb, 0, 512, T, bank)
        dstv = vaT[:, T, :].rearrange("p (h d) -> p h d", h=8)[:, :, 0:64]
        srcv = ps[bank][:].rearrange("p (h d) -> p h d", h=8)
        if T % 2 == 0:
            sch.add("act", lambda e, dstv=dstv, srcv=srcv: e.activation(out=dstv, in_=srcv, func=AF.Copy),
                    reads=[psk(bank)], writes=[("va", T)])
        else:
            sch.add("dve", lambda e, dstv=dstv, srcv=srcv: e.tensor_copy(out=dstv, in_=srcv),
                    reads=[psk(bank)], writes=[("va", T)])
    dump("qaT", qaT[:, 0, :], dbg.get("qaT"), [("qaT", 0, tc) for tc in range(4)])
    dump("kaT", kaT[:, 0, :], dbg.get("kaT"), [("kaT", 0, tc) for tc in range(4)])
    if STOP_AFTER == "p2":
        return finish()
    sch.barrier()

    selT = view(R4, 66 * KB, [P, SELT_TOTAL], BF16)
    selts = [view(R4, 100 * KB + i * 4 * KB, [P, S], BF16) for i in range(2)]
    PTb = [view(R4, 100 * KB + i * KB, [P, 512], BF16) for i in range(8)]
    junk_tk = view(R4, 108 * KB, [P, 2048], BF16)
    score_all = R1[:, :].bitcast(F32)
    thr = sb("thr", [P, NT], F32)
    thr2 = sb("thr2", [P, NT], F32)
    cnt = sb("cnt", [P, NT], F32)
    sgn = sb("sgn", [P, NT], F32)
    amax = sb("amax", [P, NT], F32)
    mrow = sb("mrow", [P, 1], F32)
    mtab = sb("mtab", [P, KITER + 1], F32)
    biasT_s = view(R4, 100 * KB, [P, 8, 2, 128], F32)
    sch.add("sp", lambda e: e.dma_start(out=biasT_s, in_=biasT_d.rearrange("p (h d t) -> p h d t", h=8, d=2)),
            writes=["biasT"], dma=True)
    sch.add("act", lambda e: e.activation(out=Etile[:], in_=biasT_s, func=AF.Exp), reads=["biasT"], writes=["Etile"])
    for h in range(8):
        sch.add("dve", lambda e, h=h: e.tensor_tensor(out=Etile[:, h, 0, :], in0=Etile[:, h, 0, :], in1=tri_bf[:], op=ALU.mult),
                reads=["Etile", "tri_bf"], writes=["Etile"])
    sch.add("dve", lambda e: e.memset(selts[0][:], 0.0), reads=["Etile"], writes=[("selts", 0)])
    sch.add("dve", lambda e: e.tensor_copy(out=selT[:, _selT_off(0):_selT_off(0) + 128], in_=tri_bf[:]), reads=["tri_bf"], writes=[("selT", 0, 0)])
    sch.add("dve", lambda e: e.memset(selT[:, _selT_off(0) + 128:_selT_off(0) + 256], 1.0), writes=[("selT", 0, 1)])
    sch.add("dve", lambda e: e.tensor_copy(out=selT[:, _selT_off(1):_selT_off(1) + 128], in_=tri_bf[:]), reads=["tri_bf"], writes=[("selT", 1, 1)])

    print("ops before topk loops", len(sch.ops))
    batches = [list(range(2, 8)), list(range(8, 12)), list(range(12, 16))]
    ACT_SHARE = [4, 2, 2]
    sumA = sb("sumA", [P, NT], F32)
    nhalf = sb("nhalf", [P, NT], F32)
    junk_act = view(R3, 0, [P, 2048], BF16)
    for j in range(NT):
        sch.add("dve", lambda e, j=j: e.memset(nhalf[:, j:j + 1], 64.0 * (j + 1)), writes=["nhalf"])
    hd_loc = []
    for ti, (h0, h1) in enumerate(IQ_TILES):
        for k in range(h1 - h0):
            hd_loc.append((ti, k))
    lbank = [0]
    for bi, batch in enumerate(batches):
        soff = {}
        o = 0
        for j in batch:
            soff[j] = o
            o += (j + 1) * 128
        nb = len(batch)
        j0 = batch[0]
        for j in batch:
            n = (j + 1) * 128
            sc_j = score_all[:, soff[j]:soff[j] + n]
            nsc = (n + 511) // 512
            for sc in range(nsc):
                w = min(512, n - sc * 512)
                ssl = slice(sc * 512, sc * 512 + w)
                for h in range(8):
                    ti, k = hd_loc[h]
                    rs = slice(k * 32, (k + 1) * 32)
                    lb = lbank[0] % 4
                    rb = 4 + lbank[0] % 4
                    lbank[0] += 1
                    sch.add("pe", lambda e, lb=lb, rs=rs, ti=ti, j=j, ssl=ssl, w=w: e.matmul(
                        ps[lb][:, 0:w], lhsT=iqT[rs, ti, j * 128:(j + 1) * 128], rhs=ikT[rs, ssl], start=True, stop=True),
                        reads=[("iqT", ti, j // 4)] + [("ikT", c) for c in range(sc * 4 // 4, (sc * 512 + w - 1) // 512 + 1)],
                        writes=[psk(lb)])
                    sch.add("act", lambda e, lb=lb, rb=rb, w=w: e.activation(out=ps[rb][:, 0:w], in_=ps[lb][:, 0:w], func=AF.Relu),
                            reads=[psk(lb)], writes=[psk(rb), psk(lb)])
                    dst = sc_j[:, ssl]
                    if h == 0:
                        sch.add("dve", lambda e, rb=rb, w=w, dst=dst, j=j, h=h: e.tensor_scalar(
                            out=dst, in0=ps[rb][:, 0:w], scalar1=iw_s[:, j, h:h + 1], scalar2=None, op0=ALU.mult),
                            reads=[psk(rb), ("iw", j)], writes=[("score", j, sc), psk(rb)])
                    else:
                        sch.add("dve", lambda e, rb=rb, w=w, dst=dst, j=j, h=h: e.scalar_tensor_tensor(
                            out=dst, in0=ps[rb][:, 0:w], scalar=iw_s[:, j, h:h + 1], in1=dst, op0=ALU.mult, op1=ALU.add),
                            reads=[psk(rb), ("iw", j), ("score", j, sc)], writes=[("score", j, sc), psk(rb)])
            skeys = [("score", j, sc) for sc in range(nsc)]
            sch.add("dve", lambda e, sc_j=sc_j, j=j: e.tensor_reduce(out=amax[:, j:j + 1], in_=sc_j, axis=AX.X, op=ALU.max,
                                                                    apply_absolute_value=True),
                    reads=skeys, writes=[("amax", j)])
            dsl = slice(soff[j] + j * 128, soff[j] + (j + 1) * 128)
            sch.add("dve", lambda e, dsl=dsl: e.tensor_tensor(out=score_all[:, dsl], in0=score_all[:, dsl], in1=negmask[:], op=ALU.add),
                    reads=skeys + ["negmask", ("amax", j)], writes=skeys)
        if "score" in dbg and bi == 1:
            sch.add("sp", lambda e, soff=soff: e.dma_start(out=dbg["score"][:, 0:1280], in_=score_all[:, soff[9]:soff[9] + 1280]),
                    reads=[("score", 9, sc) for sc in range(3)], writes=["dbg_score"], dma=True)
        bsl = slice(j0, j0 + nb)
        sch.add("dve", lambda e, bsl=bsl: e.tensor_reduce(out=mrow[:], in_=amax[:, bsl], axis=AX.X, op=ALU.max),
                reads=[("amax", j) for j in batch], writes=["mrow"])
        sch.add("dve", lambda e: e.tensor_scalar(out=mtab[:], in0=pow2[:], scalar1=mrow[:, 0:1], scalar2=None, op0=ALU.mult),
                reads=["mrow", "pow2"], writes=["mtab"])
        sch.add("dve", lambda e, bsl=bsl: e.memset(thr[:, bsl], 0.0), writes=["thr"])
        nact = ACT_SHARE[bi]
        act_tiles = batch[:nact]
        asl = slice(batch[0], batch[0] + nact)
        for k in range(KITER):
            for j in batch:
                n = (j + 1) * 128
                sc_j = score_all[:, soff[j]:soff[j] + n]
                skeys_j = [("score", j, sc) for sc in range((n + 511) // 512)]
                if j in act_tiles:
                    sch.add("act", lambda e, sc_j=sc_j, n=n, j=j: e.activation(
                        out=junk_act[:, 0:n], in_=sc_j, func=AF.Sign, scale=-1.0, bias=thr[:, j:j + 1], accum_out=sumA[:, j:j + 1]),
                        reads=skeys_j + ["thr"], writes=["junk_act", ("sumA", j)])
                else:
                    sch.add("dve", lambda e, sc_j=sc_j, n=n, j=j: e.tensor_scalar(
                        out=junk_tk[:, 0:n], in0=sc_j, scalar1=thr[:, j:j + 1], scalar2=None, op0=ALU.is_ge, op1=ALU.add,
                        accum_out=cnt[:, j:j + 1]),
                        reads=skeys_j + ["thr"], writes=["junk", ("cnt", j)])
            if nact > 0:
                sch.add("dve", lambda e, asl=asl: e.scalar_tensor_tensor(out=cnt[:, asl], in0=sumA[:, asl], scalar=-0.5, in1=nhalf[:, asl],
                                                                        op0=ALU.mult, op1=ALU.add),
                        reads=[("sumA", j) for j in act_tiles] + ["nhalf"], writes=[("cnt", j) for j in act_tiles])
            sch.add("dve", lambda e, bsl=bsl: e.tensor_scalar(out=sgn[:, bsl], in0=cnt[:, bsl], scalar1=256.0, scalar2=0.5,
                                                             op0=ALU.is_ge, op1=ALU.subtract),
                    reads=[("cnt", j) for j in batch], writes=["sgn"])
            sch.add("dve", lambda e, bsl=bsl, k=k: e.scalar_tensor_tensor(out=thr2[:, bsl], in0=sgn[:, bsl], scalar=mtab[:, k:k + 1],
                                                                       in1=thr[:, bsl], op0=ALU.mult, op1=ALU.add),
                    reads=["sgn", "mtab", "thr"], writes=["thr2"])
            sch.add("dve", lambda e, bsl=bsl: e.tensor_copy(out=thr[:, bsl], in_=thr2[:, bsl]), reads=["thr2"], writes=["thr"])
        sch.add("dve", lambda e, bsl=bsl: e.tensor_scalar(out=thr2[:, bsl], in0=thr[:, bsl], scalar1=mtab[:, KITER:KITER + 1], scalar2=None,
                                                         op0=ALU.subtract),
                reads=["thr", "mtab"], writes=["thr2"])
        if "thr" in dbg and bi == 1:
            sch.add("sp", lambda e: e.dma_start(out=dbg["thr"][:, :], in_=thr2[:]), reads=["thr2"], writes=["dbg_thr"], dma=True)
        for j in batch:
            n = (j + 1) * 128
            sc_j = score_all[:, soff[j]:soff[j] + n]
            sb_ = j % 2
            sch.add("dve", lambda e, sc_j=sc_j, n=n, j=j, sb_=sb_: e.tensor_scalar(
                out=selts[sb_][:, 0:n], in0=sc_j, scalar1=thr2[:, j:j + 1], scalar2=None, op0=ALU.is_ge),
                reads=[("score", j, sc) for sc in range((n + 511) // 512)] + ["thr2"], writes=[("selts", sb_)])
            for i0 in range(0, j + 1, 8):
                i1 = min(j + 1, i0 + 8)
                tb = next_bank(0, 8)

                def trs(e, i0=i0, i1=i1, tb=tb, sb_=sb_):
                    o = ps[tb][:].bitcast(BF16).rearrange("p (c t) -> p c t", c=8)
                    ins = None
                    for i in range(i0, i1):
                        ins = e.transpose(o[:, i - i0, :], selts[sb_][:, i * 128:(i + 1) * 128], ident_bf[:])
                    return ins
                sch.add("pe", trs, reads=[("selts", sb_), "ident_bf"], writes=[psk(tb)])
                for i in range(i0, i1):
                    o = ps[tb][:].bitcast(BF16).rearrange("p (c t) -> p c t", c=8)[:, i - i0, :]
                    off = _selT_off(i) + (j - i) * 128
                    if i % 2 == 0:
                        sch.add("act", lambda e, o=o, off=off: e.activation(out=selT[:, off:off + 128], in_=o, func=AF.Copy),
                                reads=[psk(tb)], writes=[("selT", i, j), psk(tb)])
                    else:
                        sch.add("dve", lambda e, o=o, off=off: e.tensor_copy(out=selT[:, off:off + 128], in_=o),
                                reads=[psk(tb)], writes=[("selT", i, j), psk(tb)])
    dump("selT", selT, dbg.get("selT"), [("selT", i, j) for i in range(16) for j in range(i, 16)])
    if STOP_AFTER == "topk":
        return finish()
    sch.barrier()

    mixa = view(R1, 0, [P, NT, 512], BF16)
    rden = sb("rden", [P, 4], F32)
    abank = [0]
    LOOKAHEAD = 4
    pend = []

    def flush(keep):
        while len(pend) > keep:
            pend.pop(0)()

    for h in range(8):
        p, r = h // 2, h % 2
        rs = slice(r * 64, (r + 1) * 64)
        for J in range(4):
            accb = 6 + (abank[0] % 2)
            abank[0] += 1
            accv = ps[accb][:, 0:260].rearrange("p (j d) -> p j d", j=4)
            first = [True]
            for i in range(4 * J + 4):
                jlo = max(i, 4 * J)
                t0 = jlo * 128
                n = (4 * J + 4 - jlo) * 128
                sbk = next_bank(0, 6)
                pb = next_bank(100, 108) - 100
                sch.add("pe", lambda e, sbk=sbk, rs=rs, p=p, i=i, t0=t0, n=n: e.matmul(
                    ps[sbk][:, 0:n], lhsT=kaT[rs, p, i * 128:(i + 1) * 128], rhs=qaT[rs, p, t0:t0 + n], start=True, stop=True),
                    reads=[("kaT", p, i // 4), ("qaT", p, J)], writes=[psk(sbk)])
                nnear = max(0, min(4 * J + 4, i + 2) - jlo) * 128
                if nnear > 0:
                    sch.add("act", lambda e, sbk=sbk, pb=pb, nnear=nnear: e.activation(out=PTb[pb][:, 0:nnear], in_=ps[sbk][:, 0:nnear],
                                                                                       func=AF.Exp, scale=0.125),
                            reads=[psk(sbk)], writes=[("PT", pb), psk(sbk)])
                if n > nnear:
                    sch.add("act", lambda e, sbk=sbk, pb=pb, nnear=nnear, n=n, h=h: e.activation(
                        out=PTb[pb][:, nnear:n], in_=ps[sbk][:, nnear:n], func=AF.Exp, scale=0.125, bias=rb31[:, h:h + 1]),
                        reads=[psk(sbk), "rb31"], writes=[("PT", pb), psk(sbk)])
                soff_ = _selT_off(i) + (jlo - i) * 128
                sch.add("dve", lambda e, pb=pb, n=n, soff_=soff_: e.tensor_tensor(out=PTb[pb][:, 0:n], in0=PTb[pb][:, 0:n],
                                                                                 in1=selT[:, soff_:soff_ + n], op=ALU.mult),
                        reads=[("PT", pb)] + [("selT", i, j) for j in range(jlo, 4 * J + 4)], writes=[("PT", pb)])
                for j in range(jlo, min(4 * J + 4, i + 2)):
                    dlt = j - i
                    cs = slice((j - jlo) * 128, (j - jlo + 1) * 128)
                    sch.add("dve", lambda e, pb=pb, cs=cs, h=h, dlt=dlt: e.tensor_tensor(out=PTb[pb][:, cs], in0=PTb[pb][:, cs],
                                                                                         in1=Etile[:, h, dlt, :], op=ALU.mult),
                            reads=[("PT", pb), "Etile"], writes=[("PT", pb)])

                def back(pb=pb, jlo=jlo, J=J, i=i, h=h, accv=accv, first=first, accb=accb):
                    def pv(e):
                        ins = None
                        for j in range(jlo, 4 * J + 4):
                            cs = slice((j - jlo) * 128, (j - jlo + 1) * 128)
                            ins = e.matmul(accv[:, j - 4 * J, :], lhsT=PTb[pb][:, cs], rhs=vaT[:, i, h * 65:(h + 1) * 65],
                                           start=first[0], stop=False, skip_group_check=True)
                            first[0] = False
                        return ins
                    sch.add("pe", pv, reads=[("PT", pb), ("va", i)], writes=[psk(accb)])
                    if i == 4 * J + 3:
                        sch.add("dve", lambda e: e.reciprocal(out=rden[:], in_=accv[:, :, 64]), reads=[psk(accb)], writes=["rden", psk(accb)])
                        sch.add("dve", lambda e: e.tensor_tensor(
                            out=mixa[:, 4 * J:4 * J + 4, h * 64:(h + 1) * 64], in0=accv[:, :, 0:64],
                            in1=rden[:, :].unsqueeze(2).to_broadcast([P, 4, 64]), op=ALU.mult),
                            reads=[psk(accb), "rden"], writes=[("mixa", 4 * J + jj, h) for jj in range(4)] + [psk(accb)])
                pend.append(back)
                flush(LOOKAHEAD)
    flush(0)
    dump("mixa", mixa, dbg.get("mixa").rearrange("(t p) c -> p t c", p=P) if "mixa" in dbg else None,
         [("mixa", T, h) for T in range(NT) for h in range(8)])
    for T in range(NT):
        tsl = slice(T * P, (T + 1) * P)
        tb = next_bank(0, 6)

        def tra(e, T=T, tb=tb):
            o = ps[tb][:].bitcast(BF16).rearrange("p (c t) -> p c t", c=8)
            ins = None
            for c in range(4):
                ins = e.transpose(o[:, c, :], mixa[:, T, c * P:(c + 1) * P], ident_bf[:])
            return ins
        sch.add("pe", tra, reads=[("mixa", T, h) for h in range(8)] + ["ident_bf"], writes=[psk(tb)])
        sch.add("act", lambda e, tsl=tsl, tb=tb: e.activation(
            out=mixT_a[:, :, tsl], in_=ps[tb][:].bitcast(BF16).rearrange("p (c t) -> p c t", c=8)[:, 0:4, :], func=AF.Copy),
            reads=[psk(tb)], writes=[("mixT_a", T)])
    if STOP_AFTER == "attn":
        return finish()
    sch.barrier()

    x1 = view(R4, 0, [P, NT, D], F32)
    woutb = view(R4, 64 * KB, [P, 8, D], BF16)
    xst5 = [view(R4, 80 * KB + i * 4 * KB, [P, D], F32) for i in range(2)]
    xs5 = [view(R4, 88 * KB + i * 2 * KB, [P, D], BF16) for i in range(2)]
    junk5 = view(R4, 92 * KB, [P, 2048], BF16)
    g2bc = view(R4, 96 * KB, [P, D], F32)
    wrb = view(R4, 100 * KB, [P, 8, 36], BF16)
    h2T = view(R1, 0, [P, 8, S], BF16)
    ssq2 = sb("ssq2", [P, NT], F32)
    rstd2 = sb("rstd2", [P, NT], F32)
    logit = sb("logit", [P, NT, 36], F32)
    for hh in range(2):
        sch.add("pool", lambda e, hh=hh: e.dma_start(out=woutb[:, :, hh * 512:(hh + 1) * 512],
                                                     in_=wout_d[:, hh * 512:(hh + 1) * 512].rearrange("(c p) n -> p c n", p=P)),
                writes=[("wout", hh)], dma=True)
    sch.add("pool", lambda e: e.dma_start(out=wrb, in_=wr_d.rearrange("(c p) n -> p c n", p=P)), writes=["wrb"], dma=True)
    sch.add("sp", lambda e: e.dma_start(out=g2bc, in_=g2bc_d[:, :]), writes=["g2bc"], dma=True)
    pend5 = []
    for T in range(NT):
        b = T % 2
        tsl = slice(T * P, (T + 1) * P)
        sch.add("sp", lambda e, b=b, tsl=tsl: e.dma_start(out=xst5[b], in_=x_d[tsl, :]), writes=[("xst5", b)], dma=True)
        for hh in range(2):
            bank = next_bank(0, 4)

            def om(e, T=T, hh=hh, bank=bank):
                ins = None
                for c in range(8):
                    src = mixT_a if c < 4 else mixT_b
                    ins = e.matmul(ps[bank][:], lhsT=src[:, c % 4, T * P:(T + 1) * P], rhs=woutb[:, c, hh * 512:(hh + 1) * 512],
                                   start=(c == 0), stop=(c == 7))
                return ins
            sch.add("pe", om, reads=[("mixT_a", T), ("mixT_b", T), ("wout", hh)], writes=[psk(bank)])
            sch.add("dve", lambda e, T=T, hh=hh, bank=bank, b=b: e.tensor_tensor(
                out=x1[:, T, hh * 512:(hh + 1) * 512], in0=ps[bank][:], in1=xst5[b][:, hh * 512:(hh + 1) * 512], op=ALU.add),
                reads=[psk(bank), ("xst5", b)], writes=[("x1", T, hh)])
        sch.add("act", lambda e, T=T: e.activation(out=junk5[:, 0:D], in_=x1[:, T, :], func=AF.Square, accum_out=ssq2[:, T:T + 1]),
                reads=[("x1", T, 0), ("x1", T, 1)], writes=["junk5", ("ssq2", T)])
        sch.add("act", lambda e, T=T: e.activation(out=rstd2[:, T:T + 1], in_=ssq2[:, T:T + 1], func=AF.Sqrt, scale=1.0 / D,
                                                   bias=eps_col[:, 0:1]),
                reads=[("ssq2", T), "eps_col"], writes=[("rstd2", T)])
        sch.add("dve", lambda e, T=T: e.reciprocal(out=rstd2[:, T:T + 1], in_=rstd2[:, T:T + 1]), reads=[("rstd2", T)], writes=[("rstd2", T)])
        sch.add("dve", lambda e, T=T, b=b: e.scalar_tensor_tensor(out=xs5[b], in0=x1[:, T, :], scalar=rstd2[:, T:T + 1], in1=g2bc,
                                                                  op0=ALU.mult, op1=ALU.mult),
                reads=[("x1", T, 0), ("x1", T, 1), ("rstd2", T), "g2bc"], writes=[("xs5", b)])
        def back5(T=T, b=b, tsl=tsl):
            tb = next_bank(4, 6)

            def tr5(e, b=b, tb=tb):
                o = ps[tb][:].bitcast(BF16).rearrange("p (c t) -> p c t", c=8)
                ins = None
                for c in range(8):
                    ins = e.transpose(o[:, c, :], xs5[b][:, c * P:(c + 1) * P], ident_bf[:])
                return ins
            sch.add("pe", tr5, reads=[("xs5", b), "ident_bf"], writes=[psk(tb)])
            sch.add("act", lambda e, tb=tb, tsl=tsl: e.activation(
                out=h2T[:, :, tsl], in_=ps[tb][:].bitcast(BF16).rearrange("p (c t) -> p c t", c=8), func=AF.Copy),
                reads=[psk(tb)], writes=[("h2T", T)])
            lbk = next_bank(6, 8)

            def rmm(e, T=T, lbk=lbk):
                ins = None
                for c in range(8):
                    ins = e.matmul(ps[lbk][:, 0:36], lhsT=h2T[:, c, T * P:(T + 1) * P], rhs=wrb[:, c, :], start=(c == 0), stop=(c == 7))
                return ins
            sch.add("pe", rmm, reads=[("h2T", T), "wrb"], writes=[psk(lbk)])
            sch.add("dve", lambda e, T=T, lbk=lbk: e.tensor_tensor(out=logit[:, T, :], in0=ps[lbk][:, 0:36], in1=brbc[:], op=ALU.add),
                    reads=[psk(lbk), "brbc"], writes=["logit"])
        pend5.append(back5)
        while len(pend5) > 1:
            pend5.pop(0)()
    while pend5:
        pend5.pop(0)()
    dump("x1", x1, dbg.get("x1").rearrange("(t p) c -> p t c", p=P) if "x1" in dbg else None,
         [("x1", T, hh) for T in range(NT) for hh in range(2)])
    if STOP_AFTER == "x1":
        return finish()

    gl = logit[:, :, 0:4]
    el = logit[:, :, 4:36].rearrange("p t (g e) -> p t g e", g=4)
    _ro = [101 * KB]

    def rv(shape):
        nb = int(np.prod(shape[1:])) * 4
        v = view(R4, _ro[0], shape, F32)
        _ro[0] += nb
        return v
    gmax = rv([P, NT])
    goh = rv([P, NT, 4])
    gsh = rv([P, NT, 4])
    gsum = rv([P, NT])
    gw = rv([P, NT])
    etmp = rv([P, NT, 4, 8])
    esel = rv([P, NT, 8])
    esel2 = rv([P, NT, 8])
    m1 = rv([P, NT])
    m2 = rv([P, NT])
    oh1 = rv([P, NT, 8])
    oh2 = rv([P, NT, 8])
    dd = rv([P, NT])
    w1 = rv([P, NT])
    w2 = rv([P, NT])
    gsel = rv([P, NT, 8])
    assert _ro[0] <= 110 * KB
    gates = view(R4, 110 * KB, [P, NT, 4, 8], F32)

    def D_(fn, reads, writes):
        sch.add("dve", fn, reads=reads, writes=writes)

    def bc3(ap2, n):
        return ap2.unsqueeze(2).to_broadcast([P, NT, n])
    D_(lambda e: e.tensor_reduce(out=gmax[:], in_=gl, axis=AX.X, op=ALU.max), ["logit"], ["gmax"])
    D_(lambda e: e.tensor_tensor(out=goh[:], in0=gl, in1=bc3(gmax[:, :], 4), op=ALU.is_equal), ["logit", "gmax"], ["goh"])
    D_(lambda e: e.tensor_tensor(out=gsh[:], in0=gl, in1=bc3(gmax[:, :], 4), op=ALU.subtract), ["logit", "gmax"], ["gsh"])
    sch.add("act", lambda e: e.activation(out=gsh[:], in_=gsh[:], func=AF.Exp), reads=["gsh"], writes=["gsh"])
    D_(lambda e: e.tensor_reduce(out=gsum[:], in_=gsh[:], axis=AX.X, op=ALU.add), ["gsh"], ["gsum"])
    D_(lambda e: e.reciprocal(out=gw[:], in_=gsum[:]), ["gsum"], ["gw"])
    D_(lambda e: e.tensor_tensor(out=etmp[:], in0=el, in1=goh[:, :, :].unsqueeze(3).to_broadcast([P, NT, 4, 8]), op=ALU.mult),
       ["logit", "goh"], ["etmp"])
    D_(lambda e: e.tensor_reduce(out=esel[:], in_=etmp[:, :, :, :].rearrange("p t g e -> p t e g"), axis=AX.X, op=ALU.add), ["etmp"], ["esel"])
    D_(lambda e: e.tensor_reduce(out=m1[:], in_=esel[:], axis=AX.X, op=ALU.max), ["esel"], ["m1"])
    D_(lambda e: e.tensor_tensor(out=oh1[:], in0=esel[:], in1=bc3(m1[:, :], 8), op=ALU.is_equal), ["esel", "m1"], ["oh1"])
    D_(lambda e: e.scalar_tensor_tensor(out=esel2[:], in0=oh1[:], scalar=-1e30, in1=esel[:], op0=ALU.mult, op1=ALU.add), ["oh1", "esel"], ["esel2"])
    D_(lambda e: e.tensor_reduce(out=m2[:], in_=esel2[:], axis=AX.X, op=ALU.max), ["esel2"], ["m2"])
    D_(lambda e: e.tensor_tensor(out=oh2[:], in0=esel2[:], in1=bc3(m2[:, :], 8), op=ALU.is_equal), ["esel2", "m2"], ["oh2"])
    D_(lambda e: e.tensor_tensor(out=dd[:], in0=m2[:], in1=m1[:], op=ALU.subtract), ["m1", "m2"], ["dd"])
    sch.add("act", lambda e: e.activation(out=dd[:], in_=dd[:], func=AF.Exp), reads=["dd"], writes=["dd"])
    D_(lambda e: e.tensor_scalar(out=w1[:], in0=dd[:], scalar1=1.0, scalar2=None, op0=ALU.add), ["dd"], ["w1"])
    D_(lambda e: e.reciprocal(out=w1[:], in_=w1[:]), ["w1"], ["w1"])
    D_(lambda e: e.tensor_tensor(out=w2[:], in0=dd[:], in1=w1[:], op=ALU.mult), ["dd", "w1"], ["w2"])
    D_(lambda e: e.tensor_tensor(out=w1[:], in0=w1[:], in1=gw[:], op=ALU.mult), ["w1", "gw"], ["w1"])
    D_(lambda e: e.tensor_tensor(out=w2[:], in0=w2[:], in1=gw[:], op=ALU.mult), ["w2", "gw"], ["w2"])
    D_(lambda e: e.tensor_tensor(out=oh1[:], in0=oh1[:], in1=bc3(w1[:, :], 8), op=ALU.mult), ["oh1", "w1"], ["oh1"])
    D_(lambda e: e.tensor_tensor(out=oh2[:], in0=oh2[:], in1=bc3(w2[:, :], 8), op=ALU.mult), ["oh2", "w2"], ["oh2"])
    D_(lambda e: e.tensor_tensor(out=gsel[:], in0=oh1[:], in1=oh2[:], op=ALU.add), ["oh1", "oh2"], ["gsel"])
    D_(lambda e: e.tensor_tensor(out=gates[:], in0=goh[:, :, :].unsqueeze(3).to_broadcast([P, NT, 4, 8]),
                                 in1=gsel[:, :, :].unsqueeze(2).to_broadcast([P, NT, 4, 8]), op=ALU.mult), ["goh", "gsel"], ["gates"])
    dump("gates", gates[:, :, :, :].rearrange("p t g e -> p t (g e)"),
         dbg.get("gates").rearrange("(t p) c -> p t c", p=P) if "gates" in dbg else None, ["gates"])
    if STOP_AFTER == "router":
        return finish()
    sch.barrier()

    hid = [view(R4, 64 * KB + i * 8 * KB, [P, 2, S], BF16) for i in range(2)]
    gT = view(R4, 80 * KB, [32, 2, S], BF16, parts=32)
    onehot = view(R4, 88 * KB, [32, 32, 128], BF16, parts=32)
    sa = [view(R4, 96 * KB + i * 2 * KB, [P, 512], F32) for i in range(2)]
    t1 = [view(R4, 100 * KB + i * 2 * KB, [P, 512], F32) for i in range(2)]
    wbuf = []
    for RR in (R2, R3):
        wbuf.append((view(RR, 0, [P, 8, 256], BF16), view(RR, 4 * KB, [P, 8, 256], BF16), view(RR, 8 * KB, [P, 2, D], BF16)))
    sch.add("sp", lambda e: e.dma_start(out=onehot, in_=onehot_d.rearrange("e (k m) -> e k m", k=32)), writes=["onehot"], dma=True)
    for T4 in range(4):
        tb = next_bank(0, 4)

        def trg(e, T4=T4, tb=tb):
            ins = None
            for k in range(4):
                T = T4 * 4 + k
                ins = e.transpose(ps[tb][0:32, k * 128:(k + 1) * 128], gates[:, T, :, :].rearrange("p g e -> p (g e)"), ident_f[:])
            return ins
        sch.add("pe", trg, reads=["gates", "ident_f"], writes=[psk(tb)])
        sl = slice(T4 * 512, (T4 + 1) * 512)
        sch.add("act", lambda e, tb=tb, sl=sl: e.activation(out=gT[0:32, 0, sl], in_=ps[tb][0:32, :], func=AF.Copy),
                reads=[psk(tb)], writes=[("gThi", T4), psk(tb)])
        sch.add("dve", lambda e, tb=tb, sl=sl: e.tensor_tensor(out=gT[0:32, 1, sl], in0=ps[tb][0:32, :], in1=gT[0:32, 0, sl], op=ALU.subtract),
                reads=[psk(tb), ("gThi", T4)], writes=[("gTlo", T4), psk(tb)])
    mcnt = [0]
    for pr in range(NE // 2):
      for ex in (2 * pr, 2 * pr + 1):
        wbi = ex % 2
        Wg, Wu, Wd = wbuf[wbi]
        sch.add("pool", lambda e, Wg=Wg, ex=ex: e.dma_start(out=Wg, in_=wg_d[ex].rearrange("(c p) f -> p c f", p=P)),
                writes=[("Wg", wbi)], dma=True)
        sch.add("pool", lambda e, Wu=Wu, ex=ex: e.dma_start(out=Wu, in_=wu_d[ex].rearrange("(c p) f -> p c f", p=P)),
                writes=[("Wu", wbi)], dma=True)
        for hh in range(2):
            sch.add("pool", lambda e, Wd=Wd, ex=ex, hh=hh: e.dma_start(
                out=Wd[:, :, hh * 512:(hh + 1) * 512], in_=wd_d[ex][:, hh * 512:(hh + 1) * 512].rearrange("(c p) n -> p c n", p=P)),
                writes=[("Wd", wbi, hh)], dma=True)
        hb = ex % 2
        for tc in range(4):
            sl = slice(tc * 512, (tc + 1) * 512)
            gb = mcnt[0] % 2
            mcnt[0] += 1

            def gmm(e, ex=ex, gb=gb, sl=sl):
                e.matmul(ps[gb][:], lhsT=onehot[0:32, ex, :], rhs=gT[0:32, 0, sl], start=True, stop=False)
                return e.matmul(ps[gb][:], lhsT=onehot[0:32, ex, :], rhs=gT[0:32, 1, sl], start=False, stop=True)
            sch.add("pe", gmm, reads=["onehot", ("gThi", tc), ("gTlo", tc)], writes=[psk(gb)])
            for ft in range(2):
                ab = 2 + (mcnt[0] % 2)
                ub = 4 + (mcnt[0] % 2)
                tb_ = mcnt[0] % 2
                mcnt[0] += 1

                def amm(e, Wg=Wg, ft=ft, sl=sl, ab=ab):
                    ins = None
                    for c in range(8):
                        ins = e.matmul(ps[ab][:], lhsT=Wg[:, c, ft * 128:(ft + 1) * 128], rhs=h2T[:, c, sl], start=(c == 0), stop=(c == 7))
                    return ins

                def umm2(e, Wu=Wu, ft=ft, sl=sl, ub=ub):
                    ins = None
                    for c in range(8):
                        ins = e.matmul(ps[ub][:], lhsT=Wu[:, c, ft * 128:(ft + 1) * 128], rhs=h2T[:, c, sl], start=(c == 0), stop=(c == 7))
                    return ins
                hkeys = [("h2T", tc * 4 + k) for k in range(4)]
                sch.add("pe", amm, reads=[("Wg", wbi)] + hkeys, writes=[psk(ab)])
                sch.add("pe", umm2, reads=[("Wu", wbi)] + hkeys, writes=[psk(ub)])
                sch.add("act", lambda e, ab=ab, tb_=tb_: e.activation(out=sa[tb_], in_=ps[ab][:], func=AF.Silu),
                        reads=[psk(ab)], writes=[("sa", tb_), psk(ab)])
                sch.add("dve", lambda e, ub=ub, tb_=tb_: e.tensor_tensor(out=t1[tb_], in0=ps[ub][:], in1=sa[tb_], op=ALU.mult),
                        reads=[psk(ub), ("sa", tb_)], writes=[("t1", tb_), psk(ub)])
                sch.add("dve", lambda e, gb=gb, tb_=tb_, hb=hb, ft=ft, sl=sl: e.tensor_tensor(out=hid[hb][:, ft, sl], in0=ps[gb][:], in1=t1[tb_],
                                                                                             op=ALU.mult),
                        reads=[psk(gb), ("t1", tb_)], writes=[("hid", hb, tc), psk(gb)])
      WdA, WdB = wbuf[0][2], wbuf[1][2]
      for T in range(NT):
            for hh in range(2):
                yb = 6 + (mcnt[0] % 2)
                mcnt[0] += 1

                def dmm(e, T=T, hh=hh, yb=yb):
                    ins = None
                    k = 0
                    for (hd, Wd_) in ((hid[0], WdA), (hid[1], WdB)):
                        for ft in range(2):
                            ins = e.matmul(ps[yb][:], lhsT=hd[:, ft, T * P:(T + 1) * P], rhs=Wd_[:, ft, hh * 512:(hh + 1) * 512],
                                           start=(k == 0), stop=(k == 3))
                            k += 1
                    return ins
                sch.add("pe", dmm, reads=[("hid", 0, T // 4), ("hid", 1, T // 4), ("Wd", 0, hh), ("Wd", 1, hh)], writes=[psk(yb)])
                sch.add("dve", lambda e, T=T, hh=hh, yb=yb: e.tensor_tensor(out=x1[:, T, hh * 512:(hh + 1) * 512], in0=ps[yb][:],
                                                                           in1=x1[:, T, hh * 512:(hh + 1) * 512], op=ALU.add),
                        reads=[psk(yb), ("x1", T, hh)], writes=[("x1", T, hh), psk(yb)])
    for T in range(NT):
        tsl = slice(T * P, (T + 1) * P)
        sch.add("sp", lambda e, T=T, tsl=tsl: e.dma_start(out=out_d[tsl, :], in_=x1[:, T, :]), reads=[("x1", T, 0), ("x1", T, 1)],
                writes=["out_%d" % T], dma=True)
    return finish()


_CACHE = {}


def _prep_shared(inputs):
    f32 = np.float32
    c = host_constants()
    w_in = np.asarray(inputs["w_in"][0], f32)
    shared = {}
    shared["w1"] = np.ascontiguousarray(w_in[:, W1_COLS])
    shared["wout"] = np.ascontiguousarray(np.asarray(inputs["w_out"][0], f32))
    shared["wr"] = np.ascontiguousarray(np.concatenate([np.asarray(inputs["w_router_group"][0], f32),
                                                        np.asarray(inputs["w_router_expert"][0], f32)], axis=1))
    shared["wg"] = np.ascontiguousarray(np.asarray(inputs["w_exp_gate"][0], f32))
    shared["wu"] = np.ascontiguousarray(np.asarray(inputs["w_exp_up"][0], f32))
    shared["wd"] = np.ascontiguousarray(np.asarray(inputs["w_exp_down"][0], f32))
    shared["w2aug"] = np.ascontiguousarray(np.concatenate([np.asarray(inputs["gla_gate_w2"][0], f32),
                                                           np.asarray(inputs["gla_gate_b"], f32).reshape(1, 256)], axis=0))
    shared["g1bc"] = np.ascontiguousarray(np.tile(np.asarray(inputs["norm1_g"], f32).reshape(1, D), (P, 1)))
    shared["g2bc"] = np.ascontiguousarray(np.tile(np.asarray(inputs["norm2_g"], f32).reshape(1, D), (P, 1)))
    qg = np.tile(np.asarray(inputs["q_norm_g"], f32).reshape(64), 2)
    kg = np.tile(np.asarray(inputs["k_norm_g"], f32).reshape(64), 2)
    shared["qkg"] = np.ascontiguousarray(np.stack([qg, kg], axis=1))
    shared["goutbc"] = np.ascontiguousarray(np.tile(np.asarray(inputs["gla_out_norm_g"], f32).reshape(1, 128), (P, 4)))
    br = np.concatenate([np.asarray(inputs["b_router_group"], f32).reshape(4),
                         np.asarray(inputs["b_router_expert"], f32).reshape(32)])
    shared["brbc"] = np.ascontiguousarray(np.tile(br.reshape(1, 36), (P, 1)))
    rb = np.asarray(inputs["rel_bias"], f32)
    idx = np.arange(128)
    bT = np.zeros((P, 8, 2, 128), f32)
    for dlt in range(2):
        dist = 128 * dlt + idx[None, :] - idx[:, None]
        bk = _t5_bucket_np(np.maximum(dist, 0))
        for h in range(8):
            bT[:, h, dlt, :] = rb[bk, h]
    shared["biasT"] = np.ascontiguousarray(bT.reshape(P, -1))
    shared["rb31"] = np.ascontiguousarray(np.tile(rb[31:32, :], (P, 1)))
    for k in ["ident_bf", "ident_f", "tri_bf", "tri_f", "after_f", "negmask", "blockones", "pow2"]:
        shared[k] = c[k]
    shared["onehot"] = np.ascontiguousarray(c["onehot"].reshape(32, -1))
    return shared


def kernel(**inputs):
    x = np.asarray(inputs["x"], np.float32)
    if "nc" not in _CACHE:
        _CACHE["nc"] = build_program()
    nc = _CACHE["nc"]
    shared = _prep_shared(inputs)
    in_maps = []
    for b in range(8):
        m = dict(shared)
        m["x"] = np.ascontiguousarray(x[b])
        in_maps.append(m)
    res = run_bass_kernel_spmd(nc, in_maps, core_ids=list(range(8)))
    _CACHE["last"] = res
    out = np.stack([np.asarray(r["out"], np.float32) for r in res.results], axis=0)
    return out
```

```python
import os
import numpy as np
import ml_dtypes
from contextlib import ExitStack
import concourse.bass as bass
import concourse.mybir as mybir
from concourse.bass_utils import run_bass_kernel_spmd

F32 = mybir.dt.float32
BF16 = mybir.dt.bfloat16
U8 = mybir.dt.uint8
AF = mybir.ActivationFunctionType
ALU = mybir.AluOpType
AX = mybir.AxisListType

P = 128
S = 2048
D = 1024
NT = 16
NE = 32
EPS = 1e-6
KITER = 18
DEBUG = {}
STOP_AFTER = None


class _Op:
    __slots__ = ("eng", "fn", "reads", "writes", "dma", "deps", "waits", "signal", "idx", "flag", "slotwait", "after")

    def __init__(self, eng, fn, reads, writes, dma):
        self.eng = eng
        self.fn = fn
        self.reads = reads
        self.writes = writes
        self.dma = dma
        self.deps = ()
        self.waits = []
        self.signal = None
        self.flag = False
        self.slotwait = None
        self.after = []


def _nofn(e):
    return None


class Sched:
    EPOCH = 12000
    RING = 8

    def __init__(self, nc, stack):
        self.nc = nc
        self.stack = stack
        self.ops = []

    def add(self, eng, fn, reads=(), writes=(), dma=False):
        reads = list(reads)
        writes = list(writes)
        for k in reads:
            if isinstance(k, tuple) and k and k[0] == "ps" and k not in writes:
                writes.append(k)
        self.ops.append(_Op(eng, fn, tuple(reads), tuple(writes), dma))

    def barrier(self):
        pos = getattr(self, "_barpos", 0)
        last = {}
        dmas = []
        for i, op in enumerate(self.ops):
            if i < pos:
                continue
            if op.dma:
                dmas.append(op)
            elif op.fn is not _nofn:
                last[op.eng] = op
        for eng in ("pe", "act", "dve", "pool", "sp"):
            b = _Op(eng, _nofn, (), (), False)
            b.after = [p for k, p in last.items() if k != eng] + dmas
            self.ops.append(b)
        self._barpos = len(self.ops)

    def finalize(self):
        nc = self.nc
        last_w = {}
        readers = {}
        for i, op in enumerate(self.ops):
            op.idx = i
            raw = set()
            other = set()
            for k in op.reads:
                if k in last_w:
                    raw.add(last_w[k])
            for k in op.writes:
                if k in last_w:
                    raw.add(last_w[k])
                for r in readers.get(k, ()):
                    other.add(r)
            raw.discard(i)
            other.discard(i)
            for k in op.reads:
                readers.setdefault(k, set()).add(i)
            for k in op.writes:
                last_w[k] = i
                readers[k] = set()
            best = {}
            deps = []
            for d in raw | other:
                p = self.ops[d]
                if p.dma:
                    deps.append(d)
                    continue
                if p.eng == op.eng and not op.dma:
                    if p.eng == "pe":
                        continue
                    if d not in raw:
                        continue
                if p.eng not in best or best[p.eng] < d:
                    best[p.eng] = d
            deps.extend(best.values())
            for a in op.after:
                deps.append(a.idx)
            op.deps = deps
            for d in deps:
                self.ops[d].flag = True
        cnt = {}
        self.sems = {}
        dcount = {}
        for op in self.ops:
            if op.dma:
                q = op.eng
                k = dcount.get(q, 0)
                dcount[q] = k + 1
                slot = k % self.RING
                name = "dq_%s_%d" % (q, slot)
                if name not in self.sems:
                    self.sems[name] = self.stack.enter_context(nc.semaphore(name))
                op.signal = (name, 16 * (k // self.RING + 1), 16)
                if k >= self.RING:
                    op.slotwait = (name, 16 * (k // self.RING))
            elif op.flag:
                c = cnt.get(op.eng, 0)
                ep = c // self.EPOCH
                name = "s_%s_%d" % (op.eng, ep)
                if name not in self.sems:
                    self.sems[name] = self.stack.enter_context(nc.semaphore(name))
                op.signal = (name, c % self.EPOCH + 1, 1)
                cnt[op.eng] = c + 1
        for op in self.ops:
            w = []
            if op.slotwait is not None:
                w.append(op.slotwait)
            for d in op.deps:
                sg = self.ops[d].signal
                w.append((sg[0], sg[1]))
            op.waits = w

    def emit(self, block):
        table = [("pe", block.tensor), ("act", block.scalar), ("dve", block.vector),
                 ("pool", block.gpsimd), ("sp", block.sync)]
        for engname, deco in table:
            ops = [op for op in self.ops if op.eng == engname]

            def body(e, ops=ops):
                seen = {}
                for op in ops:
                    for (sn, val) in op.waits:
                        if seen.get(sn, 0) >= val:
                            continue
                        e.wait_ge(self.sems[sn], val)
                        seen[sn] = val
                    ins = op.fn(e)
                    if op.signal is not None and ins is not None:
                        ins.then_inc(self.sems[op.signal[0]], op.signal[2])

            deco(body)


IQ_TILES = [(0, 3), (3, 6), (6, 8)]


def _t5_bucket_np(dist):
    max_exact = 16
    d_f = np.maximum(dist, 1).astype(np.float32)
    large = max_exact + (np.log(d_f / max_exact) / np.log(128 / max_exact) * (32 - max_exact)).astype(np.int32)
    large = np.minimum(large, 31)
    return np.where(dist < max_exact, dist, large)


def _w1_columns():
    A = 512
    off = {}
    o = 0
    for name, n in [("qa", 512), ("ka", 512), ("va", 512), ("iq", 256), ("ik", 32), ("iw", 8),
                    ("qb", 256), ("kb", 256), ("vb", 512), ("glr", 16), ("rg", 512)]:
        off[name] = o
        o += n
    cols = []
    groups = []

    def grp(name, kind, cl):
        groups.append((name, kind, len(cols), len(cl)))
        cols.extend(cl)

    r = lambda name, a, b: list(range(off[name] + a, off[name] + b))
    grp("qbkb", "fm", r("qb", 0, 256) + r("kb", 0, 256) + r("glr", 0, 16))
    grp("vb", "tm", r("vb", 0, 512))
    grp("rg", "tm", r("rg", 0, 512))
    grp("kbiw", "tm", r("kb", 0, 256) + r("iw", 0, 8))
    grp("qa", "fm", r("qa", 0, 512))
    grp("ka", "fm", r("ka", 0, 512))
    iqc = []
    for (h0, h1) in IQ_TILES:
        iqc += r("iq", h0 * 32, h1 * 32)
    grp("iqik", "fm", iqc + r("ik", 0, 32) * 3)
    grp("va", "tm", r("va", 0, 512))
    return np.array(cols, np.int64), groups


W1_COLS, W1_GROUPS = _w1_columns()
NW1 = len(W1_COLS)


def _selT_off(i):
    return sum((16 - ii) * 128 for ii in range(i))


SELT_TOTAL = _selT_off(16)


def host_constants():
    c = {}
    idx = np.arange(128)
    tri = (idx[:, None] <= idx[None, :])
    c["ident_bf"] = np.eye(128, dtype=np.float32).astype(ml_dtypes.bfloat16)
    c["ident_f"] = np.eye(128, dtype=np.float32)
    c["tri_bf"] = tri.astype(np.float32).astype(ml_dtypes.bfloat16)
    c["tri_f"] = tri.astype(np.float32)
    c["after_f"] = (idx[:, None] > idx[None, :]).astype(np.float32)
    c["negmask"] = np.where(idx[None, :] <= idx[:, None], 0.0, -1e30).astype(np.float32)
    bo = np.zeros((128, 128), np.float32)
    bo[:64, :64] = 1.0
    bo[64:, 64:] = 1.0
    c["blockones"] = bo.astype(ml_dtypes.bfloat16)
    c["pow2"] = np.tile((2.0 ** -np.arange(KITER + 1, dtype=np.float64)).astype(np.float32)[None, :], (128, 1))
    oh = np.zeros((32, 32, 128), np.float32)
    for e in range(32):
        oh[e, e, :] = 1.0
    c["onehot"] = oh.astype(ml_dtypes.bfloat16)
    return c


def build_program():
    nc = bass.Bass("TRN2", target_bir_lowering=False)
    stack = ExitStack()
    sch = Sched(nc, stack)

    def dram(name, shape, dt, kind="ExternalInput"):
        return nc.dram_tensor(name, list(shape), dt, kind=kind).ap()

    x_d = dram("x", [S, D], F32)
    w1_d = dram("w1", [D, NW1], F32)
    wout_d = dram("wout", [D, D], F32)
    wr_d = dram("wr", [D, 36], F32)
    wg_d = dram("wg", [NE, D, 256], F32)
    wu_d = dram("wu", [NE, D, 256], F32)
    wd_d = dram("wd", [NE, 256, D], F32)
    w2_d = dram("w2aug", [17, 256], F32)
    g1bc_d = dram("g1bc", [P, D], F32)
    g2bc_d = dram("g2bc", [P, D], F32)
    qkg_d = dram("qkg", [P, 2], F32)
    gout_d = dram("goutbc", [P, 512], F32)
    brbc_d = dram("brbc", [P, 36], F32)
    biasT_d = dram("biasT", [P, 8 * 2 * 128], F32)
    rb31_d = dram("rb31", [P, 8], F32)
    ident_bf_d = dram("ident_bf", [P, P], BF16)
    ident_f_d = dram("ident_f", [P, P], F32)
    tri_bf_d = dram("tri_bf", [P, P], BF16)
    tri_f_d = dram("tri_f", [P, P], F32)
    after_f_d = dram("after_f", [P, P], F32)
    negmask_d = dram("negmask", [P, P], F32)
    blockones_d = dram("blockones", [P, P], BF16)
    pow2_d = dram("pow2", [P, KITER + 1], F32)
    onehot_d = dram("onehot", [32, 32 * 128], BF16)
    out_d = dram("out", [S, D], F32, kind="ExternalOutput")
    dbg = {}
    for name, (shape, dt) in DEBUG.items():
        dbg[name] = dram("dbg_" + name, shape, dt, kind="ExternalOutput")

    def sb(name, shape, dt):
        return stack.enter_context(nc.sbuf_tensor(name, list(shape), dt))

    ident_bf = sb("ident_bf_s", [P, P], BF16)
    ident_f = sb("ident_f_s", [P, P], F32)
    tri_bf = sb("tri_bf_s", [P, P], BF16)
    tri_f = sb("tri_f_s", [P, P], F32)
    after_f = sb("after_f_s", [P, P], F32)
    negmask = sb("negmask_s", [P, P], F32)
    blockones = sb("blockones_s", [P, P], BF16)
    pow2 = sb("pow2_s", [P, KITER + 1], F32)
    g1bc = sb("g1bc_s", [P, D], F32)
    qkg = sb("qkg_s", [P, 2], F32)
    gout = sb("gout_s", [P, 512], F32)
    brbc = sb("brbc_s", [P, 36], F32)
    rb31 = sb("rb31_s", [P, 8], F32)
    Etile = sb("Etile", [P, 8, 2, 128], BF16)
    w2aug = sb("w2aug_s", [17, 256], BF16)
    iw_s = sb("iw_s", [P, NT, 8], F32)
    ssq1 = sb("ssq1", [P, NT], F32)
    rstd1 = sb("rstd1", [P, NT], F32)
    ones_col = sb("ones_col", [P, 1], F32)

    R1 = sb("R1", [P, 32 * 1024], U8)
    R2 = sb("R2", [P, 16 * 1024], U8)
    R3 = sb("R3", [P, 16 * 1024], U8)
    R4 = sb("R4", [P, 112 * 1024], U8)

    def view(arena, off, shape, dt, parts=P):
        nb = int(np.prod(shape[1:])) * (4 if dt == F32 else (2 if dt == BF16 else 1))
        ap = arena[0:parts, off:off + nb].bitcast(dt)
        if len(shape) == 3:
            ap = ap.rearrange("p (a b) -> p a b", a=shape[1])
        elif len(shape) == 4:
            ap = ap.rearrange("p (a b c) -> p a b c", a=shape[1], b=shape[2])
        return ap

    KB = 1024
    hT = view(R1, 0, [P, 8, S], BF16)
    mixT_b = view(R2, 0, [P, 4, S], BF16)
    mixT_a = view(R3, 0, [P, 4, S], BF16)
    qbT = view(R4, 0, [P, 2, S], BF16)
    kbT = view(R4, 8 * KB, [P, 2, S], BF16)
    glrT = view(R4, 16 * KB, [32, S], BF16, parts=32)
    vb = view(R4, 20 * KB, [P, NT, 512], BF16)
    Gt = view(R4, 36 * KB, [P, NT, 512], BF16)
    kbtm = view(R4, 52 * KB, [P, NT, 256], BF16)
    wst = [view(R4, 60 * KB + i * 9 * KB, [P, 8, 528], BF16) for i in range(2)]
    xst = [view(R4, 78 * KB + i * 4 * KB, [P, D], F32) for i in range(2)]
    xs = [view(R4, 86 * KB + i * 2 * KB, [P, D], BF16) for i in range(2)]
    junk = view(R4, 90 * KB, [P, 2048], BF16)
    tmpA = [view(R4, 94 * KB + i * 2 * KB, [P, 512], F32) for i in range(4)]
    glatmp = view(R4, 102 * KB, [P, 10 * 256], F32)

    ps = [stack.enter_context(nc.psum_tensor("ps%d" % i, [P, 512], F32)) for i in range(8)]

    def psk(i):
        return ("ps", i)

    def load_const(dst, src, key, eng="sp"):
        sch.add(eng, lambda e, dst=dst, src=src: e.dma_start(out=dst, in_=src), writes=[key], dma=True)

    load_const(ident_bf[:], ident_bf_d[:, :], "ident_bf")
    load_const(ident_f[:], ident_f_d[:, :], "ident_f")
    load_const(tri_bf[:], tri_bf_d[:, :], "tri_bf")
    load_const(tri_f[:], tri_f_d[:, :], "tri_f")
    load_const(after_f[:], after_f_d[:, :], "after_f")
    load_const(negmask[:], negmask_d[:, :], "negmask")
    load_const(blockones[:], blockones_d[:, :], "blockones")
    load_const(pow2[:], pow2_d[:, :], "pow2")
    load_const(g1bc[:], g1bc_d[:, :], "g1bc")
    load_const(qkg[:], qkg_d[:, :], "qkg")
    load_const(gout[:], gout_d[:, :], "gout")
    load_const(brbc[:], brbc_d[:, :], "brbc")
    load_const(rb31[:], rb31_d[:, :], "rb31")
    load_const(w2aug[:], w2_d[:, :], "w2aug", eng="pool")
    sch.add("dve", lambda e: e.memset(ones_col[:], 1.0), writes=["ones_col"])
    sch.add("dve", lambda e: e.memset(glrT[0:32, :], 1.0), writes=["glrT_init"])

    for T in range(NT):
        b = T % 2
        tsl = slice(T * P, (T + 1) * P)
        sch.add("sp", lambda e, b=b, tsl=tsl: e.dma_start(out=xst[b], in_=x_d[tsl, :]),
                writes=[("xst", b)], dma=True)
        sch.add("act", lambda e, b=b, T=T: e.activation(out=junk[:, 0:D], in_=xst[b], func=AF.Square,
                                                        accum_out=ssq1[:, T:T + 1]),
                reads=[("xst", b)], writes=["junk", ("ssq1", T)])
        sch.add("act", lambda e, T=T: e.activation(out=rstd1[:, T:T + 1], in_=ssq1[:, T:T + 1], func=AF.Sqrt,
                                                   scale=1.0 / D, bias=eps_col[:, 0:1]),
                reads=[("ssq1", T), "eps_col"], writes=[("rstd1", T)])
        sch.add("dve", lambda e, T=T: e.reciprocal(out=rstd1[:, T:T + 1], in_=rstd1[:, T:T + 1]),
                reads=[("rstd1", T)], writes=[("rstd1", T)])
        sch.add("dve", lambda e, b=b, T=T: e.scalar_tensor_tensor(out=xs[b], in0=xst[b], scalar=rstd1[:, T:T + 1],
                                                                  in1=g1bc[:], op0=ALU.mult, op1=ALU.mult),
                reads=[("xst", b), ("rstd1", T), "g1bc"], writes=[("xs", b)])
        pb = T % 2

        def tr(e, b=b, pb=pb):
            o = ps[pb][:].bitcast(BF16).rearrange("p (c t) -> p c t", c=8)
            ins = None
            for c in range(8):
                ins = e.transpose(o[:, c, :], xs[b][:, c * P:(c + 1) * P], ident_bf[:])
            return ins
        sch.add("pe", tr, reads=[("xs", b), "ident_bf"], writes=[psk(pb)])
        sch.add("act", lambda e, pb=pb, tsl=tsl: e.activation(
            out=hT[:, :, tsl], in_=ps[pb][:].bitcast(BF16).rearrange("p (c t) -> p c t", c=8), func=AF.Copy),
            reads=[psk(pb)], writes=[("hT", T)])

    wcount = [0]

    def load_wgroup(gi):
        name, kind, c0, n = W1_GROUPS[gi]
        b = wcount[0] % 2
        wcount[0] += 1
        src = w1_d[:, c0:c0 + n].rearrange("(c p) n -> p c n", p=P)
        dst = wst[b][:, :, 0:n]
        sch.add("pool", lambda e, dst=dst, src=src: e.dma_start(out=dst, in_=src),
                writes=[("wst", b)], dma=True)
        return b

    bankrot = {}

    def next_bank(lo=2, hi=6):
        k = (lo, hi)
        c = bankrot.get(k, 0)
        bankrot[k] = c + 1
        return lo + c % (hi - lo)

    def fm_matmul(wb, col0, m, tc, bank, parts=None):
        wt = wst[wb]

        def fn(e):
            ins = None
            for c in range(8):
                ins = e.matmul(ps[bank][0:m, :], lhsT=wt[:, c, col0:col0 + m],
                               rhs=hT[:, c, tc * 512:(tc + 1) * 512], start=(c == 0), stop=(c == 7))
            return ins
        sch.add("pe", fn, reads=[("wst", wb)] + [("hT", tc * 4 + k) for k in range(4)], writes=[psk(bank)])

    def tm_matmul(wb, col0, n, T, bank):
        wt = wst[wb]

        def fn(e):
            ins = None
            for c in range(8):
                ins = e.matmul(ps[bank][:, 0:n], lhsT=hT[:, c, T * P:(T + 1) * P],
                               rhs=wt[:, c, col0:col0 + n], start=(c == 0), stop=(c == 7))
            return ins
        sch.add("pe", fn, reads=[("wst", wb), ("hT", T)], writes=[psk(bank)])

    eps_col = sb("eps_col", [P, 1], F32)
    sch.ops.insert(0, _Op("dve", lambda e: e.memset(eps_col[:], EPS), (), ("eps_col",), False))

    wb = load_wgroup(0)
    for tc in range(4):
        tsl = slice(tc * 512, (tc + 1) * 512)
        for k in range(4):
            bank = next_bank()
            fm_matmul(wb, k * 128, 128, tc, bank)
            dst = (qbT if k < 2 else kbT)[:, k % 2, tsl]
            key = ("qbT" if k < 2 else "kbT", tc)
            eng = "act" if k % 2 == 0 else "dve"
            if eng == "act":
                sch.add("act", lambda e, dst=dst, bank=bank: e.activation(out=dst, in_=ps[bank][:], func=AF.Copy),
                        reads=[psk(bank)], writes=[key + (k % 2,)])
            else:
                sch.add("dve", lambda e, dst=dst, bank=bank: e.tensor_copy(out=dst, in_=ps[bank][:]),
                        reads=[psk(bank)], writes=[key + (k % 2,)])
        bank = next_bank()
        fm_matmul(wb, 512, 16, tc, bank)
        sch.add("dve", lambda e, tsl=tsl, bank=bank: e.tensor_copy(out=glrT[0:16, tsl], in_=ps[bank][0:16, :]),
                reads=[psk(bank), "glrT_init"], writes=[("glrT", tc)])
    wb = load_wgroup(1)
    for T in range(NT):
        bank = next_bank()
        tm_matmul(wb, 0, 512, T, bank)
        eng = "act" if T % 2 == 0 else "dve"
        if eng == "act":
            sch.add("act", lambda e, T=T, bank=bank: e.activation(out=vb[:, T, :], in_=ps[bank][:], func=AF.Copy),
                    reads=[psk(bank)], writes=[("vb", T)])
        else:
            sch.add("dve", lambda e, T=T, bank=bank: e.tensor_copy(out=vb[:, T, :], in_=ps[bank][:]),
                    reads=[psk(bank)], writes=[("vb", T)])
    wb = load_wgroup(2)
    for T in range(NT):
        bank = next_bank()
        tm_matmul(wb, 0, 512, T, bank)
        tb = T % 2
        sch.add("act", lambda e, tb=tb, bank=bank: e.activation(out=tmpA[tb][:], in_=ps[bank][:], func=AF.Silu),
                reads=[psk(bank)], writes=[("tmpA", tb)])
        sch.add("pool", lambda e, tb=tb, T=T: e.tensor_tensor(out=Gt[:, T, :], in0=tmpA[tb][:], in1=gout[:], op=ALU.mult),
                reads=[("tmpA", tb), "gout"], writes=[("Gt", T)])
    wb = load_wgroup(3)
    for T in range(NT):
        bank = next_bank()
        tm_matmul(wb, 0, 264, T, bank)
        sch.add("dve", lambda e, T=T, bank=bank: e.tensor_copy(out=kbtm[:, T, :], in_=ps[bank][:, 0:256]),
                reads=[psk(bank)], writes=[("kbtm", T)])
        sch.add("dve", lambda e, T=T, bank=bank: e.tensor_copy(out=iw_s[:, T, :], in_=ps[bank][:, 256:264]),
                reads=[psk(bank)], writes=[("iw", T)])

    def dump(name, src_ap, dst_ap, reads):
        if name in dbg:
            sch.add("sp", lambda e: e.dma_start(out=dst_ap, in_=src_ap), reads=reads, writes=["dbg_" + name], dma=True)

    def finish():
        import os
        if os.environ.get("KSTOP_OPS"):
            print("total ops", len(sch.ops))
            sch.ops = sch.ops[:int(os.environ["KSTOP_OPS"])]
        outs = ["dbg_" + n for n in dbg] + ["out_%d" % T for T in range(NT)]
        sch.add("sp", lambda e: None, reads=outs)
        sch.finalize()
        with nc.Block() as block:
            sch.emit(block)
        stack.close()
        return nc

    dump("hT", hT, dbg.get("hT").rearrange("(c p) t -> p c t", p=P) if "hT" in dbg else None, [("hT", T) for T in range(NT)])
    dump("qbT", qbT[:, 0, :], dbg.get("qbT"), [("qbT", tc, 0) for tc in range(4)])
    dump("vb", vb, dbg.get("vb").rearrange("(t p) c -> p t c", p=P) if "vb" in dbg else None, [("vb", T) for T in range(NT)])
    dump("Gt", Gt, dbg.get("Gt").rearrange("(t p) c -> p t c", p=P) if "Gt" in dbg else None, [("Gt", T) for T in range(NT)])
    if STOP_AFTER == "p1":
        return finish()

    Sst = sb("Sst", [P, 4, 128], F32)
    Sbf = sb("Sbf", [P, 4, 128], BF16)
    g_e1 = sb("g_e1", [P, 256], F32)
    g_l = sb("g_l", [P, 256], F32)
    g_eb = [sb("g_eb%d" % i, [P, 2, 128], F32) for i in range(2)]
    g_einv = sb("g_einv", [P, 2, 128], F32)
    g_erem = sb("g_erem", [P, 256], F32)
    g_qt = [sb("g_qt%d" % i, [P, 2, 128], BF16) for i in range(2)]
    g_kt = sb("g_kt", [P, 2, 128], BF16)
    g_kh = sb("g_kh", [P, 256], BF16)
    g_A = [sb("g_A%d" % i, [P, 4, 128], BF16) for i in range(2)]
    g_ob = sb("g_ob", [P, 512], BF16)
    g_ssq = sb("g_ssq", [P, 4], F32)
    g_rs = sb("g_rs", [P, 4], F32)
    sch.add("dve", lambda e: e.memset(Sst[:], 0.0), writes=[("Sst", h) for h in range(4)])
    sch.add("dve", lambda e: e.memset(Sbf[:], 0.0), writes=[("Sbf", h) for h in range(4)])
    BZ, BC, BA, BO, BT = 0, 1, 3, 4, 6
    BUS = [5, 2]
    print("ops before GLA", len(sch.ops))

    def gla_stage1(T):
        b = T % 2
        tsl = slice(T * P, (T + 1) * P)
        eb, qt, A, BU = g_eb[b], g_qt[b], g_A[b], BUS[b]
        sch.add("pe", lambda e: e.matmul(ps[BZ][:, 0:256], lhsT=glrT[0:17, tsl], rhs=w2aug[0:17, :], start=True, stop=True),
                reads=[("glrT", T // 4), "glrT_init", "w2aug"], writes=[psk(BZ)])
        sch.add("act", lambda e: e.activation(out=g_e1[:], in_=ps[BZ][:, 0:256], func=AF.Exp, scale=-1.0),
                reads=[psk(BZ)], writes=["g_e1"])
        sch.add("act", lambda e: e.activation(out=g_l[:], in_=g_e1[:], func=AF.Ln, scale=1.0, bias=ones_col[:, 0:1]),
                reads=["g_e1", "ones_col"], writes=["g_l"])

        def cum(e):
            e.matmul(ps[BC][:, 0:128], lhsT=g_l[:, 0:128], rhs=tri_f[:], start=True, stop=True)
            return e.matmul(ps[BC][:, 128:256], lhsT=g_l[:, 128:256], rhs=tri_f[:], start=True, stop=True)
        sch.add("pe", cum, reads=["g_l", "tri_f"], writes=[psk(BC)])
        sch.add("pe", lambda e: e.matmul(ps[BZ][:, 256:512], lhsT=after_f[:], rhs=g_l[:], start=True, stop=True),
                reads=["g_l", "after_f"], writes=[psk(BZ)])
        cview = ps[BC][:, 0:256].rearrange("p (a b) -> p a b", a=2)
        sch.add("act", lambda e: e.activation(out=eb[:], in_=cview, func=AF.Exp, scale=-1.0 / 16),
                reads=[psk(BC)], writes=[("g_eb", b)])
        sch.add("act", lambda e: e.activation(out=g_einv[:], in_=cview, func=AF.Exp, scale=1.0 / 16),
                reads=[psk(BC)], writes=["g_einv"])
        sch.add("act", lambda e: e.activation(out=g_erem[:], in_=ps[BZ][:, 256:512], func=AF.Exp, scale=-1.0 / 16),
                reads=[psk(BZ)], writes=["g_erem"])
        sch.add("dve", lambda e: e.scalar_tensor_tensor(out=qt[:], in0=qbT[:, :, tsl], scalar=0.125, in1=eb[:],
                                                        op0=ALU.mult, op1=ALU.mult),
                reads=[("qbT", T // 4, 0), ("qbT", T // 4, 1), ("g_eb", b)], writes=[("g_qt", b)])
        sch.add("dve", lambda e: e.scalar_tensor_tensor(out=g_kt[:], in0=kbT[:, :, tsl], scalar=1.0, in1=g_einv[:],
                                                        op0=ALU.mult, op1=ALU.mult),
                reads=[("kbT", T // 4, 0), ("kbT", T // 4, 1), "g_einv"], writes=["g_kt"])
        sch.add("dve", lambda e: e.scalar_tensor_tensor(out=g_kh[:], in0=kbtm[:, T, :], scalar=1.0, in1=g_erem[:],
                                                        op0=ALU.mult, op1=ALU.mult),
                reads=[("kbtm", T), "g_erem"], writes=["g_kh"])

        def attn(e):
            ins = None
            for h in (0, 2, 1, 3):
                p, r = h // 2, h % 2
                rs = slice(r * 64, (r + 1) * 64)
                bk = BA if r == 0 else 7
                ins = e.matmul(ps[bk][:, p * 128:(p + 1) * 128], lhsT=g_kt[rs, p, :], rhs=qt[rs, p, :], start=True, stop=True)
            return ins
        sch.add("pe", attn, reads=["g_kt", ("g_qt", b)], writes=[psk(BA), psk(7)])
        for r in range(2):
            bk = BA if r == 0 else 7
            sch.add("dve", lambda e, r=r, bk=bk: e.tensor_tensor(
                out=A[:, r::2, :], in0=ps[bk][:, 0:256].rearrange("p (h t) -> p h t", h=2),
                in1=tri_bf[:, :].unsqueeze(1).to_broadcast([P, 2, 128]), op=ALU.mult),
                reads=[psk(bk), "tri_bf"], writes=[("g_A", b, r)])

        def umm(e):
            ins = None
            for h in range(4):
                p = h // 2
                ins = e.matmul(ps[BU][:, h * 128:(h + 1) * 128], lhsT=g_kh[:, p * 128:(p + 1) * 128], rhs=vb[:, T, h * 128:(h + 1) * 128],
                               start=True, stop=True)
            return ins
        sch.add("pe", umm, reads=["g_kh", ("vb", T)], writes=[psk(BU)])

    def gla_stage2(T):
        b = T % 2
        tsl = slice(T * P, (T + 1) * P)
        eb, qt, A, BU = g_eb[b], g_qt[b], g_A[b], BUS[b]

        def omm(e):
            ins = None
            for h in range(4):
                p = h // 2
                e.matmul(ps[BO][:, h * 128:(h + 1) * 128], lhsT=A[:, h, :], rhs=vb[:, T, h * 128:(h + 1) * 128], start=True, stop=False)
                ins = e.matmul(ps[BO][:, h * 128:(h + 1) * 128], lhsT=qt[:, p, :], rhs=Sbf[:, h, :], start=False, stop=True)
            return ins
        sch.add("pe", omm, reads=[("g_A", b, 0), ("g_A", b, 1), ("vb", T), ("g_qt", b)] + [("Sbf", h) for h in range(4)], writes=[psk(BO)])
        for h in range(4):
            p, r = h // 2, h % 2
            rs = slice(r * 64, (r + 1) * 64)
            sch.add("dve", lambda e, h=h, p=p, rs=rs: e.scalar_tensor_tensor(
                out=Sst[rs, h, :], in0=Sst[rs, h, :], scalar=eb[rs, p, 127:128], in1=ps[BU][rs, h * 128:(h + 1) * 128],
                op0=ALU.mult, op1=ALU.add), reads=[psk(BU), ("g_eb", b), ("Sst", h)], writes=[("Sst", h), psk(BU)])
            sch.add("act", lambda e, h=h, rs=rs: e.activation(out=Sbf[rs, h, :], in_=Sst[rs, h, :], func=AF.Copy),
                    reads=[("Sst", h)], writes=[("Sbf", h)])
        for h in range(4):
            sch.add("act", lambda e, h=h: e.activation(out=junk[:, 0:128], in_=ps[BO][:, h * 128:(h + 1) * 128], func=AF.Square,
                                                       accum_out=g_ssq[:, h:h + 1]),
                    reads=[psk(BO)], writes=["junk", ("g_ssq", h), psk(BO)])
        sch.add("act", lambda e: e.activation(out=g_rs[:], in_=g_ssq[:], func=AF.Sqrt, scale=1.0 / 128, bias=eps_col[:, 0:1]),
                reads=[("g_ssq", h) for h in range(4)] + ["eps_col"], writes=["g_rs"])
        sch.add("dve", lambda e: e.reciprocal(out=g_rs[:], in_=g_rs[:]), reads=["g_rs"], writes=["g_rs"])
        for h in range(4):
            hs = slice(h * 128, (h + 1) * 128)
            sch.add("dve", lambda e, h=h, hs=hs: e.scalar_tensor_tensor(
                out=g_ob[:, hs], in0=ps[BO][:, hs], scalar=g_rs[:, h:h + 1], in1=Gt[:, T, hs], op0=ALU.mult, op1=ALU.mult),
                reads=[psk(BO), "g_rs", ("Gt", T)], writes=[("g_ob", h), psk(BO)])
        if "ob" in dbg:
            sch.add("sp", lambda e: e.dma_start(out=dbg["ob"][tsl, :], in_=g_ob[:]), reads=[("g_ob", h) for h in range(4)],
                    writes=["dbg_ob"], dma=True)

        def trb(e):
            o = ps[BT][:].bitcast(BF16).rearrange("p (c t) -> p c t", c=8)
            ins = None
            for c in range(4):
                ins = e.transpose(o[:, c, :], g_ob[:, c * P:(c + 1) * P], ident_bf[:])
            return ins
        sch.add("pe", trb, reads=[("g_ob", h) for h in range(4)] + ["ident_bf"], writes=[psk(BT)])
        sch.add("act", lambda e: e.activation(
            out=mixT_b[:, :, tsl], in_=ps[BT][:].bitcast(BF16).rearrange("p (c t) -> p c t", c=8)[:, 0:4, :], func=AF.Copy),
            reads=[psk(BT)], writes=[("mixT_b", T)])

    gla_stage1(0)
    for T in range(NT):
        if T + 1 < NT:
            gla_stage1(T + 1)
        gla_stage2(T)
    if STOP_AFTER == "gla":
        return finish()
    sch.barrier()

    qaT = view(R4, 0, [P, 4, S], BF16)
    kaT = view(R4, 16 * KB, [P, 4, S], BF16)
    iqT = view(R4, 32 * KB, [P, 3, S], BF16)
    ikT = view(R4, 44 * KB, [P, S], BF16)
    vaT = view(R4, 48 * KB, [P, NT, 8 * 65], BF16)
    wst2 = [view(R4, 66 * KB + i * 9 * KB, [P, 8, 528], BF16) for i in range(2)]
    n_sq = [view(R4, 84 * KB + i * KB, [P, 512], BF16) for i in range(2)]
    n_ln = [view(R4, 86 * KB + i * 2 * KB, [P, 512], F32) for i in range(2)]
    n_rs = [view(R4, 90 * KB + i * 2 * KB, [P, 512], F32) for i in range(2)]
    wst[0], wst[1] = wst2[0], wst2[1]
    sch.add("dve", lambda e: e.memset(vaT[:], 1.0), writes=[("va", T) for T in range(NT)])
    ncnt = [0]
    pend2 = []
    for gi, which in [(4, 0), (5, 1)]:
        wb = load_wgroup(gi)
        dstT = qaT if which == 0 else kaT
        kname = "qaT" if which == 0 else "kaT"
        for tc in range(4):
            tsl = slice(tc * 512, (tc + 1) * 512)
            for p in range(4):
                bank = next_bank(0, 4)
                sbank = next_bank(4, 8)
                nb = ncnt[0] % 2
                ncnt[0] += 1
                fm_matmul(wb, p * 128, 128, tc, bank)
                sch.add("act", lambda e, nb=nb, bank=bank: e.activation(out=n_sq[nb], in_=ps[bank][:], func=AF.Square),
                        reads=[psk(bank)], writes=[("n_sq", nb), psk(bank)])
                def back2(nb=nb, sbank=sbank, bank=bank, p=p, tsl=tsl, dstT=dstT, which=which, kname=kname, tc=tc):
                    sch.add("pe", lambda e: e.matmul(ps[sbank][:], lhsT=blockones[:], rhs=n_sq[nb], start=True, stop=True),
                            reads=[("n_sq", nb), "blockones"], writes=[psk(sbank)])
                    sch.add("act", lambda e: e.activation(out=n_ln[nb], in_=ps[sbank][:], func=AF.Ln, scale=1.0 / 64,
                                                          bias=eps_col[:, 0:1]),
                            reads=[psk(sbank), "eps_col"], writes=[("n_ln", nb)])
                    sch.add("act", lambda e: e.activation(out=n_rs[nb], in_=n_ln[nb], func=AF.Exp, scale=-0.5),
                            reads=[("n_ln", nb)], writes=[("n_rs", nb)])
                    sch.add("dve", lambda e: e.scalar_tensor_tensor(
                        out=dstT[:, p, tsl], in0=ps[bank][:], scalar=qkg[:, which:which + 1], in1=n_rs[nb], op0=ALU.mult, op1=ALU.mult),
                        reads=[psk(bank), ("n_rs", nb), "qkg"], writes=[(kname, p, tc), psk(bank)])
                pend2.append(back2)
                while len(pend2) > 1:
                    pend2.pop(0)()
    while pend2:
        pend2.pop(0)()
    wb = load_wgroup(6)
    for tc in range(4):
        tsl = slice(tc * 512, (tc + 1) * 512)
        col = 0
        for ti, (h0, h1) in enumerate(IQ_TILES):
            m = (h1 - h0) * 32
            bank = next_bank(0, 8)
            fm_matmul(wb, col, m, tc, bank)
            col += m
            sch.add("act", lambda e, ti=ti, m=m, tsl=tsl, bank=bank: e.activation(out=iqT[0:m, ti, tsl], in_=ps[bank][0:m, :], func=AF.Copy),
                    reads=[psk(bank)], writes=[("iqT", ti, tc)])
        bank = next_bank(0, 8)
        fm_matmul(wb, col, 96, tc, bank)
        sch.add("dve", lambda e, tsl=tsl, bank=bank: e.tensor_copy(out=ikT[0:96, tsl], in_=ps[bank][0:96, :]),
                reads=[psk(bank)], writes=[("ikT", tc)])
    wb = load_wgroup(7)
    for T in range(NT):
        bank = next_bank(0, 8)
        tm_matmul(wb, 0, 512, T, bank)
        dstv = vaT[:, T, :].rearrange("p (h d) -> p h d", h=8)[:, :, 0:64]
        srcv = ps[bank][:].rearrange("p (h d) -> p h d", h=8)
        if T % 2 == 0:
            sch.add("act", lambda e, dstv=dstv, srcv=srcv: e.activation(out=dstv, in_=srcv, func=AF.Copy),
                    reads=[psk(bank)], writes=[("va", T)])
        else:
            sch.add("dve", lambda e, dstv=dstv, srcv=srcv: e.tensor_copy(out=dstv, in_=srcv),
                    reads=[psk(bank)], writes=[("va", T)])
    dump("qaT", qaT[:, 0, :], dbg.get("qaT"), [("qaT", 0, tc) for tc in range(4)])
    dump("kaT", kaT[:, 0, :], dbg.get("kaT"), [("kaT", 0, tc) for tc in range(4)])
    if STOP_AFTER == "p2":
        return finish()
    sch.barrier()

    selT = view(R4, 66 * KB, [P, SELT_TOTAL], BF16)
    selts = [view(R4, 100 * KB + i * 4 * KB, [P, S], BF16) for i in range(2)]
    PTb = [view(R4, 100 * KB + i * KB, [P, 512], BF16) for i in range(8)]
    junk_tk = view(R4, 108 * KB, [P, 2048], BF16)
    score_all = R1[:, :].bitcast(F32)
    thr = sb("thr", [P, NT], F32)
    thr2 = sb("thr2", [P, NT], F32)
    cnt = sb("cnt", [P, NT], F32)
    sgn = sb("sgn", [P, NT], F32)
    amax = sb("amax", [P, NT], F32)
    mrow = sb("mrow", [P, 1], F32)
    mtab = sb("mtab", [P, KITER + 1], F32)
    biasT_s = view(R4, 100 * KB, [P, 8, 2, 128], F32)
    sch.add("sp", lambda e: e.dma_start(out=biasT_s, in_=biasT_d.rearrange("p (h d t) -> p h d t", h=8, d=2)),
            writes=["biasT"], dma=True)
    sch.add("act", lambda e: e.activation(out=Etile[:], in_=biasT_s, func=AF.Exp), reads=["biasT"], writes=["Etile"])
    for h in range(8):
        sch.add("dve", lambda e, h=h: e.tensor_tensor(out=Etile[:, h, 0, :], in0=Etile[:, h, 0, :], in1=tri_bf[:], op=ALU.mult),
                reads=["Etile", "tri_bf"], writes=["Etile"])
    sch.add("dve", lambda e: e.memset(selts[0][:], 0.0), reads=["Etile"], writes=[("selts", 0)])
    sch.add("dve", lambda e: e.tensor_copy(out=selT[:, _selT_off(0):_selT_off(0) + 128], in_=tri_bf[:]), reads=["tri_bf"], writes=[("selT", 0, 0)])
    sch.add("dve", lambda e: e.memset(selT[:, _selT_off(0) + 128:_selT_off(0) + 256], 1.0), writes=[("selT", 0, 1)])
    sch.add("dve", lambda e: e.tensor_copy(out=selT[:, _selT_off(1):_selT_off(1) + 128], in_=tri_bf[:]), reads=["tri_bf"], writes=[("selT", 1, 1)])

    print("ops before topk loops", len(sch.ops))
    batches = [list(range(2, 8)), list(range(8, 12)), list(range(12, 16))]
    ACT_SHARE = [4, 2, 2]
    sumA = sb("sumA", [P, NT], F32)
    nhalf = sb("nhalf", [P, NT], F32)
    junk_act = view(R3, 0, [P, 2048], BF16)
    for j in range(NT):
        sch.add("dve", lambda e, j=j: e.memset(nhalf[:, j:j + 1], 64.0 * (j + 1)), writes=["nhalf"])
    hd_loc = []
    for ti, (h0, h1) in enumerate(IQ_TILES):
        for k in range(h1 - h0):
            hd_loc.append((ti, k))
    lbank = [0]
    for bi, batch in enumerate(batches):
        soff = {}
        o = 0
        for j in batch:
            soff[j] = o
            o += (j + 1) * 128
        nb = len(batch)
        j0 = batch[0]
        for j in batch:
            n = (j + 1) * 128
            sc_j = score_all[:, soff[j]:soff[j] + n]
            nsc = (n + 511) // 512
            for sc in range(nsc):
                w = min(512, n - sc * 512)
                ssl = slice(sc * 512, sc * 512 + w)
                for h in range(8):
                    ti, k = hd_loc[h]
                    rs = slice(k * 32, (k + 1) * 32)
                    lb = lbank[0] % 4
                    rb = 4 + lbank[0] % 4
                    lbank[0] += 1
                    sch.add("pe", lambda e, lb=lb, rs=rs, ti=ti, j=j, ssl=ssl, w=w: e.matmul(
                        ps[lb][:, 0:w], lhsT=iqT[rs, ti, j * 128:(j + 1) * 128], rhs=ikT[rs, ssl], start=True, stop=True),
                        reads=[("iqT", ti, j // 4)] + [("ikT", c) for c in range(sc * 4 // 4, (sc * 512 + w - 1) // 512 + 1)],
                        writes=[psk(lb)])
                    sch.add("act", lambda e, lb=lb, rb=rb, w=w: e.activation(out=ps[rb][:, 0:w], in_=ps[lb][:, 0:w], func=AF.Relu),
                            reads=[psk(lb)], writes=[psk(rb), psk(lb)])
                    dst = sc_j[:, ssl]
                    if h == 0:
                        sch.add("dve", lambda e, rb=rb, w=w, dst=dst, j=j, h=h: e.tensor_scalar(
                            out=dst, in0=ps[rb][:, 0:w], scalar1=iw_s[:, j, h:h + 1], scalar2=None, op0=ALU.mult),
                            reads=[psk(rb), ("iw", j)], writes=[("score", j, sc), psk(rb)])
                    else:
                        sch.add("dve", lambda e, rb=rb, w=w, dst=dst, j=j, h=h: e.scalar_tensor_tensor(
                            out=dst, in0=ps[rb][:, 0:w], scalar=iw_s[:, j, h:h + 1], in1=dst, op0=ALU.mult, op1=ALU.add),
                            reads=[psk(rb), ("iw", j), ("score", j, sc)], writes=[("score", j, sc), psk(rb)])
            skeys = [("score", j, sc) for sc in range(nsc)]
            sch.add("dve", lambda e, sc_j=sc_j, j=j: e.tensor_reduce(out=amax[:, j:j + 1], in_=sc_j, axis=AX.X, op=ALU.max,
                                                                    apply_absolute_value=True),
                    reads=skeys, writes=[("amax", j)])
            dsl = slice(soff[j] + j * 128, soff[j] + (j + 1) * 128)
            sch.add("dve", lambda e, dsl=dsl: e.tensor_tensor(out=score_all[:, dsl], in0=score_all[:, dsl], in1=negmask[:], op=ALU.add),
                    reads=skeys + ["negmask", ("amax", j)], writes=skeys)
        if "score" in dbg and bi == 1:
            sch.add("sp", lambda e, soff=soff: e.dma_start(out=dbg["score"][:, 0:1280], in_=score_all[:, soff[9]:soff[9] + 1280]),
                    reads=[("score", 9, sc) for sc in range(3)], writes=["dbg_score"], dma=True)
        bsl = slice(j0, j0 + nb)
        sch.add("dve", lambda e, bsl=bsl: e.tensor_reduce(out=mrow[:], in_=amax[:, bsl], axis=AX.X, op=ALU.max),
                reads=[("amax", j) for j in batch], writes=["mrow"])
        sch.add("dve", lambda e: e.tensor_scalar(out=mtab[:], in0=pow2[:], scalar1=mrow[:, 0:1], scalar2=None, op0=ALU.mult),
                reads=["mrow", "pow2"], writes=["mtab"])
        sch.add("dve", lambda e, bsl=bsl: e.memset(thr[:, bsl], 0.0), writes=["thr"])
        nact = ACT_SHARE[bi]
        act_tiles = batch[:nact]
        asl = slice(batch[0], batch[0] + nact)
        for k in range(KITER):
            for j in batch:
                n = (j + 1) * 128
                sc_j = score_all[:, soff[j]:soff[j] + n]
                skeys_j = [("score", j, sc) for sc in range((n + 511) // 512)]
                if j in act_tiles:
                    sch.add("act", lambda e, sc_j=sc_j, n=n, j=j: e.activation(
                        out=junk_act[:, 0:n], in_=sc_j, func=AF.Sign, scale=-1.0, bias=thr[:, j:j + 1], accum_out=sumA[:, j:j + 1]),
                        reads=skeys_j + ["thr"], writes=["junk_act", ("sumA", j)])
                else:
                    sch.add("dve", lambda e, sc_j=sc_j, n=n, j=j: e.tensor_scalar(
                        out=junk_tk[:, 0:n], in0=sc_j, scalar1=thr[:, j:j + 1], scalar2=None, op0=ALU.is_ge, op1=ALU.add,
                        accum_out=cnt[:, j:j + 1]),
                        reads=skeys_j + ["thr"], writes=["junk", ("cnt", j)])
            if nact > 0:
                sch.add("dve", lambda e, asl=asl: e.scalar_tensor_tensor(out=cnt[:, asl], in0=sumA[:, asl], scalar=-0.5, in1=nhalf[:, asl],
                                                                        op0=ALU.mult, op1=ALU.add),
                        reads=[("sumA", j) for j in act_tiles] + ["nhalf"], writes=[("cnt", j) for j in act_tiles])
            sch.add("dve", lambda e, bsl=bsl: e.tensor_scalar(out=sgn[:, bsl], in0=cnt[:, bsl], scalar1=256.0, scalar2=0.5,
                                                             op0=ALU.is_ge, op1=ALU.subtract),
                    reads=[("cnt", j) for j in batch], writes=["sgn"])
            sch.add("dve", lambda e, bsl=bsl, k=k: e.scalar_tensor_tensor(out=thr2[:, bsl], in0=sgn[:, bsl], scalar=mtab[:, k:k + 1],
                                                                       in1=thr[:, bsl], op0=ALU.mult, op1=ALU.add),
                    reads=["sgn", "mtab", "thr"], writes=["thr2"])
            sch.add("dve", lambda e, bsl=bsl: e.tensor_copy(out=thr[:, bsl], in_=thr2[:, bsl]), reads=["thr2"], writes=["thr"])
        sch.add("dve", lambda e, bsl=bsl: e.tensor_scalar(out=thr2[:, bsl], in0=thr[:, bsl], scalar1=mtab[:, KITER:KITER + 1], scalar2=None,
                                                         op0=ALU.subtract),
                reads=["thr", "mtab"], writes=["thr2"])
        if "thr" in dbg and bi == 1:
            sch.add("sp", lambda e: e.dma_start(out=dbg["thr"][:, :], in_=thr2[:]), reads=["thr2"], writes=["dbg_thr"], dma=True)
        for j in batch:
            n = (j + 1) * 128
            sc_j = score_all[:, soff[j]:soff[j] + n]
            sb_ = j % 2
            sch.add("dve", lambda e, sc_j=sc_j, n=n, j=j, sb_=sb_: e.tensor_scalar(
                out=selts[sb_][:, 0:n], in0=sc_j, scalar1=thr2[:, j:j + 1], scalar2=None, op0=ALU.is_ge),
                reads=[("score", j, sc) for sc in range((n + 511) // 512)] + ["thr2"], writes=[("selts", sb_)])
            for i0 in range(0, j + 1, 8):
                i1 = min(j + 1, i0 + 8)
                tb = next_bank(0, 8)

                def trs(e, i0=i0, i1=i1, tb=tb, sb_=sb_):
                    o = ps[tb][:].bitcast(BF16).rearrange("p (c t) -> p c t", c=8)
                    ins = None
                    for i in range(i0, i1):
                        ins = e.transpose(o[:, i - i0, :], selts[sb_][:, i * 128:(i + 1) * 128], ident_bf[:])
                    return ins
                sch.add("pe", trs, reads=[("selts", sb_), "ident_bf"], writes=[psk(tb)])
                for i in range(i0, i1):
                    o = ps[tb][:].bitcast(BF16).rearrange("p (c t) -> p c t", c=8)[:, i - i0, :]
                    off = _selT_off(i) + (j - i) * 128
                    if i % 2 == 0:
                        sch.add("act", lambda e, o=o, off=off: e.activation(out=selT[:, off:off + 128], in_=o, func=AF.Copy),
                                reads=[psk(tb)], writes=[("selT", i, j), psk(tb)])
                    else:
                        sch.add("dve", lambda e, o=o, off=off: e.tensor_copy(out=selT[:, off:off + 128], in_=o),
                                reads=[psk(tb)], writes=[("selT", i, j), psk(tb)])
    dump("selT", selT, dbg.get("selT"), [("selT", i, j) for i in range(16) for j in range(i, 16)])
    if STOP_AFTER == "topk":
        return finish()
    sch.barrier()

    mixa = view(R1, 0, [P, NT, 512], BF16)
    rden = sb("rden", [P, 4], F32)
    abank = [0]
    LOOKAHEAD = 4
    pend = []

    def flush(keep):
        while len(pend) > keep:
            pend.pop(0)()

    for h in range(8):
        p, r = h // 2, h % 2
        rs = slice(r * 64, (r + 1) * 64)
        for J in range(4):
            accb = 6 + (abank[0] % 2)
            abank[0] += 1
            accv = ps[accb][:, 0:260].rearrange("p (j d) -> p j d", j=4)
            first = [True]
            for i in range(4 * J + 4):
                jlo = max(i, 4 * J)
                t0 = jlo * 128
                n = (4 * J + 4 - jlo) * 128
                sbk = next_bank(0, 6)
                pb = next_bank(100, 108) - 100
                sch.add("pe", lambda e, sbk=sbk, rs=rs, p=p, i=i, t0=t0, n=n: e.matmul(
                    ps[sbk][:, 0:n], lhsT=kaT[rs, p, i * 128:(i + 1) * 128], rhs=qaT[rs, p, t0:t0 + n], start=True, stop=True),
                    reads=[("kaT", p, i // 4), ("qaT", p, J)], writes=[psk(sbk)])
                nnear = max(0, min(4 * J + 4, i + 2) - jlo) * 128
                if nnear > 0:
                    sch.add("act", lambda e, sbk=sbk, pb=pb, nnear=nnear: e.activation(out=PTb[pb][:, 0:nnear], in_=ps[sbk][:, 0:nnear],
                                                                                       func=AF.Exp, scale=0.125),
                            reads=[psk(sbk)], writes=[("PT", pb), psk(sbk)])
                if n > nnear:
                    sch.add("act", lambda e, sbk=sbk, pb=pb, nnear=nnear, n=n, h=h: e.activation(
                        out=PTb[pb][:, nnear:n], in_=ps[sbk][:, nnear:n], func=AF.Exp, scale=0.125, bias=rb31[:, h:h + 1]),
                        reads=[psk(sbk), "rb31"], writes=[("PT", pb), psk(sbk)])
                soff_ = _selT_off(i) + (jlo - i) * 128
                sch.add("dve", lambda e, pb=pb, n=n, soff_=soff_: e.tensor_tensor(out=PTb[pb][:, 0:n], in0=PTb[pb][:, 0:n],
                                                                                 in1=selT[:, soff_:soff_ + n], op=ALU.mult),
                        reads=[("PT", pb)] + [("selT", i, j) for j in range(jlo, 4 * J + 4)], writes=[("PT", pb)])
                for j in range(jlo, min(4 * J + 4, i + 2)):
                    dlt = j - i
                    cs = slice((j - jlo) * 128, (j - jlo + 1) * 128)
                    sch.add("dve", lambda e, pb=pb, cs=cs, h=h, dlt=dlt: e.tensor_tensor(out=PTb[pb][:, cs], in0=PTb[pb][:, cs],
                                                                                         in1=Etile[:, h, dlt, :], op=ALU.mult),
                            reads=[("PT", pb), "Etile"], writes=[("PT", pb)])

                def back(pb=pb, jlo=jlo, J=J, i=i, h=h, accv=accv, first=first, accb=accb):
                    def pv(e):
                        ins = None
                        for j in range(jlo, 4 * J + 4):
                            cs = slice((j - jlo) * 128, (j - jlo + 1) * 128)
                            ins = e.matmul(accv[:, j - 4 * J, :], lhsT=PTb[pb][:, cs], rhs=vaT[:, i, h * 65:(h + 1) * 65],
                                           start=first[0], stop=False, skip_group_check=True)
                            first[0] = False
                        return ins
                    sch.add("pe", pv, reads=[("PT", pb), ("va", i)], writes=[psk(accb)])
                    if i == 4 * J + 3:
                        sch.add("dve", lambda e: e.reciprocal(out=rden[:], in_=accv[:, :, 64]), reads=[psk(accb)], writes=["rden", psk(accb)])
                        sch.add("dve", lambda e: e.tensor_tensor(
                            out=mixa[:, 4 * J:4 * J + 4, h * 64:(h + 1) * 64], in0=accv[:, :, 0:64],
                            in1=rden[:, :].unsqueeze(2).to_broadcast([P, 4, 64]), op=ALU.mult),
                            reads=[psk(accb), "rden"], writes=[("mixa", 4 * J + jj, h) for jj in range(4)] + [psk(accb)])
                pend.append(back)
                flush(LOOKAHEAD)
    flush(0)
    dump("mixa", mixa, dbg.get("mixa").rearrange("(t p) c -> p t c", p=P) if "mixa" in dbg else None,
         [("mixa", T, h) for T in range(NT) for h in range(8)])
    for T in range(NT):
        tsl = slice(T * P, (T + 1) * P)
        tb = next_bank(0, 6)

        def tra(e, T=T, tb=tb):
            o = ps[tb][:].bitcast(BF16).rearrange("p (c t) -> p c t", c=8)
            ins = None
            for c in range(4):
                ins = e.transpose(o[:, c, :], mixa[:, T, c * P:(c + 1) * P], ident_bf[:])
            return ins
        sch.add("pe", tra, reads=[("mixa", T, h) for h in range(8)] + ["ident_bf"], writes=[psk(tb)])
        sch.add("act", lambda e, tsl=tsl, tb=tb: e.activation(
            out=mixT_a[:, :, tsl], in_=ps[tb][:].bitcast(BF16).rearrange("p (c t) -> p c t", c=8)[:, 0:4, :], func=AF.Copy),
            reads=[psk(tb)], writes=[("mixT_a", T)])
    if STOP_AFTER == "attn":
        return finish()
    sch.barrier()

    x1 = view(R4, 0, [P, NT, D], F32)
    woutb = view(R4, 64 * KB, [P, 8, D], BF16)
    xst5 = [view(R4, 80 * KB + i * 4 * KB, [P, D], F32) for i in range(2)]
    xs5 = [view(R4, 88 * KB + i * 2 * KB, [P, D], BF16) for i in range(2)]
    junk5 = view(R4, 92 * KB, [P, 2048], BF16)
    g2bc = view(R4, 96 * KB, [P, D], F32)
    wrb = view(R4, 100 * KB, [P, 8, 36], BF16)
    h2T = view(R1, 0, [P, 8, S], BF16)
    ssq2 = sb("ssq2", [P, NT], F32)
    rstd2 = sb("rstd2", [P, NT], F32)
    logit = sb("logit", [P, NT, 36], F32)
    for hh in range(2):
        sch.add("pool", lambda e, hh=hh: e.dma_start(out=woutb[:, :, hh * 512:(hh + 1) * 512],
                                                     in_=wout_d[:, hh * 512:(hh + 1) * 512].rearrange("(c p) n -> p c n", p=P)),
                writes=[("wout", hh)], dma=True)
    sch.add("pool", lambda e: e.dma_start(out=wrb, in_=wr_d.rearrange("(c p) n -> p c n", p=P)), writes=["wrb"], dma=True)
    sch.add("sp", lambda e: e.dma_start(out=g2bc, in_=g2bc_d[:, :]), writes=["g2bc"], dma=True)
    pend5 = []
    for T in range(NT):
        b = T % 2
        tsl = slice(T * P, (T + 1) * P)
        sch.add("sp", lambda e, b=b, tsl=tsl: e.dma_start(out=xst5[b], in_=x_d[tsl, :]), writes=[("xst5", b)], dma=True)
        for hh in range(2):
            bank = next_bank(0, 4)

            def om(e, T=T, hh=hh, bank=bank):
                ins = None
                for c in range(8):
                    src = mixT_a if c < 4 else mixT_b
                    ins = e.matmul(ps[bank][:], lhsT=src[:, c % 4, T * P:(T + 1) * P], rhs=woutb[:, c, hh * 512:(hh + 1) * 512],
                                   start=(c == 0), stop=(c == 7))
                return ins
            sch.add("pe", om, reads=[("mixT_a", T), ("mixT_b", T), ("wout", hh)], writes=[psk(bank)])
            sch.add("dve", lambda e, T=T, hh=hh, bank=bank, b=b: e.tensor_tensor(
                out=x1[:, T, hh * 512:(hh + 1) * 512], in0=ps[bank][:], in1=xst5[b][:, hh * 512:(hh + 1) * 512], op=ALU.add),
                reads=[psk(bank), ("xst5", b)], writes=[("x1", T, hh)])
        sch.add("act", lambda e, T=T: e.activation(out=junk5[:, 0:D], in_=x1[:, T, :], func=AF.Square, accum_out=ssq2[:, T:T + 1]),
                reads=[("x1", T, 0), ("x1", T, 1)], writes=["junk5", ("ssq2", T)])
        sch.add("act", lambda e, T=T: e.activation(out=rstd2[:, T:T + 1], in_=ssq2[:, T:T + 1], func=AF.Sqrt, scale=1.0 / D,
                                                   bias=eps_col[:, 0:1]),
                reads=[("ssq2", T), "eps_col"], writes=[("rstd2", T)])
        sch.add("dve", lambda e, T=T: e.reciprocal(out=rstd2[:, T:T + 1], in_=rstd2[:, T:T + 1]), reads=[("rstd2", T)], writes=[("rstd2", T)])
        sch.add("dve", lambda e, T=T, b=b: e.scalar_tensor_tensor(out=xs5[b], in0=x1[:, T, :], scalar=rstd2[:, T:T + 1], in1=g2bc,
                                                                  op0=ALU.mult, op1=ALU.mult),
                reads=[("x1", T, 0), ("x1", T, 1), ("rstd2", T), "g2bc"], writes=[("xs5", b)])
        def back5(T=T, b=b, tsl=tsl):
            tb = next_bank(4, 6)

            def tr5(e, b=b, tb=tb):
                o = ps[tb][:].bitcast(BF16).rearrange("p (c t) -> p c t", c=8)
                ins = None
                for c in range(8):
                    ins = e.transpose(o[:, c, :], xs5[b][:, c * P:(c + 1) * P], ident_bf[:])
                return ins
            sch.add("pe", tr5, reads=[("xs5", b), "ident_bf"], writes=[psk(tb)])
            sch.add("act", lambda e, tb=tb, tsl=tsl: e.activation(
                out=h2T[:, :, tsl], in_=ps[tb][:].bitcast(BF16).rearrange("p (c t) -> p c t", c=8), func=AF.Copy),
                reads=[psk(tb)], writes=[("h2T", T)])
            lbk = next_bank(6, 8)

            def rmm(e, T=T, lbk=lbk):
                ins = None
                for c in range(8):
                    ins = e.matmul(ps[lbk][:, 0:36], lhsT=h2T[:, c, T * P:(T + 1) * P], rhs=wrb[:, c, :], start=(c == 0), stop=(c == 7))
                return ins
            sch.add("pe", rmm, reads=[("h2T", T), "wrb"], writes=[psk(lbk)])
            sch.add("dve", lambda e, T=T, lbk=lbk: e.tensor_tensor(out=logit[:, T, :], in0=ps[lbk][:, 0:36], in1=brbc[:], op=ALU.add),
                    reads=[psk(lbk), "brbc"], writes=["logit"])
        pend5.append(back5)
        while len(pend5) > 1:
            pend5.pop(0)()
    while pend5:
        pend5.pop(0)()
    dump("x1", x1, dbg.get("x1").rearrange("(t p) c -> p t c", p=P) if "x1" in dbg else None,
         [("x1", T, hh) for T in range(NT) for hh in range(2)])
    if STOP_AFTER == "x1":
        return finish()

    gl = logit[:, :, 0:4]
    el = logit[:, :, 4:36].rearrange("p t (g e) -> p t g e", g=4)
    _ro = [101 * KB]

    def rv(shape):
        nb = int(np.prod(shape[1:])) * 4
        v = view(R4, _ro[0], shape, F32)
        _ro[0] += nb
        return v
    gmax = rv([P, NT])
    goh = rv([P, NT, 4])
    gsh = rv([P, NT, 4])
    gsum = rv([P, NT])
    gw = rv([P, NT])
    etmp = rv([P, NT, 4, 8])
    esel = rv([P, NT, 8])
    esel2 = rv([P, NT, 8])
    m1 = rv([P, NT])
    m2 = rv([P, NT])
    oh1 = rv([P, NT, 8])
    oh2 = rv([P, NT, 8])
    dd = rv([P, NT])
    w1 = rv([P, NT])
    w2 = rv([P, NT])
    gsel = rv([P, NT, 8])
    assert _ro[0] <= 110 * KB
    gates = view(R4, 110 * KB, [P, NT, 4, 8], F32)

    def D_(fn, reads, writes):
        sch.add("dve", fn, reads=reads, writes=writes)

    def bc3(ap2, n):
        return ap2.unsqueeze(2).to_broadcast([P, NT, n])
    D_(lambda e: e.tensor_reduce(out=gmax[:], in_=gl, axis=AX.X, op=ALU.max), ["logit"], ["gmax"])
    D_(lambda e: e.tensor_tensor(out=goh[:], in0=gl, in1=bc3(gmax[:, :], 4), op=ALU.is_equal), ["logit", "gmax"], ["goh"])
    D_(lambda e: e.tensor_tensor(out=gsh[:], in0=gl, in1=bc3(gmax[:, :], 4), op=ALU.subtract), ["logit", "gmax"], ["gsh"])
    sch.add("act", lambda e: e.activation(out=gsh[:], in_=gsh[:], func=AF.Exp), reads=["gsh"], writes=["gsh"])
    D_(lambda e: e.tensor_reduce(out=gsum[:], in_=gsh[:], axis=AX.X, op=ALU.add), ["gsh"], ["gsum"])
    D_(lambda e: e.reciprocal(out=gw[:], in_=gsum[:]), ["gsum"], ["gw"])
    D_(lambda e: e.tensor_tensor(out=etmp[:], in0=el, in1=goh[:, :, :].unsqueeze(3).to_broadcast([P, NT, 4, 8]), op=ALU.mult),
       ["logit", "goh"], ["etmp"])
    D_(lambda e: e.tensor_reduce(out=esel[:], in_=etmp[:, :, :, :].rearrange("p t g e -> p t e g"), axis=AX.X, op=ALU.add), ["etmp"], ["esel"])
    D_(lambda e: e.tensor_reduce(out=m1[:], in_=esel[:], axis=AX.X, op=ALU.max), ["esel"], ["m1"])
    D_(lambda e: e.tensor_tensor(out=oh1[:], in0=esel[:], in1=bc3(m1[:, :], 8), op=ALU.is_equal), ["esel", "m1"], ["oh1"])
    D_(lambda e: e.scalar_tensor_tensor(out=esel2[:], in0=oh1[:], scalar=-1e30, in1=esel[:], op0=ALU.mult, op1=ALU.add), ["oh1", "esel"], ["esel2"])
    D_(lambda e: e.tensor_reduce(out=m2[:], in_=esel2[:], axis=AX.X, op=ALU.max), ["esel2"], ["m2"])
    D_(lambda e: e.tensor_tensor(out=oh2[:], in0=esel2[:], in1=bc3(m2[:, :], 8), op=ALU.is_equal), ["esel2", "m2"], ["oh2"])
    D_(lambda e: e.tensor_tensor(out=dd[:], in0=m2[:], in1=m1[:], op=ALU.subtract), ["m1", "m2"], ["dd"])
    sch.add("act", lambda e: e.activation(out=dd[:], in_=dd[:], func=AF.Exp), reads=["dd"], writes=["dd"])
    D_(lambda e: e.tensor_scalar(out=w1[:], in0=dd[:], scalar1=1.0, scalar2=None, op0=ALU.add), ["dd"], ["w1"])
    D_(lambda e: e.reciprocal(out=w1[:], in_=w1[:]), ["w1"], ["w1"])
    D_(lambda e: e.tensor_tensor(out=w2[:], in0=dd[:], in1=w1[:], op=ALU.mult), ["dd", "w1"], ["w2"])
    D_(lambda e: e.tensor_tensor(out=w1[:], in0=w1[:], in1=gw[:], op=ALU.mult), ["w1", "gw"], ["w1"])
    D_(lambda e: e.tensor_tensor(out=w2[:], in0=w2[:], in1=gw[:], op=ALU.mult), ["w2", "gw"], ["w2"])
    D_(lambda e: e.tensor_tensor(out=oh1[:], in0=oh1[:], in1=bc3(w1[:, :], 8), op=ALU.mult), ["oh1", "w1"], ["oh1"])
    D_(lambda e: e.tensor_tensor(out=oh2[:], in0=oh2[:], in1=bc3(w2[:, :], 8), op=ALU.mult), ["oh2", "w2"], ["oh2"])
    D_(lambda e: e.tensor_tensor(out=gsel[:], in0=oh1[:], in1=oh2[:], op=ALU.add), ["oh1", "oh2"], ["gsel"])
    D_(lambda e: e.tensor_tensor(out=gates[:], in0=goh[:, :, :].unsqueeze(3).to_broadcast([P, NT, 4, 8]),
                                 in1=gsel[:, :, :].unsqueeze(2).to_broadcast([P, NT, 4, 8]), op=ALU.mult), ["goh", "gsel"], ["gates"])
    dump("gates", gates[:, :, :, :].rearrange("p t g e -> p t (g e)"),
         dbg.get("gates").rearrange("(t p) c -> p t c", p=P) if "gates" in dbg else None, ["gates"])
    if STOP_AFTER == "router":
        return finish()
    sch.barrier()

    hid = [view(R4, 64 * KB + i * 8 * KB, [P, 2, S], BF16) for i in range(2)]
    gT = view(R4, 80 * KB, [32, 2, S], BF16, parts=32)
    onehot = view(R4, 88 * KB, [32, 32, 128], BF16, parts=32)
    sa = [view(R4, 96 * KB + i * 2 * KB, [P, 512], F32) for i in range(2)]
    t1 = [view(R4, 100 * KB + i * 2 * KB, [P, 512], F32) for i in range(2)]
    wbuf = []
    for RR in (R2, R3):
        wbuf.append((view(RR, 0, [P, 8, 256], BF16), view(RR, 4 * KB, [P, 8, 256], BF16), view(RR, 8 * KB, [P, 2, D], BF16)))
    sch.add("sp", lambda e: e.dma_start(out=onehot, in_=onehot_d.rearrange("e (k m) -> e k m", k=32)), writes=["onehot"], dma=True)
    for T4 in range(4):
        tb = next_bank(0, 4)

        def trg(e, T4=T4, tb=tb):
            ins = None
            for k in range(4):
                T = T4 * 4 + k
                ins = e.transpose(ps[tb][0:32, k * 128:(k + 1) * 128], gates[:, T, :, :].rearrange("p g e -> p (g e)"), ident_f[:])
            return ins
        sch.add("pe", trg, reads=["gates", "ident_f"], writes=[psk(tb)])
        sl = slice(T4 * 512, (T4 + 1) * 512)
        sch.add("act", lambda e, tb=tb, sl=sl: e.activation(out=gT[0:32, 0, sl], in_=ps[tb][0:32, :], func=AF.Copy),
                reads=[psk(tb)], writes=[("gThi", T4), psk(tb)])
        sch.add("dve", lambda e, tb=tb, sl=sl: e.tensor_tensor(out=gT[0:32, 1, sl], in0=ps[tb][0:32, :], in1=gT[0:32, 0, sl], op=ALU.subtract),
                reads=[psk(tb), ("gThi", T4)], writes=[("gTlo", T4), psk(tb)])
    mcnt = [0]
    for pr in range(NE // 2):
      for ex in (2 * pr, 2 * pr + 1):
        wbi = ex % 2
        Wg, Wu, Wd = wbuf[wbi]
        sch.add("pool", lambda e, Wg=Wg, ex=ex: e.dma_start(out=Wg, in_=wg_d[ex].rearrange("(c p) f -> p c f", p=P)),
                writes=[("Wg", wbi)], dma=True)
        sch.add("pool", lambda e, Wu=Wu, ex=ex: e.dma_start(out=Wu, in_=wu_d[ex].rearrange("(c p) f -> p c f", p=P)),
                writes=[("Wu", wbi)], dma=True)
        for hh in range(2):
            sch.add("pool", lambda e, Wd=Wd, ex=ex, hh=hh: e.dma_start(
                out=Wd[:, :, hh * 512:(hh + 1) * 512], in_=wd_d[ex][:, hh * 512:(hh + 1) * 512].rearrange("(c p) n -> p c n", p=P)),
                writes=[("Wd", wbi, hh)], dma=True)
        hb = ex % 2
        for tc in range(4):
            sl = slice(tc * 512, (tc + 1) * 512)
            gb = mcnt[0] % 2
            mcnt[0] += 1

            def gmm(e, ex=ex, gb=gb, sl=sl):
                e.matmul(ps[gb][:], lhsT=onehot[0:32, ex, :], rhs=gT[0:32, 0, sl], start=True, stop=False)
                return e.matmul(ps[gb][:], lhsT=onehot[0:32, ex, :], rhs=gT[0:32, 1, sl], start=False, stop=True)
            sch.add("pe", gmm, reads=["onehot", ("gThi", tc), ("gTlo", tc)], writes=[psk(gb)])
            for ft in range(2):
                ab = 2 + (mcnt[0] % 2)
                ub = 4 + (mcnt[0] % 2)
                tb_ = mcnt[0] % 2
                mcnt[0] += 1

                def amm(e, Wg=Wg, ft=ft, sl=sl, ab=ab):
                    ins = None
                    for c in range(8):
                        ins = e.matmul(ps[ab][:], lhsT=Wg[:, c, ft * 128:(ft + 1) * 128], rhs=h2T[:, c, sl], start=(c == 0), stop=(c == 7))
                    return ins

                def umm2(e, Wu=Wu, ft=ft, sl=sl, ub=ub):
                    ins = None
                    for c in range(8):
                        ins = e.matmul(ps[ub][:], lhsT=Wu[:, c, ft * 128:(ft + 1) * 128], rhs=h2T[:, c, sl], start=(c == 0), stop=(c == 7))
                    return ins
                hkeys = [("h2T", tc * 4 + k) for k in range(4)]
                sch.add("pe", amm, reads=[("Wg", wbi)] + hkeys, writes=[psk(ab)])
                sch.add("pe", umm2, reads=[("Wu", wbi)] + hkeys, writes=[psk(ub)])
                sch.add("act", lambda e, ab=ab, tb_=tb_: e.activation(out=sa[tb_], in_=ps[ab][:], func=AF.Silu),
                        reads=[psk(ab)], writes=[("sa", tb_), psk(ab)])
                sch.add("dve", lambda e, ub=ub, tb_=tb_: e.tensor_tensor(out=t1[tb_], in0=ps[ub][:], in1=sa[tb_], op=ALU.mult),
                        reads=[psk(ub), ("sa", tb_)], writes=[("t1", tb_), psk(ub)])
                sch.add("dve", lambda e, gb=gb, tb_=tb_, hb=hb, ft=ft, sl=sl: e.tensor_tensor(out=hid[hb][:, ft, sl], in0=ps[gb][:], in1=t1[tb_],
                                                                                             op=ALU.mult),
                        reads=[psk(gb), ("t1", tb_)], writes=[("hid", hb, tc), psk(gb)])
      WdA, WdB = wbuf[0][2], wbuf[1][2]
      for T in range(NT):
            for hh in range(2):
                yb = 6 + (mcnt[0] % 2)
                mcnt[0] += 1

                def dmm(e, T=T, hh=hh, yb=yb):
                    ins = None
                    k = 0
                    for (hd, Wd_) in ((hid[0], WdA), (hid[1], WdB)):
                        for ft in range(2):
                            ins = e.matmul(ps[yb][:], lhsT=hd[:, ft, T * P:(T + 1) * P], rhs=Wd_[:, ft, hh * 512:(hh + 1) * 512],
                                           start=(k == 0), stop=(k == 3))
                            k += 1
                    return ins
                sch.add("pe", dmm, reads=[("hid", 0, T // 4), ("hid", 1, T // 4), ("Wd", 0, hh), ("Wd", 1, hh)], writes=[psk(yb)])
                sch.add("dve", lambda e, T=T, hh=hh, yb=yb: e.tensor_tensor(out=x1[:, T, hh * 512:(hh + 1) * 512], in0=ps[yb][:],
                                                                           in1=x1[:, T, hh * 512:(hh + 1) * 512], op=ALU.add),
                        reads=[psk(yb), ("x1", T, hh)], writes=[("x1", T, hh), psk(yb)])
    for T in range(NT):
        tsl = slice(T * P, (T + 1) * P)
        sch.add("sp", lambda e, T=T, tsl=tsl: e.dma_start(out=out_d[tsl, :], in_=x1[:, T, :]), reads=[("x1", T, 0), ("x1", T, 1)],
                writes=["out_%d" % T], dma=True)
    return finish()


_CACHE = {}


def _prep_shared(inputs):
    f32 = np.float32
    c = host_constants()
    w_in = np.asarray(inputs["w_in"][0], f32)
    shared = {}
    shared["w1"] = np.ascontiguousarray(w_in[:, W1_COLS])
    shared["wout"] = np.ascontiguousarray(np.asarray(inputs["w_out"][0], f32))
    shared["wr"] = np.ascontiguousarray(np.concatenate([np.asarray(inputs["w_router_group"][0], f32),
                                                        np.asarray(inputs["w_router_expert"][0], f32)], axis=1))
    shared["wg"] = np.ascontiguousarray(np.asarray(inputs["w_exp_gate"][0], f32))
    shared["wu"] = np.ascontiguousarray(np.asarray(inputs["w_exp_up"][0], f32))
    shared["wd"] = np.ascontiguousarray(np.asarray(inputs["w_exp_down"][0], f32))
    shared["w2aug"] = np.ascontiguousarray(np.concatenate([np.asarray(inputs["gla_gate_w2"][0], f32),
                                                           np.asarray(inputs["gla_gate_b"], f32).reshape(1, 256)], axis=0))
    shared["g1bc"] = np.ascontiguousarray(np.tile(np.asarray(inputs["norm1_g"], f32).reshape(1, D), (P, 1)))
    shared["g2bc"] = np.ascontiguousarray(np.tile(np.asarray(inputs["norm2_g"], f32).reshape(1, D), (P, 1)))
    qg = np.tile(np.asarray(inputs["q_norm_g"], f32).reshape(64), 2)
    kg = np.tile(np.asarray(inputs["k_norm_g"], f32).reshape(64), 2)
    shared["qkg"] = np.ascontiguousarray(np.stack([qg, kg], axis=1))
    shared["goutbc"] = np.ascontiguousarray(np.tile(np.asarray(inputs["gla_out_norm_g"], f32).reshape(1, 128), (P, 4)))
    br = np.concatenate([np.asarray(inputs["b_router_group"], f32).reshape(4),
                         np.asarray(inputs["b_router_expert"], f32).reshape(32)])
    shared["brbc"] = np.ascontiguousarray(np.tile(br.reshape(1, 36), (P, 1)))
    rb = np.asarray(inputs["rel_bias"], f32)
    idx = np.arange(128)
    bT = np.zeros((P, 8, 2, 128), f32)
    for dlt in range(2):
        dist = 128 * dlt + idx[None, :] - idx[:, None]
        bk = _t5_bucket_np(np.maximum(dist, 0))
        for h in range(8):
            bT[:, h, dlt, :] = rb[bk, h]
    shared["biasT"] = np.ascontiguousarray(bT.reshape(P, -1))
    shared["rb31"] = np.ascontiguousarray(np.tile(rb[31:32, :], (P, 1)))
    for k in ["ident_bf", "ident_f", "tri_bf", "tri_f", "after_f", "negmask", "blockones", "pow2"]:
        shared[k] = c[k]
    shared["onehot"] = np.ascontiguousarray(c["onehot"].reshape(32, -1))
    return shared


def kernel(**inputs):
    x = np.asarray(inputs["x"], np.float32)
    if "nc" not in _CACHE:
        _CACHE["nc"] = build_program()
    nc = _CACHE["nc"]
    shared = _prep_shared(inputs)
    in_maps = []
    for b in range(8):
        m = dict(shared)
        m["x"] = np.ascontiguousarray(x[b])
        in_maps.append(m)
    res = run_bass_kernel_spmd(nc, in_maps, core_ids=list(range(8)))
    _CACHE["last"] = res
    out = np.stack([np.asarray(r["out"], np.float32) for r in res.results], axis=0)
    return out
```

```python
import os
import numpy as np
import ml_dtypes
from contextlib import ExitStack
import concourse.bass as bass
import concourse.mybir as mybir
from concourse.bass_utils import run_bass_kernel_spmd

F32 = mybir.dt.float32
BF16 = mybir.dt.bfloat16
U8 = mybir.dt.uint8
AF = mybir.ActivationFunctionType
ALU = mybir.AluOpType
AX = mybir.AxisListType

P = 128
S = 2048
D = 1024
NT = 16
NE = 32
EPS = 1e-6
KITER = 18
DEBUG = {}
STOP_AFTER = None


class _Op:
    __slots__ = ("eng", "fn", "reads", "writes", "dma", "deps", "waits", "signal", "idx", "flag", "slotwait", "after")

    def __init__(self, eng, fn, reads, writes, dma):
        self.eng = eng
        self.fn = fn
        self.reads = reads
        self.writes = writes
        self.dma = dma
        self.deps = ()
        self.waits = []
        self.signal = None
        self.flag = False
        self.slotwait = None
        self.after = []


def _nofn(e):
    return None


class Sched:
    EPOCH = 12000
    RING = 8

    def __init__(self, nc, stack):
        self.nc = nc
        self.stack = stack
        self.ops = []

    def add(self, eng, fn, reads=(), writes=(), dma=False):
        reads = list(reads)
        writes = list(writes)
        for k in reads:
            if isinstance(k, tuple) and k and k[0] == "ps" and k not in writes:
                writes.append(k)
        self.ops.append(_Op(eng, fn, tuple(reads), tuple(writes), dma))

    def barrier(self):
        pos = getattr(self, "_barpos", 0)
        last = {}
        dmas = []
        for i, op in enumerate(self.ops):
            if i < pos:
                continue
            if op.dma:
                dmas.append(op)
            elif op.fn is not _nofn:
                last[op.eng] = op
        for eng in ("pe", "act", "dve", "pool", "sp"):
            b = _Op(eng, _nofn, (), (), False)
            b.after = [p for k, p in last.items() if k != eng] + dmas
            self.ops.append(b)
        self._barpos = len(self.ops)

    def finalize(self):
        nc = self.nc
        last_w = {}
        readers = {}
        for i, op in enumerate(self.ops):
            op.idx = i
            raw = set()
            other = set()
            for k in op.reads:
                if k in last_w:
                    raw.add(last_w[k])
            for k in op.writes:
                if k in last_w:
                    raw.add(last_w[k])
                for r in readers.get(k, ()):
                    other.add(r)
            raw.discard(i)
            other.discard(i)
            for k in op.reads:
                readers.setdefault(k, set()).add(i)
            for k in op.writes:
                last_w[k] = i
                readers[k] = set()
            best = {}
            deps = []
            for d in raw | other:
                p = self.ops[d]
                if p.dma:
                    deps.append(d)
                    continue
                if p.eng == op.eng and not op.dma:
                    if p.eng == "pe":
                        continue
                    if d not in raw:
                        continue
                if p.eng not in best or best[p.eng] < d:
                    best[p.eng] = d
            deps.extend(best.values())
            for a in op.after:
                deps.append(a.idx)
            op.deps = deps
            for d in deps:
                self.ops[d].flag = True
        cnt = {}
        self.sems = {}
        dcount = {}
        for op in self.ops:
            if op.dma:
                q = op.eng
                k = dcount.get(q, 0)
                dcount[q] = k + 1
                slot = k % self.RING
                name = "dq_%s_%d" % (q, slot)
                if name not in self.sems:
                    self.sems[name] = self.stack.enter_context(nc.semaphore(name))
                op.signal = (name, 16 * (k // self.RING + 1), 16)
                if k >= self.RING:
                    op.slotwait = (name, 16 * (k // self.RING))
            elif op.flag:
                c = cnt.get(op.eng, 0)
                ep = c // self.EPOCH
                name = "s_%s_%d" % (op.eng, ep)
                if name not in self.sems:
                    self.sems[name] = self.stack.enter_context(nc.semaphore(name))
                op.signal = (name, c % self.EPOCH + 1, 1)
                cnt[op.eng] = c + 1
        for op in self.ops:
            w = []
            if op.slotwait is not None:
                w.append(op.slotwait)
            for d in op.deps:
                sg = self.ops[d].signal
                w.append((sg[0], sg[1]))
            op.waits = w

    def emit(self, block):
        table = [("pe", block.tensor), ("act", block.scalar), ("dve", block.vector),
                 ("pool", block.gpsimd), ("sp", block.sync)]
        for engname, deco in table:
            ops = [op for op in self.ops if op.eng == engname]

            def body(e, ops=ops):
                seen = {}
                for op in ops:
                    for (sn, val) in op.waits:
                        if seen.get(sn, 0) >= val:
                            continue
                        e.wait_ge(self.sems[sn], val)
                        seen[sn] = val
                    ins = op.fn(e)
                    if op.signal is not None and ins is not None:
                        ins.then_inc(self.sems[op.signal[0]], op.signal[2])

            deco(body)


IQ_TILES = [(0, 3), (3, 6), (6, 8)]


def _t5_bucket_np(dist):
    max_exact = 16
    d_f = np.maximum(dist, 1).astype(np.float32)
    large = max_exact + (np.log(d_f / max_exact) / np.log(128 / max_exact) * (32 - max_exact)).astype(np.int32)
    large = np.minimum(large, 31)
    return np.where(dist < max_exact, dist, large)


def _w1_columns():
    A = 512
    off = {}
    o = 0
    for name, n in [("qa", 512), ("ka", 512), ("va", 512), ("iq", 256), ("ik", 32), ("iw", 8),
                    ("qb", 256), ("kb", 256), ("vb", 512), ("glr", 16), ("rg", 512)]:
        off[name] = o
        o += n
    cols = []
    groups = []

    def grp(name, kind, cl):
        groups.append((name, kind, len(cols), len(cl)))
        cols.extend(cl)

    r = lambda name, a, b: list(range(off[name] + a, off[name] + b))
    grp("qbkb", "fm", r("qb", 0, 256) + r("kb", 0, 256) + r("glr", 0, 16))
    grp("vb", "tm", r("vb", 0, 512))
    grp("rg", "tm", r("rg", 0, 512))
    grp("kbiw", "tm", r("kb", 0, 256) + r("iw", 0, 8))
    grp("qa", "fm", r("qa", 0, 512))
    grp("ka", "fm", r("ka", 0, 512))
    iqc = []
    for (h0, h1) in IQ_TILES:
        iqc += r("iq", h0 * 32, h1 * 32)
    grp("iqik", "fm", iqc + r("ik", 0, 32) * 3)
    grp("va", "tm", r("va", 0, 512))
    return np.array(cols, np.int64), groups


W1_COLS, W1_GROUPS = _w1_columns()
NW1 = len(W1_COLS)


def _selT_off(i):
    return sum((16 - ii) * 128 for ii in range(i))


SELT_TOTAL = _selT_off(16)


def host_constants():
    c = {}
    idx = np.arange(128)
    tri = (idx[:, None] <= idx[None, :])
    c["ident_bf"] = np.eye(128, dtype=np.float32).astype(ml_dtypes.bfloat16)
    c["ident_f"] = np.eye(128, dtype=np.float32)
    c["tri_bf"] = tri.astype(np.float32).astype(ml_dtypes.bfloat16)
    c["tri_f"] = tri.astype(np.float32)
    c["after_f"] = (idx[:, None] > idx[None, :]).astype(np.float32)
    c["negmask"] = np.where(idx[None, :] <= idx[:, None], 0.0, -1e30).astype(np.float32)
    bo = np.zeros((128, 128), np.float32)
    bo[:64, :64] = 1.0
    bo[64:, 64:] = 1.0
    c["blockones"] = bo.astype(ml_dtypes.bfloat16)
    c["pow2"] = np.tile((2.0 ** -np.arange(KITER + 1, dtype=np.float64)).astype(np.float32)[None, :], (128, 1))
    oh = np.zeros((32, 32, 128), np.float32)
    for e in range(32):
        oh[e, e, :] = 1.0
    c["onehot"] = oh.astype(ml_dtypes.bfloat16)
    return c


def build_program():
    nc = bass.Bass("TRN2", target_bir_lowering=False)
    stack = ExitStack()
    sch = Sched(nc, stack)

    def dram(name, shape, dt, kind="ExternalInput"):
        return nc.dram_tensor(name, list(shape), dt, kind=kind).ap()

    x_d = dram("x", [S, D], F32)
    w1_d = dram("w1", [D, NW1], F32)
    wout_d = dram("wout", [D, D], F32)
    wr_d = dram("wr", [D, 36], F32)
    wg_d = dram("wg", [NE, D, 256], F32)
    wu_d = dram("wu", [NE, D, 256], F32)
    wd_d = dram("wd", [NE, 256, D], F32)
    w2_d = dram("w2aug", [17, 256], F32)
    g1bc_d = dram("g1bc", [P, D], F32)
    g2bc_d = dram("g2bc", [P, D], F32)
    qkg_d = dram("qkg", [P, 2], F32)
    gout_d = dram("goutbc", [P, 512], F32)
    brbc_d = dram("brbc", [P, 36], F32)
    biasT_d = dram("biasT", [P, 8 * 2 * 128], F32)
    rb31_d = dram("rb31", [P, 8], F32)
    ident_bf_d = dram("ident_bf", [P, P], BF16)
    ident_f_d = dram("ident_f", [P, P], F32)
    tri_bf_d = dram("tri_bf", [P, P], BF16)
    tri_f_d = dram("tri_f", [P, P], F32)
    after_f_d = dram("after_f", [P, P], F32)
    negmask_d = dram("negmask", [P, P], F32)
    blockones_d = dram("blockones", [P, P], BF16)
    pow2_d = dram("pow2", [P, KITER + 1], F32)
    onehot_d = dram("onehot", [32, 32 * 128], BF16)
    out_d = dram("out", [S, D], F32, kind="ExternalOutput")
    dbg = {}
    for name, (shape, dt) in DEBUG.items():
        dbg[name] = dram("dbg_" + name, shape, dt, kind="ExternalOutput")

    def sb(name, shape, dt):
        return stack.enter_context(nc.sbuf_tensor(name, list(shape), dt))

    ident_bf = sb("ident_bf_s", [P, P], BF16)
    ident_f = sb("ident_f_s", [P, P], F32)
    tri_bf = sb("tri_bf_s", [P, P], BF16)
    tri_f = sb("tri_f_s", [P, P], F32)
    after_f = sb("after_f_s", [P, P], F32)
    negmask = sb("negmask_s", [P, P], F32)
    blockones = sb("blockones_s", [P, P], BF16)
    pow2 = sb("pow2_s", [P, KITER + 1], F32)
    g1bc = sb("g1bc_s", [P, D], F32)
    qkg = sb("qkg_s", [P, 2], F32)
    gout = sb("gout_s", [P, 512], F32)
    brbc = sb("brbc_s", [P, 36], F32)
    rb31 = sb("rb31_s", [P, 8], F32)
    Etile = sb("Etile", [P, 8, 2, 128], BF16)
    w2aug = sb("w2aug_s", [17, 256], BF16)
    iw_s = sb("iw_s", [P, NT, 8], F32)
    ssq1 = sb("ssq1", [P, NT], F32)
    rstd1 = sb("rstd1", [P, NT], F32)
    ones_col = sb("ones_col", [P, 1], F32)

    R1 = sb("R1", [P, 32 * 1024], U8)
    R2 = sb("R2", [P, 16 * 1024], U8)
    R3 = sb("R3", [P, 16 * 1024], U8)
    R4 = sb("R4", [P, 112 * 1024], U8)

    def view(arena, off, shape, dt, parts=P):
        nb = int(np.prod(shape[1:])) * (4 if dt == F32 else (2 if dt == BF16 else 1))
        ap = arena[0:parts, off:off + nb].bitcast(dt)
        if len(shape) == 3:
            ap = ap.rearrange("p (a b) -> p a b", a=shape[1])
        elif len(shape) == 4:
            ap = ap.rearrange("p (a b c) -> p a b c", a=shape[1], b=shape[2])
        return ap

    KB = 1024
    hT = view(R1, 0, [P, 8, S], BF16)
    mixT_b = view(R2, 0, [P, 4, S], BF16)
    mixT_a = view(R3, 0, [P, 4, S], BF16)
    qbT = view(R4, 0, [P, 2, S], BF16)
    kbT = view(R4, 8 * KB, [P, 2, S], BF16)
    glrT = view(R4, 16 * KB, [32, S], BF16, parts=32)
    vb = view(R4, 20 * KB, [P, NT, 512], BF16)
    Gt = view(R4, 36 * KB, [P, NT, 512], BF16)
    kbtm = view(R4, 52 * KB, [P, NT, 256], BF16)
    wst = [view(R4, 60 * KB + i * 9 * KB, [P, 8, 528], BF16) for i in range(2)]
    xst = [view(R4, 78 * KB + i * 4 * KB, [P, D], F32) for i in range(2)]
    xs = [view(R4, 86 * KB + i * 2 * KB, [P, D], BF16) for i in range(2)]
    junk = view(R4, 90 * KB, [P, 2048], BF16)
    tmpA = [view(R4, 94 * KB + i * 2 * KB, [P, 512], F32) for i in range(4)]
    glatmp = view(R4, 102 * KB, [P, 10 * 256], F32)

    ps = [stack.enter_context(nc.psum_tensor("ps%d" % i, [P, 512], F32)) for i in range(8)]

    def psk(i):
        return ("ps", i)

    def load_const(dst, src, key, eng="sp"):
        sch.add(eng, lambda e, dst=dst, src=src: e.dma_start(out=dst, in_=src), writes=[key], dma=True)

    load_const(ident_bf[:], ident_bf_d[:, :], "ident_bf")
    load_const(ident_f[:], ident_f_d[:, :], "ident_f")
    load_const(tri_bf[:], tri_bf_d[:, :], "tri_bf")
    load_const(tri_f[:], tri_f_d[:, :], "tri_f")
    load_const(after_f[:], after_f_d[:, :], "after_f")
    load_const(negmask[:], negmask_d[:, :], "negmask")
    load_const(blockones[:], blockones_d[:, :], "blockones")
    load_const(pow2[:], pow2_d[:, :], "pow2")
    load_const(g1bc[:], g1bc_d[:, :], "g1bc")
    load_const(qkg[:], qkg_d[:, :], "qkg")
    load_const(gout[:], gout_d[:, :], "gout")
    load_const(brbc[:], brbc_d[:, :], "brbc")
    load_const(rb31[:], rb31_d[:, :], "rb31")
    load_const(w2aug[:], w2_d[:, :], "w2aug", eng="pool")
    sch.add("dve", lambda e: e.memset(ones_col[:], 1.0), writes=["ones_col"])
    sch.add("dve", lambda e: e.memset(glrT[0:32, :], 1.0), writes=["glrT_init"])

    for T in range(NT):
        b = T % 2
        tsl = slice(T * P, (T + 1) * P)
        sch.add("sp", lambda e, b=b, tsl=tsl: e.dma_start(out=xst[b], in_=x_d[tsl, :]),
                writes=[("xst", b)], dma=True)
        sch.add("act", lambda e, b=b, T=T: e.activation(out=junk[:, 0:D], in_=xst[b], func=AF.Square,
                                                        accum_out=ssq1[:, T:T + 1]),
                reads=[("xst", b)], writes=["junk", ("ssq1", T)])
        sch.add("act", lambda e, T=T: e.activation(out=rstd1[:, T:T + 1], in_=ssq1[:, T:T + 1], func=AF.Sqrt,
                                                   scale=1.0 / D, bias=eps_col[:, 0:1]),
                reads=[("ssq1", T), "eps_col"], writes=[("rstd1", T)])
        sch.add("dve", lambda e, T=T: e.reciprocal(out=rstd1[:, T:T + 1], in_=rstd1[:, T:T + 1]),
                reads=[("rstd1", T)], writes=[("rstd1", T)])
        sch.add("dve", lambda e, b=b, T=T: e.scalar_tensor_tensor(out=xs[b], in0=xst[b], scalar=rstd1[:, T:T + 1],
                                                                  in1=g1bc[:], op0=ALU.mult, op1=ALU.mult),
                reads=[("xst", b), ("rstd1", T), "g1bc"], writes=[("xs", b)])
        pb = T % 2

        def tr(e, b=b, pb=pb):
            o = ps[pb][:].bitcast(BF16).rearrange("p (c t) -> p c t", c=8)
            ins = None
            for c in range(8):
                ins = e.transpose(o[:, c, :], xs[b][:, c * P:(c + 1) * P], ident_bf[:])
            return ins
        sch.add("pe", tr, reads=[("xs", b), "ident_bf"], writes=[psk(pb)])
        sch.add("act", lambda e, pb=pb, tsl=tsl: e.activation(
            out=hT[:, :, tsl], in_=ps[pb][:].bitcast(BF16).rearrange("p (c t) -> p c t", c=8), func=AF.Copy),
            reads=[psk(pb)], writes=[("hT", T)])

    wcount = [0]

    def load_wgroup(gi):
        name, kind, c0, n = W1_GROUPS[gi]
        b = wcount[0] % 2
        wcount[0] += 1
        src = w1_d[:, c0:c0 + n].rearrange("(c p) n -> p c n", p=P)
        dst = wst[b][:, :, 0:n]
        sch.add("pool", lambda e, dst=dst, src=src: e.dma_start(out=dst, in_=src),
                writes=[("wst", b)], dma=True)
        return b

    bankrot = {}

    def next_bank(lo=2, hi=6):
        k = (lo, hi)
        c = bankrot.get(k, 0)
        bankrot[k] = c + 1
        return lo + c % (hi - lo)

    def fm_matmul(wb, col0, m, tc, bank, parts=None):
        wt = wst[wb]

        def fn(e):
            ins = None
            for c in range(8):
                ins = e.matmul(ps[bank][0:m, :], lhsT=wt[:, c, col0:col0 + m],
                               rhs=hT[:, c, tc * 512:(tc + 1) * 512], start=(c == 0), stop=(c == 7))
            return ins
        sch.add("pe", fn, reads=[("wst", wb)] + [("hT", tc * 4 + k) for k in range(4)], writes=[psk(bank)])

    def tm_matmul(wb, col0, n, T, bank):
        wt = wst[wb]

        def fn(e):
            ins = None
            for c in range(8):
                ins = e.matmul(ps[bank][:, 0:n], lhsT=hT[:, c, T * P:(T + 1) * P],
                               rhs=wt[:, c, col0:col0 + n], start=(c == 0), stop=(c == 7))
            return ins
        sch.add("pe", fn, reads=[("wst", wb), ("hT", T)], writes=[psk(bank)])

    eps_col = sb("eps_col", [P, 1], F32)
    sch.ops.insert(0, _Op("dve", lambda e: e.memset(eps_col[:], EPS), (), ("eps_col",), False))

    wb = load_wgroup(0)
    for tc in range(4):
        tsl = slice(tc * 512, (tc + 1) * 512)
        for k in range(4):
            bank = next_bank()
            fm_matmul(wb, k * 128, 128, tc, bank)
            dst = (qbT if k < 2 else kbT)[:, k % 2, tsl]
            key = ("qbT" if k < 2 else "kbT", tc)
            eng = "act" if k % 2 == 0 else "dve"
            if eng == "act":
                sch.add("act", lambda e, dst=dst, bank=bank: e.activation(out=dst, in_=ps[bank][:], func=AF.Copy),
                        reads=[psk(bank)], writes=[key + (k % 2,)])
            else:
                sch.add("dve", lambda e, dst=dst, bank=bank: e.tensor_copy(out=dst, in_=ps[bank][:]),
                        reads=[psk(bank)], writes=[key + (k % 2,)])
        bank = next_bank()
        fm_matmul(wb, 512, 16, tc, bank)
        sch.add("dve", lambda e, tsl=tsl, bank=bank: e.tensor_copy(out=glrT[0:16, tsl], in_=ps[bank][0:16, :]),
                reads=[psk(bank), "glrT_init"], writes=[("glrT", tc)])
    wb = load_wgroup(1)
    for T in range(NT):
        bank = next_bank()
        tm_matmul(wb, 0, 512, T, bank)
        eng = "act" if T % 2 == 0 else "dve"
        if eng == "act":
            sch.add("act", lambda e, T=T, bank=bank: e.activation(out=vb[:, T, :], in_=ps[bank][:], func=AF.Copy),
                    reads=[psk(bank)], writes=[("vb", T)])
        else:
            sch.add("dve", lambda e, T=T, bank=bank: e.tensor_copy(out=vb[:, T, :], in_=ps[bank][:]),
                    reads=[psk(bank)], writes=[("vb", T)])
    wb = load_wgroup(2)
    for T in range(NT):
        bank = next_bank()
        tm_matmul(wb, 0, 512, T, bank)
        tb = T % 2
        sch.add("act", lambda e, tb=tb, bank=bank: e.activation(out=tmpA[tb][:], in_=ps[bank][:], func=AF.Silu),
                reads=[psk(bank)], writes=[("tmpA", tb)])
        sch.add("pool", lambda e, tb=tb, T=T: e.tensor_tensor(out=Gt[:, T, :], in0=tmpA[tb][:], in1=gout[:], op=ALU.mult),
                reads=[("tmpA", tb), "gout"], writes=[("Gt", T)])
    wb = load_wgroup(3)
    for T in range(NT):
        bank = next_bank()
        tm_matmul(wb, 0, 264, T, bank)
        sch.add("dve", lambda e, T=T, bank=bank: e.tensor_copy(out=kbtm[:, T, :], in_=ps[bank][:, 0:256]),
                reads=[psk(bank)], writes=[("kbtm", T)])
        sch.add("dve", lambda e, T=T, bank=bank: e.tensor_copy(out=iw_s[:, T, :], in_=ps[bank][:, 256:264]),
                reads=[psk(bank)], writes=[("iw", T)])

    def dump(name, src_ap, dst_ap, reads):
        if name in dbg:
            sch.add("sp", lambda e: e.dma_start(out=dst_ap, in_=src_ap), reads=reads, writes=["dbg_" + name], dma=True)

    def finish():
        import os
        if os.environ.get("KSTOP_OPS"):
            print("total ops", len(sch.ops))
            sch.ops = sch.ops[:int(os.environ["KSTOP_OPS"])]
        outs = ["dbg_" + n for n in dbg] + ["out_%d" % T for T in range(NT)]
        sch.add("sp", lambda e: None, reads=outs)
        sch.finalize()
        with nc.Block() as block:
            sch.emit(block)
        stack.close()
        return nc

    dump("hT", hT, dbg.get("hT").rearrange("(c p) t -> p c t", p=P) if "hT" in dbg else None, [("hT", T) for T in range(NT)])
    dump("qbT", qbT[:, 0, :], dbg.get("qbT"), [("qbT", tc, 0) for tc in range(4)])
    dump("vb", vb, dbg.get("vb").rearrange("(t p) c -> p t c", p=P) if "vb" in dbg else None, [("vb", T) for T in range(NT)])
    dump("Gt", Gt, dbg.get("Gt").rearrange("(t p) c -> p t c", p=P) if "Gt" in dbg else None, [("Gt", T) for T in range(NT)])
    if STOP_AFTER == "p1":
        return finish()

    Sst = sb("Sst", [P, 4, 128], F32)
    Sbf = sb("Sbf", [P, 4, 128], BF16)
    g_e1 = sb("g_e1", [P, 256], F32)
    g_l = sb("g_l", [P, 256], F32)
    g_eb = [sb("g_eb%d" % i, [P, 2, 128], F32) for i in range(2)]
    g_einv = sb("g_einv", [P, 2, 128], F32)
    g_erem = sb("g_erem", [P, 256], F32)
    g_qt = [sb("g_qt%d" % i, [P, 2, 128], BF16) for i in range(2)]
    g_kt = sb("g_kt", [P, 2, 128], BF16)
    g_kh = sb("g_kh", [P, 256], BF16)
    g_A = [sb("g_A%d" % i, [P, 4, 128], BF16) for i in range(2)]
    g_ob = sb("g_ob", [P, 512], BF16)
    g_ssq = sb("g_ssq", [P, 4], F32)
    g_rs = sb("g_rs", [P, 4], F32)
    sch.add("dve", lambda e: e.memset(Sst[:], 0.0), writes=[("Sst", h) for h in range(4)])
    sch.add("dve", lambda e: e.memset(Sbf[:], 0.0), writes=[("Sbf", h) for h in range(4)])
    BZ, BC, BA, BO, BT = 0, 1, 3, 4, 6
    BUS = [5, 2]
    print("ops before GLA", len(sch.ops))

    def gla_stage1(T):
        b = T % 2
        tsl = slice(T * P, (T + 1) * P)
        eb, qt, A, BU = g_eb[b], g_qt[b], g_A[b], BUS[b]
        sch.add("pe", lambda e: e.matmul(ps[BZ][:, 0:256], lhsT=glrT[0:17, tsl], rhs=w2aug[0:17, :], start=True, stop=True),
                reads=[("glrT", T // 4), "glrT_init", "w2aug"], writes=[psk(BZ)])
        sch.add("act", lambda e: e.activation(out=g_e1[:], in_=ps[BZ][:, 0:256], func=AF.Exp, scale=-1.0),
                reads=[psk(BZ)], writes=["g_e1"])
        sch.add("act", lambda e: e.activation(out=g_l[:], in_=g_e1[:], func=AF.Ln, scale=1.0, bias=ones_col[:, 0:1]),
                reads=["g_e1", "ones_col"], writes=["g_l"])

        def cum(e):
            e.matmul(ps[BC][:, 0:128], lhsT=g_l[:, 0:128], rhs=tri_f[:], start=True, stop=True)
            return e.matmul(ps[BC][:, 128:256], lhsT=g_l[:, 128:256], rhs=tri_f[:], start=True, stop=True)
        sch.add("pe", cum, reads=["g_l", "tri_f"], writes=[psk(BC)])
        sch.add("pe", lambda e: e.matmul(ps[BZ][:, 256:512], lhsT=after_f[:], rhs=g_l[:], start=True, stop=True),
                reads=["g_l", "after_f"], writes=[psk(BZ)])
        cview = ps[BC][:, 0:256].rearrange("p (a b) -> p a b", a=2)
        sch.add("act", lambda e: e.activation(out=eb[:], in_=cview, func=AF.Exp, scale=-1.0 / 16),
                reads=[psk(BC)], writes=[("g_eb", b)])
        sch.add("act", lambda e: e.activation(out=g_einv[:], in_=cview, func=AF.Exp, scale=1.0 / 16),
                reads=[psk(BC)], writes=["g_einv"])
        sch.add("act", lambda e: e.activation(out=g_erem[:], in_=ps[BZ][:, 256:512], func=AF.Exp, scale=-1.0 / 16),
                reads=[psk(BZ)], writes=["g_erem"])
        sch.add("dve", lambda e: e.scalar_tensor_tensor(out=qt[:], in0=qbT[:, :, tsl], scalar=0.125, in1=eb[:],
                                                        op0=ALU.mult, op1=ALU.mult),
                reads=[("qbT", T // 4, 0), ("qbT", T // 4, 1), ("g_eb", b)], writes=[("g_qt", b)])
        sch.add("dve", lambda e: e.scalar_tensor_tensor(out=g_kt[:], in0=kbT[:, :, tsl], scalar=1.0, in1=g_einv[:],
                                                        op0=ALU.mult, op1=ALU.mult),
                reads=[("kbT", T // 4, 0), ("kbT", T // 4, 1), "g_einv"], writes=["g_kt"])
        sch.add("dve", lambda e: e.scalar_tensor_tensor(out=g_kh[:], in0=kbtm[:, T, :], scalar=1.0, in1=g_erem[:],
                                                        op0=ALU.mult, op1=ALU.mult),
                reads=[("kbtm", T), "g_erem"], writes=["g_kh"])

        def attn(e):
            ins = None
            for h in (0, 2, 1, 3):
                p, r = h // 2, h % 2
                rs = slice(r * 64, (r + 1) * 64)
                bk = BA if r == 0 else 7
                ins = e.matmul(ps[bk][:, p * 128:(p + 1) * 128], lhsT=g_kt[rs, p, :], rhs=qt[rs, p, :], start=True, stop=True)
            return ins
        sch.add("pe", attn, reads=["g_kt", ("g_qt", b)], writes=[psk(BA), psk(7)])
        for r in range(2):
            bk = BA if r == 0 else 7
            sch.add("dve", lambda e, r=r, bk=bk: e.tensor_tensor(
                out=A[:, r::2, :], in0=ps[bk][:, 0:256].rearrange("p (h t) -> p h t", h=2),
                in1=tri_bf[:, :].unsqueeze(1).to_broadcast([P, 2, 128]), op=ALU.mult),
                reads=[psk(bk), "tri_bf"], writes=[("g_A", b, r)])

        def umm(e):
            ins = None
            for h in range(4):
                p = h // 2
                ins = e.matmul(ps[BU][:, h * 128:(h + 1) * 128], lhsT=g_kh[:, p * 128:(p + 1) * 128], rhs=vb[:, T, h * 128:(h + 1) * 128],
                               start=True, stop=True)
            return ins
        sch.add("pe", umm, reads=["g_kh", ("vb", T)], writes=[psk(BU)])

    def gla_stage2(T):
        b = T % 2
        tsl = slice(T * P, (T + 1) * P)
        eb, qt, A, BU = g_eb[b], g_qt[b], g_A[b], BUS[b]

        def omm(e):
            ins = None
            for h in range(4):
                p = h // 2
                e.matmul(ps[BO][:, h * 128:(h + 1) * 128], lhsT=A[:, h, :], rhs=vb[:, T, h * 128:(h + 1) * 128], start=True, stop=False)
                ins = e.matmul(ps[BO][:, h * 128:(h + 1) * 128], lhsT=qt[:, p, :], rhs=Sbf[:, h, :], start=False, stop=True)
            return ins
        sch.add("pe", omm, reads=[("g_A", b, 0), ("g_A", b, 1), ("vb", T), ("g_qt", b)] + [("Sbf", h) for h in range(4)], writes=[psk(BO)])
        for h in range(4):
            p, r = h // 2, h % 2
            rs = slice(r * 64, (r + 1) * 64)
            sch.add("dve", lambda e, h=h, p=p, rs=rs: e.scalar_tensor_tensor(
                out=Sst[rs, h, :], in0=Sst[rs, h, :], scalar=eb[rs, p, 127:128], in1=ps[BU][rs, h * 128:(h + 1) * 128],
                op0=ALU.mult, op1=ALU.add), reads=[psk(BU), ("g_eb", b), ("Sst", h)], writes=[("Sst", h), psk(BU)])
            sch.add("act", lambda e, h=h, rs=rs: e.activation(out=Sbf[rs, h, :], in_=Sst[rs, h, :], func=AF.Copy),
                    reads=[("Sst", h)], writes=[("Sbf", h)])
        for h in range(4):
            sch.add("act", lambda e, h=h: e.activation(out=junk[:, 0:128], in_=ps[BO][:, h * 128:(h + 1) * 128], func=AF.Square,
                                                       accum_out=g_ssq[:, h:h + 1]),
                    reads=[psk(BO)], writes=["junk", ("g_ssq", h), psk(BO)])
        sch.add("act", lambda e: e.activation(out=g_rs[:], in_=g_ssq[:], func=AF.Sqrt, scale=1.0 / 128, bias=eps_col[:, 0:1]),
                reads=[("g_ssq", h) for h in range(4)] + ["eps_col"], writes=["g_rs"])
        sch.add("dve", lambda e: e.reciprocal(out=g_rs[:], in_=g_rs[:]), reads=["g_rs"], writes=["g_rs"])
        for h in range(4):
            hs = slice(h * 128, (h + 1) * 128)
            sch.add("dve", lambda e, h=h, hs=hs: e.scalar_tensor_tensor(
                out=g_ob[:, hs], in0=ps[BO][:, hs], scalar=g_rs[:, h:h + 1], in1=Gt[:, T, hs], op0=ALU.mult, op1=ALU.mult),
                reads=[psk(BO), "g_rs", ("Gt", T)], writes=[("g_ob", h), psk(BO)])
        if "ob" in dbg:
            sch.add("sp", lambda e: e.dma_start(out=dbg["ob"][tsl, :], in_=g_ob[:]), reads=[("g_ob", h) for h in range(4)],
                    writes=["dbg_ob"], dma=True)

        def trb(e):
            o = ps[BT][:].bitcast(BF16).rearrange("p (c t) -> p c t", c=8)
            ins = None
            for c in range(4):
                ins = e.transpose(o[:, c, :], g_ob[:, c * P:(c + 1) * P], ident_bf[:])
            return ins
        sch.add("pe", trb, reads=[("g_ob", h) for h in range(4)] + ["ident_bf"], writes=[psk(BT)])
        sch.add("act", lambda e: e.activation(
            out=mixT_b[:, :, tsl], in_=ps[BT][:].bitcast(BF16).rearrange("p (c t) -> p c t", c=8)[:, 0:4, :], func=AF.Copy),
            reads=[psk(BT)], writes=[("mixT_b", T)])

    gla_stage1(0)
    for T in range(NT):
        if T + 1 < NT:
            gla_stage1(T + 1)
        gla_stage2(T)
    if STOP_AFTER == "gla":
        return finish()
    sch.barrier()

    qaT = view(R4, 0, [P, 4, S], BF16)
    kaT = view(R4, 16 * KB, [P, 4, S], BF16)
    iqT = view(R4, 32 * KB, [P, 3, S], BF16)
    ikT = view(R4, 44 * KB, [P, S], BF16)
    vaT = view(R4, 48 * KB, [P, NT, 8 * 65], BF16)
    wst2 = [view(R4, 66 * KB + i * 9 * KB, [P, 8, 528], BF16) for i in range(2)]
    n_sq = [view(R4, 84 * KB + i * KB, [P, 512], BF16) for i in range(2)]
    n_ln = [view(R4, 86 * KB + i * 2 * KB, [P, 512], F32) for i in range(2)]
    n_rs = [view(R4, 90 * KB + i * 2 * KB, [P, 512], F32) for i in range(2)]
    wst[0], wst[1] = wst2[0], wst2[1]
    sch.add("dve", lambda e: e.memset(vaT[:], 1.0), writes=[("va", T) for T in range(NT)])
    ncnt = [0]
    pend2 = []
    for gi, which in [(4, 0), (5, 1)]:
        wb = load_wgroup(gi)
        dstT = qaT if which == 0 else kaT
        kname = "qaT" if which == 0 else "kaT"
        for tc in range(4):
            tsl = slice(tc * 512, (tc + 1) * 512)
            for p in range(4):
                bank = next_bank(0, 4)
                sbank = next_bank(4, 8)
                nb = ncnt[0] % 2
                ncnt[0] += 1
                fm_matmul(wb, p * 128, 128, tc, bank)
                sch.add("act", lambda e, nb=nb, bank=bank: e.activation(out=n_sq[nb], in_=ps[bank][:], func=AF.Square),
                        reads=[psk(bank)], writes=[("n_sq", nb), psk(bank)])
                def back2(nb=nb, sbank=sbank, bank=bank, p=p, tsl=tsl, dstT=dstT, which=which, kname=kname, tc=tc):
                    sch.add("pe", lambda e: e.matmul(ps[sbank][:], lhsT=blockones[:], rhs=n_sq[nb], start=True, stop=True),
                            reads=[("n_sq", nb), "blockones"], writes=[psk(sbank)])
                    sch.add("act", lambda e: e.activation(out=n_ln[nb], in_=ps[sbank][:], func=AF.Ln, scale=1.0 / 64,
                                                          bias=eps_col[:, 0:1]),
                            reads=[psk(sbank), "eps_col"], writes=[("n_ln", nb)])
                    sch.add("act", lambda e: e.activation(out=n_rs[nb], in_=n_ln[nb], func=AF.Exp, scale=-0.5),
                            reads=[("n_ln", nb)], writes=[("n_rs", nb)])
                    sch.add("dve", lambda e: e.scalar_tensor_tensor(
                        out=dstT[:, p, tsl], in0=ps[bank][:], scalar=qkg[:, which:which + 1], in1=n_rs[nb], op0=ALU.mult, op1=ALU.mult),
                        reads=[psk(bank), ("n_rs", nb), "qkg"], writes=[(kname, p, tc), psk(bank)])
                pend2.append(back2)
                while len(pend2) > 1:
                    pend2.pop(0)()
    while pend2:
        pend2.pop(0)()
    wb = load_wgroup(6)
    for tc in range(4):
        tsl = slice(tc * 512, (tc + 1) * 512)
        col = 0
        for ti, (h0, h1) in enumerate(IQ_TILES):
            m = (h1 - h0) * 32
            bank = next_bank(0, 8)
            fm_matmul(wb, col, m, tc, bank)
            col += m
            sch.add("act", lambda e, ti=ti, m=m, tsl=tsl, bank=bank: e.activation(out=iqT[0:m, ti, tsl], in_=ps[bank][0:m, :], func=AF.Copy),
                    reads=[psk(bank)], writes=[("iqT", ti, tc)])
        bank = next_bank(0, 8)
        fm_matmul(wb, col, 96, tc, bank)
        sch.add("dve", lambda e, tsl=tsl, bank=bank: e.tensor_copy(out=ikT[0:96, tsl], in_=ps[bank][0:96, :]),
                reads=[psk(bank)], writes=[("ikT", tc)])
    wb = load_wgroup(7)
    for T in range(NT):
        bank = next_bank(0, 8)
        tm_matmul(wb, 0, 512, T, bank)
        dstv = vaT[:, T, :].rearrange("p (h d) -> p h d", h=8)[:, :, 0:64]
        srcv = ps[bank][:].rearrange("p (h d) -> p h d", h=8)
        if T % 2 == 0:
            sch.add("act", lambda e, dstv=dstv, srcv=srcv: e.activation(out=dstv, in_=srcv, func=AF.Copy),
                    reads=[psk(bank)], writes=[("va", T)])
        else:
            sch.add("dve", lambda e, dstv=dstv, srcv=srcv: e.tensor_copy(out=dstv, in_=srcv),
                    reads=[psk(bank)], writes=[("va", T)])
    dump("qaT", qaT[:, 0, :], dbg.get("qaT"), [("qaT", 0, tc) for tc in range(4)])
    dump("kaT", kaT[:, 0, :], dbg.get("kaT"), [("kaT", 0, tc) for tc in range(4)])
    if STOP_AFTER == "p2":
        return finish()
    sch.barrier()

    selT = view(R4, 66 * KB, [P, SELT_TOTAL], BF16)
    selts = [view(R4, 100 * KB + i * 4 * KB, [P, S], BF16) for i in range(2)]
    PTb = [view(R4, 100 * KB + i * KB, [P, 512], BF16) for i in range(8)]
    junk_tk = view(R4, 108 * KB, [P, 2048], BF16)
    score_all = R1[:, :].bitcast(F32)
    thr = sb("thr", [P, NT], F32)
    thr2 = sb("thr2", [P, NT], F32)
    cnt = sb("cnt", [P, NT], F32)
    sgn = sb("sgn", [P, NT], F32)
    amax = sb("amax", [P, NT], F32)
    mrow = sb("mrow", [P, 1], F32)
    mtab = sb("mtab", [P, KITER + 1], F32)
    biasT_s = view(R4, 100 * KB, [P, 8, 2, 128], F32)
    sch.add("sp", lambda e: e.dma_start(out=biasT_s, in_=biasT_d.rearrange("p (h d t) -> p h d t", h=8, d=2)),
            writes=["biasT"], dma=True)
    sch.add("act", lambda e: e.activation(out=Etile[:], in_=biasT_s, func=AF.Exp), reads=["biasT"], writes=["Etile"])
    for h in range(8):
        sch.add("dve", lambda e, h=h: e.tensor_tensor(out=Etile[:, h, 0, :], in0=Etile[:, h, 0, :], in1=tri_bf[:], op=ALU.mult),
                reads=["Etile", "tri_bf"], writes=["Etile"])
    sch.add("dve", lambda e: e.memset(selts[0][:], 0.0), reads=["Etile"], writes=[("selts", 0)])
    sch.add("dve", lambda e: e.tensor_copy(out=selT[:, _selT_off(0):_selT_off(0) + 128], in_=tri_bf[:]), reads=["tri_bf"], writes=[("selT", 0, 0)])
    sch.add("dve", lambda e: e.memset(selT[:, _selT_off(0) + 128:_selT_off(0) + 256], 1.0), writes=[("selT", 0, 1)])
    sch.add("dve", lambda e: e.tensor_copy(out=selT[:, _selT_off(1):_selT_off(1) + 128], in_=tri_bf[:]), reads=["tri_bf"], writes=[("selT", 1, 1)])

    print("ops before topk loops", len(sch.ops))
    batches = [list(range(2, 8)), list(range(8, 12)), list(range(12, 16))]
    ACT_SHARE = [4, 2, 2]
    sumA = sb("sumA", [P, NT], F32)
    nhalf = sb("nhalf", [P, NT], F32)
    junk_act = view(R3, 0, [P, 2048], BF16)
    for j in range(NT):
        sch.add("dve", lambda e, j=j: e.memset(nhalf[:, j:j + 1], 64.0 * (j + 1)), writes=["nhalf"])
    hd_loc = []
    for ti, (h0, h1) in enumerate(IQ_TILES):
        for k in range(h1 - h0):
            hd_loc.append((ti, k))
    lbank = [0]
    for bi, batch in enumerate(batches):
        soff = {}
        o = 0
        for j in batch:
            soff[j] = o
            o += (j + 1) * 128
        nb = len(batch)
        j0 = batch[0]
        for j in batch:
            n = (j + 1) * 128
            sc_j = score_all[:, soff[j]:soff[j] + n]
            nsc = (n + 511) // 512
            for sc in range(nsc):
                w = min(512, n - sc * 512)
                ssl = slice(sc * 512, sc * 512 + w)
                for h in range(8):
                    ti, k = hd_loc[h]
                    rs = slice(k * 32, (k + 1) * 32)
                    lb = lbank[0] % 4
                    rb = 4 + lbank[0] % 4
                    lbank[0] += 1
                    sch.add("pe", lambda e, lb=lb, rs=rs, ti=ti, j=j, ssl=ssl, w=w: e.matmul(
                        ps[lb][:, 0:w], lhsT=iqT[rs, ti, j * 128:(j + 1) * 128], rhs=ikT[rs, ssl], start=True, stop=True),
                        reads=[("iqT", ti, j // 4)] + [("ikT", c) for c in range(sc * 4 // 4, (sc * 512 + w - 1) // 512 + 1)],
                        writes=[psk(lb)])
                    sch.add("act", lambda e, lb=lb, rb=rb, w=w: e.activation(out=ps[rb][:, 0:w], in_=ps[lb][:, 0:w], func=AF.Relu),
                            reads=[psk(lb)], writes=[psk(rb), psk(lb)])
                    dst = sc_j[:, ssl]
                    if h == 0:
                        sch.add("dve", lambda e, rb=rb, w=w, dst=dst, j=j, h=h: e.tensor_scalar(
                            out=dst, in0=ps[rb][:, 0:w], scalar1=iw_s[:, j, h:h + 1], scalar2=None, op0=ALU.mult),
                            reads=[psk(rb), ("iw", j)], writes=[("score", j, sc), psk(rb)])
                    else:
                        sch.add("dve", lambda e, rb=rb, w=w, dst=dst, j=j, h=h: e.scalar_tensor_tensor(
                            out=dst, in0=ps[rb][:, 0:w], scalar=iw_s[:, j, h:h + 1], in1=dst, op0=ALU.mult, op1=ALU.add),
                            reads=[psk(rb), ("iw", j), ("score", j, sc)], writes=[("score", j, sc), psk(rb)])
            skeys = [("score", j, sc) for sc in range(nsc)]
            sch.add("dve", lambda e, sc_j=sc_j, j=j: e.tensor_reduce(out=amax[:, j:j + 1], in_=sc_j, axis=AX.X, op=ALU.max,
                                                                    apply_absolute_value=True),
                    reads=skeys, writes=[("amax", j)])
            dsl = slice(soff[j] + j * 128, soff[j] + (j + 1) * 128)
            sch.add("dve", lambda e, dsl=dsl: e.tensor_tensor(out=score_all[:, dsl], in0=score_all[:, dsl], in1=negmask[:], op=ALU.add),
                    reads=skeys + ["negmask", ("amax", j)], writes=skeys)
        if "score" in dbg and bi == 1:
            sch.add("sp", lambda e, soff=soff: e.dma_start(out=dbg["score"][:, 0:1280], in_=score_all[:, soff[9]:soff[9] + 1280]),
                    reads=[("score", 9, sc) for sc in range(3)], writes=["dbg_score"], dma=True)
        bsl = slice(j0, j0 + nb)
        sch.add("dve", lambda e, bsl=bsl: e.tensor_reduce(out=mrow[:], in_=amax[:, bsl], axis=AX.X, op=ALU.max),
                reads=[("amax", j) for j in batch], writes=["mrow"])
        sch.add("dve", lambda e: e.tensor_scalar(out=mtab[:], in0=pow2[:], scalar1=mrow[:, 0:1], scalar2=None, op0=ALU.mult),
                reads=["mrow", "pow2"], writes=["mtab"])
        sch.add("dve", lambda e, bsl=bsl: e.memset(thr[:, bsl], 0.0), writes=["thr"])
        nact = ACT_SHARE[bi]
        act_tiles = batch[:nact]
        asl = slice(batch[0], batch[0] + nact)
        for k in range(KITER):
            for j in batch:
                n = (j + 1) * 128
                sc_j = score_all[:, soff[j]:soff[j] + n]
                skeys_j = [("score", j, sc) for sc in range((n + 511) // 512)]
                if j in act_tiles:
                    sch.add("act", lambda e, sc_j=sc_j, n=n, j=j: e.activation(
                        out=junk_act[:, 0:n], in_=sc_j, func=AF.Sign, scale=-1.0, bias=thr[:, j:j + 1], accum_out=sumA[:, j:j + 1]),
                        reads=skeys_j + ["thr"], writes=["junk_act", ("sumA", j)])
                else:
                    sch.add("dve", lambda e, sc_j=sc_j, n=n, j=j: e.tensor_scalar(
                        out=junk_tk[:, 0:n], in0=sc_j, scalar1=thr[:, j:j + 1], scalar2=None, op0=ALU.is_ge, op1=ALU.add,
                        accum_out=cnt[:, j:j + 1]),
                        reads=skeys_j + ["thr"], writes=["junk", ("cnt", j)])
            if nact > 0:
                sch.add("dve", lambda e, asl=asl: e.scalar_tensor_tensor(out=cnt[:, asl], in0=sumA[:, asl], scalar=-0.5, in1=nhalf[:, asl],
                                                                        op0=ALU.mult, op1=ALU.add),
                        reads=[("sumA", j) for j in act_tiles] + ["nhalf"], writes=[("cnt", j) for j in act_tiles])
            sch.add("dve", lambda e, bsl=bsl: e.tensor_scalar(out=sgn[:, bsl], in0=cnt[:, bsl], scalar1=256.0, scalar2=0.5,
                                                             op0=ALU.is_ge, op1=ALU.subtract),
                    reads=[("cnt", j) for j in batch], writes=["sgn"])
            sch.add("dve", lambda e, bsl=bsl, k=k: e.scalar_tensor_tensor(out=thr2[:, bsl], in0=sgn[:, bsl], scalar=mtab[:, k:k + 1],
                                                                       in1=thr[:, bsl], op0=ALU.mult, op1=ALU.add),
                    reads=["sgn", "mtab", "thr"], writes=["thr2"])
            sch.add("dve", lambda e, bsl=bsl: e.tensor_copy(out=thr[:, bsl], in_=thr2[:, bsl]), reads=["thr2"], writes=["thr"])
        sch.add("dve", lambda e, bsl=bsl: e.tensor_scalar(out=thr2[:, bsl], in0=thr[:, bsl], scalar1=mtab[:, KITER:KITER + 1], scalar2=None,
                                                         op0=ALU.subtract),
                reads=["thr", "mtab"], writes=["thr2"])
        if "thr" in dbg and bi == 1:
            sch.add("sp", lambda e: e.dma_start(out=dbg["thr"][:, :], in_=thr2[:]), reads=["thr2"], writes=["dbg_thr"], dma=True)
        for j in batch:
            n = (j + 1) * 128
            sc_j = score_all[:, soff[j]:soff[j] + n]
            sb_ = j % 2
            sch.add("dve", lambda e, sc_j=sc_j, n=n, j=j, sb_=sb_: e.tensor_scalar(
                out=selts[sb_][:, 0:n], in0=sc_j, scalar1=thr2[:, j:j + 1], scalar2=None, op0=ALU.is_ge),
                reads=[("score", j, sc) for sc in range((n + 511) // 512)] + ["thr2"], writes=[("selts", sb_)])
            for i0 in range(0, j + 1, 8):
                i1 = min(j + 1, i0 + 8)
                tb = next_bank(0, 8)

                def trs(e, i0=i0, i1=i1, tb=tb, sb_=sb_):
                    o = ps[tb][:].bitcast(BF16).rearrange("p (c t) -> p c t", c=8)
                    ins = None
                    for i in range(i0, i1):
                        ins = e.transpose(o[:, i - i0, :], selts[sb_][:, i * 128:(i + 1) * 128], ident_bf[:])
                    return ins
                sch.add("pe", trs, reads=[("selts", sb_), "ident_bf"], writes=[psk(tb)])
                for i in range(i0, i1):
                    o = ps[tb][:].bitcast(BF16).rearrange("p (c t) -> p c t", c=8)[:, i - i0, :]
                    off = _selT_off(i) + (j - i) * 128
                    if i % 2 == 0:
                        sch.add("act", lambda e, o=o, off=off: e.activation(out=selT[:, off:off + 128], in_=o, func=AF.Copy),
                                reads=[psk(tb)], writes=[("selT", i, j), psk(tb)])
                    else:
                        sch.add("dve", lambda e, o=o, off=off: e.tensor_copy(out=selT[:, off:off + 128], in_=o),
                                reads=[psk(tb)], writes=[("selT", i, j), psk(tb)])
    dump("selT", selT, dbg.get("selT"), [("selT", i, j) for i in range(16) for j in range(i, 16)])
    if STOP_AFTER == "topk":
        return finish()
    sch.barrier()

    mixa = view(R1, 0, [P, NT, 512], BF16)
    rden = sb("rden", [P, 4], F32)
    abank = [0]
    LOOKAHEAD = 5
    pend = []

    def flush(keep):
        while len(pend) > keep:
            pend.pop(0)()

    for h in range(8):
        p, r = h // 2, h % 2
        rs = slice(r * 64, (r + 1) * 64)
        for J in range(4):
            accb = 6 + (abank[0] % 2)
            abank[0] += 1
            accv = ps[accb][:, 0:260].rearrange("p (j d) -> p j d", j=4)
            first = [True]
            for i in range(4 * J + 4):
                jlo = max(i, 4 * J)
                t0 = jlo * 128
                n = (4 * J + 4 - jlo) * 128
                sbk = next_bank(0, 6)
                pb = next_bank(100, 108) - 100
                sch.add("pe", lambda e, sbk=sbk, rs=rs, p=p, i=i, t0=t0, n=n: e.matmul(
                    ps[sbk][:, 0:n], lhsT=kaT[rs, p, i * 128:(i + 1) * 128], rhs=qaT[rs, p, t0:t0 + n], start=True, stop=True),
                    reads=[("kaT", p, i // 4), ("qaT", p, J)], writes=[psk(sbk)])
                nnear = max(0, min(4 * J + 4, i + 2) - jlo) * 128
                if nnear > 0:
                    sch.add("act", lambda e, sbk=sbk, pb=pb, nnear=nnear: e.activation(out=PTb[pb][:, 0:nnear], in_=ps[sbk][:, 0:nnear],
                                                                                       func=AF.Exp, scale=0.125),
                            reads=[psk(sbk)], writes=[("PT", pb), psk(sbk)])
                if n > nnear:
                    sch.add("act", lambda e, sbk=sbk, pb=pb, nnear=nnear, n=n, h=h: e.activation(
                        out=PTb[pb][:, nnear:n], in_=ps[sbk][:, nnear:n], func=AF.Exp, scale=0.125, bias=rb31[:, h:h + 1]),
                        reads=[psk(sbk), "rb31"], writes=[("PT", pb), psk(sbk)])
                soff_ = _selT_off(i) + (jlo - i) * 128
                sch.add("dve", lambda e, pb=pb, n=n, soff_=soff_: e.tensor_tensor(out=PTb[pb][:, 0:n], in0=PTb[pb][:, 0:n],
                                                                                 in1=selT[:, soff_:soff_ + n], op=ALU.mult),
                        reads=[("PT", pb)] + [("selT", i, j) for j in range(jlo, 4 * J + 4)], writes=[("PT", pb)])
                for j in range(jlo, min(4 * J + 4, i + 2)):
                    dlt = j - i
                    cs = slice((j - jlo) * 128, (j - jlo + 1) * 128)
                    sch.add("dve", lambda e, pb=pb, cs=cs, h=h, dlt=dlt: e.tensor_tensor(out=PTb[pb][:, cs], in0=PTb[pb][:, cs],
                                                                                         in1=Etile[:, h, dlt, :], op=ALU.mult),
                            reads=[("PT", pb), "Etile"], writes=[("PT", pb)])

                def back(pb=pb, jlo=jlo, J=J, i=i, h=h, accv=accv, first=first, accb=accb):
                    def pv(e):
                        ins = None
                        for j in range(jlo, 4 * J + 4):
                            cs = slice((j - jlo) * 128, (j - jlo + 1) * 128)
                            ins = e.matmul(accv[:, j - 4 * J, :], lhsT=PTb[pb][:, cs], rhs=vaT[:, i, h * 65:(h + 1) * 65],
                                           start=first[0], stop=False, skip_group_check=True)
                            first[0] = False
                        return ins
                    sch.add("pe", pv, reads=[("PT", pb), ("va", i)], writes=[psk(accb)])
                    if i == 4 * J + 3:
                        sch.add("dve", lambda e: e.reciprocal(out=rden[:], in_=accv[:, :, 64]), reads=[psk(accb)], writes=["rden", psk(accb)])
                        sch.add("dve", lambda e: e.tensor_tensor(
                            out=mixa[:, 4 * J:4 * J + 4, h * 64:(h + 1) * 64], in0=accv[:, :, 0:64],
                            in1=rden[:, :].unsqueeze(2).to_broadcast([P, 4, 64]), op=ALU.mult),
                            reads=[psk(accb), "rden"], writes=[("mixa", 4 * J + jj, h) for jj in range(4)] + [psk(accb)])
                pend.append(back)
                flush(LOOKAHEAD)
    flush(0)
    dump("mixa", mixa, dbg.get("mixa").rearrange("(t p) c -> p t c", p=P) if "mixa" in dbg else None,
         [("mixa", T, h) for T in range(NT) for h in range(8)])
    for T in range(NT):
        tsl = slice(T * P, (T + 1) * P)
        tb = next_bank(0, 6)

        def tra(e, T=T, tb=tb):
            o = ps[tb][:].bitcast(BF16).rearrange("p (c t) -> p c t", c=8)
            ins = None
            for c in range(4):
                ins = e.transpose(o[:, c, :], mixa[:, T, c * P:(c + 1) * P], ident_bf[:])
            return ins
        sch.add("pe", tra, reads=[("mixa", T, h) for h in range(8)] + ["ident_bf"], writes=[psk(tb)])
        sch.add("act", lambda e, tsl=tsl, tb=tb: e.activation(
            out=mixT_a[:, :, tsl], in_=ps[tb][:].bitcast(BF16).rearrange("p (c t) -> p c t", c=8)[:, 0:4, :], func=AF.Copy),
            reads=[psk(tb)], writes=[("mixT_a", T)])
    if STOP_AFTER == "attn":
        return finish()
    sch.barrier()

    x1 = view(R4, 0, [P, NT, D], F32)
    woutb = view(R4, 64 * KB, [P, 8, D], BF16)
    xst5 = [view(R4, 80 * KB + i * 4 * KB, [P, D], F32) for i in range(2)]
    xs5 = [view(R4, 88 * KB + i * 2 * KB, [P, D], BF16) for i in range(2)]
    junk5 = view(R4, 92 * KB, [P, 2048], BF16)
    g2bc = view(R4, 96 * KB, [P, D], F32)
    wrb = view(R4, 100 * KB, [P, 8, 36], BF16)
    h2T = view(R1, 0, [P, 8, S], BF16)
    ssq2 = sb("ssq2", [P, NT], F32)
    rstd2 = sb("rstd2", [P, NT], F32)
    logit = sb("logit", [P, NT, 36], F32)
    for hh in range(2):
        sch.add("pool", lambda e, hh=hh: e.dma_start(out=woutb[:, :, hh * 512:(hh + 1) * 512],
                                                     in_=wout_d[:, hh * 512:(hh + 1) * 512].rearrange("(c p) n -> p c n", p=P)),
                writes=[("wout", hh)], dma=True)
    sch.add("pool", lambda e: e.dma_start(out=wrb, in_=wr_d.rearrange("(c p) n -> p c n", p=P)), writes=["wrb"], dma=True)
    sch.add("sp", lambda e: e.dma_start(out=g2bc, in_=g2bc_d[:, :]), writes=["g2bc"], dma=True)
    pend5 = []
    for T in range(NT):
        b = T % 2
        tsl = slice(T * P, (T + 1) * P)
        sch.add("sp", lambda e, b=b, tsl=tsl: e.dma_start(out=xst5[b], in_=x_d[tsl, :]), writes=[("xst5", b)], dma=True)
        for hh in range(2):
            bank = next_bank(0, 4)

            def om(e, T=T, hh=hh, bank=bank):
                ins = None
                for c in range(8):
                    src = mixT_a if c < 4 else mixT_b
                    ins = e.matmul(ps[bank][:], lhsT=src[:, c % 4, T * P:(T + 1) * P], rhs=woutb[:, c, hh * 512:(hh + 1) * 512],
                                   start=(c == 0), stop=(c == 7))
                return ins
            sch.add("pe", om, reads=[("mixT_a", T), ("mixT_b", T), ("wout", hh)], writes=[psk(bank)])
            sch.add("dve", lambda e, T=T, hh=hh, bank=bank, b=b: e.tensor_tensor(
                out=x1[:, T, hh * 512:(hh + 1) * 512], in0=ps[bank][:], in1=xst5[b][:, hh * 512:(hh + 1) * 512], op=ALU.add),
                reads=[psk(bank), ("xst5", b)], writes=[("x1", T, hh)])
        sch.add("act", lambda e, T=T: e.activation(out=junk5[:, 0:D], in_=x1[:, T, :], func=AF.Square, accum_out=ssq2[:, T:T + 1]),
                reads=[("x1", T, 0), ("x1", T, 1)], writes=["junk5", ("ssq2", T)])
        sch.add("act", lambda e, T=T: e.activation(out=rstd2[:, T:T + 1], in_=ssq2[:, T:T + 1], func=AF.Sqrt, scale=1.0 / D,
                                                   bias=eps_col[:, 0:1]),
                reads=[("ssq2", T), "eps_col"], writes=[("rstd2", T)])
        sch.add("dve", lambda e, T=T: e.reciprocal(out=rstd2[:, T:T + 1], in_=rstd2[:, T:T + 1]), reads=[("rstd2", T)], writes=[("rstd2", T)])
        sch.add("dve", lambda e, T=T, b=b: e.scalar_tensor_tensor(out=xs5[b], in0=x1[:, T, :], scalar=rstd2[:, T:T + 1], in1=g2bc,
                                                                  op0=ALU.mult, op1=ALU.mult),
                reads=[("x1", T, 0), ("x1", T, 1), ("rstd2", T), "g2bc"], writes=[("xs5", b)])
        def back5(T=T, b=b, tsl=tsl):
            tb = next_bank(4, 6)

            def tr5(e, b=b, tb=tb):
                o = ps[tb][:].bitcast(BF16).rearrange("p (c t) -> p c t", c=8)
                ins = None
                for c in range(8):
                    ins = e.transpose(o[:, c, :], xs5[b][:, c * P:(c + 1) * P], ident_bf[:])
                return ins
            sch.add("pe", tr5, reads=[("xs5", b), "ident_bf"], writes=[psk(tb)])
            sch.add("act", lambda e, tb=tb, tsl=tsl: e.activation(
                out=h2T[:, :, tsl], in_=ps[tb][:].bitcast(BF16).rearrange("p (c t) -> p c t", c=8), func=AF.Copy),
                reads=[psk(tb)], writes=[("h2T", T)])
            lbk = next_bank(6, 8)

            def rmm(e, T=T, lbk=lbk):
                ins = None
                for c in range(8):
                    ins = e.matmul(ps[lbk][:, 0:36], lhsT=h2T[:, c, T * P:(T + 1) * P], rhs=wrb[:, c, :], start=(c == 0), stop=(c == 7))
                return ins
            sch.add("pe", rmm, reads=[("h2T", T), "wrb"], writes=[psk(lbk)])
            sch.add("dve", lambda e, T=T, lbk=lbk: e.tensor_tensor(out=logit[:, T, :], in0=ps[lbk][:, 0:36], in1=brbc[:], op=ALU.add),
                    reads=[psk(lbk), "brbc"], writes=["logit"])
        pend5.append(back5)
        while len(pend5) > 1:
            pend5.pop(0)()
    while pend5:
        pend5.pop(0)()
    dump("x1", x1, dbg.get("x1").rearrange("(t p) c -> p t c", p=P) if "x1" in dbg else None,
         [("x1", T, hh) for T in range(NT) for hh in range(2)])
    if STOP_AFTER == "x1":
        return finish()

    gl = logit[:, :, 0:4]
    el = logit[:, :, 4:36].rearrange("p t (g e) -> p t g e", g=4)
    _ro = [101 * KB]

    def rv(shape):
        nb = int(np.prod(shape[1:])) * 4
        v = view(R4, _ro[0], shape, F32)
        _ro[0] += nb
        return v
    gmax = rv([P, NT])
    goh = rv([P, NT, 4])
    gsh = rv([P, NT, 4])
    gsum = rv([P, NT])
    gw = rv([P, NT])
    etmp = rv([P, NT, 4, 8])
    esel = rv([P, NT, 8])
    esel2 = rv([P, NT, 8])
    m1 = rv([P, NT])
    m2 = rv([P, NT])
    oh1 = rv([P, NT, 8])
    oh2 = rv([P, NT, 8])
    dd = rv([P, NT])
    w1 = rv([P, NT])
    w2 = rv([P, NT])
    gsel = rv([P, NT, 8])
    assert _ro[0] <= 110 * KB
    gates = view(R4, 110 * KB, [P, NT, 4, 8], F32)

    def D_(fn, reads, writes):
        sch.add("dve", fn, reads=reads, writes=writes)

    def bc3(ap2, n):
        return ap2.unsqueeze(2).to_broadcast([P, NT, n])
    D_(lambda e: e.tensor_reduce(out=gmax[:], in_=gl, axis=AX.X, op=ALU.max), ["logit"], ["gmax"])
    D_(lambda e: e.tensor_tensor(out=goh[:], in0=gl, in1=bc3(gmax[:, :], 4), op=ALU.is_equal), ["logit", "gmax"], ["goh"])
    D_(lambda e: e.tensor_tensor(out=gsh[:], in0=gl, in1=bc3(gmax[:, :], 4), op=ALU.subtract), ["logit", "gmax"], ["gsh"])
    sch.add("act", lambda e: e.activation(out=gsh[:], in_=gsh[:], func=AF.Exp), reads=["gsh"], writes=["gsh"])
    D_(lambda e: e.tensor_reduce(out=gsum[:], in_=gsh[:], axis=AX.X, op=ALU.add), ["gsh"], ["gsum"])
    D_(lambda e: e.reciprocal(out=gw[:], in_=gsum[:]), ["gsum"], ["gw"])
    D_(lambda e: e.tensor_tensor(out=etmp[:], in0=el, in1=goh[:, :, :].unsqueeze(3).to_broadcast([P, NT, 4, 8]), op=ALU.mult),
       ["logit", "goh"], ["etmp"])
    D_(lambda e: e.tensor_reduce(out=esel[:], in_=etmp[:, :, :, :].rearrange("p t g e -> p t e g"), axis=AX.X, op=ALU.add), ["etmp"], ["esel"])
    D_(lambda e: e.tensor_reduce(out=m1[:], in_=esel[:], axis=AX.X, op=ALU.max), ["esel"], ["m1"])
    D_(lambda e: e.tensor_tensor(out=oh1[:], in0=esel[:], in1=bc3(m1[:, :], 8), op=ALU.is_equal), ["esel", "m1"], ["oh1"])
    D_(lambda e: e.scalar_tensor_tensor(out=esel2[:], in0=oh1[:], scalar=-1e30, in1=esel[:], op0=ALU.mult, op1=ALU.add), ["oh1", "esel"], ["esel2"])
    D_(lambda e: e.tensor_reduce(out=m2[:], in_=esel2[:], axis=AX.X, op=ALU.max), ["esel2"], ["m2"])
    D_(lambda e: e.tensor_tensor(out=oh2[:], in0=esel2[:], in1=bc3(m2[:, :], 8), op=ALU.is_equal), ["esel2", "m2"], ["oh2"])
    D_(lambda e: e.tensor_tensor(out=dd[:], in0=m2[:], in1=m1[:], op=ALU.subtract), ["m1", "m2"], ["dd"])
    sch.add("act", lambda e: e.activation(out=dd[:], in_=dd[:], func=AF.Exp), reads=["dd"], writes=["dd"])
    D_(lambda e: e.tensor_scalar(out=w1[:], in0=dd[:], scalar1=1.0, scalar2=None, op0=ALU.add), ["dd"], ["w1"])
    D_(lambda e: e.reciprocal(out=w1[:], in_=w1[:]), ["w1"], ["w1"])
    D_(lambda e: e.tensor_tensor(out=w2[:], in0=dd[:], in1=w1[:], op=ALU.mult), ["dd", "w1"], ["w2"])
    D_(lambda e: e.tensor_tensor(out=w1[:], in0=w1[:], in1=gw[:], op=ALU.mult), ["w1", "gw"], ["w1"])
    D_(lambda e: e.tensor_tensor(out=w2[:], in0=w2[:], in1=gw[:], op=ALU.mult), ["w2", "gw"], ["w2"])
    D_(lambda e: e.tensor_tensor(out=oh1[:], in0=oh1[:], in1=bc3(w1[:, :], 8), op=ALU.mult), ["oh1", "w1"], ["oh1"])
    D_(lambda e: e.tensor_tensor(out=oh2[:], in0=oh2[:], in1=bc3(w2[:, :], 8), op=ALU.mult), ["oh2", "w2"], ["oh2"])
    D_(lambda e: e.tensor_tensor(out=gsel[:], in0=oh1[:], in1=oh2[:], op=ALU.add), ["oh1", "oh2"], ["gsel"])
    D_(lambda e: e.tensor_tensor(out=gates[:], in0=goh[:, :, :].unsqueeze(3).to_broadcast([P, NT, 4, 8]),
                                 in1=gsel[:, :, :].unsqueeze(2).to_broadcast([P, NT, 4, 8]), op=ALU.mult), ["goh", "gsel"], ["gates"])
    dump("gates", gates[:, :, :, :].rearrange("p t g e -> p t (g e)"),
         dbg.get("gates").rearrange("(t p) c -> p t c", p=P) if "gates" in dbg else None, ["gates"])
    if STOP_AFTER == "router":
        return finish()
    sch.barrier()

    hid = [view(R4, 64 * KB + i * 8 * KB, [P, 2, S], BF16) for i in range(2)]
    gT = view(R4, 80 * KB, [32, 2, S], BF16, parts=32)
    onehot = view(R4, 88 * KB, [32, 32, 128], BF16, parts=32)
    sa = [view(R4, 96 * KB + i * 2 * KB, [P, 512], F32) for i in range(2)]
    t1 = [view(R4, 100 * KB + i * 2 * KB, [P, 512], F32) for i in range(2)]
    wbuf = []
    for RR in (R2, R3):
        wbuf.append((view(RR, 0, [P, 8, 256], BF16), view(RR, 4 * KB, [P, 8, 256], BF16), view(RR, 8 * KB, [P, 2, D], BF16)))
    sch.add("sp", lambda e: e.dma_start(out=onehot, in_=onehot_d.rearrange("e (k m) -> e k m", k=32)), writes=["onehot"], dma=True)
    for T4 in range(4):
        tb = next_bank(0, 4)

        def trg(e, T4=T4, tb=tb):
            ins = None
            for k in range(4):
                T = T4 * 4 + k
                ins = e.transpose(ps[tb][0:32, k * 128:(k + 1) * 128], gates[:, T, :, :].rearrange("p g e -> p (g e)"), ident_f[:])
            return ins
        sch.add("pe", trg, reads=["gates", "ident_f"], writes=[psk(tb)])
        sl = slice(T4 * 512, (T4 + 1) * 512)
        sch.add("act", lambda e, tb=tb, sl=sl: e.activation(out=gT[0:32, 0, sl], in_=ps[tb][0:32, :], func=AF.Copy),
                reads=[psk(tb)], writes=[("gThi", T4), psk(tb)])
        sch.add("dve", lambda e, tb=tb, sl=sl: e.tensor_tensor(out=gT[0:32, 1, sl], in0=ps[tb][0:32, :], in1=gT[0:32, 0, sl], op=ALU.subtract),
                reads=[psk(tb), ("gThi", T4)], writes=[("gTlo", T4), psk(tb)])
    mcnt = [0]
    for pr in range(NE // 2):
      for ex in (2 * pr, 2 * pr + 1):
        wbi = ex % 2
        Wg, Wu, Wd = wbuf[wbi]
        sch.add("pool", lambda e, Wg=Wg, ex=ex: e.dma_start(out=Wg, in_=wg_d[ex].rearrange("(c p) f -> p c f", p=P)),
                writes=[("Wg", wbi)], dma=True)
        sch.add("pool", lambda e, Wu=Wu, ex=ex: e.dma_start(out=Wu, in_=wu_d[ex].rearrange("(c p) f -> p c f", p=P)),
                writes=[("Wu", wbi)], dma=True)
        for hh in range(2):
            sch.add("pool", lambda e, Wd=Wd, ex=ex, hh=hh: e.dma_start(
                out=Wd[:, :, hh * 512:(hh + 1) * 512], in_=wd_d[ex][:, hh * 512:(hh + 1) * 512].rearrange("(c p) n -> p c n", p=P)),
                writes=[("Wd", wbi, hh)], dma=True)
        hb = ex % 2
        for tc in range(4):
            sl = slice(tc * 512, (tc + 1) * 512)
            gb = mcnt[0] % 2
            mcnt[0] += 1

            def gmm(e, ex=ex, gb=gb, sl=sl):
                e.matmul(ps[gb][:], lhsT=onehot[0:32, ex, :], rhs=gT[0:32, 0, sl], start=True, stop=False)
                return e.matmul(ps[gb][:], lhsT=onehot[0:32, ex, :], rhs=gT[0:32, 1, sl], start=False, stop=True)
            sch.add("pe", gmm, reads=["onehot", ("gThi", tc), ("gTlo", tc)], writes=[psk(gb)])
            for ft in range(2):
                ab = 2 + (mcnt[0] % 2)
                ub = 4 + (mcnt[0] % 2)
                tb_ = mcnt[0] % 2
                mcnt[0] += 1

                def amm(e, Wg=Wg, ft=ft, sl=sl, ab=ab):
                    ins = None
                    for c in range(8):
                        ins = e.matmul(ps[ab][:], lhsT=Wg[:, c, ft * 128:(ft + 1) * 128], rhs=h2T[:, c, sl], start=(c == 0), stop=(c == 7))
                    return ins

                def umm2(e, Wu=Wu, ft=ft, sl=sl, ub=ub):
                    ins = None
                    for c in range(8):
                        ins = e.matmul(ps[ub][:], lhsT=Wu[:, c, ft * 128:(ft + 1) * 128], rhs=h2T[:, c, sl], start=(c == 0), stop=(c == 7))
                    return ins
                hkeys = [("h2T", tc * 4 + k) for k in range(4)]
                sch.add("pe", amm, reads=[("Wg", wbi)] + hkeys, writes=[psk(ab)])
                sch.add("pe", umm2, reads=[("Wu", wbi)] + hkeys, writes=[psk(ub)])
                sch.add("act", lambda e, ab=ab, tb_=tb_: e.activation(out=sa[tb_], in_=ps[ab][:], func=AF.Silu),
                        reads=[psk(ab)], writes=[("sa", tb_), psk(ab)])
                sch.add("dve", lambda e, ub=ub, tb_=tb_: e.tensor_tensor(out=t1[tb_], in0=ps[ub][:], in1=sa[tb_], op=ALU.mult),
                        reads=[psk(ub), ("sa", tb_)], writes=[("t1", tb_), psk(ub)])
                sch.add("dve", lambda e, gb=gb, tb_=tb_, hb=hb, ft=ft, sl=sl: e.tensor_tensor(out=hid[hb][:, ft, sl], in0=ps[gb][:], in1=t1[tb_],
                                                                                             op=ALU.mult),
                        reads=[psk(gb), ("t1", tb_)], writes=[("hid", hb, tc), psk(gb)])
      WdA, WdB = wbuf[0][2], wbuf[1][2]
      for T in range(NT):
            for hh in range(2):
                yb = 6 + (mcnt[0] % 2)
                mcnt[0] += 1

                def dmm(e, T=T, hh=hh, yb=yb):
                    ins = None
                    k = 0
                    for (hd, Wd_) in ((hid[0], WdA), (hid[1], WdB)):
                        for ft in range(2):
                            ins = e.matmul(ps[yb][:], lhsT=hd[:, ft, T * P:(T + 1) * P], rhs=Wd_[:, ft, hh * 512:(hh + 1) * 512],
                                           start=(k == 0), stop=(k == 3))
                            k += 1
                    return ins
                sch.add("pe", dmm, reads=[("hid", 0, T // 4), ("hid", 1, T // 4), ("Wd", 0, hh), ("Wd", 1, hh)], writes=[psk(yb)])
                sch.add("dve", lambda e, T=T, hh=hh, yb=yb: e.tensor_tensor(out=x1[:, T, hh * 512:(hh + 1) * 512], in0=ps[yb][:],
                                                                           in1=x1[:, T, hh * 512:(hh + 1) * 512], op=ALU.add),
                        reads=[psk(yb), ("x1", T, hh)], writes=[("x1", T, hh), psk(yb)])
    for T in range(NT):
        tsl = slice(T * P, (T + 1) * P)
        sch.add("sp", lambda e, T=T, tsl=tsl: e.dma_start(out=out_d[tsl, :], in_=x1[:, T, :]), reads=[("x1", T, 0), ("x1", T, 1)],
                writes=["out_%d" % T], dma=True)
    return finish()


_CACHE = {}


def _prep_shared(inputs):
    f32 = np.float32
    c = host_constants()
    w_in = np.asarray(inputs["w_in"][0], f32)
    shared = {}
    shared["w1"] = np.ascontiguousarray(w_in[:, W1_COLS])
    shared["wout"] = np.ascontiguousarray(np.asarray(inputs["w_out"][0], f32))
    shared["wr"] = np.ascontiguousarray(np.concatenate([np.asarray(inputs["w_router_group"][0], f32),
                                                        np.asarray(inputs["w_router_expert"][0], f32)], axis=1))
    shared["wg"] = np.ascontiguousarray(np.asarray(inputs["w_exp_gate"][0], f32))
    shared["wu"] = np.ascontiguousarray(np.asarray(inputs["w_exp_up"][0], f32))
    shared["wd"] = np.ascontiguousarray(np.asarray(inputs["w_exp_down"][0], f32))
    shared["w2aug"] = np.ascontiguousarray(np.concatenate([np.asarray(inputs["gla_gate_w2"][0], f32),
                                                           np.asarray(inputs["gla_gate_b"], f32).reshape(1, 256)], axis=0))
    shared["g1bc"] = np.ascontiguousarray(np.tile(np.asarray(inputs["norm1_g"], f32).reshape(1, D), (P, 1)))
    shared["g2bc"] = np.ascontiguousarray(np.tile(np.asarray(inputs["norm2_g"], f32).reshape(1, D), (P, 1)))
    qg = np.tile(np.asarray(inputs["q_norm_g"], f32).reshape(64), 2)
    kg = np.tile(np.asarray(inputs["k_norm_g"], f32).reshape(64), 2)
    shared["qkg"] = np.ascontiguousarray(np.stack([qg, kg], axis=1))
    shared["goutbc"] = np.ascontiguousarray(np.tile(np.asarray(inputs["gla_out_norm_g"], f32).reshape(1, 128), (P, 4)))
    br = np.concatenate([np.asarray(inputs["b_router_group"], f32).reshape(4),
                         np.asarray(inputs["b_router_expert"], f32).reshape(32)])
    shared["brbc"] = np.ascontiguousarray(np.tile(br.reshape(1, 36), (P, 1)))
    rb = np.asarray(inputs["rel_bias"], f32)
    idx = np.arange(128)
    bT = np.zeros((P, 8, 2, 128), f32)
    for dlt in range(2):
        dist = 128 * dlt + idx[None, :] - idx[:, None]
        bk = _t5_bucket_np(np.maximum(dist, 0))
        for h in range(8):
            bT[:, h, dlt, :] = rb[bk, h]
    shared["biasT"] = np.ascontiguousarray(bT.reshape(P, -1))
    shared["rb31"] = np.ascontiguousarray(np.tile(rb[31:32, :], (P, 1)))
    for k in ["ident_bf", "ident_f", "tri_bf", "tri_f", "after_f", "negmask", "blockones", "pow2"]:
        shared[k] = c[k]
    shared["onehot"] = np.ascontiguousarray(c["onehot"].reshape(32, -1))
    return shared


def kernel(**inputs):
    x = np.asarray(inputs["x"], np.float32)
    if "nc" not in _CACHE:
        _CACHE["nc"] = build_program()
    nc = _CACHE["nc"]
    shared = _prep_shared(inputs)
    in_maps = []
    for b in range(8):
        m = dict(shared)
        m["x"] = np.ascontiguousarray(x[b])
        in_maps.append(m)
    res = run_bass_kernel_spmd(nc, in_maps, core_ids=list(range(8)))
    _CACHE["last"] = res
    out = np.stack([np.asarray(r["out"], np.float32) for r in res.results], axis=0)
    return out
```

```python
import os
import numpy as np
import ml_dtypes
from contextlib import ExitStack
import concourse.bass as bass
import concourse.mybir as mybir
from concourse.bass_utils import run_bass_kernel_spmd

F32 = mybir.dt.float32
BF16 = mybir.dt.bfloat16
U8 = mybir.dt.uint8
AF = mybir.ActivationFunctionType
ALU = mybir.AluOpType
AX = mybir.AxisListType

P = 128
S = 2048
D = 1024
NT = 16
NE = 32
EPS = 1e-6
KITER = 18
DEBUG = {}
STOP_AFTER = None


class _Op:
    __slots__ = ("eng", "fn", "reads", "writes", "dma", "deps", "waits", "signal", "idx", "flag", "slotwait", "after")

    def __init__(self, eng, fn, reads, writes, dma):
        self.eng = eng
        self.fn = fn
        self.reads = reads
        self.writes = writes
        self.dma = dma
        self.deps = ()
        self.waits = []
        self.signal = None
        self.flag = False
        self.slotwait = None
        self.after = []


def _nofn(e):
    return None


class Sched:
    EPOCH = 12000
    RING = 8

    def __init__(self, nc, stack):
        self.nc = nc
        self.stack = stack
        self.ops = []

    def add(self, eng, fn, reads=(), writes=(), dma=False):
        reads = list(reads)
        writes = list(writes)
        for k in reads:
            if isinstance(k, tuple) and k and k[0] == "ps" and k not in writes:
                writes.append(k)
        self.ops.append(_Op(eng, fn, tuple(reads), tuple(writes), dma))

    def barrier(self):
        pos = getattr(self, "_barpos", 0)
        last = {}
        dmas = []
        for i, op in enumerate(self.ops):
            if i < pos:
                continue
            if op.dma:
                dmas.append(op)
            elif op.fn is not _nofn:
                last[op.eng] = op
        for eng in ("pe", "act", "dve", "pool", "sp"):
            b = _Op(eng, _nofn, (), (), False)
            b.after = [p for k, p in last.items() if k != eng] + dmas
            self.ops.append(b)
        self._barpos = len(self.ops)

    def finalize(self):
        nc = self.nc
        last_w = {}
        readers = {}
        for i, op in enumerate(self.ops):
            op.idx = i
            raw = set()
            other = set()
            for k in op.reads:
                if k in last_w:
                    raw.add(last_w[k])
            for k in op.writes:
                if k in last_w:
                    raw.add(last_w[k])
                for r in readers.get(k, ()):
                    other.add(r)
            raw.discard(i)
            other.discard(i)
            for k in op.reads:
                readers.setdefault(k, set()).add(i)
            for k in op.writes:
                last_w[k] = i
                readers[k] = set()
            best = {}
            deps = []
            for d in raw | other:
                p = self.ops[d]
                if p.dma:
                    deps.append(d)
                    continue
                if p.eng == op.eng and not op.dma:
                    if p.eng == "pe":
                        continue
                    if d not in raw:
                        continue
                if p.eng not in best or best[p.eng] < d:
                    best[p.eng] = d
            deps.extend(best.values())
            for a in op.after:
                deps.append(a.idx)
            op.deps = deps
            for d in deps:
                self.ops[d].flag = True
        cnt = {}
        self.sems = {}
        dcount = {}
        for op in self.ops:
            if op.dma:
                q = op.eng
                k = dcount.get(q, 0)
                dcount[q] = k + 1
                slot = k % self.RING
                name = "dq_%s_%d" % (q, slot)
                if name not in self.sems:
                    self.sems[name] = self.stack.enter_context(nc.semaphore(name))
                op.signal = (name, 16 * (k // self.RING + 1), 16)
                if k >= self.RING:
                    op.slotwait = (name, 16 * (k // self.RING))
            elif op.flag:
                c = cnt.get(op.eng, 0)
                ep = c // self.EPOCH
                name = "s_%s_%d" % (op.eng, ep)
                if name not in self.sems:
                    self.sems[name] = self.stack.enter_context(nc.semaphore(name))
                op.signal = (name, c % self.EPOCH + 1, 1)
                cnt[op.eng] = c + 1
        for op in self.ops:
            w = []
            if op.slotwait is not None:
                w.append(op.slotwait)
            for d in op.deps:
                sg = self.ops[d].signal
                w.append((sg[0], sg[1]))
            op.waits = w

    def emit(self, block):
        table = [("pe", block.tensor), ("act", block.scalar), ("dve", block.vector),
                 ("pool", block.gpsimd), ("sp", block.sync)]
        for engname, deco in table:
            ops = [op for op in self.ops if op.eng == engname]

            def body(e, ops=ops):
                seen = {}
                for op in ops:
                    for (sn, val) in op.waits:
                        if seen.get(sn, 0) >= val:
                            continue
                        e.wait_ge(self.sems[sn], val)
                        seen[sn] = val
                    ins = op.fn(e)
                    if op.signal is not None and ins is not None:
                        ins.then_inc(self.sems[op.signal[0]], op.signal[2])

            deco(body)


IQ_TILES = [(0, 3), (3, 6), (6, 8)]


def _t5_bucket_np(dist):
    max_exact = 16
    d_f = np.maximum(dist, 1).astype(np.float32)
    large = max_exact + (np.log(d_f / max_exact) / np.log(128 / max_exact) * (32 - max_exact)).astype(np.int32)
    large = np.minimum(large, 31)
    return np.where(dist < max_exact, dist, large)


def _w1_columns():
    A = 512
    off = {}
    o = 0
    for name, n in [("qa", 512), ("ka", 512), ("va", 512), ("iq", 256), ("ik", 32), ("iw", 8),
                    ("qb", 256), ("kb", 256), ("vb", 512), ("glr", 16), ("rg", 512)]:
        off[name] = o
        o += n
    cols = []
    groups = []

    def grp(name, kind, cl):
        groups.append((name, kind, len(cols), len(cl)))
        cols.extend(cl)

    r = lambda name, a, b: list(range(off[name] + a, off[name] + b))
    grp("qbkb", "fm", r("qb", 0, 256) + r("kb", 0, 256) + r("glr", 0, 16))
    grp("vb", "tm", r("vb", 0, 512))
    grp("rg", "tm", r("rg", 0, 512))
    grp("kbiw", "tm", r("kb", 0, 256) + r("iw", 0, 8))
    grp("qa", "fm", r("qa", 0, 512))
    grp("ka", "fm", r("ka", 0, 512))
    iqc = []
    for (h0, h1) in IQ_TILES:
        iqc += r("iq", h0 * 32, h1 * 32)
    grp("iqik", "fm", iqc + r("ik", 0, 32) * 3)
    grp("va", "tm", r("va", 0, 512))
    return np.array(cols, np.int64), groups


W1_COLS, W1_GROUPS = _w1_columns()
NW1 = len(W1_COLS)


def _selT_off(i):
    return sum((16 - ii) * 128 for ii in range(i))


SELT_TOTAL = _selT_off(16)


def host_constants():
    c = {}
    idx = np.arange(128)
    tri = (idx[:, None] <= idx[None, :])
    c["ident_bf"] = np.eye(128, dtype=np.float32).astype(ml_dtypes.bfloat16)
    c["ident_f"] = np.eye(128, dtype=np.float32)
    c["tri_bf"] = tri.astype(np.float32).astype(ml_dtypes.bfloat16)
    c["tri_f"] = tri.astype(np.float32)
    c["after_f"] = (idx[:, None] > idx[None, :]).astype(np.float32)
    c["negmask"] = np.where(idx[None, :] <= idx[:, None], 0.0, -1e30).astype(np.float32)
    bo = np.zeros((128, 128), np.float32)
    bo[:64, :64] = 1.0
    bo[64:, 64:] = 1.0
    c["blockones"] = bo.astype(ml_dtypes.bfloat16)
    c["pow2"] = np.tile((2.0 ** -np.arange(KITER + 1, dtype=np.float64)).astype(np.float32)[None, :], (128, 1))
    oh = np.zeros((32, 32, 128), np.float32)
    for e in range(32):
        oh[e, e, :] = 1.0
    c["onehot"] = oh.astype(ml_dtypes.bfloat16)
    return c


def build_program():
    nc = bass.Bass("TRN2", target_bir_lowering=False)
    stack = ExitStack()
    sch = Sched(nc, stack)

    def dram(name, shape, dt, kind="ExternalInput"):
        return nc.dram_tensor(name, list(shape), dt, kind=kind).ap()

    x_d = dram("x", [S, D], F32)
    w1_d = dram("w1", [D, NW1], F32)
    wout_d = dram("wout", [D, D], F32)
    wr_d = dram("wr", [D, 36], F32)
    wg_d = dram("wg", [NE, D, 256], F32)
    wu_d = dram("wu", [NE, D, 256], F32)
    wd_d = dram("wd", [NE, 256, D], F32)
    w2_d = dram("w2aug", [17, 256], F32)
    g1bc_d = dram("g1bc", [P, D], F32)
    g2bc_d = dram("g2bc", [P, D], F32)
    qkg_d = dram("qkg", [P, 2], F32)
    gout_d = dram("goutbc", [P, 512], F32)
    brbc_d = dram("brbc", [P, 36], F32)
    biasT_d = dram("biasT", [P, 8 * 2 * 128], F32)
    rb31_d = dram("rb31", [P, 8], F32)
    ident_bf_d = dram("ident_bf", [P, P], BF16)
    ident_f_d = dram("ident_f", [P, P], F32)
    tri_bf_d = dram("tri_bf", [P, P], BF16)
    tri_f_d = dram("tri_f", [P, P], F32)
    after_f_d = dram("after_f", [P, P], F32)
    negmask_d = dram("negmask", [P, P], F32)
    blockones_d = dram("blockones", [P, P], BF16)
    pow2_d = dram("pow2", [P, KITER + 1], F32)
    onehot_d = dram("onehot", [32, 32 * 128], BF16)
    out_d = dram("out", [S, D], F32, kind="ExternalOutput")
    dbg = {}
    for name, (shape, dt) in DEBUG.items():
        dbg[name] = dram("dbg_" + name, shape, dt, kind="ExternalOutput")

    def sb(name, shape, dt):
        return stack.enter_context(nc.sbuf_tensor(name, list(shape), dt))

    ident_bf = sb("ident_bf_s", [P, P], BF16)
    ident_f = sb("ident_f_s", [P, P], F32)
    tri_bf = sb("tri_bf_s", [P, P], BF16)
    tri_f = sb("tri_f_s", [P, P], F32)
    after_f = sb("after_f_s", [P, P], F32)
    negmask = sb("negmask_s", [P, P], F32)
    blockones = sb("blockones_s", [P, P], BF16)
    pow2 = sb("pow2_s", [P, KITER + 1], F32)
    g1bc = sb("g1bc_s", [P, D], F32)
    qkg = sb("qkg_s", [P, 2], F32)
    gout = sb("gout_s", [P, 512], F32)
    brbc = sb("brbc_s", [P, 36], F32)
    rb31 = sb("rb31_s", [P, 8], F32)
    Etile = sb("Etile", [P, 8, 2, 128], BF16)
    w2aug = sb("w2aug_s", [17, 256], BF16)
    iw_s = sb("iw_s", [P, NT, 8], F32)
    ssq1 = sb("ssq1", [P, NT], F32)
    rstd1 = sb("rstd1", [P, NT], F32)
    ones_col = sb("ones_col", [P, 1], F32)

    R1 = sb("R1", [P, 32 * 1024], U8)
    R2 = sb("R2", [P, 16 * 1024], U8)
    R3 = sb("R3", [P, 16 * 1024], U8)
    R4 = sb("R4", [P, 112 * 1024], U8)

    def view(arena, off, shape, dt, parts=P):
        nb = int(np.prod(shape[1:])) * (4 if dt == F32 else (2 if dt == BF16 else 1))
        ap = arena[0:parts, off:off + nb].bitcast(dt)
        if len(shape) == 3:
            ap = ap.rearrange("p (a b) -> p a b", a=shape[1])
        elif len(shape) == 4:
            ap = ap.rearrange("p (a b c) -> p a b c", a=shape[1], b=shape[2])
        return ap

    KB = 1024
    hT = view(R1, 0, [P, 8, S], BF16)
    mixT_b = view(R2, 0, [P, 4, S], BF16)
    mixT_a = view(R3, 0, [P, 4, S], BF16)
    qbT = view(R4, 0, [P, 2, S], BF16)
    kbT = view(R4, 8 * KB, [P, 2, S], BF16)
    glrT = view(R4, 16 * KB, [32, S], BF16, parts=32)
    vb = view(R4, 20 * KB, [P, NT, 512], BF16)
    Gt = view(R4, 36 * KB, [P, NT, 512], BF16)
    kbtm = view(R4, 52 * KB, [P, NT, 256], BF16)
    wst = [view(R4, 60 * KB + i * 9 * KB, [P, 8, 528], BF16) for i in range(2)]
    xst = [view(R4, 78 * KB + i * 4 * KB, [P, D], F32) for i in range(2)]
    xs = [view(R4, 86 * KB + i * 2 * KB, [P, D], BF16) for i in range(2)]
    junk = view(R4, 90 * KB, [P, 2048], BF16)
    tmpA = [view(R4, 94 * KB + i * 2 * KB, [P, 512], F32) for i in range(4)]
    glatmp = view(R4, 102 * KB, [P, 10 * 256], F32)

    ps = [stack.enter_context(nc.psum_tensor("ps%d" % i, [P, 512], F32)) for i in range(8)]

    def psk(i):
        return ("ps", i)

    def load_const(dst, src, key, eng="sp"):
        sch.add(eng, lambda e, dst=dst, src=src: e.dma_start(out=dst, in_=src), writes=[key], dma=True)

    load_const(ident_bf[:], ident_bf_d[:, :], "ident_bf")
    load_const(ident_f[:], ident_f_d[:, :], "ident_f")
    load_const(tri_bf[:], tri_bf_d[:, :], "tri_bf")
    load_const(tri_f[:], tri_f_d[:, :], "tri_f")
    load_const(after_f[:], after_f_d[:, :], "after_f")
    load_const(negmask[:], negmask_d[:, :], "negmask")
    load_const(blockones[:], blockones_d[:, :], "blockones")
    load_const(pow2[:], pow2_d[:, :], "pow2")
    load_const(g1bc[:], g1bc_d[:, :], "g1bc")
    load_const(qkg[:], qkg_d[:, :], "qkg")
    load_const(gout[:], gout_d[:, :], "gout")
    load_const(brbc[:], brbc_d[:, :], "brbc")
    load_const(rb31[:], rb31_d[:, :], "rb31")
    load_const(w2aug[:], w2_d[:, :], "w2aug", eng="pool")
    sch.add("dve", lambda e: e.memset(ones_col[:], 1.0), writes=["ones_col"])
    sch.add("dve", lambda e: e.memset(glrT[0:32, :], 1.0), writes=["glrT_init"])

    for T in range(NT):
        b = T % 2
        tsl = slice(T * P, (T + 1) * P)
        sch.add("sp", lambda e, b=b, tsl=tsl: e.dma_start(out=xst[b], in_=x_d[tsl, :]),
                writes=[("xst", b)], dma=True)
        sch.add("act", lambda e, b=b, T=T: e.activation(out=junk[:, 0:D], in_=xst[b], func=AF.Square,
                                                        accum_out=ssq1[:, T:T + 1]),
                reads=[("xst", b)], writes=["junk", ("ssq1", T)])
        sch.add("act", lambda e, T=T: e.activation(out=rstd1[:, T:T + 1], in_=ssq1[:, T:T + 1], func=AF.Sqrt,
                                                   scale=1.0 / D, bias=eps_col[:, 0:1]),
                reads=[("ssq1", T), "eps_col"], writes=[("rstd1", T)])
        sch.add("dve", lambda e, T=T: e.reciprocal(out=rstd1[:, T:T + 1], in_=rstd1[:, T:T + 1]),
                reads=[("rstd1", T)], writes=[("rstd1", T)])
        sch.add("dve", lambda e, b=b, T=T: e.scalar_tensor_tensor(out=xs[b], in0=xst[b], scalar=rstd1[:, T:T + 1],
                                                                  in1=g1bc[:], op0=ALU.mult, op1=ALU.mult),
                reads=[("xst", b), ("rstd1", T), "g1bc"], writes=[("xs", b)])
        pb = T % 2

        def tr(e, b=b, pb=pb):
            o = ps[pb][:].bitcast(BF16).rearrange("p (c t) -> p c t", c=8)
            ins = None
            for c in range(8):
                ins = e.transpose(o[:, c, :], xs[b][:, c * P:(c + 1) * P], ident_bf[:])
            return ins
        sch.add("pe", tr, reads=[("xs", b), "ident_bf"], writes=[psk(pb)])
        sch.add("act", lambda e, pb=pb, tsl=tsl: e.activation(
            out=hT[:, :, tsl], in_=ps[pb][:].bitcast(BF16).rearrange("p (c t) -> p c t", c=8), func=AF.Copy),
            reads=[psk(pb)], writes=[("hT", T)])

    wcount = [0]

    def load_wgroup(gi):
        name, kind, c0, n = W1_GROUPS[gi]
        b = wcount[0] % 2
        wcount[0] += 1
        src = w1_d[:, c0:c0 + n].rearrange("(c p) n -> p c n", p=P)
        dst = wst[b][:, :, 0:n]
        sch.add("pool", lambda e, dst=dst, src=src: e.dma_start(out=dst, in_=src),
                writes=[("wst", b)], dma=True)
        return b

    bankrot = {}

    def next_bank(lo=2, hi=6):
        k = (lo, hi)
        c = bankrot.get(k, 0)
        bankrot[k] = c + 1
        return lo + c % (hi - lo)

    def fm_matmul(wb, col0, m, tc, bank, parts=None):
        wt = wst[wb]

        def fn(e):
            ins = None
            for c in range(8):
                ins = e.matmul(ps[bank][0:m, :], lhsT=wt[:, c, col0:col0 + m],
                               rhs=hT[:, c, tc * 512:(tc + 1) * 512], start=(c == 0), stop=(c == 7))
            return ins
        sch.add("pe", fn, reads=[("wst", wb)] + [("hT", tc * 4 + k) for k in range(4)], writes=[psk(bank)])

    def tm_matmul(wb, col0, n, T, bank):
        wt = wst[wb]

        def fn(e):
            ins = None
            for c in range(8):
                ins = e.matmul(ps[bank][:, 0:n], lhsT=hT[:, c, T * P:(T + 1) * P],
                               rhs=wt[:, c, col0:col0 + n], start=(c == 0), stop=(c == 7))
            return ins
        sch.add("pe", fn, reads=[("wst", wb), ("hT", T)], writes=[psk(bank)])

    eps_col = sb("eps_col", [P, 1], F32)
    sch.ops.insert(0, _Op("dve", lambda e: e.memset(eps_col[:], EPS), (), ("eps_col",), False))

    wb = load_wgroup(0)
    for tc in range(4):
        tsl = slice(tc * 512, (tc + 1) * 512)
        for k in range(4):
            bank = next_bank()
            fm_matmul(wb, k * 128, 128, tc, bank)
            dst = (qbT if k < 2 else kbT)[:, k % 2, tsl]
            key = ("qbT" if k < 2 else "kbT", tc)
            eng = "act" if k % 2 == 0 else "dve"
            if eng == "act":
                sch.add("act", lambda e, dst=dst, bank=bank: e.activation(out=dst, in_=ps[bank][:], func=AF.Copy),
                        reads=[psk(bank)], writes=[key + (k % 2,)])
            else:
                sch.add("dve", lambda e, dst=dst, bank=bank: e.tensor_copy(out=dst, in_=ps[bank][:]),
                        reads=[psk(bank)], writes=[key + (k % 2,)])
        bank = next_bank()
        fm_matmul(wb, 512, 16, tc, bank)
        sch.add("dve", lambda e, tsl=tsl, bank=bank: e.tensor_copy(out=glrT[0:16, tsl], in_=ps[bank][0:16, :]),
                reads=[psk(bank), "glrT_init"], writes=[("glrT", tc)])
    wb = load_wgroup(1)
    for T in range(NT):
        bank = next_bank()
        tm_matmul(wb, 0, 512, T, bank)
        eng = "act" if T % 2 == 0 else "dve"
        if eng == "act":
            sch.add("act", lambda e, T=T, bank=bank: e.activation(out=vb[:, T, :], in_=ps[bank][:], func=AF.Copy),
                    reads=[psk(bank)], writes=[("vb", T)])
        else:
            sch.add("dve", lambda e, T=T, bank=bank: e.tensor_copy(out=vb[:, T, :], in_=ps[bank][:]),
                    reads=[psk(bank)], writes=[("vb", T)])
    wb = load_wgroup(2)
    for T in range(NT):
        bank = next_bank()
        tm_matmul(wb, 0, 512, T, bank)
        tb = T % 2
        sch.add("act", lambda e, tb=tb, bank=bank: e.activation(out=tmpA[tb][:], in_=ps[bank][:], func=AF.Silu),
                reads=[psk(bank)], writes=[("tmpA", tb)])
        sch.add("pool", lambda e, tb=tb, T=T: e.tensor_tensor(out=Gt[:, T, :], in0=tmpA[tb][:], in1=gout[:], op=ALU.mult),
                reads=[("tmpA", tb), "gout"], writes=[("Gt", T)])
    wb = load_wgroup(3)
    for T in range(NT):
        bank = next_bank()
        tm_matmul(wb, 0, 264, T, bank)
        sch.add("dve", lambda e, T=T, bank=bank: e.tensor_copy(out=kbtm[:, T, :], in_=ps[bank][:, 0:256]),
                reads=[psk(bank)], writes=[("kbtm", T)])
        sch.add("dve", lambda e, T=T, bank=bank: e.tensor_copy(out=iw_s[:, T, :], in_=ps[bank][:, 256:264]),
                reads=[psk(bank)], writes=[("iw", T)])

    def dump(name, src_ap, dst_ap, reads):
        if name in dbg:
            sch.add("sp", lambda e: e.dma_start(out=dst_ap, in_=src_ap), reads=reads, writes=["dbg_" + name], dma=True)

    def finish():
        import os
        if os.environ.get("KSTOP_OPS"):
            print("total ops", len(sch.ops))
            sch.ops = sch.ops[:int(os.environ["KSTOP_OPS"])]
        outs = ["dbg_" + n for n in dbg] + ["out_%d" % T for T in range(NT)]
        sch.add("sp", lambda e: None, reads=outs)
        sch.finalize()
        with nc.Block() as block:
            sch.emit(block)
        stack.close()
        return nc

    dump("hT", hT, dbg.get("hT").rearrange("(c p) t -> p c t", p=P) if "hT" in dbg else None, [("hT", T) for T in range(NT)])
    dump("qbT", qbT[:, 0, :], dbg.get("qbT"), [("qbT", tc, 0) for tc in range(4)])
    dump("vb", vb, dbg.get("vb").rearrange("(t p) c -> p t c", p=P) if "vb" in dbg else None, [("vb", T) for T in range(NT)])
    dump("Gt", Gt, dbg.get("Gt").rearrange("(t p) c -> p t c", p=P) if "Gt" in dbg else None, [("Gt", T) for T in range(NT)])
    if STOP_AFTER == "p1":
        return finish()

    Sst = sb("Sst", [P, 4, 128], F32)
    Sbf = sb("Sbf", [P, 4, 128], BF16)
    g_e1 = sb("g_e1", [P, 256], F32)
    g_l = sb("g_l", [P, 256], F32)
    g_eb = [sb("g_eb%d" % i, [P, 2, 128], F32) for i in range(2)]
    g_einv = sb("g_einv", [P, 2, 128], F32)
    g_erem = sb("g_erem", [P, 256], F32)
    g_qt = [sb("g_qt%d" % i, [P, 2, 128], BF16) for i in range(2)]
    g_kt = sb("g_kt", [P, 2, 128], BF16)
    g_kh = sb("g_kh", [P, 256], BF16)
    g_A = [sb("g_A%d" % i, [P, 4, 128], BF16) for i in range(2)]
    g_ob = sb("g_ob", [P, 512], BF16)
    g_ssq = sb("g_ssq", [P, 4], F32)
    g_rs = sb("g_rs", [P, 4], F32)
    sch.add("dve", lambda e: e.memset(Sst[:], 0.0), writes=[("Sst", h) for h in range(4)])
    sch.add("dve", lambda e: e.memset(Sbf[:], 0.0), writes=[("Sbf", h) for h in range(4)])
    BZ, BC, BA, BO, BT = 0, 1, 3, 4, 6
    BUS = [5, 2]
    print("ops before GLA", len(sch.ops))

    def gla_stage1(T):
        b = T % 2
        tsl = slice(T * P, (T + 1) * P)
        eb, qt, A, BU = g_eb[b], g_qt[b], g_A[b], BUS[b]
        sch.add("pe", lambda e: e.matmul(ps[BZ][:, 0:256], lhsT=glrT[0:17, tsl], rhs=w2aug[0:17, :], start=True, stop=True),
                reads=[("glrT", T // 4), "glrT_init", "w2aug"], writes=[psk(BZ)])
        sch.add("act", lambda e: e.activation(out=g_e1[:], in_=ps[BZ][:, 0:256], func=AF.Exp, scale=-1.0),
                reads=[psk(BZ)], writes=["g_e1"])
        sch.add("act", lambda e: e.activation(out=g_l[:], in_=g_e1[:], func=AF.Ln, scale=1.0, bias=ones_col[:, 0:1]),
                reads=["g_e1", "ones_col"], writes=["g_l"])

        def cum(e):
            e.matmul(ps[BC][:, 0:128], lhsT=g_l[:, 0:128], rhs=tri_f[:], start=True, stop=True)
            return e.matmul(ps[BC][:, 128:256], lhsT=g_l[:, 128:256], rhs=tri_f[:], start=True, stop=True)
        sch.add("pe", cum, reads=["g_l", "tri_f"], writes=[psk(BC)])
        sch.add("pe", lambda e: e.matmul(ps[BZ][:, 256:512], lhsT=after_f[:], rhs=g_l[:], start=True, stop=True),
                reads=["g_l", "after_f"], writes=[psk(BZ)])
        cview = ps[BC][:, 0:256].rearrange("p (a b) -> p a b", a=2)
        sch.add("act", lambda e: e.activation(out=eb[:], in_=cview, func=AF.Exp, scale=-1.0 / 16),
                reads=[psk(BC)], writes=[("g_eb", b)])
        sch.add("act", lambda e: e.activation(out=g_einv[:], in_=cview, func=AF.Exp, scale=1.0 / 16),
                reads=[psk(BC)], writes=["g_einv"])
        sch.add("act", lambda e: e.activation(out=g_erem[:], in_=ps[BZ][:, 256:512], func=AF.Exp, scale=-1.0 / 16),
                reads=[psk(BZ)], writes=["g_erem"])
        sch.add("dve", lambda e: e.scalar_tensor_tensor(out=qt[:], in0=qbT[:, :, tsl], scalar=0.125, in1=eb[:],
                                                        op0=ALU.mult, op1=ALU.mult),
                reads=[("qbT", T // 4, 0), ("qbT", T // 4, 1), ("g_eb", b)], writes=[("g_qt", b)])
        sch.add("dve", lambda e: e.scalar_tensor_tensor(out=g_kt[:], in0=kbT[:, :, tsl], scalar=1.0, in1=g_einv[:],
                                                        op0=ALU.mult, op1=ALU.mult),
                reads=[("kbT", T // 4, 0), ("kbT", T // 4, 1), "g_einv"], writes=["g_kt"])
        sch.add("dve", lambda e: e.scalar_tensor_tensor(out=g_kh[:], in0=kbtm[:, T, :], scalar=1.0, in1=g_erem[:],
                                                        op0=ALU.mult, op1=ALU.mult),
                reads=[("kbtm", T), "g_erem"], writes=["g_kh"])

        def attn(e):
            ins = None
            for h in (0, 2, 1, 3):
                p, r = h // 2, h % 2
                rs = slice(r * 64, (r + 1) * 64)
                bk = BA if r == 0 else 7
                ins = e.matmul(ps[bk][:, p * 128:(p + 1) * 128], lhsT=g_kt[rs, p, :], rhs=qt[rs, p, :], start=True, stop=True)
            return ins
        sch.add("pe", attn, reads=["g_kt", ("g_qt", b)], writes=[psk(BA), psk(7)])
        for r in range(2):
            bk = BA if r == 0 else 7
            sch.add("dve", lambda e, r=r, bk=bk: e.tensor_tensor(
                out=A[:, r::2, :], in0=ps[bk][:, 0:256].rearrange("p (h t) -> p h t", h=2),
                in1=tri_bf[:, :].unsqueeze(1).to_broadcast([P, 2, 128]), op=ALU.mult),
                reads=[psk(bk), "tri_bf"], writes=[("g_A", b, r)])

        def umm(e):
            ins = None
            for h in range(4):
                p = h // 2
                ins = e.matmul(ps[BU][:, h * 128:(h + 1) * 128], lhsT=g_kh[:, p * 128:(p + 1) * 128], rhs=vb[:, T, h * 128:(h + 1) * 128],
                               start=True, stop=True)
            return ins
        sch.add("pe", umm, reads=["g_kh", ("vb", T)], writes=[psk(BU)])

    def gla_stage2(T):
        b = T % 2
        tsl = slice(T * P, (T + 1) * P)
        eb, qt, A, BU = g_eb[b], g_qt[b], g_A[b], BUS[b]

        def omm(e):
            ins = None
            for h in range(4):
                p = h // 2
                e.matmul(ps[BO][:, h * 128:(h + 1) * 128], lhsT=A[:, h, :], rhs=vb[:, T, h * 128:(h + 1) * 128], start=True, stop=False)
                ins = e.matmul(ps[BO][:, h * 128:(h + 1) * 128], lhsT=qt[:, p, :], rhs=Sbf[:, h, :], start=False, stop=True)
            return ins
        sch.add("pe", omm, reads=[("g_A", b, 0), ("g_A", b, 1), ("vb", T), ("g_qt", b)] + [("Sbf", h) for h in range(4)], writes=[psk(BO)])
        for h in range(4):
            p, r = h // 2, h % 2
            rs = slice(r * 64, (r + 1) * 64)
            sch.add("dve", lambda e, h=h, p=p, rs=rs: e.scalar_tensor_tensor(
                out=Sst[rs, h, :], in0=Sst[rs, h, :], scalar=eb[rs, p, 127:128], in1=ps[BU][rs, h * 128:(h + 1) * 128],
                op0=ALU.mult, op1=ALU.add), reads=[psk(BU), ("g_eb", b), ("Sst", h)], writes=[("Sst", h), psk(BU)])
            sch.add("act", lambda e, h=h, rs=rs: e.activation(out=Sbf[rs, h, :], in_=Sst[rs, h, :], func=AF.Copy),
                    reads=[("Sst", h)], writes=[("Sbf", h)])
        for h in range(4):
            sch.add("act", lambda e, h=h: e.activation(out=junk[:, 0:128], in_=ps[BO][:, h * 128:(h + 1) * 128], func=AF.Square,
                                                       accum_out=g_ssq[:, h:h + 1]),
                    reads=[psk(BO)], writes=["junk", ("g_ssq", h), psk(BO)])
        sch.add("act", lambda e: e.activation(out=g_rs[:], in_=g_ssq[:], func=AF.Sqrt, scale=1.0 / 128, bias=eps_col[:, 0:1]),
                reads=[("g_ssq", h) for h in range(4)] + ["eps_col"], writes=["g_rs"])
        sch.add("dve", lambda e: e.reciprocal(out=g_rs[:], in_=g_rs[:]), reads=["g_rs"], writes=["g_rs"])
        for h in range(4):
            hs = slice(h * 128, (h + 1) * 128)
            sch.add("dve", lambda e, h=h, hs=hs: e.scalar_tensor_tensor(
                out=g_ob[:, hs], in0=ps[BO][:, hs], scalar=g_rs[:, h:h + 1], in1=Gt[:, T, hs], op0=ALU.mult, op1=ALU.mult),
                reads=[psk(BO), "g_rs", ("Gt", T)], writes=[("g_ob", h), psk(BO)])
        if "ob" in dbg:
            sch.add("sp", lambda e: e.dma_start(out=dbg["ob"][tsl, :], in_=g_ob[:]), reads=[("g_ob", h) for h in range(4)],
                    writes=["dbg_ob"], dma=True)

        def trb(e):
            o = ps[BT][:].bitcast(BF16).rearrange("p (c t) -> p c t", c=8)
            ins = None
            for c in range(4):
                ins = e.transpose(o[:, c, :], g_ob[:, c * P:(c + 1) * P], ident_bf[:])
            return ins
        sch.add("pe", trb, reads=[("g_ob", h) for h in range(4)] + ["ident_bf"], writes=[psk(BT)])
        sch.add("act", lambda e: e.activation(
            out=mixT_b[:, :, tsl], in_=ps[BT][:].bitcast(BF16).rearrange("p (c t) -> p c t", c=8)[:, 0:4, :], func=AF.Copy),
            reads=[psk(BT)], writes=[("mixT_b", T)])

    gla_stage1(0)
    for T in range(NT):
        if T + 1 < NT:
            gla_stage1(T + 1)
        gla_stage2(T)
    if STOP_AFTER == "gla":
        return finish()
    sch.barrier()

    qaT = view(R4, 0, [P, 4, S], BF16)
    kaT = view(R4, 16 * KB, [P, 4, S], BF16)
    iqT = view(R4, 32 * KB, [P, 3, S], BF16)
    ikT = view(R4, 44 * KB, [P, S], BF16)
    vaT = view(R4, 48 * KB, [P, NT, 8 * 65], BF16)
    wst2 = [view(R4, 66 * KB + i * 9 * KB, [P, 8, 528], BF16) for i in range(2)]
    n_sq = [view(R4, 84 * KB + i * KB, [P, 512], BF16) for i in range(2)]
    n_ln = [view(R4, 86 * KB + i * 2 * KB, [P, 512], F32) for i in range(2)]
    n_rs = [view(R4, 90 * KB + i * 2 * KB, [P, 512], F32) for i in range(2)]
    wst[0], wst[1] = wst2[0], wst2[1]
    sch.add("dve", lambda e: e.memset(vaT[:], 1.0), writes=[("va", T) for T in range(NT)])
    ncnt = [0]
    pend2 = []
    for gi, which in [(4, 0), (5, 1)]:
        wb = load_wgroup(gi)
        dstT = qaT if which == 0 else kaT
        kname = "qaT" if which == 0 else "kaT"
        for tc in range(4):
            tsl = slice(tc * 512, (tc + 1) * 512)
            for p in range(4):
                bank = next_bank(0, 4)
                sbank = next_bank(4, 8)
                nb = ncnt[0] % 2
                ncnt[0] += 1
                fm_matmul(wb, p * 128, 128, tc, bank)
                sch.add("act", lambda e, nb=nb, bank=bank: e.activation(out=n_sq[nb], in_=ps[bank][:], func=AF.Square),
                        reads=[psk(bank)], writes=[("n_sq", nb), psk(bank)])
                def back2(nb=nb, sbank=sbank, bank=bank, p=p, tsl=tsl, dstT=dstT, which=which, kname=kname, tc=tc):
                    sch.add("pe", lambda e: e.matmul(ps[sbank][:], lhsT=blockones[:], rhs=n_sq[nb], start=True, stop=True),
                            reads=[("n_sq", nb), "blockones"], writes=[psk(sbank)])
                    sch.add("act", lambda e: e.activation(out=n_ln[nb], in_=ps[sbank][:], func=AF.Ln, scale=1.0 / 64,
                                                          bias=eps_col[:, 0:1]),
                            reads=[psk(sbank), "eps_col"], writes=[("n_ln", nb)])
                    sch.add("act", lambda e: e.activation(out=n_rs[nb], in_=n_ln[nb], func=AF.Exp, scale=-0.5),
                            reads=[("n_ln", nb)], writes=[("n_rs", nb)])
                    sch.add("dve", lambda e: e.scalar_tensor_tensor(
                        out=dstT[:, p, tsl], in0=ps[bank][:], scalar=qkg[:, which:which + 1], in1=n_rs[nb], op0=ALU.mult, op1=ALU.mult),
                        reads=[psk(bank), ("n_rs", nb), "qkg"], writes=[(kname, p, tc), psk(bank)])
                pend2.append(back2)
                while len(pend2) > 1:
                    pend2.pop(0)()
    while pend2:
        pend2.pop(0)()
    wb = load_wgroup(6)
    for tc in range(4):
        tsl = slice(tc * 512, (tc + 1) * 512)
        col = 0
        for ti, (h0, h1) in enumerate(IQ_TILES):
            m = (h1 - h0) * 32
            bank = next_bank(0, 8)
            fm_matmul(wb, col, m, tc, bank)
            col += m
            sch.add("act", lambda e, ti=ti, m=m, tsl=tsl, bank=bank: e.activation(out=iqT[0:m, ti, tsl], in_=ps[bank][0:m, :], func=AF.Copy),
                    reads=[psk(bank)], writes=[("iqT", ti, tc)])
        bank = next_bank(0, 8)
        fm_matmul(wb, col, 96, tc, bank)
        sch.add("dve", lambda e, tsl=tsl, bank=bank: e.tensor_copy(out=ikT[0:96, tsl], in_=ps[bank][0:96, :]),
                reads=[psk(bank)], writes=[("ikT", tc)])
    wb = load_wgroup(7)
    for T in range(NT):
        bank = next_bank(0, 8)
        tm_matmul(wb, 0, 512, T, bank)
        dstv = vaT[:, T, :].rearrange("p (h d) -> p h d", h=8)[:, :, 0:64]
        srcv = ps[bank][:].rearrange("p (h d) -> p h d", h=8)
        if T % 2 == 0:
            sch.add("act", lambda e, dstv=dstv, srcv=srcv: e.activation(out=dstv, in_=srcv, func=AF.Copy),
                    reads=[psk(bank)], writes=[("va", T)])
        else:
            sch.add("dve", lambda e, dstv=dstv, srcv=srcv: e.tensor_copy(out=dstv, in_=srcv),
                    reads=[psk(bank)], writes=[("va", T)])
    dump("qaT", qaT[:, 0, :], dbg.get("qaT"), [("qaT", 0, tc) for tc in range(4)])
    dump("kaT", kaT[:, 0, :], dbg.get("kaT"), [("kaT", 0, tc) for tc in range(4)])
    if STOP_AFTER == "p2":
        return finish()
    sch.barrier()

    selT = view(R4, 66 * KB, [P, SELT_TOTAL], BF16)
    selts = [view(R4, 100 * KB + i * 4 * KB, [P, S], BF16) for i in range(2)]
    PTb = [view(R4, 100 * KB + i * KB, [P, 512], BF16) for i in range(8)]
    junk_tk = view(R4, 108 * KB, [P, 2048], BF16)
    score_all = R1[:, :].bitcast(F32)
    thr = sb("thr", [P, NT], F32)
    thr2 = sb("thr2", [P, NT], F32)
    cnt = sb("cnt", [P, NT], F32)
    sgn = sb("sgn", [P, NT], F32)
    amax = sb("amax", [P, NT], F32)
    mrow = sb("mrow", [P, 1], F32)
    mtab = sb("mtab", [P, KITER + 1], F32)
    biasT_s = view(R4, 100 * KB, [P, 8, 2, 128], F32)
    sch.add("sp", lambda e: e.dma_start(out=biasT_s, in_=biasT_d.rearrange("p (h d t) -> p h d t", h=8, d=2)),
            writes=["biasT"], dma=True)
    sch.add("act", lambda e: e.activation(out=Etile[:], in_=biasT_s, func=AF.Exp), reads=["biasT"], writes=["Etile"])
    for h in range(8):
        sch.add("dve", lambda e, h=h: e.tensor_tensor(out=Etile[:, h, 0, :], in0=Etile[:, h, 0, :], in1=tri_bf[:], op=ALU.mult),
                reads=["Etile", "tri_bf"], writes=["Etile"])
    sch.add("dve", lambda e: e.memset(selts[0][:], 0.0), reads=["Etile"], writes=[("selts", 0)])
    sch.add("dve", lambda e: e.tensor_copy(out=selT[:, _selT_off(0):_selT_off(0) + 128], in_=tri_bf[:]), reads=["tri_bf"], writes=[("selT", 0, 0)])
    sch.add("dve", lambda e: e.memset(selT[:, _selT_off(0) + 128:_selT_off(0) + 256], 1.0), writes=[("selT", 0, 1)])
    sch.add("dve", lambda e: e.tensor_copy(out=selT[:, _selT_off(1):_selT_off(1) + 128], in_=tri_bf[:]), reads=["tri_bf"], writes=[("selT", 1, 1)])

    print("ops before topk loops", len(sch.ops))
    batches = [list(range(2, 8)), list(range(8, 12)), list(range(12, 16))]
    ACT_SHARE = [4, 2, 2]
    sumA = sb("sumA", [P, NT], F32)
    nhalf = sb("nhalf", [P, NT], F32)
    junk_act = view(R3, 0, [P, 2048], BF16)
    for j in range(NT):
        sch.add("dve", lambda e, j=j: e.memset(nhalf[:, j:j + 1], 64.0 * (j + 1)), writes=["nhalf"])
    hd_loc = []
    for ti, (h0, h1) in enumerate(IQ_TILES):
        for k in range(h1 - h0):
            hd_loc.append((ti, k))
    lbank = [0]
    for bi, batch in enumerate(batches):
        soff = {}
        o = 0
        for j in batch:
            soff[j] = o
            o += (j + 1) * 128
        nb = len(batch)
        j0 = batch[0]
        for j in batch:
            n = (j + 1) * 128
            sc_j = score_all[:, soff[j]:soff[j] + n]
            nsc = (n + 511) // 512
            for sc in range(nsc):
                w = min(512, n - sc * 512)
                ssl = slice(sc * 512, sc * 512 + w)
                for h in range(8):
                    ti, k = hd_loc[h]
                    rs = slice(k * 32, (k + 1) * 32)
                    lb = lbank[0] % 4
                    rb = 4 + lbank[0] % 4
                    lbank[0] += 1
                    sch.add("pe", lambda e, lb=lb, rs=rs, ti=ti, j=j, ssl=ssl, w=w: e.matmul(
                        ps[lb][:, 0:w], lhsT=iqT[rs, ti, j * 128:(j + 1) * 128], rhs=ikT[rs, ssl], start=True, stop=True),
                        reads=[("iqT", ti, j // 4)] + [("ikT", c) for c in range(sc * 4 // 4, (sc * 512 + w - 1) // 512 + 1)],
                        writes=[psk(lb)])
                    sch.add("act", lambda e, lb=lb, rb=rb, w=w: e.activation(out=ps[rb][:, 0:w], in_=ps[lb][:, 0:w], func=AF.Relu),
                            reads=[psk(lb)], writes=[psk(rb), psk(lb)])
                    dst = sc_j[:, ssl]
                    if h == 0:
                        sch.add("dve", lambda e, rb=rb, w=w, dst=dst, j=j, h=h: e.tensor_scalar(
                            out=dst, in0=ps[rb][:, 0:w], scalar1=iw_s[:, j, h:h + 1], scalar2=None, op0=ALU.mult),
                            reads=[psk(rb), ("iw", j)], writes=[("score", j, sc), psk(rb)])
                    else:
                        sch.add("dve", lambda e, rb=rb, w=w, dst=dst, j=j, h=h: e.scalar_tensor_tensor(
                            out=dst, in0=ps[rb][:, 0:w], scalar=iw_s[:, j, h:h + 1], in1=dst, op0=ALU.mult, op1=ALU.add),
                            reads=[psk(rb), ("iw", j), ("score", j, sc)], writes=[("score", j, sc), psk(rb)])
            skeys = [("score", j, sc) for sc in range(nsc)]
            sch.add("dve", lambda e, sc_j=sc_j, j=j: e.tensor_reduce(out=amax[:, j:j + 1], in_=sc_j, axis=AX.X, op=ALU.max,
                                                                    apply_absolute_value=True),
                    reads=skeys, writes=[("amax", j)])
            dsl = slice(soff[j] + j * 128, soff[j] + (j + 1) * 128)
            sch.add("dve", lambda e, dsl=dsl: e.tensor_tensor(out=score_all[:, dsl], in0=score_all[:, dsl], in1=negmask[:], op=ALU.add),
                    reads=skeys + ["negmask", ("amax", j)], writes=skeys)
        if "score" in dbg and bi == 1:
            sch.add("sp", lambda e, soff=soff: e.dma_start(out=dbg["score"][:, 0:1280], in_=score_all[:, soff[9]:soff[9] + 1280]),
                    reads=[("score", 9, sc) for sc in range(3)], writes=["dbg_score"], dma=True)
        bsl = slice(j0, j0 + nb)
        sch.add("dve", lambda e, bsl=bsl: e.tensor_reduce(out=mrow[:], in_=amax[:, bsl], axis=AX.X, op=ALU.max),
                reads=[("amax", j) for j in batch], writes=["mrow"])
        sch.add("dve", lambda e: e.tensor_scalar(out=mtab[:], in0=pow2[:], scalar1=mrow[:, 0:1], scalar2=None, op0=ALU.mult),
                reads=["mrow", "pow2"], writes=["mtab"])
        sch.add("dve", lambda e, bsl=bsl: e.memset(thr[:, bsl], 0.0), writes=["thr"])
        nact = ACT_SHARE[bi]
        act_tiles = batch[:nact]
        asl = slice(batch[0], batch[0] + nact)
        for k in range(KITER):
            for j in batch:
                n = (j + 1) * 128
                sc_j = score_all[:, soff[j]:soff[j] + n]
                skeys_j = [("score", j, sc) for sc in range((n + 511) // 512)]
                if j in act_tiles:
                    sch.add("act", lambda e, sc_j=sc_j, n=n, j=j: e.activation(
                        out=junk_act[:, 0:n], in_=sc_j, func=AF.Sign, scale=-1.0, bias=thr[:, j:j + 1], accum_out=sumA[:, j:j + 1]),
                        reads=skeys_j + ["thr"], writes=["junk_act", ("sumA", j)])
                else:
                    sch.add("dve", lambda e, sc_j=sc_j, n=n, j=j: e.tensor_scalar(
                        out=junk_tk[:, 0:n], in0=sc_j, scalar1=thr[:, j:j + 1], scalar2=None, op0=ALU.is_ge, op1=ALU.add,
                        accum_out=cnt[:, j:j + 1]),
                        reads=skeys_j + ["thr"], writes=["junk", ("cnt", j)])
            if nact > 0:
                sch.add("dve", lambda e, asl=asl: e.scalar_tensor_tensor(out=cnt[:, asl], in0=sumA[:, asl], scalar=-0.5, in1=nhalf[:, asl],
                                                                        op0=ALU.mult, op1=ALU.add),
                        reads=[("sumA", j) for j in act_tiles] + ["nhalf"], writes=[("cnt", j) for j in act_tiles])
            sch.add("dve", lambda e, bsl=bsl: e.tensor_scalar(out=sgn[:, bsl], in0=cnt[:, bsl], scalar1=256.0, scalar2=0.5,
                                                             op0=ALU.is_ge, op1=ALU.subtract),
                    reads=[("cnt", j) for j in batch], writes=["sgn"])
            sch.add("dve", lambda e, bsl=bsl, k=k: e.scalar_tensor_tensor(out=thr2[:, bsl], in0=sgn[:, bsl], scalar=mtab[:, k:k + 1],
                                                                       in1=thr[:, bsl], op0=ALU.mult, op1=ALU.add),
                    reads=["sgn", "mtab", "thr"], writes=["thr2"])
            sch.add("dve", lambda e, bsl=bsl: e.tensor_copy(out=thr[:, bsl], in_=thr2[:, bsl]), reads=["thr2"], writes=["thr"])
        sch.add("dve", lambda e, bsl=bsl: e.tensor_scalar(out=thr2[:, bsl], in0=thr[:, bsl], scalar1=mtab[:, KITER:KITER + 1], scalar2=None,
                                                         op0=ALU.subtract),
                reads=["thr", "mtab"], writes=["thr2"])
        if "thr" in dbg and bi == 1:
            sch.add("sp", lambda e: e.dma_start(out=dbg["thr"][:, :], in_=thr2[:]), reads=["thr2"], writes=["dbg_thr"], dma=True)
        for j in batch:
            n = (j + 1) * 128
            sc_j = score_all[:, soff[j]:soff[j] + n]
            sb_ = j % 2
            sch.add("dve", lambda e, sc_j=sc_j, n=n, j=j, sb_=sb_: e.tensor_scalar(
                out=selts[sb_][:, 0:n], in0=sc_j, scalar1=thr2[:, j:j + 1], scalar2=None, op0=ALU.is_ge),
                reads=[("score", j, sc) for sc in range((n + 511) // 512)] + ["thr2"], writes=[("selts", sb_)])
            for i0 in range(0, j + 1, 8):
                i1 = min(j + 1, i0 + 8)
                tb = next_bank(0, 8)

                def trs(e, i0=i0, i1=i1, tb=tb, sb_=sb_):
                    o = ps[tb][:].bitcast(BF16).rearrange("p (c t) -> p c t", c=8)
                    ins = None
                    for i in range(i0, i1):
                        ins = e.transpose(o[:, i - i0, :], selts[sb_][:, i * 128:(i + 1) * 128], ident_bf[:])
                    return ins
                sch.add("pe", trs, reads=[("selts", sb_), "ident_bf"], writes=[psk(tb)])
                for i in range(i0, i1):
                    o = ps[tb][:].bitcast(BF16).rearrange("p (c t) -> p c t", c=8)[:, i - i0, :]
                    off = _selT_off(i) + (j - i) * 128
                    if i % 2 == 0:
                        sch.add("act", lambda e, o=o, off=off: e.activation(out=selT[:, off:off + 128], in_=o, func=AF.Copy),
                                reads=[psk(tb)], writes=[("selT", i, j), psk(tb)])
                    else:
                        sch.add("dve", lambda e, o=o, off=off: e.tensor_copy(out=selT[:, off:off + 128], in_=o),
                                reads=[psk(tb)], writes=[("selT", i, j), psk(tb)])
    dump("selT", selT, dbg.get("selT"), [("selT", i, j) for i in range(16) for j in range(i, 16)])
    if STOP_AFTER == "topk":
        return finish()
    sch.barrier()

    mixa = view(R1, 0, [P, NT, 512], BF16)
    rden = sb("rden", [P, 4], F32)
    abank = [0]
    LOOKAHEAD = 4
    pend = []

    def flush(keep):
        while len(pend) > keep:
            pend.pop(0)()

    for h in range(8):
        p, r = h // 2, h % 2
        rs = slice(r * 64, (r + 1) * 64)
        for J in range(4):
            accb = 6 + (abank[0] % 2)
            abank[0] += 1
            accv = ps[accb][:, 0:260].rearrange("p (j d) -> p j d", j=4)
            first = [True]
            for i in range(4 * J + 4):
                jlo = max(i, 4 * J)
                t0 = jlo * 128
                n = (4 * J + 4 - jlo) * 128
                sbk = next_bank(0, 6)
                pb = next_bank(100, 108) - 100
                sch.add("pe", lambda e, sbk=sbk, rs=rs, p=p, i=i, t0=t0, n=n: e.matmul(
                    ps[sbk][:, 0:n], lhsT=kaT[rs, p, i * 128:(i + 1) * 128], rhs=qaT[rs, p, t0:t0 + n], start=True, stop=True),
                    reads=[("kaT", p, i // 4), ("qaT", p, J)], writes=[psk(sbk)])
                nnear = max(0, min(4 * J + 4, i + 2) - jlo) * 128
                if nnear > 0:
                    sch.add("act", lambda e, sbk=sbk, pb=pb, nnear=nnear: e.activation(out=PTb[pb][:, 0:nnear], in_=ps[sbk][:, 0:nnear],
                                                                                       func=AF.Exp, scale=0.125),
                            reads=[psk(sbk)], writes=[("PT", pb), psk(sbk)])
                if n > nnear:
                    sch.add("act", lambda e, sbk=sbk, pb=pb, nnear=nnear, n=n, h=h: e.activation(
                        out=PTb[pb][:, nnear:n], in_=ps[sbk][:, nnear:n], func=AF.Exp, scale=0.125, bias=rb31[:, h:h + 1]),
                        reads=[psk(sbk), "rb31"], writes=[("PT", pb), psk(sbk)])
                soff_ = _selT_off(i) + (jlo - i) * 128
                sch.add("dve", lambda e, pb=pb, n=n, soff_=soff_: e.tensor_tensor(out=PTb[pb][:, 0:n], in0=PTb[pb][:, 0:n],
                                                                                 in1=selT[:, soff_:soff_ + n], op=ALU.mult),
                        reads=[("PT", pb)] + [("selT", i, j) for j in range(jlo, 4 * J + 4)], writes=[("PT", pb)])
                for j in range(jlo, min(4 * J + 4, i + 2)):
                    dlt = j - i
                    cs = slice((j - jlo) * 128, (j - jlo + 1) * 128)
                    sch.add("dve", lambda e, pb=pb, cs=cs, h=h, dlt=dlt: e.tensor_tensor(out=PTb[pb][:, cs], in0=PTb[pb][:, cs],
                                                                                         in1=Etile[:, h, dlt, :], op=ALU.mult),
                            reads=[("PT", pb), "Etile"], writes=[("PT", pb)])

                def back(pb=pb, jlo=jlo, J=J, i=i, h=h, accv=accv, first=first, accb=accb):
                    def pv(e):
                        ins = None
                        for j in range(jlo, 4 * J + 4):
                            cs = slice((j - jlo) * 128, (j - jlo + 1) * 128)
                            ins = e.matmul(accv[:, j - 4 * J, :], lhsT=PTb[pb][:, cs], rhs=vaT[:, i, h * 65:(h + 1) * 65],
                                           start=first[0], stop=False, skip_group_check=True)
                            first[0] = False
                        return ins
                    sch.add("pe", pv, reads=[("PT", pb), ("va", i)], writes=[psk(accb)])
                    if i == 4 * J + 3:
                        sch.add("dve", lambda e: e.reciprocal(out=rden[:], in_=accv[:, :, 64]), reads=[psk(accb)], writes=["rden", psk(accb)])
                        sch.add("dve", lambda e: e.tensor_tensor(
                            out=mixa[:, 4 * J:4 * J + 4, h * 64:(h + 1) * 64], in0=accv[:, :, 0:64],
                            in1=rden[:, :].unsqueeze(2).to_broadcast([P, 4, 64]), op=ALU.mult),
                            reads=[psk(accb), "rden"], writes=[("mixa", 4 * J + jj, h) for jj in range(4)] + [psk(accb)])
                pend.append(back)
                flush(LOOKAHEAD)
    flush(0)
    dump("mixa", mixa, dbg.get("mixa").rearrange("(t p) c -> p t c", p=P) if "mixa" in dbg else None,
         [("mixa", T, h) for T in range(NT) for h in range(8)])
    for T in range(NT):
        tsl = slice(T * P, (T + 1) * P)
        tb = next_bank(0, 6)

        def tra(e, T=T, tb=tb):
            o = ps[tb][:].bitcast(BF16).rearrange("p (c t) -> p c t", c=8)
            ins = None
            for c in range(4):
                ins = e.transpose(o[:, c, :], mixa[:, T, c * P:(c + 1) * P], ident_bf[:])
            return ins
        sch.add("pe", tra, reads=[("mixa", T, h) for h in range(8)] + ["ident_bf"], writes=[psk(tb)])
        sch.add("act", lambda e, tsl=tsl, tb=tb: e.activation(
            out=mixT_a[:, :, tsl], in_=ps[tb][:].bitcast(BF16).rearrange("p (c t) -> p c t", c=8)[:, 0:4, :], func=AF.Copy),
            reads=[psk(tb)], writes=[("mixT_a", T)])
    if STOP_AFTER == "attn":
        return finish()
    sch.barrier()

    x1 = view(R4, 0, [P, NT, D], F32)
    woutb = view(R4, 64 * KB, [P, 8, D], BF16)
    xst5 = [view(R4, 80 * KB + i * 4 * KB, [P, D], F32) for i in range(2)]
    xs5 = [view(R4, 88 * KB + i * 2 * KB, [P, D], BF16) for i in range(2)]
    junk5 = view(R4, 92 * KB, [P, 2048], BF16)
    g2bc = view(R4, 96 * KB, [P, D], F32)
    wrb = view(R4, 100 * KB, [P, 8, 36], BF16)
    h2T = view(R1, 0, [P, 8, S], BF16)
    ssq2 = sb("ssq2", [P, NT], F32)
    rstd2 = sb("rstd2", [P, NT], F32)
    logit = sb("logit", [P, NT, 36], F32)
    for hh in range(2):
        sch.add("pool", lambda e, hh=hh: e.dma_start(out=woutb[:, :, hh * 512:(hh + 1) * 512],
                                                     in_=wout_d[:, hh * 512:(hh + 1) * 512].rearrange("(c p) n -> p c n", p=P)),
                writes=[("wout", hh)], dma=True)
    sch.add("pool", lambda e: e.dma_start(out=wrb, in_=wr_d.rearrange("(c p) n -> p c n", p=P)), writes=["wrb"], dma=True)
    sch.add("sp", lambda e: e.dma_start(out=g2bc, in_=g2bc_d[:, :]), writes=["g2bc"], dma=True)
    pend5 = []
    for T in range(NT):
        b = T % 2
        tsl = slice(T * P, (T + 1) * P)
        sch.add("sp", lambda e, b=b, tsl=tsl: e.dma_start(out=xst5[b], in_=x_d[tsl, :]), writes=[("xst5", b)], dma=True)
        for hh in range(2):
            bank = next_bank(0, 4)

            def om(e, T=T, hh=hh, bank=bank):
                ins = None
                for c in range(8):
                    src = mixT_a if c < 4 else mixT_b
                    ins = e.matmul(ps[bank][:], lhsT=src[:, c % 4, T * P:(T + 1) * P], rhs=woutb[:, c, hh * 512:(hh + 1) * 512],
                                   start=(c == 0), stop=(c == 7))
                return ins
            sch.add("pe", om, reads=[("mixT_a", T), ("mixT_b", T), ("wout", hh)], writes=[psk(bank)])
            sch.add("dve", lambda e, T=T, hh=hh, bank=bank, b=b: e.tensor_tensor(
                out=x1[:, T, hh * 512:(hh + 1) * 512], in0=ps[bank][:], in1=xst5[b][:, hh * 512:(hh + 1) * 512], op=ALU.add),
                reads=[psk(bank), ("xst5", b)], writes=[("x1", T, hh)])
        sch.add("act", lambda e, T=T: e.activation(out=junk5[:, 0:D], in_=x1[:, T, :], func=AF.Square, accum_out=ssq2[:, T:T + 1]),
                reads=[("x1", T, 0), ("x1", T, 1)], writes=["junk5", ("ssq2", T)])
        sch.add("act", lambda e, T=T: e.activation(out=rstd2[:, T:T + 1], in_=ssq2[:, T:T + 1], func=AF.Sqrt, scale=1.0 / D,
                                                   bias=eps_col[:, 0:1]),
                reads=[("ssq2", T), "eps_col"], writes=[("rstd2", T)])
        sch.add("dve", lambda e, T=T: e.reciprocal(out=rstd2[:, T:T + 1], in_=rstd2[:, T:T + 1]), reads=[("rstd2", T)], writes=[("rstd2", T)])
        sch.add("dve", lambda e, T=T, b=b: e.scalar_tensor_tensor(out=xs5[b], in0=x1[:, T, :], scalar=rstd2[:, T:T + 1], in1=g2bc,
                                                                  op0=ALU.mult, op1=ALU.mult),
                reads=[("x1", T, 0), ("x1", T, 1), ("rstd2", T), "g2bc"], writes=[("xs5", b)])
        def back5(T=T, b=b, tsl=tsl):
            tb = next_bank(4, 6)

            def tr5(e, b=b, tb=tb):
                o = ps[tb][:].bitcast(BF16).rearrange("p (c t) -> p c t", c=8)
                ins = None
                for c in range(8):
                    ins = e.transpose(o[:, c, :], xs5[b][:, c * P:(c + 1) * P], ident_bf[:])
                return ins
            sch.add("pe", tr5, reads=[("xs5", b), "ident_bf"], writes=[psk(tb)])
            sch.add("act", lambda e, tb=tb, tsl=tsl: e.activation(
                out=h2T[:, :, tsl], in_=ps[tb][:].bitcast(BF16).rearrange("p (c t) -> p c t", c=8), func=AF.Copy),
                reads=[psk(tb)], writes=[("h2T", T)])
            lbk = next_bank(6, 8)

            def rmm(e, T=T, lbk=lbk):
                ins = None
                for c in range(8):
                    ins = e.matmul(ps[lbk][:, 0:36], lhsT=h2T[:, c, T * P:(T + 1) * P], rhs=wrb[:, c, :], start=(c == 0), stop=(c == 7))
                return ins
            sch.add("pe", rmm, reads=[("h2T", T), "wrb"], writes=[psk(lbk)])
            sch.add("dve", lambda e, T=T, lbk=lbk: e.tensor_tensor(out=logit[:, T, :], in0=ps[lbk][:, 0:36], in1=brbc[:], op=ALU.add),
                    reads=[psk(lbk), "brbc"], writes=["logit"])
        pend5.append(back5)
        while len(pend5) > 1:
            pend5.pop(0)()
    while pend5:
        pend5.pop(0)()
    dump("x1", x1, dbg.get("x1").rearrange("(t p) c -> p t c", p=P) if "x1" in dbg else None,
         [("x1", T, hh) for T in range(NT) for hh in range(2)])
    if STOP_AFTER == "x1":
        return finish()

    gl = logit[:, :, 0:4]
    el = logit[:, :, 4:36].rearrange("p t (g e) -> p t g e", g=4)
    _ro = [101 * KB]

    def rv(shape):
        nb = int(np.prod(shape[1:])) * 4
        v = view(R4, _ro[0], shape, F32)
        _ro[0] += nb
        return v
    gmax = rv([P, NT])
    goh = rv([P, NT, 4])
    gsh = rv([P, NT, 4])
    gsum = rv([P, NT])
    gw = rv([P, NT])
    etmp = rv([P, NT, 4, 8])
    esel = rv([P, NT, 8])
    esel2 = rv([P, NT, 8])
    m1 = rv([P, NT])
    m2 = rv([P, NT])
    oh1 = rv([P, NT, 8])
    oh2 = rv([P, NT, 8])
    dd = rv([P, NT])
    w1 = rv([P, NT])
    w2 = rv([P, NT])
    gsel = rv([P, NT, 8])
    assert _ro[0] <= 110 * KB
    gates = view(R4, 110 * KB, [P, NT, 4, 8], F32)

    def D_(fn, reads, writes):
        sch.add("dve", fn, reads=reads, writes=writes)

    def bc3(ap2, n):
        return ap2.unsqueeze(2).to_broadcast([P, NT, n])
    D_(lambda e: e.tensor_reduce(out=gmax[:], in_=gl, axis=AX.X, op=ALU.max), ["logit"], ["gmax"])
    D_(lambda e: e.tensor_tensor(out=goh[:], in0=gl, in1=bc3(gmax[:, :], 4), op=ALU.is_equal), ["logit", "gmax"], ["goh"])
    D_(lambda e: e.tensor_tensor(out=gsh[:], in0=gl, in1=bc3(gmax[:, :], 4), op=ALU.subtract), ["logit", "gmax"], ["gsh"])
    sch.add("act", lambda e: e.activation(out=gsh[:], in_=gsh[:], func=AF.Exp), reads=["gsh"], writes=["gsh"])
    D_(lambda e: e.tensor_reduce(out=gsum[:], in_=gsh[:], axis=AX.X, op=ALU.add), ["gsh"], ["gsum"])
    D_(lambda e: e.reciprocal(out=gw[:], in_=gsum[:]), ["gsum"], ["gw"])
    D_(lambda e: e.tensor_tensor(out=etmp[:], in0=el, in1=goh[:, :, :].unsqueeze(3).to_broadcast([P, NT, 4, 8]), op=ALU.mult),
       ["logit", "goh"], ["etmp"])
    D_(lambda e: e.tensor_reduce(out=esel[:], in_=etmp[:, :, :, :].rearrange("p t g e -> p t e g"), axis=AX.X, op=ALU.add), ["etmp"], ["esel"])
    D_(lambda e: e.tensor_reduce(out=m1[:], in_=esel[:], axis=AX.X, op=ALU.max), ["esel"], ["m1"])
    D_(lambda e: e.tensor_tensor(out=oh1[:], in0=esel[:], in1=bc3(m1[:, :], 8), op=ALU.is_equal), ["esel", "m1"], ["oh1"])
    D_(lambda e: e.scalar_tensor_tensor(out=esel2[:], in0=oh1[:], scalar=-1e30, in1=esel[:], op0=ALU.mult, op1=ALU.add), ["oh1", "esel"], ["esel2"])
    D_(lambda e: e.tensor_reduce(out=m2[:], in_=esel2[:], axis=AX.X, op=ALU.max), ["esel2"], ["m2"])
    D_(lambda e: e.tensor_tensor(out=oh2[:], in0=esel2[:], in1=bc3(m2[:, :], 8), op=ALU.is_equal), ["esel2", "m2"], ["oh2"])
    D_(lambda e: e.tensor_tensor(out=dd[:], in0=m2[:], in1=m1[:], op=ALU.subtract), ["m1", "m2"], ["dd"])
    sch.add("act", lambda e: e.activation(out=dd[:], in_=dd[:], func=AF.Exp), reads=["dd"], writes=["dd"])
    D_(lambda e: e.tensor_scalar(out=w1[:], in0=dd[:], scalar1=1.0, scalar2=None, op0=ALU.add), ["dd"], ["w1"])
    D_(lambda e: e.reciprocal(out=w1[:], in_=w1[:]), ["w1"], ["w1"])
    D_(lambda e: e.tensor_tensor(out=w2[:], in0=dd[:], in1=w1[:], op=ALU.mult), ["dd", "w1"], ["w2"])
    D_(lambda e: e.tensor_tensor(out=w1[:], in0=w1[:], in1=gw[:], op=ALU.mult), ["w1", "gw"], ["w1"])
    D_(lambda e: e.tensor_tensor(out=w2[:], in0=w2[:], in1=gw[:], op=ALU.mult), ["w2", "gw"], ["w2"])
    D_(lambda e: e.tensor_tensor(out=oh1[:], in0=oh1[:], in1=bc3(w1[:, :], 8), op=ALU.mult), ["oh1", "w1"], ["oh1"])
    D_(lambda e: e.tensor_tensor(out=oh2[:], in0=oh2[:], in1=bc3(w2[:, :], 8), op=ALU.mult), ["oh2", "w2"], ["oh2"])
    D_(lambda e: e.tensor_tensor(out=gsel[:], in0=oh1[:], in1=oh2[:], op=ALU.add), ["oh1", "oh2"], ["gsel"])
    D_(lambda e: e.tensor_tensor(out=gates[:], in0=goh[:, :, :].unsqueeze(3).to_broadcast([P, NT, 4, 8]),
                                 in1=gsel[:, :, :].unsqueeze(2).to_broadcast([P, NT, 4, 8]), op=ALU.mult), ["goh", "gsel"], ["gates"])
    dump("gates", gates[:, :, :, :].rearrange("p t g e -> p t (g e)"),
         dbg.get("gates").rearrange("(t p) c -> p t c", p=P) if "gates" in dbg else None, ["gates"])
    if STOP_AFTER == "router":
        return finish()
    sch.barrier()

    hid = [view(R4, 64 * KB + i * 8 * KB, [P, 2, S], BF16) for i in range(2)]
    gT = view(R4, 80 * KB, [32, 2, S], BF16, parts=32)
    onehot = view(R4, 88 * KB, [32, 32, 128], BF16, parts=32)
    sa = [view(R4, 96 * KB + i * 2 * KB, [P, 512], F32) for i in range(2)]
    t1 = [view(R4, 100 * KB + i * 2 * KB, [P, 512], F32) for i in range(2)]
    wbuf = []
    for RR in (R2, R3):
        wbuf.append((view(RR, 0, [P, 8, 256], BF16), view(RR, 4 * KB, [P, 8, 256], BF16), view(RR, 8 * KB, [P, 2, D], BF16)))
    sch.add("sp", lambda e: e.dma_start(out=onehot, in_=onehot_d.rearrange("e (k m) -> e k m", k=32)), writes=["onehot"], dma=True)
    for T4 in range(4):
        tb = next_bank(0, 4)

        def trg(e, T4=T4, tb=tb):
            ins = None
            for k in range(4):
                T = T4 * 4 + k
                ins = e.transpose(ps[tb][0:32, k * 128:(k + 1) * 128], gates[:, T, :, :].rearrange("p g e -> p (g e)"), ident_f[:])
            return ins
        sch.add("pe", trg, reads=["gates", "ident_f"], writes=[psk(tb)])
        sl = slice(T4 * 512, (T4 + 1) * 512)
        sch.add("act", lambda e, tb=tb, sl=sl: e.activation(out=gT[0:32, 0, sl], in_=ps[tb][0:32, :], func=AF.Copy),
                reads=[psk(tb)], writes=[("gThi", T4), psk(tb)])
        sch.add("dve", lambda e, tb=tb, sl=sl: e.tensor_tensor(out=gT[0:32, 1, sl], in0=ps[tb][0:32, :], in1=gT[0:32, 0, sl], op=ALU.subtract),
                reads=[psk(tb), ("gThi", T4)], writes=[("gTlo", T4), psk(tb)])
    mcnt = [0]
    for pr in range(NE // 2):
      for ex in (2 * pr, 2 * pr + 1):
        wbi = ex % 2
        Wg, Wu, Wd = wbuf[wbi]
        sch.add("pool", lambda e, Wg=Wg, ex=ex: e.dma_start(out=Wg, in_=wg_d[ex].rearrange("(c p) f -> p c f", p=P)),
                writes=[("Wg", wbi)], dma=True)
        sch.add("pool", lambda e, Wu=Wu, ex=ex: e.dma_start(out=Wu, in_=wu_d[ex].rearrange("(c p) f -> p c f", p=P)),
                writes=[("Wu", wbi)], dma=True)
        for hh in range(2):
            sch.add("pool", lambda e, Wd=Wd, ex=ex, hh=hh: e.dma_start(
                out=Wd[:, :, hh * 512:(hh + 1) * 512], in_=wd_d[ex][:, hh * 512:(hh + 1) * 512].rearrange("(c p) n -> p c n", p=P)),
                writes=[("Wd", wbi, hh)], dma=True)
        hb = ex % 2
        for tc in range(4):
            sl = slice(tc * 512, (tc + 1) * 512)
            gb = mcnt[0] % 2
            mcnt[0] += 1

            def gmm(e, ex=ex, gb=gb, sl=sl):
                e.matmul(ps[gb][:], lhsT=onehot[0:32, ex, :], rhs=gT[0:32, 0, sl], start=True, stop=False)
                return e.matmul(ps[gb][:], lhsT=onehot[0:32, ex, :], rhs=gT[0:32, 1, sl], start=False, stop=True)
            sch.add("pe", gmm, reads=["onehot", ("gThi", tc), ("gTlo", tc)], writes=[psk(gb)])
            for ft in range(2):
                ab = 2 + (mcnt[0] % 2)
                ub = 4 + (mcnt[0] % 2)
                tb_ = mcnt[0] % 2
                mcnt[0] += 1

                def amm(e, Wg=Wg, ft=ft, sl=sl, ab=ab):
                    ins = None
                    for c in range(8):
                        ins = e.matmul(ps[ab][:], lhsT=Wg[:, c, ft * 128:(ft + 1) * 128], rhs=h2T[:, c, sl], start=(c == 0), stop=(c == 7))
                    return ins

                def umm2(e, Wu=Wu, ft=ft, sl=sl, ub=ub):
                    ins = None
                    for c in range(8):
                        ins = e.matmul(ps[ub][:], lhsT=Wu[:, c, ft * 128:(ft + 1) * 128], rhs=h2T[:, c, sl], start=(c == 0), stop=(c == 7))
                    return ins
                hkeys = [("h2T", tc * 4 + k) for k in range(4)]
                sch.add("pe", amm, reads=[("Wg", wbi)] + hkeys, writes=[psk(ab)])
                sch.add("pe", umm2, reads=[("Wu", wbi)] + hkeys, writes=[psk(ub)])
                sch.add("act", lambda e, ab=ab, tb_=tb_: e.activation(out=sa[tb_], in_=ps[ab][:], func=AF.Silu),
                        reads=[psk(ab)], writes=[("sa", tb_), psk(ab)])
                sch.add("dve", lambda e, ub=ub, tb_=tb_: e.tensor_tensor(out=t1[tb_], in0=ps[ub][:], in1=sa[tb_], op=ALU.mult),
                        reads=[psk(ub), ("sa", tb_)], writes=[("t1", tb_), psk(ub)])
                sch.add("dve", lambda e, gb=gb, tb_=tb_, hb=hb, ft=ft, sl=sl: e.tensor_tensor(out=hid[hb][:, ft, sl], in0=ps[gb][:], in1=t1[tb_],
                                                                                             op=ALU.mult),
                        reads=[psk(gb), ("t1", tb_)], writes=[("hid", hb, tc), psk(gb)])
      WdA, WdB = wbuf[0][2], wbuf[1][2]
      for T in range(NT):
            for hh in range(2):
                yb = (6, 7, 0, 1)[mcnt[0] % 4]
                mcnt[0] += 1

                def dmm(e, T=T, hh=hh, yb=yb):
                    ins = None
                    k = 0
                    for (hd, Wd_) in ((hid[0], WdA), (hid[1], WdB)):
                        for ft in range(2):
                            ins = e.matmul(ps[yb][:], lhsT=hd[:, ft, T * P:(T + 1) * P], rhs=Wd_[:, ft, hh * 512:(hh + 1) * 512],
                                           start=(k == 0), stop=(k == 3))
                            k += 1
                    return ins
                sch.add("pe", dmm, reads=[("hid", 0, T // 4), ("hid", 1, T // 4), ("Wd", 0, hh), ("Wd", 1, hh)], writes=[psk(yb)])
                sch.add("dve", lambda e, T=T, hh=hh, yb=yb: e.tensor_tensor(out=x1[:, T, hh * 512:(hh + 1) * 512], in0=ps[yb][:],
                                                                           in1=x1[:, T, hh * 512:(hh + 1) * 512], op=ALU.add),
                        reads=[psk(yb), ("x1", T, hh)], writes=[("x1", T, hh), psk(yb)])
    for T in range(NT):
        tsl = slice(T * P, (T + 1) * P)
        sch.add("sp", lambda e, T=T, tsl=tsl: e.dma_start(out=out_d[tsl, :], in_=x1[:, T, :]), reads=[("x1", T, 0), ("x1", T, 1)],
                writes=["out_%d" % T], dma=True)
    return finish()


_CACHE = {}


def _prep_shared(inputs):
    f32 = np.float32
    c = host_constants()
    w_in = np.asarray(inputs["w_in"][0], f32)
    shared = {}
    shared["w1"] = np.ascontiguousarray(w_in[:, W1_COLS])
    shared["wout"] = np.ascontiguousarray(np.asarray(inputs["w_out"][0], f32))
    shared["wr"] = np.ascontiguousarray(np.concatenate([np.asarray(inputs["w_router_group"][0], f32),
                                                        np.asarray(inputs["w_router_expert"][0], f32)], axis=1))
    shared["wg"] = np.ascontiguousarray(np.asarray(inputs["w_exp_gate"][0], f32))
    shared["wu"] = np.ascontiguousarray(np.asarray(inputs["w_exp_up"][0], f32))
    shared["wd"] = np.ascontiguousarray(np.asarray(inputs["w_exp_down"][0], f32))
    shared["w2aug"] = np.ascontiguousarray(np.concatenate([np.asarray(inputs["gla_gate_w2"][0], f32),
                                                           np.asarray(inputs["gla_gate_b"], f32).reshape(1, 256)], axis=0))
    shared["g1bc"] = np.ascontiguousarray(np.tile(np.asarray(inputs["norm1_g"], f32).reshape(1, D), (P, 1)))
    shared["g2bc"] = np.ascontiguousarray(np.tile(np.asarray(inputs["norm2_g"], f32).reshape(1, D), (P, 1)))
    qg = np.tile(np.asarray(inputs["q_norm_g"], f32).reshape(64), 2)
    kg = np.tile(np.asarray(inputs["k_norm_g"], f32).reshape(64), 2)
    shared["qkg"] = np.ascontiguousarray(np.stack([qg, kg], axis=1))
    shared["goutbc"] = np.ascontiguousarray(np.tile(np.asarray(inputs["gla_out_norm_g"], f32).reshape(1, 128), (P, 4)))
    br = np.concatenate([np.asarray(inputs["b_router_group"], f32).reshape(4),
                         np.asarray(inputs["b_router_expert"], f32).reshape(32)])
    shared["brbc"] = np.ascontiguousarray(np.tile(br.reshape(1, 36), (P, 1)))
    rb = np.asarray(inputs["rel_bias"], f32)
    idx = np.arange(128)
    bT = np.zeros((P, 8, 2, 128), f32)
    for dlt in range(2):
        dist = 128 * dlt + idx[None, :] - idx[:, None]
        bk = _t5_bucket_np(np.maximum(dist, 0))
        for h in range(8):
            bT[:, h, dlt, :] = rb[bk, h]
    shared["biasT"] = np.ascontiguousarray(bT.reshape(P, -1))
    shared["rb31"] = np.ascontiguousarray(np.tile(rb[31:32, :], (P, 1)))
    for k in ["ident_bf", "ident_f", "tri_bf", "tri_f", "after_f", "negmask", "blockones", "pow2"]:
        shared[k] = c[k]
    shared["onehot"] = np.ascontiguousarray(c["onehot"].reshape(32, -1))
    return shared


def kernel(**inputs):
    x = np.asarray(inputs["x"], np.float32)
    if "nc" not in _CACHE:
        _CACHE["nc"] = build_program()
    nc = _CACHE["nc"]
    shared = _prep_shared(inputs)
    in_maps = []
    for b in range(8):
        m = dict(shared)
        m["x"] = np.ascontiguousarray(x[b])
        in_maps.append(m)
    res = run_bass_kernel_spmd(nc, in_maps, core_ids=list(range(8)))
    _CACHE["last"] = res
    out = np.stack([np.asarray(r["out"], np.float32) for r in res.results], axis=0)
    return out
```
